# Optimizing a Trainium2 kernel written in Bass

```python
import math
import jax, jax.numpy as jnp
from jax import lax
import numpy as np

D_MODEL = 1024
BATCH = 8
SEQ = 8192
DEPTH = 2

CHUNK = 64
N_META = 16
SSM_HEAD_DIM = 64
SSM_INNER = D_MODEL
SSM_HEADS = SSM_INNER // SSM_HEAD_DIM
SSM_GROUPS = 2
SSM_STATE = 128
SSM_CONV = 4
SSM_CONV_DIM = SSM_INNER + 2 * SSM_GROUPS * SSM_STATE
RWKV_HEAD_DIM = 64
RWKV_DIM = D_MODEL
RWKV_HEADS = RWKV_DIM // RWKV_HEAD_DIM
RWKV_DECAY_LORA = 64
RWKV_AAA_LORA = 64
RWKV_GATE_LORA = 128
RWKV_COLS = 3 * RWKV_DIM + RWKV_DECAY_LORA + RWKV_AAA_LORA + RWKV_GATE_LORA
RWKV_LN_EPS = 64e-5
IN0 = SSM_INNER + SSM_CONV_DIM + SSM_HEADS + RWKV_COLS
MIX0 = SSM_INNER + RWKV_DIM
LRU_DIM = D_MODEL
LRU_BLOCKS = 8
LRU_BLOCK = LRU_DIM // LRU_BLOCKS
LRU_CONV = 4
LRU_C = 8.0
D_FF = 2816
N_EXPERTS = 8
TOP_K = 2
MOE_BLOCK = 128
N_EVEN = (DEPTH + 1) // 2
N_ODD = DEPTH // 2
DEEPNORM_ALPHA = (2 * DEPTH) ** 0.25
DEEPNORM_BETA = (8 * DEPTH) ** -0.25
LN_EPS = 1e-5

kernel_name = 'hybrid_ssd_rwkv7_rglru_moe_deepnorm'


def layer_norm(x, g, b):
    xf = x.astype(jnp.float32)
    mu = jnp.mean(xf, -1, keepdims=True)
    var = jnp.mean(jnp.square(xf - mu), -1, keepdims=True)
    return ((xf - mu) * lax.rsqrt(var + LN_EPS) * g + b).astype(x.dtype)


def causal_dwconv(x, w, b):
    k = w.shape[0]
    y = lax.conv_general_dilated(x, w[:, None, :].astype(x.dtype), (1,), [(k - 1, 0)],
                                 dimension_numbers=('NWC', 'WIO', 'NWC'),
                                 feature_group_count=x.shape[-1])
    return y + b


def swiglu(x, w_gu, w_down):
    g, u = jnp.split(x @ w_gu, 2, axis=-1)
    return (jax.nn.silu(g) * u) @ w_down


def ssd_chunked(x, dt, A, Bm, Cm):
    b, l, g, e, p = x.shape
    n = Bm.shape[-1]
    c, q = l // CHUNK, CHUNK
    xdt = (x * dt[..., None]).reshape(b, c, q, g, e, p)
    Bc = Bm.reshape(b, c, q, g, n)
    Cc = Cm.reshape(b, c, q, g, n)
    acs = jnp.cumsum(jnp.moveaxis((dt * A).reshape(b, c, q, g, e), 2, -1), axis=-1)
    causal = jnp.tril(jnp.ones((q, q), bool))
    decay_in = jnp.exp(jnp.where(causal, acs[..., :, None] - acs[..., None, :], -jnp.inf))
    scores = jnp.einsum('bclgn,bcsgn->bcgls', Cc, Bc)
    y_diag = jnp.einsum('bcgels,bcsgep->bclgep', scores[:, :, :, None] * decay_in, xdt)
    to_end = jnp.moveaxis(jnp.exp(acs[..., -1:] - acs), -1, 2)[..., None]
    states = jnp.einsum('bcsgn,bcsgep->bcgepn', Bc, xdt * to_end)
    chunk_decay = jnp.exp(acs[..., -1])

    def carry_state(h, inp):
        s, d = inp
        return h * d[..., None, None] + s, h

    _, prev = lax.scan(carry_state, jnp.zeros_like(states[:, 0]),
                       (jnp.moveaxis(states, 1, 0), jnp.moveaxis(chunk_decay, 1, 0)))
    prev = jnp.moveaxis(prev, 0, 1)
    y_off = jnp.einsum('bclgn,bcgepn->bclgep', Cc, prev) * jnp.moveaxis(jnp.exp(acs), -1, 2)[..., None]
    return (y_diag + y_off).reshape(b, l, g, e, p)


def ssd_branch(z, xbc, dt_raw, conv_w, conv_b, dt_bias, a_log, d_skip, norm_w):
    b, l, _ = z.shape
    pad = (-l) % CHUNK
    lp = l + pad
    xbc = jax.nn.silu(causal_dwconv(jnp.pad(xbc, ((0, 0), (pad, 0), (0, 0))), conv_w, conv_b))
    xs, bm, cm = jnp.split(xbc.astype(jnp.float32), [SSM_INNER, SSM_INNER + SSM_GROUPS * SSM_STATE], axis=-1)
    valid = (jnp.arange(lp) >= pad).astype(jnp.float32)[None, :, None]
    dt = jax.nn.softplus(jnp.pad(dt_raw.astype(jnp.float32), ((0, 0), (pad, 0), (0, 0))) + dt_bias) * valid
    g, e = SSM_GROUPS, SSM_HEADS // SSM_GROUPS
    xs = xs.reshape(b, lp, g, e, SSM_HEAD_DIM)
    A = -jnp.exp(a_log.astype(jnp.float32)).reshape(g, e)
    y = ssd_chunked(xs, dt.reshape(b, lp, g, e), A,
                    bm.reshape(b, lp, g, SSM_STATE), cm.reshape(b, lp, g, SSM_STATE))
    y = (y + d_skip.reshape(g, e, 1) * xs)[:, pad:].reshape(b, l, g, e * SSM_HEAD_DIM)
    y = y * jax.nn.silu(z.astype(jnp.float32)).reshape(b, l, g, e * SSM_HEAD_DIM)
    y = y * lax.rsqrt(jnp.mean(y * y, -1, keepdims=True) + LN_EPS) * norm_w.reshape(g, -1)
    return y.reshape(b, l, SSM_INNER).astype(z.dtype)


def rwkv7_recurrence(r, decay, k, v, kk, a):
    def step(S, inp):
        r_t, w_t, k_t, v_t, kk_t, a_t = inp
        sa = jnp.einsum('bhvk,bhk->bhv', S, kk_t)
        S = S * w_t[:, :, None, :] - sa[..., None] * (kk_t * a_t)[:, :, None, :] + v_t[..., None] * k_t[:, :, None, :]
        return S, jnp.einsum('bhvk,bhk->bhv', S, r_t)

    b, l, h, n = r.shape
    seq = tuple(jnp.moveaxis(t, 1, 0) for t in (r, decay, k, v, kk, a))
    _, out = lax.scan(step, jnp.zeros((b, h, n, n), jnp.float32), seq)
    return jnp.moveaxis(out, 0, 1)


def rwkv7_branch(cols, shift_mu, w0, w_up, a0, a_up, g_up, k_k, k_a, r_k, lnx_g, lnx_b):
    b, l, _ = cols.shape
    hs = (RWKV_HEADS, RWKV_HEAD_DIM)
    prev = jnp.pad(cols, ((0, 0), (1, 0), (0, 0)))[:, :l]
    mixed = (cols + (prev - cols) * shift_mu).astype(jnp.float32)
    s1, s2, s3 = RWKV_DIM, 2 * RWKV_DIM, 3 * RWKV_DIM
    s4 = s3 + RWKV_DECAY_LORA
    s5 = s4 + RWKV_AAA_LORA
    r, k, v, w_lo, a_lo, g_lo = jnp.split(mixed, [s1, s2, s3, s4, s5], axis=-1)
    w = -jax.nn.softplus(-(w0 + jnp.tanh(w_lo) @ w_up)) - 0.5
    decay = jnp.exp(-jnp.exp(w))
    a = jax.nn.sigmoid(a0 + a_lo @ a_up)
    gate = jax.nn.sigmoid(g_lo) @ g_up
    r, k, v, decay, a = (t.reshape(b, l, *hs) for t in (r, k, v, decay, a))
    kk = k * k_k.reshape(hs)
    kk = kk * lax.rsqrt(jnp.maximum(jnp.sum(kk * kk, -1, keepdims=True), 1e-24))
    k = k * (1.0 + (a - 1.0) * k_a.reshape(hs))
    o = rwkv7_recurrence(r, decay, k, v, kk, a)
    mu = jnp.mean(o, -1, keepdims=True)
    var = jnp.mean(jnp.square(o - mu), -1, keepdims=True)
    o = (o - mu) * lax.rsqrt(var + RWKV_LN_EPS) * lnx_g.reshape(hs) + lnx_b.reshape(hs)
    o = o + jnp.sum(r * k * r_k, -1, keepdims=True) * v
    return (o.reshape(b, l, RWKV_DIM) * gate).astype(cols.dtype)


def ssd_rwkv_mixer(h, w_in, conv_w, conv_b, dt_bias, a_log, d_skip, ssm_norm, shift_mu, w0, w_up,
                   a0, a_up, g_up, k_k, k_a, r_k, lnx_g, lnx_b, w_out):
    proj = h @ w_in
    z, xbc, dt_raw, cols = jnp.split(
        proj, [SSM_INNER, SSM_INNER + SSM_CONV_DIM, SSM_INNER + SSM_CONV_DIM + SSM_HEADS], axis=-1)
    y_a = ssd_branch(z, xbc, dt_raw, conv_w, conv_b, dt_bias, a_log, d_skip, ssm_norm)
    y_b = rwkv7_branch(cols, shift_mu, w0, w_up, a0, a_up, g_up, k_k, k_a, r_k, lnx_g, lnx_b)
    return jnp.concatenate([y_a, y_b], axis=-1) @ w_out


def rglru_block(h, w_in, conv_w, conv_b, gx_w, gx_b, ga_w, ga_b, lam, w_out):
    b, l, _ = h.shape
    gate_branch, xr = jnp.split(h @ w_in, 2, axis=-1)
    xf = causal_dwconv(xr, conv_w, conv_b).astype(jnp.float32)
    xb = xf.reshape(b, l, LRU_BLOCKS, LRU_BLOCK)
    gate_x = jax.nn.sigmoid(jnp.einsum('blhi,hij->blhj', xb, gx_w).reshape(b, l, LRU_DIM) + gx_b)
    gate_a = jax.nn.sigmoid(jnp.einsum('blhi,hij->blhj', xb, ga_w).reshape(b, l, LRU_DIM) + ga_b)
    log_a = -LRU_C * gate_a * jax.nn.softplus(-lam)
    a = jnp.exp(log_a)
    u = jnp.sqrt(-jnp.expm1(2.0 * log_a)) * (gate_x * xf)

    def combine(lhs, rhs):
        a1, b1 = lhs
        a2, b2 = rhs
        return a1 * a2, a2 * b1 + b2

    _, hs = lax.associative_scan(combine, (a, u), axis=1)
    y = hs.astype(h.dtype) * jax.nn.gelu(gate_branch)
    return y @ w_out


def moe_swiglu(x, w_router, w_gu, w_down):
    b, l, d = x.shape
    n_tok = b * l
    n_slot = n_tok * TOP_K
    xt = x.reshape(n_tok, d)
    logits = (xt @ w_router).astype(jnp.float32)
    top_logit, top_idx = lax.top_k(logits, TOP_K)
    top_gate = jax.nn.softmax(top_logit, axis=-1)
    slot_e = top_idx.reshape(n_slot)
    order = jnp.argsort(slot_e, stable=True)
    sorted_e = slot_e[order]
    sorted_tok = order // TOP_K
    sorted_gate = top_gate.reshape(n_slot)[order]
    counts = jnp.bincount(slot_e, length=N_EXPERTS)
    starts = jnp.cumsum(counts) - counts
    padded = (counts + MOE_BLOCK - 1) // MOE_BLOCK * MOE_BLOCK
    padded_end = jnp.cumsum(padded)
    dest = (padded_end - padded)[sorted_e] + jnp.arange(n_slot) - starts[sorted_e]
    n_blocks = -(-(n_slot + N_EXPERTS * (MOE_BLOCK - 1)) // MOE_BLOCK)
    buf = jnp.zeros((n_blocks * MOE_BLOCK, d), x.dtype).at[dest].set(xt[sorted_tok])
    block_e = jnp.minimum(jnp.searchsorted(padded_end, jnp.arange(n_blocks) * MOE_BLOCK, side='right'),
                          N_EXPERTS - 1)

    def expert_block(args):
        xb, e = args
        return swiglu(xb, w_gu[e], w_down[e])

    out = lax.map(expert_block, (buf.reshape(n_blocks, MOE_BLOCK, d), block_e)).reshape(n_blocks * MOE_BLOCK, d)
    y = jnp.zeros((n_tok, d), x.dtype).at[sorted_tok].add(out[dest] * sorted_gate[:, None].astype(x.dtype))
    return y.reshape(b, l, d)


def setup_inputs(seed: int = 0) -> dict:
    key = jax.random.key(seed)
    keys = jax.random.split(key, 64)
    counter = [0]
    f32 = jnp.float32

    def nxt():
        k = keys[counter[0]]
        counter[0] += 1
        return k

    def nrm(shape, scale):
        return jax.random.normal(nxt(), shape, f32) * scale

    def unif(shape, lo, hi):
        return jax.random.uniform(nxt(), shape, f32, lo, hi)

    def gain(shape):
        return 1.0 + nrm(shape, 0.05)

    NE, NO = N_EVEN, N_ODD
    dt0 = jnp.exp(unif((NE, SSM_HEADS), math.log(1e-3), math.log(1e-1)))
    a_root = unif((NO, LRU_DIM), 0.9, 0.999) ** (1.0 / LRU_C)
    return {
        'x': nrm((BATCH, SEQ, D_MODEL), 1.0),
        'meta': nrm((N_META, D_MODEL), 1.0),
        'ev_w_in': nrm((NE, D_MODEL, IN0), D_MODEL ** -0.5),
        'ev_conv_w': nrm((NE, SSM_CONV, SSM_CONV_DIM), 0.5),
        'ev_conv_b': nrm((NE, SSM_CONV_DIM), 0.1),
        'ev_dt_bias': dt0 + jnp.log(-jnp.expm1(-dt0)),
        'ev_a_log': jnp.log(unif((NE, SSM_HEADS), 1.0, 16.0)),
        'ev_d_skip': 1.0 + nrm((NE, SSM_HEADS), 0.1),
        'ev_ssm_norm': gain((NE, SSM_INNER)),
        'ev_shift_mu': unif((NE, RWKV_COLS), 0.0, 1.0),
        'ev_w0': unif((NE, RWKV_DIM), -6.0, -1.0),
        'ev_w_up': nrm((NE, RWKV_DECAY_LORA, RWKV_DIM), 0.5 * RWKV_DECAY_LORA ** -0.5),
        'ev_a0': nrm((NE, RWKV_DIM), 0.5),
        'ev_a_up': nrm((NE, RWKV_AAA_LORA, RWKV_DIM), 0.5 * RWKV_AAA_LORA ** -0.5),
        'ev_g_up': nrm((NE, RWKV_GATE_LORA, RWKV_DIM), RWKV_GATE_LORA ** -0.5),
        'ev_k_k': 0.85 + nrm((NE, RWKV_DIM), 0.05),
        'ev_k_a': gain((NE, RWKV_DIM)),
        'ev_r_k': nrm((NE, RWKV_HEADS, RWKV_HEAD_DIM), 0.1),
        'ev_lnx_g': gain((NE, RWKV_DIM)),
        'ev_lnx_b': nrm((NE, RWKV_DIM), 0.02),
        'ev_w_out': nrm((NE, MIX0, D_MODEL), DEEPNORM_BETA * MIX0 ** -0.5),
        'ev_ln1_g': gain((NE, D_MODEL)),
        'ev_ln1_b': nrm((NE, D_MODEL), 0.02),
        'ev_ffn_w_gu': nrm((NE, D_MODEL, 2 * D_FF), D_MODEL ** -0.5),
        'ev_ffn_w_down': nrm((NE, D_FF, D_MODEL), DEEPNORM_BETA * D_FF ** -0.5),
        'ev_ln2_g': gain((NE, D_MODEL)),
        'ev_ln2_b': nrm((NE, D_MODEL), 0.02),
        'od_w_in': nrm((NO, D_MODEL, 2 * LRU_DIM), D_MODEL ** -0.5),
        'od_conv_w': nrm((NO, LRU_CONV, LRU_DIM), 0.5),
        'od_conv_b': nrm((NO, LRU_DIM), 0.1),
        'od_gx_w': nrm((NO, LRU_BLOCKS, LRU_BLOCK, LRU_BLOCK), LRU_BLOCK ** -0.5),
        'od_gx_b': nrm((NO, LRU_DIM), 0.1),
        'od_ga_w': nrm((NO, LRU_BLOCKS, LRU_BLOCK, LRU_BLOCK), LRU_BLOCK ** -0.5),
        'od_ga_b': nrm((NO, LRU_DIM), 0.1),
        'od_lambda': jnp.log(a_root) - jnp.log1p(-a_root),
        'od_w_out': nrm((NO, LRU_DIM, D_MODEL), DEEPNORM_BETA * LRU_DIM ** -0.5),
        'od_ln1_g': gain((NO, D_MODEL)),
        'od_ln1_b': nrm((NO, D_MODEL), 0.02),
        'od_router': nrm((NO, D_MODEL, N_EXPERTS), D_MODEL ** -0.5),
        'od_exp_w_gu': nrm((NO, N_EXPERTS, D_MODEL, 2 * D_FF), D_MODEL ** -0.5),
        'od_exp_w_down': nrm((NO, N_EXPERTS, D_FF, D_MODEL), DEEPNORM_BETA * D_FF ** -0.5),
        'od_ln2_g': gain((NO, D_MODEL)),
        'od_ln2_b': nrm((NO, D_MODEL), 0.02),
    }


def reference(x, meta, ev_w_in, ev_conv_w, ev_conv_b, ev_dt_bias, ev_a_log, ev_d_skip, ev_ssm_norm,
              ev_shift_mu, ev_w0, ev_w_up, ev_a0, ev_a_up, ev_g_up, ev_k_k, ev_k_a, ev_r_k, ev_lnx_g,
              ev_lnx_b, ev_w_out, ev_ln1_g, ev_ln1_b, ev_ffn_w_gu, ev_ffn_w_down, ev_ln2_g, ev_ln2_b,
              od_w_in, od_conv_w, od_conv_b, od_gx_w, od_gx_b, od_ga_w, od_ga_b, od_lambda, od_w_out,
              od_ln1_g, od_ln1_b, od_router, od_exp_w_gu, od_exp_w_down, od_ln2_g, od_ln2_b):
    b = x.shape[0]
    h = jnp.concatenate([jnp.broadcast_to(meta.astype(x.dtype)[None], (b, N_META, D_MODEL)), x], axis=1)
    for layer in range(DEPTH):
        i = layer // 2
        if layer % 2 == 0:
            mix = ssd_rwkv_mixer(h, ev_w_in[i], ev_conv_w[i], ev_conv_b[i], ev_dt_bias[i], ev_a_log[i],
                                 ev_d_skip[i], ev_ssm_norm[i], ev_shift_mu[i], ev_w0[i], ev_w_up[i],
                                 ev_a0[i], ev_a_up[i], ev_g_up[i], ev_k_k[i], ev_k_a[i], ev_r_k[i],
                                 ev_lnx_g[i], ev_lnx_b[i], ev_w_out[i])
            h = layer_norm(DEEPNORM_ALPHA * h + mix, ev_ln1_g[i], ev_ln1_b[i])
            h = layer_norm(DEEPNORM_ALPHA * h + swiglu(h, ev_ffn_w_gu[i], ev_ffn_w_down[i]),
                           ev_ln2_g[i], ev_ln2_b[i])
        else:
            mix = rglru_block(h, od_w_in[i], od_conv_w[i], od_conv_b[i], od_gx_w[i], od_gx_b[i],
                              od_ga_w[i], od_ga_b[i], od_lambda[i], od_w_out[i])
            h = layer_norm(DEEPNORM_ALPHA * h + mix, od_ln1_g[i], od_ln1_b[i])
            h = layer_norm(DEEPNORM_ALPHA * h + moe_swiglu(h, od_router[i], od_exp_w_gu[i], od_exp_w_down[i]),
                           od_ln2_g[i], od_ln2_b[i])
    return h[:, N_META:]
```

```python
from contextlib import ExitStack
import concourse.bass as bass
import concourse.mybir as mybir

F32 = mybir.dt.float32
BF16 = mybir.dt.bfloat16
AF = mybir.ActivationFunctionType
OP = mybir.AluOpType
ENGS = ["pe", "dve", "act", "pool", "sp"]
NDMA = 40


class KB:
    def __init__(self, nc, es: ExitStack):
        self.nc = nc
        self.es = es
        self.ops = {e: [] for e in ENGS}
        self.cnt = {e: 0 for e in ENGS}
        self.waited = {e: {} for e in ENGS}
        self.last_w = {}
        self.readers = {}
        self.sem = {e: es.enter_context(nc.semaphore("s_" + e)) for e in ENGS}
        self.dsem = [es.enter_context(nc.semaphore("d%d" % i)) for i in range(NDMA)]
        self.dval = [0] * NDMA
        self.dnext = 0
        self.semid = {}
        for e in ENGS:
            self.semid[("e", e)] = self.sem[e]
        for i in range(NDMA):
            self.semid[("d", i)] = self.dsem[i]
        self.ntile = 0
        self.psb = [self.psum_t("psb%d" % i, [128, 512], F32) for i in range(8)]
        self.psi = 0
        self.final_tokens = []

    def sb(self, name, shape, dt=F32):
        self.ntile += 1
        es = self.pes if getattr(self, "pes", None) is not None else self.es
        return es.enter_context(self.nc.sbuf_tensor(name, list(shape), dt))

    def barrier(self):
        allw = []
        for i in range(NDMA):
            if self.dval[i] > 0:
                allw.append((("d", i), self.dval[i]))
        for e in ENGS:
            if self.cnt[e] > 0:
                allw.append((("e", e), self.cnt[e]))
        semid = self.semid
        for e in ENGS:
            waits = self._waits(e, allw, skip_self=True)

            def run(h, waits=waits):
                for s, v in waits:
                    h.wait_ge(semid[s], v)

            self.ops[e].append(run)

    def psum_t(self, name, shape, dt=F32):
        return self.es.enter_context(self.nc.psum_tensor(name, list(shape), dt))

    def ps(self):
        i = self.psi
        self.psi = (self.psi + 1) % 8
        return self.psb[i], "psb%d" % i

    def _deps(self, reads, writes):
        deps = []
        for k in reads:
            if k in self.last_w:
                deps.append(self.last_w[k])
            if isinstance(k, str) and k.startswith("psb"):
                deps.extend(self.readers.get(k, {}).values())
        for k in writes:
            if k in self.last_w:
                deps.append(self.last_w[k])
            deps.extend(self.readers.get(k, {}).values())
        return deps

    def _waits(self, eng, deps, skip_self=False):
        w = {}
        for (s, v) in deps:
            if skip_self and s == ("e", eng):
                continue
            if self.waited[eng].get(s, 0) < v and w.get(s, 0) < v:
                w[s] = v
        for s, v in w.items():
            self.waited[eng][s] = v
        return list(w.items())

    def _commit(self, tok, reads, writes):
        for k in writes:
            self.last_w[k] = tok
            self.readers[k] = {}
        for k in reads:
            if k in writes:
                continue
            r = self.readers.setdefault(k, {})
            if r.get(tok[0], (None, 0))[1] < tok[1]:
                r[tok[0]] = tok

    def op(self, eng, fn, reads=(), writes=()):
        deps = self._deps(reads, writes)
        waits = self._waits(eng, deps, skip_self=(eng == "pe"))
        self.cnt[eng] += 1
        tok = (("e", eng), self.cnt[eng])
        semid = self.semid
        mysem = self.sem[eng]

        def run(h):
            for s, v in waits:
                h.wait_ge(semid[s], v)
            fn(h).then_inc(mysem, 1)

        self.ops[eng].append(run)
        self._commit(tok, reads, writes)
        return tok

    def dma(self, q, out, in_, reads=(), writes=(), **kw):
        slot = self.dnext
        self.dnext = (self.dnext + 1) % NDMA
        deps = self._deps(reads, writes)
        if self.dval[slot] > 0:
            deps.append((("d", slot), self.dval[slot]))
        waits = self._waits(q, deps)
        self.dval[slot] += 16
        tok = (("d", slot), self.dval[slot])
        semid = self.semid
        ds = self.dsem[slot]

        def run(h):
            for s, v in waits:
                h.wait_ge(semid[s], v)
            h.dma_start(out=out, in_=in_, **kw).then_inc(ds, 16)

        self.ops[q].append(run)
        self._commit(tok, reads, writes)
        return tok

    def finish(self, eng="sp"):
        waits = []
        for i in range(NDMA):
            if self.dval[i] > 0:
                waits.append((("d", i), self.dval[i]))
        for e in ENGS:
            if self.cnt[e] > 0:
                waits.append((("e", e), self.cnt[e]))
        semid = self.semid

        def run(h):
            for s, v in waits:
                h.wait_ge(semid[s], v)

        self.ops[eng].append(run)

    def emit(self):
        nc = self.nc
        with nc.Block() as block:
            @block.tensor
            def _(h):
                for f in self.ops["pe"]:
                    f(h)

            @block.vector
            def _(h):
                for f in self.ops["dve"]:
                    f(h)

            @block.scalar
            def _(h):
                for f in self.ops["act"]:
                    f(h)

            @block.gpsimd
            def _(h):
                for f in self.ops["pool"]:
                    f(h)

            @block.sync
            def _(h):
                for f in self.ops["sp"]:
                    f(h)

    def mm(self, out, lhsT, rhs, start, stop, reads, writes):
        return self.op("pe", lambda h: h.matmul(out, lhsT, rhs, start=start, stop=stop), reads, writes)

    def tr(self, out, in_, ident, reads, writes):
        return self.op("pe", lambda h: h.transpose(out, in_, ident), reads, writes)

    def act(self, out, in_, func, reads, writes, bias=None, scale=None, eng="act"):
        kw = {}
        if bias is not None:
            kw["bias"] = bias
        if scale is not None:
            kw["scale"] = scale
        return self.op("act", lambda h: h.activation(out=out, in_=in_, func=func, **kw), reads, writes)

    def tt(self, out, in0, in1, op, reads, writes, eng="dve"):
        return self.op(eng, lambda h: h.tensor_tensor(out=out, in0=in0, in1=in1, op=op), reads, writes)

    def ts(self, out, in0, s1, op0, reads, writes, s2=None, op1=None, eng="dve"):
        if op1 is None:
            return self.op(eng, lambda h: h.tensor_scalar(out=out, in0=in0, scalar1=s1, scalar2=None, op0=op0), reads, writes)
        return self.op(eng, lambda h: h.tensor_scalar(out=out, in0=in0, scalar1=s1, scalar2=s2, op0=op0, op1=op1), reads, writes)

    def stt(self, out, in0, scalar, in1, op0, op1, reads, writes):
        return self.op("dve", lambda h: h.scalar_tensor_tensor(out=out, in0=in0, scalar=scalar, in1=in1, op0=op0, op1=op1), reads, writes)

    def copy(self, out, in_, reads, writes, eng="dve"):
        if eng == "act":
            return self.op("act", lambda h: h.copy(out=out, in_=in_), reads, writes)
        return self.op(eng, lambda h: h.tensor_copy(out=out, in_=in_), reads, writes)

    def memset(self, ap, val, writes, eng="dve"):
        return self.op(eng, lambda h: h.memset(ap, val), (), writes)
import numpy as np
from contextlib import ExitStack
import concourse.bass as bass
import concourse.mybir as mybir
from concourse.bass_utils import run_bass_kernel_spmd

D = 1024
NMETA = 16
DFF = 2816
NEXP = 8
ALPHA = 4 ** 0.25
LN_EPS = 1e-5
RWKV_EPS = 64e-5

C_XS, C_B, C_C, C_R, C_K, C_V, C_LO, C_G = 0, 8, 10, 12, 20, 28, 36, 37
NFC = 38


def supertiles(LP):
    out = []
    t = 0
    while t < LP:
        n = min(512, LP - t)
        out.append((t, n))
        t += n
    return out


class Prog:
    def __init__(self, LP, dbg=()):
        self.LP = LP
        self.dbg = set(dbg)
        self.nc = bass.Bass("TRN2", target_bir_lowering=False)
        self.es = ExitStack()
        self.k = None
        self.dram = {}

    def din(self, name, shape, dt=F32):
        t = self.nc.dram_tensor(name, list(shape), dt, kind="ExternalInput").ap()
        self.dram[name] = t
        return t

    def dscr(self, name, shape, dt=F32):
        kind = "ExternalOutput" if name in self.dbg else "Internal"
        t = self.nc.dram_tensor(name, list(shape), dt, kind=kind).ap()
        self.dram[name] = t
        return t

    def dout(self, name, shape, dt=F32):
        t = self.nc.dram_tensor(name, list(shape), dt, kind="ExternalOutput").ap()
        self.dram[name] = t
        return t


def cast_weights(P, src, dst, nrows, key):
    k = P.k
    step = 512
    for r0 in range(0, nrows, step):
        r1 = min(nrows, r0 + step)
        k.dma("pool", dst[r0:r1, :], src[r0:r1, :], reads=(), writes=((key, r0 // step),))


def phase1(P):
    k, LP = P.k, P.LP
    d = P.dram
    xt = [k.sb("xt%d" % i, [128, 1024]) for i in range(4)]
    hT = k.sb("hT", [128, 8, 512], BF16)
    wf = [k.sb("wf%d" % i, [128, 8, 128], BF16) for i in range(3)]
    wz = k.sb("wz", [128, 8, 1040], BF16)
    stg = [k.sb("stg%d" % i, [128, 512]) for i in range(3)]
    zst = [k.sb("zst%d" % i, [128, 1040]) for i in range(2)]
    ident = P.ident
    k.dma("sp", wz[:].rearrange("p a b -> p (a b)"), d["WINZb"][:, :], reads=[("WINZb", i) for i in range(1)], writes=["wz"])
    wi = 0
    si = 0
    zi = 0
    for (t0, n) in supertiles(LP):
        nj = n // 128
        for j in range(nj):
            k.dma("sp", xt[j][:], d["xin"][t0 + j * 128:t0 + (j + 1) * 128, :], reads=[], writes=["xt%d" % j])
        for kc in range(8):
            ps, pk = k.ps()
            for j in range(nj):
                k.tr(ps[:, j * 128:(j + 1) * 128], xt[j][:, kc * 128:(kc + 1) * 128], ident[:], reads=["xt%d" % j, "ident"], writes=[pk])
            k.copy(hT[:, kc, :n], ps[:, :n], reads=[pk], writes=[("hT", kc)], eng=("act" if kc % 2 else "dve"))
        for c in range(NFC):
            w = wf[wi % 3]
            wk = "wf%d" % (wi % 3)
            wi += 1
            k.dma("sp", w[:].rearrange("p a b -> p (a b)"), d["WINFb"][c * 128:(c + 1) * 128, :], reads=[("WINFb", (c * 128) // 512)], writes=[wk])
            ps, pk = k.ps()
            for kc in range(8):
                k.mm(ps[:, :n], w[:, kc, :], hT[:, kc, :n], kc == 0, kc == 7, reads=[wk, ("hT", kc)], writes=[pk])
            s = stg[si % 3]
            sk = "stg%d" % (si % 3)
            si += 1
            k.copy(s[:, :n], ps[:, :n], reads=[pk], writes=[sk], eng=("act" if c % 2 else "dve"))
            k.dma("pool", d["PT"][c * 128:(c + 1) * 128, t0:t0 + n], s[:, :n], reads=[sk], writes=[("PT", c)])
        for j in range(nj):
            z = zst[zi % 2]
            zk = "zst%d" % (zi % 2)
            zi += 1
            for (c0, c1) in ((0, 512), (512, 1024), (1024, 1040)):
                ps, pk = k.ps()
                for kc in range(8):
                    k.mm(ps[:, :c1 - c0], hT[:, kc, j * 128:(j + 1) * 128], wz[:, kc, c0:c1], kc == 0, kc == 7,
                         reads=["wz", ("hT", kc)], writes=[pk])
                k.copy(z[:, c0:c1], ps[:, :c1 - c0], reads=[pk], writes=[zk], eng=("act" if c0 == 512 else "dve"))
            k.dma("pool", d["Z"][t0 + j * 128:t0 + (j + 1) * 128, :], z[:], reads=[zk], writes=[("Z", 0)])


import os
LVL = int(os.environ.get('LVL', '99'))
R3 = int(os.environ.get('R3', '99'))
R3C = int(os.environ.get('R3C', '99'))
R3L = int(os.environ.get('R3L', '99'))
NVT = 32 + 2048
NVF = 64


def build(LP, dbg=(), upto=99):
    P = Prog(LP, dbg)
    nc = P.nc
    P.din("xin", [LP, D])
    P.din("CMd", [128, 640])
    P.din("VTd", [1, NVT])
    P.din("VFd", [128, NVF])
    P.din("WINF", [NFC * 128, 1024])
    P.din("WINZ", [128, 8 * 1040])
    P.din("CRd", [128, NCR])
    P.din("VRd", [128, NVR])
    P.din("WUAd", [128, 1024])
    P.din("GUPd", [128, 1024])
    P.din("LNGd", [128, 512])
    P.din("LNBd", [128, 512])
    P.din("VT2d", [1, 8192])
    P.din("WO", [128, 16 * 1024])
    P.din("WGU0", [44 * 128, 1024])
    P.din("WD0", [128, 22 * 1024])
    P.din("W1IN", [16 * 128, 1024])
    P.din("GWd", [128, 16 * 128])
    P.din("VLd", [128, NVL])
    P.din("W1O", [128, 8 * 1024])
    P.din("WRd", [128, 64])
    for e in range(NEXP):
        P.din("WGUE%d" % e, [44 * 128, 1024])
        P.din("WDE%d" % e, [128, 22 * 1024])
    P.dscr("WOb", [128, 16 * 1024], BF16)
    P.dscr("WGU0b", [44 * 128, 1024], BF16)
    P.dscr("WD0b", [128, 22 * 1024], BF16)
    P.dscr("W1INb", [16 * 128, 1024], BF16)
    P.dscr("W1Ob", [128, 8 * 1024], BF16)
    for e in range(NEXP):
        P.dscr("WGUEb%d" % e, [44 * 128, 1024], BF16)
        P.dscr("WDEb%d" % e, [128, 22 * 1024], BF16)
    P.dscr("H1", [LP, D])
    P.dscr("H1T", [D, LP], BF16)
    P.dscr("H2", [LP, D])
    P.dscr("H2T", [D, LP], BF16)
    P.dscr("XRT", [D, LP])
    P.dscr("Y1T", [D, LP], BF16)
    P.dscr("H3", [LP, D])
    P.dscr("H3T", [D, LP], BF16)
    P.dscr("H3T32", [D, LP])
    P.dout("OUT", [LP, D])
    P.dscr("WINFb", [NFC * 128, 1024], BF16)
    P.dscr("WINZb", [128, 8 * 1040], BF16)
    P.dscr("PT", [NFC * 128, LP])
    P.dscr("Z", [LP, 1040])
    P.dscr("XS", [LP, 1024])
    P.dscr("BTOK", [LP, 256])
    P.dscr("BT", [256, LP])
    P.dscr("CT", [256, LP])
    P.dscr("YT", [2048, LP], BF16)
    with P.es:
        k = KB(nc, P.es)
        P.k = k
        P.CM = k.sb("CM", [128, 640])
        P.ident = P.CM[:, 0:128]
        P.VT = k.sb("VT", [128, NVT])
        P.VF = k.sb("VF", [128, NVF])
        k.dma("sp", P.CM[:], P.dram["CMd"][:, :], writes=["CM", "ident"])
        k.dma("sp", P.VT[:], P.dram["VTd"][0:1, :].partition_broadcast(128), writes=["VT"])
        k.dma("sp", P.VF[:], P.dram["VFd"][:, :], writes=["VF"])
        cast_weights(P, P.dram["WINF"], P.dram["WINFb"], NFC * 128, "WINFb")
        k.dma("pool", P.dram["WINZb"][:, :].rearrange("p (a b) -> p a b", b=1040), P.dram["WINZ"][:, :].rearrange("p (a b) -> p a b", b=1040), writes=[("WINZb", 0)])
        if upto >= 1:
            with ExitStack() as pes:
                k.pes = pes
                phase1(P)
            k.pes = None
            k.barrier()
        if upto >= 1.5:
            phase2a(P)
        if upto >= 2 and upto != 3.5:
            phase2b(P)
        if upto >= 3:
            phase3(P)
        if upto >= 4:
            def cast_rhs(src, dst, nk):
                k.dma("pool", P.dram[dst][:, :].rearrange("p (a b) -> p a b", b=1024), P.dram[src][:, :].rearrange("p (a b) -> p a b", b=1024), writes=[(dst, 0)])
            cast_rhs("WO", "WOb", 16)
            cast_weights(P, P.dram["WGU0"], P.dram["WGU0b"], 44 * 128, "WGU0b")
            cast_rhs("WD0", "WD0b", 22)
            cast_weights(P, P.dram["W1IN"], P.dram["W1INb"], 16 * 128, "W1INb")
            cast_rhs("W1O", "W1Ob", 8)
            for e in range(NEXP):
                cast_weights(P, P.dram["WGUE%d" % e], P.dram["WGUEb%d" % e], 44 * 128, "WGUEb%d" % e)
                cast_rhs("WDE%d" % e, "WDEb%d" % e, 22)
            proj_res_ln(P, "p4", "YT", 16, "WOb", "xin", 0, 1024, "H1", "H1T")
            ffn_phase(P, "f0", "H1T", "H1", ["WGU0b"], ["WD0b"], 2048, 3072, "H2", "H2T")
        if upto >= 5:
            phase5(P)
            proj_res_ln(P, "p5", "Y1T", 8, "W1Ob", "H2", 4096, 5120, "H3", "H3T", "H3T32")
        if upto >= 6:
            ffn_phase(P, "f1", "H3T", "H3", ["WGUEb%d" % e for e in range(NEXP)], ["WDEb%d" % e for e in range(NEXP)], 6144, 7168, "OUT", None, router="H3T32")
        k.finish("sp")
        k.emit()
    return P


def host_consts(inp):
    t = np.arange(128)
    CM = np.zeros((128, 640), np.float32)
    CM[:, 0:128] = np.eye(128)
    CM[:, 128:256] = (t[:, None] <= t[None, :])
    CM[:, 256:384] = (t[:, None] > t[None, :])
    CM[:, 384:512] = 1.0
    CM[:, 512:640] = np.where(t[None, :] >= t[:, None], 0.0, -30000.0)
    VT = np.zeros((1, NVT), np.float32)
    VT[0, 0:16] = inp["ev_dt_bias"][0]
    VT[0, 16:32] = inp["ev_a_log"][0]
    VT[0, 32:32 + 1024] = np.repeat(inp["ev_d_skip"][0], 64)
    VT[0, 32 + 1024:32 + 2048] = inp["ev_ssm_norm"][0]
    VF = np.zeros((128, NVF), np.float32)
    cw = inp["ev_conv_w"][0]
    VF[:, 0:48] = cw.reshape(4, 12, 128).transpose(2, 1, 0).reshape(128, 48)
    VF[:, 48:60] = inp["ev_conv_b"][0].reshape(12, 128).T
    return CM, VT, VF


def fcols():
    return np.concatenate([np.arange(1024, 2048), np.arange(2048, 2304), np.arange(2304, 2560),
                           np.arange(2576, 3600), np.arange(3600, 4624), np.arange(4624, 5648),
                           np.arange(5648, 5776), np.arange(5776, 5904)])


def zcols():
    return np.concatenate([np.arange(0, 1024), np.arange(2560, 2576)])


def pack_lhsT(w, cols):
    ws = w[:, cols]
    nch = ws.shape[1] // 128
    a = ws.reshape(8, 128, nch, 128)
    return np.ascontiguousarray(a.transpose(2, 1, 0, 3)).reshape(nch * 128, 1024)


def pack_rhs(w, cols):
    ws = w[:, cols]
    n = ws.shape[1]
    return np.ascontiguousarray(ws.reshape(8, 128, n).transpose(1, 0, 2)).reshape(128, 8 * n)


CM_ID, CM_TRI, CM_TRIS, CM_ONES, CM_MB = 0, 128, 256, 384, 512
VT_DTB, VT_ALOG, VT_D, VT_NW = 0, 16, 32, 32 + 1024
VF_CW, VF_CB = 0, 48


def phase2a(P):
    k, LP, d = P.k, P.LP, P.dram
    with ExitStack() as pes:
        k.pes = pes
        raw = [k.sb("c_raw%d" % i, [128, LP + 3]) for i in range(2)]
        acc = [k.sb("c_acc%d" % i, [128, LP]) for i in range(2)]
        stg = [k.sb("c_stg%d" % i, [128, 512]) for i in range(3)]
        VF = P.VF
        for i in range(2):
            k.memset(raw[i][:, 0:3], 0.0, writes=["c_raw%d" % i])
        si = 0
        for c in range(12):
            r, rk = raw[c % 2], "c_raw%d" % (c % 2)
            a, ak = acc[c % 2], "c_acc%d" % (c % 2)
            k.dma("sp", r[:, 3:3 + LP], d["PT"][c * 128:(c + 1) * 128, :], reads=[("PT", c)], writes=[rk])
            cw = VF_CW + c * 4
            k.ts(a[:], r[:, 3:3 + LP], VF[:, cw + 3:cw + 4], OP.mult, reads=[rk, "VF"], writes=[ak])
            for kk in (2, 1, 0):
                k.stt(a[:], r[:, kk:kk + LP], VF[:, cw + kk:cw + kk + 1], a[:], OP.mult, OP.add, reads=[rk, ak, "VF"], writes=[ak])
            k.act(a[:], a[:], AF.Silu, reads=[ak, "VF"], writes=[ak], bias=VF[:, VF_CB + c:VF_CB + c + 1])
            if c >= 8:
                dst = d["BT"] if c < 10 else d["CT"]
                cc = (c - 8) % 2
                k.dma("pool", dst[cc * 128:(cc + 1) * 128, :], a[:], reads=[ak], writes=[("BT" if c < 10 else "CT", cc)])
            if c < 10:
                for (t0, n) in supertiles(LP):
                    nj = n // 128
                    ps, pk = k.ps()
                    for j in range(nj):
                        k.tr(ps[:, j * 128:(j + 1) * 128], a[:, t0 + j * 128:t0 + (j + 1) * 128], P.CM[:, CM_ID:CM_ID + 128], reads=[ak, "CM"], writes=[pk])
                    s, sk = stg[si % 3], "c_stg%d" % (si % 3)
                    si += 1
                    k.copy(s[:, :n], ps[:, :n], reads=[pk], writes=[sk], eng=("act" if si % 2 else "dve"))
                    if c < 8:
                        dd = d["XS"][t0:t0 + n, c * 128:(c + 1) * 128]
                        key = ("XS", c)
                    else:
                        dd = d["BTOK"][t0:t0 + n, (c - 8) * 128:(c - 7) * 128]
                        key = ("BTOK", c - 8)
                    k.dma("pool", dd.rearrange("(j p) m -> p j m", p=128), s[:, :n].rearrange("p (j m) -> p j m", m=128), reads=[sk], writes=[key])
    k.pes = None
    k.barrier()


def phase2b(P):
    k, LP, d = P.k, P.LP, P.dram
    CM, VT = P.CM, P.VT
    with ExitStack() as pes:
        k.pes = pes
        nb = 2
        xs = [k.sb("s_xs%d" % i, [128, 1024]) for i in range(nb)]
        btok = [k.sb("s_btok%d" % i, [128, 256]) for i in range(nb)]
        bt = [k.sb("s_bt%d" % i, [128, 2, 128]) for i in range(nb)]
        ct = [k.sb("s_ct%d" % i, [128, 2, 128]) for i in range(nb)]
        z = [k.sb("s_z%d" % i, [128, 1040]) for i in range(nb)]
        HT = [k.sb("s_HT%d" % g, [128, 512]) for g in range(2)]
        Abc = k.sb("s_Abc", [128, 16])
        dt = k.sb("s_dt", [128, 16])
        dtA = k.sb("s_dtA", [128, 16])
        E = k.sb("s_E", [128, 64])
        ncum = E[:, 48:64]
        LT = k.sb("s_LT", [128, 16, 128])
        SLT = k.sb("s_SLT", [128, 16, 128])
        xdt = k.sb("s_xdt", [128, 1024])
        xde = k.sb("s_xde", [128, 1024])
        y = k.sb("s_y", [128, 1024])
        t1 = k.sb("s_t1", [128, 1024])
        zs = k.sb("s_zs", [128, 1024])
        sq = k.sb("s_sq", [128, 512])
        ss = k.sb("s_ss", [128, 2])
        rstd = k.sb("s_rstd", [128, 2])
        yTs = [k.sb("s_yT%d" % i, [128, 8, 128], BF16) for i in range(2)]
        for g in range(2):
            k.memset(HT[g][:], 0.0, writes=["s_HT%d" % g])
        k.act(Abc[:], VT[:, VT_ALOG:VT_ALOG + 16], AF.Exp, reads=["VT"], writes=["s_Abc"])
        k.ts(Abc[:], Abc[:], -1.0, OP.mult, reads=["s_Abc"], writes=["s_Abc"])
        nch = LP // 128
        for ci in range(nch):
            t0 = ci * 128
            b = ci % nb
            kx, kb, kbt, kct, kz = "s_xs%d" % b, "s_btok%d" % b, "s_bt%d" % b, "s_ct%d" % b, "s_z%d" % b
            k.dma("sp", xs[b][:], d["XS"][t0:t0 + 128, :], reads=[("XS", c) for c in range(8)], writes=[kx])
            k.dma("sp", btok[b][:], d["BTOK"][t0:t0 + 128, :], reads=[("BTOK", 0), ("BTOK", 1)], writes=[kb])
            k.dma("sp", bt[b][:], d["BT"][:, t0:t0 + 128].rearrange("(g n) t -> n g t", n=128), reads=[("BT", 0), ("BT", 1)], writes=[kbt])
            k.dma("sp", ct[b][:], d["CT"][:, t0:t0 + 128].rearrange("(g n) t -> n g t", n=128), reads=[("CT", 0), ("CT", 1)], writes=[kct])
            k.dma("sp", z[b][:], d["Z"][t0:t0 + 128, :], reads=[("Z", 0)], writes=[kz])
            if LVL < 1: continue
            k.tt(dt[:], z[b][:, 1024:1040], VT[:, VT_DTB:VT_DTB + 16], OP.add, reads=[kz, "VT"], writes=["s_dt"])
            k.act(dt[:], dt[:], AF.Exp, reads=["s_dt"], writes=["s_dt"])
            k.act(dt[:], dt[:], AF.Ln, reads=["s_dt"], writes=["s_dt"], bias=1.0)
            k.tt(dtA[:], dt[:], Abc[:], OP.mult, reads=["s_dt", "s_Abc"], writes=["s_dtA"])
            if LVL < 2: continue
            psc, pkc = k.ps()
            k.mm(psc[:, 0:16], CM[:, CM_TRI:CM_TRI + 128], dtA[:], True, True, reads=["CM", "s_dtA"], writes=[pkc])
            k.mm(psc[:, 16:32], CM[:, CM_TRIS:CM_TRIS + 128], dtA[:], True, True, reads=["CM", "s_dtA"], writes=[pkc])
            k.mm(psc[:, 32:48], CM[:, CM_ONES:CM_ONES + 128], dtA[:], True, True, reads=["CM", "s_dtA"], writes=[pkc])
            SUB = int(os.environ.get('SUB', '3'))
            if SUB >= 1:
                k.act(E[:, 0:48], psc[:, 0:48], AF.Exp, reads=[pkc], writes=["s_E"])
            if SUB >= 2:
                k.ts(ncum, psc[:, 0:16], -1.0, OP.mult, reads=[pkc, "s_E"], writes=["s_ncum"])
            if LVL < 3: continue
            k.tt(xdt[:].rearrange("p (h e) -> p h e", e=64), xs[b][:].rearrange("p (h e) -> p h e", e=64),
                 dt[:].unsqueeze(2).broadcast_to([128, 16, 64]), OP.mult, reads=[kx, "s_dt"], writes=["s_xdt"])
            k.tt(xde[:].rearrange("p (h e) -> p h e", e=64), xdt[:].rearrange("p (h e) -> p h e", e=64),
                 E[:, 16:32].unsqueeze(2).broadcast_to([128, 16, 64]), OP.mult, reads=["s_xdt", "s_E"], writes=["s_xde"])
            if LVL < 4: continue
            for hq in range(4):
                psa, pka = k.ps()
                for hh in range(4):
                    h = hq * 4 + hh
                    k.mm(psa[:, hh * 128:(hh + 1) * 128], dtA[:, h:h + 1].broadcast_to([128, 128]), CM[:, CM_TRI:CM_TRI + 128], True, True,
                         reads=["s_dtA", "CM"], writes=[pka])
                for hh in range(4):
                    h = hq * 4 + hh
                    k.stt(LT[:, h, :], psa[:, hh * 128:(hh + 1) * 128], E[:, 48 + h:49 + h], CM[:, CM_MB:CM_MB + 128], OP.add, OP.add,
                          reads=[pka, "s_ncum", "CM"], writes=[("s_LT", hq)])
            for hq in range(4):
                k.act(LT[:, hq * 4:(hq + 1) * 4, :], LT[:, hq * 4:(hq + 1) * 4, :], AF.Exp, reads=[("s_LT", hq)], writes=[("s_LT", hq)])
            if LVL < 5: continue
            pss, pks = k.ps()
            for g in range(2):
                k.mm(pss[:, g * 128:(g + 1) * 128], bt[b][:, g, :], ct[b][:, g, :], True, True, reads=[kbt, kct], writes=[pks])
            for g in range(2):
                k.tt(SLT[:, g * 8:(g + 1) * 8, :], LT[:, g * 8:(g + 1) * 8, :],
                     pss[:, g * 128:(g + 1) * 128].unsqueeze(1).broadcast_to([128, 8, 128]), OP.mult,
                     reads=[("s_LT", 2 * g), ("s_LT", 2 * g + 1), pks], writes=[("s_SLT", g)])
            if LVL < 6: continue
            for g in range(2):
                psy, pky = k.ps()
                for hh in range(8):
                    h = g * 8 + hh
                    k.mm(psy[:, hh * 64:(hh + 1) * 64], SLT[:, h, :], xdt[:, h * 64:(h + 1) * 64], True, True, reads=[("s_SLT", g), "s_xdt"], writes=[pky])
                if R3L < 4: continue
                pso, pko = k.ps()
                k.mm(pso[:, :], ct[b][:, g, :], HT[g][:], True, True, reads=[kct, "s_HT%d" % g], writes=[pko])
                gs = slice(g * 512, (g + 1) * 512)
                k.tt(t1[:, gs].rearrange("p (h e) -> p h e", e=64), pso[:, :].rearrange("p (h e) -> p h e", e=64),
                     E[:, g * 8:(g + 1) * 8].unsqueeze(2).broadcast_to([128, 8, 64]), OP.mult, reads=[pko, "s_E"], writes=[("s_t1", g)])
                k.tt(y[:, gs], psy[:, :], t1[:, gs], OP.add, reads=[pky, ("s_t1", g)], writes=[("s_y", g)])
                psh, pkh = k.ps()
                k.mm(psh[:, :], btok[b][:, g * 128:(g + 1) * 128], xde[:, gs], True, True, reads=[kb, "s_xde"], writes=[pkh])
                k.tt(HT[g][:].rearrange("p (h e) -> p h e", e=64), HT[g][:].rearrange("p (h e) -> p h e", e=64),
                     E[:, 32 + g * 8:32 + (g + 1) * 8].unsqueeze(2).broadcast_to([128, 8, 64]), OP.mult, reads=["s_HT%d" % g, "s_E"], writes=["s_HT%d" % g])
                k.tt(HT[g][:], HT[g][:], psh[:, :], OP.add, reads=["s_HT%d" % g, pkh], writes=["s_HT%d" % g])
            if LVL < 7: continue
            k.tt(t1[:], xs[b][:], VT[:, VT_D:VT_D + 1024], OP.mult, reads=[kx, "VT", ("s_t1", 0), ("s_t1", 1)], writes=[("s_t1", 0), ("s_t1", 1)], eng="pool")
            k.tt(y[:], y[:], t1[:], OP.add, reads=[("s_y", 0), ("s_y", 1), ("s_t1", 0), ("s_t1", 1)], writes=[("s_y", 0), ("s_y", 1)])
            k.act(zs[:], z[b][:, 0:1024], AF.Silu, reads=[kz], writes=["s_zs"])
            k.tt(y[:], y[:], zs[:], OP.mult, reads=[("s_y", 0), ("s_y", 1), "s_zs"], writes=[("s_y", 0), ("s_y", 1)])
            for g in range(2):
                gs = slice(g * 512, (g + 1) * 512)
                k.op("act", lambda h, g=g, gs=gs: h.activation(out=sq[:], in_=y[:, gs], func=AF.Square, accum_out=ss[:, g:g + 1]),
                     reads=[("s_y", 0), ("s_y", 1)], writes=["s_sq", ("s_ss", g)])
            k.act(rstd[:], ss[:], AF.Sqrt, reads=[("s_ss", 0), ("s_ss", 1)], writes=["s_rstd"], bias=LN_EPS, scale=1.0 / 512)
            k.op("dve", lambda h: h.reciprocal(out=rstd[:], in_=rstd[:]), reads=["s_rstd"], writes=["s_rstd"])
            for g in range(2):
                gs = slice(g * 512, (g + 1) * 512)
                k.stt(y[:, gs], y[:, gs], rstd[:, g:g + 1], VT[:, VT_NW + g * 512:VT_NW + (g + 1) * 512], OP.mult, OP.mult,
                      reads=[("s_y", g), "s_rstd", "VT"], writes=[("s_y", g)])
            if LVL < 8: continue
            yT, kyT = yTs[ci % 2], "s_yT%d" % (ci % 2)
            for half in range(2):
                pst, pkt = k.ps()
                for cc in range(4):
                    c = half * 4 + cc
                    k.tr(pst[:, cc * 128:(cc + 1) * 128], y[:, c * 128:(c + 1) * 128], CM[:, CM_ID:CM_ID + 128], reads=[("s_y", 0), ("s_y", 1), "CM"], writes=[pkt])
                k.copy(yT[:, half * 4:(half + 1) * 4, :], pst[:, :].rearrange("p (c t) -> p c t", t=128), reads=[pkt], writes=[kyT], eng=("act" if half else "dve"))
            k.dma("pool", d["YT"][0:1024, t0:t0 + 128].rearrange("(c p) t -> p c t", p=128), yT[:], reads=[kyT], writes=[("YT", 0)])
    k.pes = None
    k.barrier()


CR_M1, CR_M2, CR_SLN, CR_ISEL, CR_BD, CR_BONES, CR_RESET = 0, 256, 512, 640, 704, 832, 960
NCR = 960 + 512
VR_MUR, VR_MUK, VR_MUV, VR_W0, VR_A0, VR_KK, VR_KA, VR_RK, VR_MULO, VR_MUG = 0, 8, 16, 24, 32, 40, 48, 56, 64, 65
NVR = 66


def phase3(P):
    k, LP, d = P.k, P.LP, P.dram
    CM = P.CM
    with ExitStack() as pes:
        k.pes = pes
        CR = k.sb("CR", [128, NCR])
        VR = k.sb("VR", [128, NVR])
        OMKA = k.sb("OMKA", [128, 8])
        WUA = k.sb("WUA", [128, 1024])
        GUP = k.sb("GUP", [128, 1024])
        LNG = k.sb("LNG", [128, 512])
        LNB = k.sb("LNB", [128, 512])
        k.dma("sp", CR[:], d["CRd"][:, :], writes=["CR"])
        k.dma("sp", VR[:], d["VRd"][:, :], writes=["VR"])
        k.dma("sp", WUA[:], d["WUAd"][:, :], writes=["WUA"])
        k.dma("sp", GUP[:], d["GUPd"][:, :], writes=["GUP"])
        k.dma("sp", LNG[:], d["LNGd"][:, :], writes=["LNG"])
        k.dma("sp", LNB[:], d["LNBd"][:, :], writes=["LNB"])
        k.ts(OMKA[:], VR[:, VR_KA:VR_KA + 8], -1.0, OP.mult, reads=["VR"], writes=["OMKA"], s2=1.0, op1=OP.add)
        T = [k.sb("r_T%d" % j, [128, 64]) for j in range(8)]
        for j in range(8):
            k.memset(T[j][:], 0.0, writes=["r_T%d" % j])
        ident = CM[:, CM_ID:CM_ID + 128]
        MASK1 = CR[:, CR_M1:CR_M1 + 256]
        MASK2 = CR[:, CR_M2:CR_M2 + 256]
        MSLN = CR[:, CR_SLN:CR_SLN + 128]
        ISEL = CR[:, CR_ISEL:CR_ISEL + 64]
        BONES = CR[:, CR_BONES:CR_BONES + 128]

        def T_(name, shape, dt=F32):
            return k.sb(name, shape, dt), name

        lo_raw, klo_raw = T_("r_loraw", [128, 513])
        g_raw, kg_raw = T_("r_graw", [128, 513])
        LOm, kLOm = T_("r_LOm", [128, 512])
        Gs, kGs = T_("r_Gs", [128, 512])
        raw = {nm: [T_("r_raw%s%d" % (nm, i), [128, 513]) for i in range(2)] for nm in "rkv"}
        mx = {nm: T_("r_mx" + nm, [128, 512]) for nm in "rkv"}
        tmp, ktmp = T_("r_tmp", [128, 512])
        tmp2, ktmp2 = T_("r_tmp2", [128, 512])
        lw, klw = T_("r_lw", [128, 512])
        av, kav = T_("r_a", [128, 512])
        gate, kgate = T_("r_gate", [128, 512])
        kkv, kkk = T_("r_kk", [128, 512])
        kmod, kkmod = T_("r_kmod", [128, 512])
        bv, kbv = T_("r_b", [128, 512])
        bonus, kbonus = T_("r_bonus", [128, 512])
        cw, kcw = T_("r_cw", [128, 512])
        ecw, kecw = T_("r_ecw", [128, 512])
        ecwx, kecwx = T_("r_ecwx", [128, 512])
        eneg, keneg = T_("r_eneg", [128, 512])
        eend, keend = T_("r_eend", [128, 512])
        prod, kprod = T_("r_prod", [128, 512])
        QRbd, kQR = T_("r_QRbd", [128, 8, 2, 128])
        kibd, kki = T_("r_kibd", [128, 8, 128])
        bibd, kbi = T_("r_bibd", [128, 8, 128])
        vbd, kvb = T_("r_vbd", [128, 8, 128])
        kebd, kke = T_("r_kebd", [128, 8, 128])
        bebd, kbe = T_("r_bebd", [128, 8, 128])
        A1, kA1 = T_("r_A1", [128, 256])
        A2, kA2 = T_("r_A2", [128, 256])
        PP = [T_("r_PP%d" % i, [128, 256]) for i in range(2)]
        QT = [T_("r_QT%d" % i, [128, 128]) for i in range(2)]
        Vs, kVs = T_("r_Vs", [128, 64])
        Rs, kRs = T_("r_Rs", [128, 64])
        Us, kUs = T_("r_Us", [128, 64])
        KT, kKT = T_("r_KT", [128, 256])
        Yall, kY = T_("r_Yall", [128, 8, 64])
        Ysq, kYsq = T_("r_Ysq", [128, 8, 64])
        st1, kst1 = T_("r_st1", [128, 8])
        st2, kst2 = T_("r_st2", [128, 8])
        obd, kobd = T_("r_obd", [128, 8, 128])
        yb = [T_("r_yb%d" % i, [128, 512], BF16) for i in range(2)]
        k.memset(lo_raw[:, 0:1], 0.0, writes=[klo_raw])
        k.memset(g_raw[:, 0:1], 0.0, writes=[kg_raw])
        for nm in "rkv":
            for i in range(2):
                k.memset(raw[nm][i][0][:, 0:1], 0.0, writes=[raw[nm][i][1]])
        BDM = CR[:, CR_BD:CR_BD + 128].rearrange("p (a b) -> p a b", b=64)
        it = 0

        def load_shift(tile, key, chunk, t0, n, mucol, outt, outk):
            if t0 == 0:
                k.dma("sp", tile[:, 1:n + 1], d["PT"][chunk * 128:(chunk + 1) * 128, 0:n], reads=[("PT", chunk)], writes=[key])
            else:
                k.dma("sp", tile[:, 0:n + 1], d["PT"][chunk * 128:(chunk + 1) * 128, t0 - 1:t0 + n], reads=[("PT", chunk)], writes=[key])
            k.tt(outt[:, :n], tile[:, 0:n], tile[:, 1:n + 1], OP.subtract, reads=[key], writes=[outk])
            k.stt(outt[:, :n], outt[:, :n], VR[:, mucol:mucol + 1], tile[:, 1:n + 1], OP.mult, OP.add, reads=[outk, key, "VR"], writes=[outk])

        def bd_expand(dst, kdst, src, ksrc, nc_, eng="dve"):
            k.tt(dst[:, :nc_].rearrange("p c (a b) -> p c a b", b=64) if len(dst.shape) == 3 else dst,
                 src.rearrange("p (c t) -> p c t", t=64).unsqueeze(2).broadcast_to([128, nc_, 2, 64]),
                 BDM.unsqueeze(1).broadcast_to([128, nc_, 2, 64]), OP.mult, reads=[ksrc, "CR"], writes=[kdst], eng=eng)

        for (t0, n) in supertiles(LP):
            nc_ = n // 64
            load_shift(lo_raw, klo_raw, C_LO, t0, n, VR_MULO, LOm, kLOm)
            k.act(LOm[0:64, :n], LOm[0:64, :n], AF.Tanh, reads=[kLOm], writes=[kLOm])
            load_shift(g_raw, kg_raw, C_G, t0, n, VR_MUG, Gs, kGs)
            k.act(Gs[:, :n], Gs[:, :n], AF.Sigmoid, reads=[kGs], writes=[kGs])
            for j in range(8):
                it += 1
                for nm, ch, mu in (("r", C_R, VR_MUR), ("k", C_K, VR_MUK), ("v", C_V, VR_MUV)):
                    rt, rk_ = raw[nm][it % 2]
                    load_shift(rt, rk_, ch + j, t0, n, mu + j, mx[nm][0], mx[nm][1])
                r_, kr_ = mx["r"]
                k_, kk_ = mx["k"]
                v_, kv_ = mx["v"]
                js = slice(j * 128, (j + 1) * 128)
                if R3 < 1: continue
                ps, pk = k.ps()
                k.mm(ps[:, :n], WUA[0:64, js], LOm[0:64, :n], True, True, reads=["WUA", kLOm], writes=[pk])
                k.act(lw[:, :n], ps[:, :n], AF.Sigmoid, reads=[pk, "VR"], writes=[klw], bias=VR[:, VR_W0 + j:VR_W0 + j + 1])
                k.ts(lw[:, :n], lw[:, :n], -float(np.exp(-0.5)), OP.mult, reads=[klw], writes=[klw])
                ps, pk = k.ps()
                k.mm(ps[:, :n], WUA[64:128, js], LOm[64:128, :n], True, True, reads=["WUA", kLOm], writes=[pk])
                k.act(av[:, :n], ps[:, :n], AF.Sigmoid, reads=[pk, "VR"], writes=[kav], bias=VR[:, VR_A0 + j:VR_A0 + j + 1])
                ps, pk = k.ps()
                k.mm(ps[:, :n], GUP[:, js], Gs[:, :n], True, True, reads=["GUP", kGs], writes=[pk])
                k.copy(gate[:, :n], ps[:, :n], reads=[pk], writes=[kgate], eng="act")
                if R3 < 2: continue
                k.ts(kkv[:, :n], k_[:, :n], VR[:, VR_KK + j:VR_KK + j + 1], OP.mult, reads=[kk_, "VR"], writes=[kkk])
                k.tt(tmp[:, :n], kkv[:, :n], kkv[:, :n], OP.mult, reads=[kkk], writes=[ktmp])
                ps, pk = k.ps()
                k.mm(ps[:, :n], BONES, tmp[:, :n], True, True, reads=["CR", ktmp], writes=[pk])
                k.ts(tmp2[:, :n], ps[:, :n], 1e-24, OP.max, reads=[pk], writes=[ktmp2])
                k.act(tmp2[:, :n], tmp2[:, :n], AF.Sqrt, reads=[ktmp2], writes=[ktmp2])
                k.op("dve", lambda h, n=n: h.reciprocal(out=tmp2[:, :n], in_=tmp2[:, :n]), reads=[ktmp2], writes=[ktmp2])
                k.tt(kkv[:, :n], kkv[:, :n], tmp2[:, :n], OP.mult, reads=[kkk, ktmp2], writes=[kkk])
                k.ts(tmp[:, :n], av[:, :n], VR[:, VR_KA + j:VR_KA + j + 1], OP.mult, reads=[kav, "VR", "OMKA"], writes=[ktmp], s2=OMKA[:, j:j + 1], op1=OP.add)
                k.tt(kmod[:, :n], k_[:, :n], tmp[:, :n], OP.mult, reads=[kk_, ktmp], writes=[kkmod])
                k.tt(bv[:, :n], av[:, :n], kkv[:, :n], OP.mult, reads=[kav, kkk], writes=[kbv], eng="pool")
                k.stt(tmp[:, :n], r_[:, :n], VR[:, VR_RK + j:VR_RK + j + 1], kmod[:, :n], OP.mult, OP.mult, reads=[kr_, kkmod, "VR"], writes=[ktmp])
                ps, pk = k.ps()
                k.mm(ps[:, :n], BONES, tmp[:, :n], True, True, reads=["CR", ktmp], writes=[pk])
                k.tt(bonus[:, :n], ps[:, :n], v_[:, :n], OP.mult, reads=[pk, kv_], writes=[kbonus])
                if R3 < 3: continue
                k.op("dve", lambda h, n=n: h.tensor_tensor_scan(out=cw[:, :n], data0=CR[:, CR_RESET:CR_RESET + n], data1=lw[:, :n], initial=0.0, op0=OP.mult, op1=OP.add),
                     reads=["CR", klw], writes=[kcw])
                k.act(ecw[:, :n], cw[:, :n], AF.Exp, reads=[kcw], writes=[kecw])
                k.tt(tmp2[:, :n], cw[:, :n], lw[:, :n], OP.subtract, reads=[kcw, klw], writes=[ktmp2], eng="pool")
                k.act(ecwx[:, :n], tmp2[:, :n], AF.Exp, reads=[ktmp2], writes=[kecwx])
                k.act(eneg[:, :n], cw[:, :n], AF.Exp, reads=[kcw], writes=[keneg], scale=-1.0)
                cw3 = cw[:, :n].rearrange("p (c t) -> p c t", t=64)
                k.tt(tmp[:, :n].rearrange("p (c t) -> p c t", t=64), cw3[:, :, 63:64].broadcast_to([128, nc_, 64]), cw3, OP.subtract, reads=[kcw], writes=[ktmp])
                k.act(eend[:, :n], tmp[:, :n], AF.Exp, reads=[ktmp], writes=[keend])
                if R3 < 4: continue
                k.tt(prod[:, :n], kkv[:, :n], ecwx[:, :n], OP.mult, reads=[kkk, kecwx], writes=[kprod])
                k.tt(QRbd[:, :nc_, 0, :].rearrange("p c (a b) -> p c a b", b=64), prod[:, :n].rearrange("p (c t) -> p c t", t=64).unsqueeze(2).broadcast_to([128, nc_, 2, 64]),
                     BDM.unsqueeze(1).broadcast_to([128, nc_, 2, 64]), OP.mult, reads=[kprod, "CR"], writes=[(kQR, 0)])
                k.tt(prod[:, :n], r_[:, :n], ecw[:, :n], OP.mult, reads=[kr_, kecw, (kQR, 0)], writes=[kprod])
                k.tt(QRbd[:, :nc_, 1, :].rearrange("p c (a b) -> p c a b", b=64), prod[:, :n].rearrange("p (c t) -> p c t", t=64).unsqueeze(2).broadcast_to([128, nc_, 2, 64]),
                     BDM.unsqueeze(1).broadcast_to([128, nc_, 2, 64]), OP.mult, reads=[kprod, "CR"], writes=[(kQR, 1)])
                for (dst, kdst, src, ksrc, ex, kex, neg) in ((kibd, kki, kmod, kkmod, eneg, keneg, False), (bibd, kbi, bv, kbv, eneg, keneg, False),
                                                          (kebd, kke, kmod, kkmod, eend, keend, False), (bebd, kbe, bv, kbv, eend, keend, True)):
                    if neg:
                        k.stt(prod[:, :n], src[:, :n], -1.0, ex[:, :n], OP.mult, OP.mult, reads=[ksrc, kex, kprod, (kQR, 1), kki, kbi, kke], writes=[kprod])
                    else:
                        k.tt(prod[:, :n], src[:, :n], ex[:, :n], OP.mult, reads=[ksrc, kex, kprod, (kQR, 1), kki, kbi, kke], writes=[kprod])
                    k.tt(dst[:, :nc_].rearrange("p c (a b) -> p c a b", b=64), prod[:, :n].rearrange("p (c t) -> p c t", t=64).unsqueeze(2).broadcast_to([128, nc_, 2, 64]),
                         BDM.unsqueeze(1).broadcast_to([128, nc_, 2, 64]), OP.mult, reads=[kprod, "CR"], writes=[kdst])
                k.tt(vbd[:, :nc_].rearrange("p c (a b) -> p c a b", b=64), v_[:, :n].rearrange("p (c t) -> p c t", t=64).unsqueeze(2).broadcast_to([128, nc_, 2, 64]),
                     BDM.unsqueeze(1).broadcast_to([128, nc_, 2, 64]), OP.mult, reads=[kv_, "CR"], writes=[kvb], eng="pool")
                if R3 < 5: continue
                Tj, kTj = T[j], "r_T%d" % j
                for c in range(nc_):
                    QRc = QRbd[:, c].rearrange("p a b -> p (a b)")
                    ps1, pk1 = k.ps()
                    k.mm(ps1[:, 0:256], kibd[:, c], QRc, True, True, reads=[kki, (kQR, 0), (kQR, 1)], writes=[pk1])
                    k.tt(A1[:], ps1[:, 0:256], MASK1, OP.mult, reads=[pk1, "CR"], writes=[kA1])
                    ps2, pk2 = k.ps()
                    k.mm(ps2[:, 0:256], bibd[:, c], QRc, True, True, reads=[kbi, (kQR, 0), (kQR, 1)], writes=[pk2])
                    k.tt(A2[:], ps2[:, 0:256], MASK2, OP.mult, reads=[pk2, "CR"], writes=[kA2])
                    ps3, pk3 = k.ps()
                    k.mm(ps3[:, 0:128], QRbd[:, c, 0], bibd[:, c], True, True, reads=[kbi, (kQR, 0)], writes=[pk3])
                    if R3C < 1: continue
                    Pc, kPc = PP[0]
                    k.tt(Pc[:, 0:128], ps3[:, 0:128], MSLN, OP.mult, reads=[pk3, "CR"], writes=[kPc])
                    k.copy(Pc[:, 128:256], A2[:, 0:128], reads=[kA2, kPc], writes=[kPc], eng="pool")
                    Qc, kQc = QT[0]
                    k.tt(Qc[:], A2[:, 0:128], ident, OP.add, reads=[kA2, "CM"], writes=[kQc], eng="pool")
                    if R3C < 2: continue
                    for lv in range(1, 6):
                        Pn, kPn = PP[lv % 2]
                        psq, pkq = k.ps()
                        k.mm(psq[:, 0:128], Pc[:, 128:256], Pc[:, 0:128], True, True, reads=[kPc], writes=[pkq])
                        k.mm(psq[:, 128:256], Pc[:, 0:128], Pc[:, 128:256], True, True, reads=[kPc], writes=[pkq])
                        k.copy(Pn[:], psq[:, 0:256], reads=[pkq], writes=[kPn], eng="act")
                        Qn, kQn = QT[lv % 2]
                        psu, pku = k.ps()
                        k.mm(psu[:, 0:128], Pn[:, 0:128], Qc[:], True, True, reads=[kPn, kQc], writes=[pku])
                        k.tt(Qn[:], psu[:, 0:128], Qc[:], OP.add, reads=[pku, kQc], writes=[kQn])
                        Pc, kPc, Qc, kQc = Pn, kPn, Qn, kQn
                    if R3C < 3: continue
                    psv, pkv = k.ps()
                    k.mm(psv[:, 0:64], vbd[:, c], ISEL, True, True, reads=[kvb, "CR"], writes=[pkv])
                    k.copy(Vs[:], psv[:, 0:64], reads=[pkv], writes=[kVs], eng="act")
                    if R3C < 4: continue
                    psr, pkr = k.ps()
                    k.mm(psr[:, 0:64], QRbd[:, c, 0], Tj[:], True, False, reads=[(kQR, 0), kTj], writes=[pkr])
                    k.mm(psr[:, 0:64], A1[:, 0:128], Vs[:], False, True, reads=[kA1, kVs], writes=[pkr])
                    k.copy(Rs[:], psr[:, 0:64], reads=[pkr], writes=[kRs])
                    psu, pku = k.ps()
                    k.mm(psu[:, 0:64], Qc[:], Rs[:], True, True, reads=[kQc, kRs], writes=[pku])
                    k.copy(Us[:], psu[:, 0:64], reads=[pku], writes=[kUs])
                    if R3C < 5: continue
                    psy, pky = k.ps()
                    k.mm(psy[:, 0:64], QRbd[:, c, 1], Tj[:], True, False, reads=[(kQR, 1), kTj], writes=[pky])
                    k.mm(psy[:, 0:64], A1[:, 128:256], Vs[:], False, False, reads=[kA1, kVs], writes=[pky])
                    k.mm(psy[:, 0:64], A2[:, 128:256], Us[:], False, True, reads=[kA2, kUs], writes=[pky])
                    k.copy(Yall[:, c, :], psy[:, 0:64], reads=[pky], writes=[kY], eng="act")
                    if R3C < 6: continue
                    pst, pkt = k.ps()
                    k.tr(pst[:, 0:128], kebd[:, c], ident, reads=[kke, "CM"], writes=[pkt])
                    k.tr(pst[:, 128:256], bebd[:, c], ident, reads=[kbe, "CM"], writes=[pkt])
                    k.copy(KT[:], pst[:, 0:256], reads=[pkt], writes=[kKT])
                    psn, pkn = k.ps()
                    k.mm(psn[:, 0:64], KT[:, 0:128], Vs[:], True, False, reads=[kKT, kVs], writes=[pkn])
                    k.mm(psn[:, 0:64], KT[:, 128:256], Us[:], False, True, reads=[kKT, kUs], writes=[pkn])
                    k.stt(Tj[:], Tj[:], ecw[:, c * 64 + 63:c * 64 + 64], psn[:, 0:64], OP.mult, OP.add, reads=[kTj, kecw, pkn], writes=[kTj])
                if R3 < 6: continue
                Yv = Yall[:, :nc_, :]
                k.op("dve", lambda h, Yv=Yv, nc_=nc_: h.tensor_reduce(out=st1[:, :nc_], in_=Yv, axis=mybir.AxisListType.X, op=OP.add), reads=[kY], writes=[kst1])
                k.ts(st1[:, :nc_], st1[:, :nc_], 1.0 / 64, OP.mult, reads=[kst1], writes=[kst1])
                k.tt(Yv, Yv, st1[:, :nc_].unsqueeze(2).broadcast_to([128, nc_, 64]), OP.subtract, reads=[kY, kst1], writes=[kY])
                if R3L < 1: continue
                k.tt(Ysq[:, :nc_, :], Yv, Yv, OP.mult, reads=[kY], writes=[kYsq])
                k.op("dve", lambda h, nc_=nc_: h.tensor_reduce(out=st2[:, :nc_], in_=Ysq[:, :nc_, :], axis=mybir.AxisListType.X, op=OP.add), reads=[kYsq], writes=[kst2])
                k.act(st2[:, :nc_], st2[:, :nc_], AF.Sqrt, reads=[kst2], writes=[kst2], bias=RWKV_EPS, scale=1.0 / 64)
                k.op("dve", lambda h, nc_=nc_: h.reciprocal(out=st2[:, :nc_], in_=st2[:, :nc_]), reads=[kst2], writes=[kst2])
                k.tt(Yv, Yv, st2[:, :nc_].unsqueeze(2).broadcast_to([128, nc_, 64]), OP.mult, reads=[kY, kst2], writes=[kY])
                if R3L < 2: continue
                for c in range(nc_):
                    k.tt(Yall[:, c, :], Yall[:, c, :], LNG[:, j * 64:(j + 1) * 64], OP.mult, reads=[kY, "LNG"], writes=[kY])
                    k.tt(Yall[:, c, :], Yall[:, c, :], LNB[:, j * 64:(j + 1) * 64], OP.add, reads=[kY, "LNB"], writes=[kY])
                if R3L < 3: continue
                k.tt(obd[:, :nc_].rearrange("p c (a b) -> p c a b", b=64), Yv.unsqueeze(2).broadcast_to([128, nc_, 2, 64]),
                     BDM.unsqueeze(1).broadcast_to([128, nc_, 2, 64]), OP.mult, reads=[kY, "CR"], writes=[kobd])
                pso, pko = k.ps()
                for c in range(nc_):
                    k.mm(pso[:, c * 64:(c + 1) * 64], obd[:, c], ISEL, True, True, reads=[kobd, "CR"], writes=[pko])
                if R3L < 5: continue
                k.tt(tmp[:, :n], pso[:, :n], bonus[:, :n], OP.add, reads=[pko, kbonus], writes=[ktmp])
                ybt, kyb = yb[it % 2]
                k.tt(ybt[:, :n], tmp[:, :n], gate[:, :n], OP.mult, reads=[ktmp, kgate], writes=[kyb])
                k.dma("pool", d["YT"][1024 + j * 128:1024 + (j + 1) * 128, t0:t0 + n], ybt[:, :n], reads=[kyb], writes=[("YT", 1)])
    k.pes = None
    k.barrier()


def host_consts_rwkv(inp):
    p = np.arange(128)
    q = np.arange(128)
    same = (p[:, None] // 64) == (q[None, :] // 64)
    SU = (same & ((p[:, None] % 64) < (q[None, :] % 64))).astype(np.float32)
    IU = (same & ((p[:, None] % 64) <= (q[None, :] % 64))).astype(np.float32)
    SL = (same & ((q[None, :] % 64) < (p[:, None] % 64))).astype(np.float32)
    CR = np.zeros((128, NCR), np.float32)
    CR[:, 0:128] = SU
    CR[:, 128:256] = IU
    CR[:, 256:384] = -SU
    CR[:, 384:512] = -IU
    CR[:, 512:640] = -SL
    CR[:, 640:704] = ((p[:, None] % 64) == np.arange(64)[None, :])
    CR[:, 704:832] = ((p[:, None] // 64) == (np.arange(128)[None, :] // 64))
    CR[:, 832:960] = same
    CR[:, 960:960 + 512] = (np.arange(512) % 64 != 0)[None, :]
    mu = inp["ev_shift_mu"][0]
    VR = np.zeros((128, NVR), np.float32)

    def pairs(v):
        return v.reshape(8, 128).T

    VR[:, 0:8] = pairs(mu[0:1024])
    VR[:, 8:16] = pairs(mu[1024:2048])
    VR[:, 16:24] = pairs(mu[2048:3072])
    VR[:, 24:32] = pairs(inp["ev_w0"][0])
    VR[:, 32:40] = pairs(inp["ev_a0"][0])
    VR[:, 40:48] = pairs(inp["ev_k_k"][0])
    VR[:, 48:56] = pairs(inp["ev_k_a"][0])
    VR[:, 56:64] = pairs(inp["ev_r_k"][0].reshape(1024))
    VR[:, 64] = mu[3072:3200]
    VR[:, 65] = mu[3200:3328]
    WUA = np.concatenate([inp["ev_w_up"][0], inp["ev_a_up"][0]], 0).astype(np.float32)
    GUP = inp["ev_g_up"][0].astype(np.float32)

    def stack(v):
        a = v.reshape(8, 2, 64)
        return np.ascontiguousarray(np.repeat(a.transpose(1, 0, 2)[:, None], 64, axis=1).reshape(128, 512))

    return dict(CRd=CR, VRd=VR, WUAd=WUA, GUPd=GUP, LNGd=stack(inp["ev_lnx_g"][0]), LNBd=stack(inp["ev_lnx_b"][0]))


def layer_norm_rows(k, pfx, src, ksrc, dst, kdst, gcol, bcol, VT2, tiles):
    stats, mv, rs = tiles
    for i in range(2):
        k.op("dve", lambda h, i=i: h.bn_stats(out=stats[:, i * 6:(i + 1) * 6], in_=src[:, i * 512:(i + 1) * 512]), reads=[ksrc], writes=[pfx + "st"])
    k.op("dve", lambda h: h.bn_aggr(out=mv[:], in_=stats[:]), reads=[pfx + "st"], writes=[pfx + "mv"])
    k.act(rs[:], mv[:, 1:2], AF.Sqrt, reads=[pfx + "mv"], writes=[pfx + "rs"], bias=LN_EPS)
    k.op("dve", lambda h: h.reciprocal(out=rs[:], in_=rs[:]), reads=[pfx + "rs"], writes=[pfx + "rs"])
    k.ts(dst[:], src[:], mv[:, 0:1], OP.subtract, reads=[ksrc, pfx + "mv", pfx + "rs"], writes=[kdst], s2=rs[:, 0:1], op1=OP.mult)
    k.tt(dst[:], dst[:], VT2[:, gcol:gcol + 1024], OP.mult, reads=[kdst, "VT2"], writes=[kdst])
    k.tt(dst[:], dst[:], VT2[:, bcol:bcol + 1024], OP.add, reads=[kdst, "VT2"], writes=[kdst])


def proj_res_ln(P, pfx, srcT, nkc, wname, res_name, gcol, bcol, out_name, outT_name, outT32_name=None):
    k, LP, d = P.k, P.LP, P.dram
    with ExitStack() as pes:
        k.pes = pes
        VT2 = k.sb(pfx + "VT2", [128, 2048])
        k.dma("sp", VT2[:], d["VT2d"][0:1, gcol:gcol + 2048].partition_broadcast(128), writes=["VT2"])
        P.VT2 = VT2
        gcol, bcol = 0, 1024
        W = k.sb(pfx + "W", [128, nkc, 1024], BF16)
        k.dma("sp", W[:].rearrange("p a b -> p (a b)"), d[wname][:, :], reads=[(wname, 0)], writes=[pfx + "W"])
        yT = [k.sb(pfx + "yT%d" % i, [128, nkc, 512], BF16) for i in range(2)]
        xr = [k.sb(pfx + "x%d" % i, [128, 1024]) for i in range(2)]
        hp = k.sb(pfx + "hp", [128, 1024])
        ho = [k.sb(pfx + "ho%d" % i, [128, 1024]) for i in range(2)]
        hT = [k.sb(pfx + "hT%d" % i, [128, 8, 128], BF16) for i in range(2)]
        hT32 = [k.sb(pfx + "hTf%d" % i, [128, 8, 128]) for i in range(2)]
        lnt = (k.sb(pfx + "st", [128, 12]), k.sb(pfx + "mv", [128, 2]), k.sb(pfx + "rs", [128, 1]))
        it = 0
        for si, (t0, n) in enumerate(supertiles(LP)):
            y, ky = yT[si % 2], pfx + "yT%d" % (si % 2)
            k.dma("sp", y[:, :, :n], d[srcT][:, t0:t0 + n].rearrange("(c p) t -> p c t", p=128), reads=[(srcT, 0), (srcT, 1)], writes=[ky])
            for j in range(n // 128):
                it += 1
                x, kx = xr[it % 2], pfx + "x%d" % (it % 2)
                r0 = t0 + j * 128
                k.dma("sp", x[:], d[res_name][r0:r0 + 128, :], reads=[(res_name, 0)], writes=[kx])
                for half in range(2):
                    ps, pk = k.ps()
                    for kc in range(nkc):
                        k.mm(ps[:, :], y[:, kc, j * 128:(j + 1) * 128], W[:, kc, half * 512:(half + 1) * 512], kc == 0, kc == nkc - 1, reads=[ky, pfx + "W"], writes=[pk])
                    k.stt(hp[:, half * 512:(half + 1) * 512], x[:, half * 512:(half + 1) * 512], ALPHA, ps[:, :], OP.mult, OP.add, reads=[kx, pk], writes=[pfx + "hp"])
                o, ko = ho[it % 2], pfx + "ho%d" % (it % 2)
                layer_norm_rows(k, pfx, hp, pfx + "hp", o, ko, gcol, bcol, P.VT2, lnt)
                k.dma("pool", d[out_name][r0:r0 + 128, :], o[:], reads=[ko], writes=[(out_name, 0)])
                t_, kt = hT[it % 2], pfx + "hT%d" % (it % 2)
                tf, ktf = hT32[it % 2], pfx + "hTf%d" % (it % 2)
                for half in range(2):
                    ps, pk = k.ps()
                    for cc in range(4):
                        c = half * 4 + cc
                        k.tr(ps[:, cc * 128:(cc + 1) * 128], o[:, c * 128:(c + 1) * 128], P.ident, reads=[ko, "CM"], writes=[pk])
                    k.copy(t_[:, half * 4:(half + 1) * 4, :], ps[:, :].rearrange("p (c t) -> p c t", t=128), reads=[pk], writes=[kt], eng="act")
                    if outT32_name:
                        k.copy(tf[:, half * 4:(half + 1) * 4, :], ps[:, :].rearrange("p (c t) -> p c t", t=128), reads=[pk], writes=[ktf])
                k.dma("pool", d[outT_name][:, r0:r0 + 128].rearrange("(c p) t -> p c t", p=128), t_[:], reads=[kt], writes=[(outT_name, 0)])
                if outT32_name:
                    k.dma("pool", d[outT32_name][:, r0:r0 + 128].rearrange("(c p) t -> p c t", p=128), tf[:], reads=[ktf], writes=[(outT32_name, 0)])
    k.pes = None
    k.barrier()


def ffn_phase(P, pfx, srcT, res_name, wgu_names, wd_names, gcol, bcol, out_name, outT_name=None, router=None):
    k, LP, d = P.k, P.LP, P.dram
    ne = len(wgu_names)
    with ExitStack() as pes:
        k.pes = pes
        VT2 = k.sb(pfx + "VT2", [128, 2048])
        k.dma("sp", VT2[:], d["VT2d"][0:1, gcol:gcol + 2048].partition_broadcast(128), writes=["VT2"])
        P.VT2 = VT2
        gcol, bcol = 0, 1024
        hT = [k.sb(pfx + "hT%d" % i, [128, 8, 512], BF16) for i in range(2)]
        wg = [k.sb(pfx + "wg%d" % i, [128, 8, 128], BF16) for i in range(4)]
        wd = [k.sb(pfx + "wd%d" % i, [128, 22, 1024], BF16) for i in range(1)]
        actT = k.sb(pfx + "actT", [128, 22, 512], BF16)
        sg = [k.sb(pfx + "sg%d" % i, [128, 512]) for i in range(2)]
        acc = [k.sb(pfx + "acc%d" % i, [128, 1024]) for i in range(4)]
        o = [k.sb(pfx + "o%d" % i, [128, 1024]) for i in range(2)]
        oT = [k.sb(pfx + "oT%d" % i, [128, 8, 128], BF16) for i in range(2)]
        lnt = (k.sb(pfx + "st", [128, 12]), k.sb(pfx + "mv", [128, 2]), k.sb(pfx + "rs", [128, 1]))
        if router:
            hT32 = k.sb(pfx + "hT32", [128, 8, 512])
            WR = k.sb(pfx + "WR", [128, 8, 8])
            k.dma("sp", WR[:].rearrange("p a b -> p (a b)"), d["WRd"][:, :], writes=[pfx + "WR"])
            lg = k.sb(pfx + "lg", [128, 8])
            m8 = k.sb(pfx + "m8", [128, 8])
            msk = k.sb(pfx + "msk", [128, 8])
            nm0 = k.sb(pfx + "nm0", [128, 1])
            den = k.sb(pfx + "den", [128, 1])
            G = [k.sb(pfx + "G%d" % i, [128, 8]) for i in range(4)]
        if ne == 1:
            k.dma("sp", wd[0][:].rearrange("p a b -> p (a b)"), d[wd_names[0]][:, :], reads=[(wd_names[0], 0)], writes=[pfx + "wd0"])
        wi = 0
        wdi = 0
        oi = 0
        for si, (t0, n) in enumerate(supertiles(LP)):
            nj = n // 128
            h, kh = hT[si % 2], pfx + "hT%d" % (si % 2)
            k.dma("sp", h[:, :, :n], d[srcT][:, t0:t0 + n].rearrange("(c p) t -> p c t", p=128), reads=[(srcT, 0)], writes=[kh])
            if router:
                k.dma("sp", hT32[:, :, :n], d[router][:, t0:t0 + n].rearrange("(c p) t -> p c t", p=128), reads=[(router, 0)], writes=[pfx + "hT32"])
            for j in range(nj):
                r0 = t0 + j * 128
                k.dma("sp", acc[j][:], d[res_name][r0:r0 + 128, :], reads=[(res_name, 0)], writes=[pfx + "acc%d" % j])
                k.ts(acc[j][:], acc[j][:], ALPHA, OP.mult, reads=[pfx + "acc%d" % j], writes=[pfx + "acc%d" % j], eng="pool")
                if router:
                    ps, pk = k.ps()
                    for kc in range(8):
                        k.mm(ps[:, 0:8], hT32[:, kc, j * 128:(j + 1) * 128], WR[:, kc, :], kc == 0, kc == 7, reads=[pfx + "hT32", pfx + "WR"], writes=[pk])
                    k.copy(lg[:], ps[:, 0:8], reads=[pk], writes=[pfx + "lg"])
                    k.op("dve", lambda hh: hh.max(out=m8[:], in_=lg[:]), reads=[pfx + "lg"], writes=[pfx + "m8"])
                    k.ts(msk[:], lg[:], m8[:, 1:2], OP.is_ge, reads=[pfx + "lg", pfx + "m8"], writes=[pfx + "msk"])
                    k.ts(nm0[:], m8[:, 0:1], -1.0, OP.mult, reads=[pfx + "m8"], writes=[pfx + "nm0"])
                    k.act(lg[:], lg[:], AF.Exp, reads=[pfx + "lg", pfx + "nm0"], writes=[pfx + "lg"], bias=nm0[:, 0:1])
                    k.tt(lg[:], lg[:], msk[:], OP.mult, reads=[pfx + "lg", pfx + "msk"], writes=[pfx + "lg"])
                    k.op("dve", lambda hh: hh.tensor_reduce(out=den[:], in_=lg[:], axis=mybir.AxisListType.X, op=OP.add), reads=[pfx + "lg"], writes=[pfx + "den"])
                    k.op("dve", lambda hh: hh.reciprocal(out=den[:], in_=den[:]), reads=[pfx + "den"], writes=[pfx + "den"])
                    k.ts(G[j][:], lg[:], den[:, 0:1], OP.mult, reads=[pfx + "lg", pfx + "den"], writes=[pfx + "G%d" % j])
            for e in range(ne):
                if ne > 1:
                    wdt, kwd = wd[0], pfx + "wd0"
                    wdi += 1
                    k.dma("sp", wdt[:].rearrange("p a b -> p (a b)"), d[wd_names[e]][:, :], reads=[(wd_names[e], 0)], writes=[kwd])
                else:
                    wdt, kwd = wd[0], pfx + "wd0"
                for i in range(22):
                    pss = []
                    for part in range(2):
                        c = part * 22 + i
                        w, kw = wg[wi % 4], pfx + "wg%d" % (wi % 4)
                        wi += 1
                        k.dma("sp", w[:].rearrange("p a b -> p (a b)"), d[wgu_names[e]][c * 128:(c + 1) * 128, :], reads=[(wgu_names[e], (c * 128) // 512)], writes=[kw])
                        ps, pk = k.ps()
                        for kc in range(8):
                            k.mm(ps[:, :n], w[:, kc, :], h[:, kc, :n], kc == 0, kc == 7, reads=[kw, kh], writes=[pk])
                        pss.append((ps, pk))
                    s, ks = sg[i % 2], pfx + "sg%d" % (i % 2)
                    k.act(s[:, :n], pss[0][0][:, :n], AF.Silu, reads=[pss[0][1]], writes=[ks])
                    k.tt(actT[:, i, :n], s[:, :n], pss[1][0][:, :n], OP.mult, reads=[ks, pss[1][1]], writes=[(pfx + "actT", i)])
                for j in range(nj):
                    for half in range(2):
                        ps, pk = k.ps()
                        for i in range(22):
                            k.mm(ps[:, :], actT[:, i, j * 128:(j + 1) * 128], wdt[:, i, half * 512:(half + 1) * 512], i == 0, i == 21, reads=[(pfx + "actT", i), kwd], writes=[pk])
                        hs = slice(half * 512, (half + 1) * 512)
                        if router:
                            k.stt(acc[j][:, hs], ps[:, :], G[j][:, e:e + 1], acc[j][:, hs], OP.mult, OP.add, reads=[pk, pfx + "G%d" % j, pfx + "acc%d" % j], writes=[pfx + "acc%d" % j])
                        else:
                            k.tt(acc[j][:, hs], ps[:, :], acc[j][:, hs], OP.add, reads=[pk, pfx + "acc%d" % j], writes=[pfx + "acc%d" % j])
            for j in range(nj):
                r0 = t0 + j * 128
                oi += 1
                ot, ko = o[oi % 2], pfx + "o%d" % (oi % 2)
                layer_norm_rows(k, pfx, acc[j], pfx + "acc%d" % j, ot, ko, gcol, bcol, P.VT2, lnt)
                k.dma("pool", d[out_name][r0:r0 + 128, :], ot[:], reads=[ko], writes=[(out_name, 0)])
                if outT_name:
                    t_, kt = oT[oi % 2], pfx + "oT%d" % (oi % 2)
                    for half in range(2):
                        ps, pk = k.ps()
                        for cc in range(4):
                            c = half * 4 + cc
                            k.tr(ps[:, cc * 128:(cc + 1) * 128], ot[:, c * 128:(c + 1) * 128], P.ident, reads=[ko, "CM"], writes=[pk])
                        k.copy(t_[:, half * 4:(half + 1) * 4, :], ps[:, :].rearrange("p (c t) -> p c t", t=128), reads=[pk], writes=[kt], eng="act")
                    k.dma("pool", d[outT_name][:, r0:r0 + 128].rearrange("(c p) t -> p c t", p=128), t_[:], reads=[kt], writes=[(outT_name, 0)])
    k.pes = None
    k.barrier()


VL_CW, VL_CB, VL_GXB, VL_GAB, VL_LAM = 0, 32, 40, 48, 56
NVL = 64


def phase5(P):
    k, LP, d = P.k, P.LP, P.dram
    with ExitStack() as pes:
        k.pes = pes
        W = k.sb("l_W", [128, 16, 8, 128], BF16)
        k.dma("sp", W[:].rearrange("p c a b -> p c (a b)"), d["W1INb"][:, :].rearrange("(c p) f -> p c f", p=128), reads=[("W1INb", i) for i in range(4)], writes=["l_W"])
        GW = k.sb("l_GW", [128, 16, 128])
        k.dma("sp", GW[:].rearrange("p c b -> p (c b)"), d["GWd"][:, :], writes=["l_GW"])
        VL = k.sb("l_VL", [128, NVL])
        k.dma("sp", VL[:], d["VLd"][:, :], writes=["l_VL"])
        SPm8 = k.sb("l_sp8", [128, 8])
        SPm16 = k.sb("l_sp16", [128, 8])
        k.act(SPm8[:], VL[:, VL_LAM:VL_LAM + 8], AF.Exp, reads=["l_VL"], writes=["l_sp8"], scale=-1.0)
        k.act(SPm8[:], SPm8[:], AF.Ln, reads=["l_sp8"], writes=["l_sp8"], bias=1.0)
        k.ts(SPm16[:], SPm8[:], -16.0, OP.mult, reads=["l_sp8"], writes=["l_sp16"])
        k.ts(SPm8[:], SPm8[:], -8.0, OP.mult, reads=["l_sp8", "l_sp16"], writes=["l_sp8"])
        hT = [k.sb("l_hT%d" % i, [128, 8, 512], BF16) for i in range(2)]
        gb = [k.sb("l_gb%d" % i, [128, 512]) for i in range(2)]
        g2 = k.sb("l_g2", [128, 512])
        xr = [k.sb("l_xr%d" % i, [128, 515]) for i in range(2)]
        xf = k.sb("l_xf", [128, 512])
        gx = k.sb("l_gx", [128, 512])
        ga = k.sb("l_ga", [128, 512])
        av = k.sb("l_a", [128, 512])
        uv = k.sb("l_u", [128, 512])
        hs = [k.sb("l_hs%d" % c, [128, 512]) for c in range(8)]
        carry = [k.sb("l_cy%d" % c, [128, 1]) for c in range(8)]
        yb = [k.sb("l_yb%d" % i, [128, 512], BF16) for i in range(2)]
        for c in range(8):
            k.memset(carry[c][:], 0.0, writes=["l_cy%d" % c])
        it = 0
        for si, (t0, n) in enumerate(supertiles(LP)):
            h, kh = hT[si % 2], "l_hT%d" % (si % 2)
            k.dma("sp", h[:, :, :n], d["H2T"][:, t0:t0 + n].rearrange("(c p) t -> p c t", p=128), reads=[("H2T", 0)], writes=[kh])
            for c in range(8):
                it += 1
                ps, pk = k.ps()
                for kc in range(8):
                    k.mm(ps[:, :n], W[:, c, kc, :], h[:, kc, :n], kc == 0, kc == 7, reads=["l_W", kh], writes=[pk])
                g, kg = gb[it % 2], "l_gb%d" % (it % 2)
                k.copy(g[:, :n], ps[:, :n], reads=[pk], writes=[kg], eng="act")
                k.tt(g2[:, :n], g[:, :n], g[:, :n], OP.mult, reads=[kg], writes=["l_g2"])
                k.ts(g2[:, :n], g2[:, :n], 0.044715, OP.mult, reads=["l_g2"], writes=["l_g2"], s2=1.0, op1=OP.add)
                k.tt(g2[:, :n], g2[:, :n], g[:, :n], OP.mult, reads=["l_g2", kg], writes=["l_g2"])
                k.act(g2[:, :n], g2[:, :n], AF.Sigmoid, reads=["l_g2"], writes=["l_g2"], scale=1.5957691216057308)
                k.tt(g[:, :n], g[:, :n], g2[:, :n], OP.mult, reads=[kg, "l_g2"], writes=[kg])
                x, kx = xr[it % 2], "l_xr%d" % (it % 2)
                ps, pk = k.ps()
                for kc in range(8):
                    k.mm(ps[:, :n], W[:, 8 + c, kc, :], h[:, kc, :n], kc == 0, kc == 7, reads=["l_W", kh], writes=[pk])
                xp, kxp = xr[(it + 1) % 2], "l_xr%d" % ((it + 1) % 2)
                k.copy(x[:, 3:3 + n], ps[:, :n], reads=[pk], writes=[kx])
                k.dma("pool", d["XRT"][c * 128:(c + 1) * 128, t0:t0 + n], x[:, 3:3 + n], reads=[kx], writes=[("XRT", c)])
                if t0 == 0:
                    k.memset(x[:, 0:3], 0.0, writes=[kx])
                else:
                    k.dma("sp", x[:, 0:3], d["XRT"][c * 128:(c + 1) * 128, t0 - 3:t0], reads=[("XRT", c)], writes=[kx])
                cwc = VL_CW + c * 4
                k.ts(xf[:, :n], x[:, 3:3 + n], VL[:, cwc + 3:cwc + 4], OP.mult, reads=[kx, "l_VL"], writes=["l_xf"])
                for kk in (2, 1, 0):
                    k.stt(xf[:, :n], x[:, kk:kk + n], VL[:, cwc + kk:cwc + kk + 1], xf[:, :n], OP.mult, OP.add, reads=[kx, "l_xf", "l_VL"], writes=["l_xf"])
                k.ts(xf[:, :n], xf[:, :n], VL[:, VL_CB + c:VL_CB + c + 1], OP.add, reads=["l_xf", "l_VL"], writes=["l_xf"])
                ps, pk = k.ps()
                k.mm(ps[:, :n], GW[:, c, :], xf[:, :n], True, True, reads=["l_GW", "l_xf"], writes=[pk])
                k.act(gx[:, :n], ps[:, :n], AF.Sigmoid, reads=[pk, "l_VL"], writes=["l_gx"], bias=VL[:, VL_GXB + c:VL_GXB + c + 1])
                ps, pk = k.ps()
                k.mm(ps[:, :n], GW[:, 8 + c, :], xf[:, :n], True, True, reads=["l_GW", "l_xf"], writes=[pk])
                k.act(ga[:, :n], ps[:, :n], AF.Sigmoid, reads=[pk, "l_VL"], writes=["l_ga"], bias=VL[:, VL_GAB + c:VL_GAB + c + 1])
                k.act(av[:, :n], ga[:, :n], AF.Exp, reads=["l_ga", "l_sp8"], writes=["l_a"], scale=SPm8[:, c:c + 1])
                k.act(uv[:, :n], ga[:, :n], AF.Exp, reads=["l_ga", "l_sp16"], writes=["l_u"], scale=SPm16[:, c:c + 1])
                k.act(uv[:, :n], uv[:, :n], AF.Sqrt, reads=["l_u"], writes=["l_u"], scale=-1.0, bias=1.0)
                k.tt(gx[:, :n], gx[:, :n], xf[:, :n], OP.mult, reads=["l_gx", "l_xf"], writes=["l_gx"])
                k.tt(uv[:, :n], uv[:, :n], gx[:, :n], OP.mult, reads=["l_u", "l_gx"], writes=["l_u"])
                k.op("dve", lambda hh, c=c, n=n: hh.tensor_tensor_scan(out=hs[c][:, :n], data0=av[:, :n], data1=uv[:, :n], initial=carry[c][:, 0:1], op0=OP.mult, op1=OP.add),
                     reads=["l_a", "l_u", "l_cy%d" % c], writes=["l_hs%d" % c])
                k.copy(carry[c][:], hs[c][:, n - 1:n], reads=["l_hs%d" % c], writes=["l_cy%d" % c], eng="pool")
                y, ky = yb[it % 2], "l_yb%d" % (it % 2)
                k.tt(y[:, :n], hs[c][:, :n], g[:, :n], OP.mult, reads=["l_hs%d" % c, kg], writes=[ky])
                k.dma("pool", d["Y1T"][c * 128:(c + 1) * 128, t0:t0 + n], y[:, :n], reads=[ky], writes=[("Y1T", 0)])
    k.pes = None
    k.barrier()


def pack_rhs_k(w):
    K = w.shape[0]
    return np.ascontiguousarray(w.reshape(K // 128, 128, w.shape[1]).transpose(1, 0, 2)).reshape(128, -1)


def host_inputs_weights(inp):
    f = np.float32
    im = {}
    CM, VT, VF = host_consts(inp)
    im.update(CMd=CM, VTd=VT, VFd=VF)
    w_in = inp["ev_w_in"][0]
    im["WINF"] = pack_lhsT(w_in, fcols())
    im["WINZ"] = pack_rhs(w_in, zcols())
    im.update(host_consts_rwkv(inp))
    im["VT2d"] = np.concatenate([inp[n][0] for n in ("ev_ln1_g", "ev_ln1_b", "ev_ln2_g", "ev_ln2_b", "od_ln1_g", "od_ln1_b", "od_ln2_g", "od_ln2_b")])[None, :].astype(f)
    im["WO"] = pack_rhs_k(inp["ev_w_out"][0])
    im["WGU0"] = pack_lhsT(inp["ev_ffn_w_gu"][0], np.arange(5632))
    im["WD0"] = pack_rhs_k(inp["ev_ffn_w_down"][0])
    im["W1IN"] = pack_lhsT(inp["od_w_in"][0], np.arange(2048))
    gw = np.concatenate([inp["od_gx_w"][0], inp["od_ga_w"][0]], 0)
    im["GWd"] = np.ascontiguousarray(gw.transpose(1, 0, 2)).reshape(128, 16 * 128).astype(f)
    VL = np.zeros((128, NVL), f)
    VL[:, 0:32] = inp["od_conv_w"][0].reshape(4, 8, 128).transpose(2, 1, 0).reshape(128, 32)
    for off, n in ((32, "od_conv_b"), (40, "od_gx_b"), (48, "od_ga_b"), (56, "od_lambda")):
        VL[:, off:off + 8] = inp[n][0].reshape(8, 128).T
    im["VLd"] = VL
    im["W1O"] = pack_rhs_k(inp["od_w_out"][0])
    im["WRd"] = np.ascontiguousarray(inp["od_router"][0].reshape(8, 128, 8).transpose(1, 0, 2)).reshape(128, 64).astype(f)
    for e in range(NEXP):
        im["WGUE%d" % e] = pack_lhsT(inp["od_exp_w_gu"][0, e], np.arange(5632))
        im["WDE%d" % e] = pack_rhs_k(inp["od_exp_w_down"][0, e])
    return im


_CACHE = {}


def kernel(**inputs):
    inp = {k_: np.asarray(v) for k_, v in inputs.items()}
    x = inp["x"]
    B, S, _ = x.shape
    L = S + NMETA
    LP = ((L + 127) // 128) * 128
    if LP not in _CACHE:
        _CACHE[LP] = build(LP)
    P = _CACHE[LP]
    wim = host_inputs_weights(inp)
    in_maps = []
    for b in range(B):
        xin = np.zeros((LP, D), np.float32)
        xin[:NMETA] = inp["meta"]
        xin[NMETA:L] = x[b]
        m = dict(wim)
        m["xin"] = xin
        in_maps.append(m)
    res = run_bass_kernel_spmd(P.nc, in_maps, core_ids=list(range(B)))
    out = np.stack([np.asarray(r["OUT"])[NMETA:L] for r in res.results], 0)
    return out.astype(np.float32)
```

```python
from contextlib import ExitStack
import concourse.bass as bass
import concourse.mybir as mybir

F32 = mybir.dt.float32
BF16 = mybir.dt.bfloat16
AF = mybir.ActivationFunctionType
OP = mybir.AluOpType
ENGS = ["pe", "dve", "act", "pool", "sp"]
NDMA = 40


class KB:
    def __init__(self, nc, es: ExitStack):
        self.nc = nc
        self.es = es
        self.ops = {e: [] for e in ENGS}
        self.cnt = {e: 0 for e in ENGS}
        self.waited = {e: {} for e in ENGS}
        self.last_w = {}
        self.readers = {}
        self.sem = {e: es.enter_context(nc.semaphore("s_" + e)) for e in ENGS}
        self.dsem = [es.enter_context(nc.semaphore("d%d" % i)) for i in range(NDMA)]
        self.dval = [0] * NDMA
        self.dnext = 0
        self.semid = {}
        for e in ENGS:
            self.semid[("e", e)] = self.sem[e]
        for i in range(NDMA):
            self.semid[("d", i)] = self.dsem[i]
        self.ntile = 0
        self.psb = [self.psum_t("psb%d" % i, [128, 512], F32) for i in range(8)]
        self.psi = 0
        self.final_tokens = []

    def sb(self, name, shape, dt=F32):
        self.ntile += 1
        es = self.pes if getattr(self, "pes", None) is not None else self.es
        return es.enter_context(self.nc.sbuf_tensor(name, list(shape), dt))

    def barrier(self):
        allw = []
        for i in range(NDMA):
            if self.dval[i] > 0:
                allw.append((("d", i), self.dval[i]))
        for e in ENGS:
            if self.cnt[e] > 0:
                allw.append((("e", e), self.cnt[e]))
        semid = self.semid
        for e in ENGS:
            waits = self._waits(e, allw, skip_self=True)

            def run(h, waits=waits):
                for s, v in waits:
                    h.wait_ge(semid[s], v)

            self.ops[e].append(run)

    def psum_t(self, name, shape, dt=F32):
        return self.es.enter_context(self.nc.psum_tensor(name, list(shape), dt))

    def ps(self):
        i = self.psi
        self.psi = (self.psi + 1) % 7
        return self.psb[i], "psb%d" % i

    def _deps(self, reads, writes):
        deps = []
        for k in reads:
            if k in self.last_w:
                deps.append(self.last_w[k])
            if isinstance(k, str) and k.startswith("psb"):
                deps.extend(self.readers.get(k, {}).values())
        for k in writes:
            if k in self.last_w:
                deps.append(self.last_w[k])
            deps.extend(self.readers.get(k, {}).values())
        return deps

    def _waits(self, eng, deps, skip_self=False):
        w = {}
        for (s, v) in deps:
            if skip_self and s == ("e", eng):
                continue
            if self.waited[eng].get(s, 0) < v and w.get(s, 0) < v:
                w[s] = v
        for s, v in w.items():
            self.waited[eng][s] = v
        return list(w.items())

    def _commit(self, tok, reads, writes):
        for k in writes:
            self.last_w[k] = tok
            self.readers[k] = {}
        for k in reads:
            if k in writes:
                continue
            r = self.readers.setdefault(k, {})
            if r.get(tok[0], (None, 0))[1] < tok[1]:
                r[tok[0]] = tok

    def op(self, eng, fn, reads=(), writes=()):
        deps = self._deps(reads, writes)
        waits = self._waits(eng, deps, skip_self=(eng == "pe"))
        self.cnt[eng] += 1
        tok = (("e", eng), self.cnt[eng])
        semid = self.semid
        mysem = self.sem[eng]

        def run(h):
            for s, v in waits:
                h.wait_ge(semid[s], v)
            fn(h).then_inc(mysem, 1)

        self.ops[eng].append(run)
        self._commit(tok, reads, writes)
        return tok

    def dma(self, q, out, in_, reads=(), writes=(), **kw):
        slot = self.dnext
        self.dnext = (self.dnext + 1) % NDMA
        deps = self._deps(reads, writes)
        if self.dval[slot] > 0:
            deps.append((("d", slot), self.dval[slot]))
        waits = self._waits(q, deps)
        self.dval[slot] += 16
        tok = (("d", slot), self.dval[slot])
        semid = self.semid
        ds = self.dsem[slot]

        def run(h):
            for s, v in waits:
                h.wait_ge(semid[s], v)
            h.dma_start(out=out, in_=in_, **kw).then_inc(ds, 16)

        self.ops[q].append(run)
        self._commit(tok, reads, writes)
        return tok

    def finish(self, eng="sp"):
        waits = []
        for i in range(NDMA):
            if self.dval[i] > 0:
                waits.append((("d", i), self.dval[i]))
        for e in ENGS:
            if self.cnt[e] > 0:
                waits.append((("e", e), self.cnt[e]))
        semid = self.semid

        def run(h):
            for s, v in waits:
                h.wait_ge(semid[s], v)

        self.ops[eng].append(run)

    def emit(self):
        nc = self.nc
        with nc.Block() as block:
            @block.tensor
            def _(h):
                for f in self.ops["pe"]:
                    f(h)

            @block.vector
            def _(h):
                for f in self.ops["dve"]:
                    f(h)

            @block.scalar
            def _(h):
                for f in self.ops["act"]:
                    f(h)

            @block.gpsimd
            def _(h):
                for f in self.ops["pool"]:
                    f(h)

            @block.sync
            def _(h):
                for f in self.ops["sp"]:
                    f(h)

    def mm(self, out, lhsT, rhs, start, stop, reads, writes):
        return self.op("pe", lambda h: h.matmul(out, lhsT, rhs, start=start, stop=stop), reads, writes)

    def tr(self, out, in_, ident, reads, writes):
        return self.op("pe", lambda h: h.transpose(out, in_, ident), reads, writes)

    def act(self, out, in_, func, reads, writes, bias=None, scale=None, eng="act"):
        kw = {}
        if bias is not None:
            kw["bias"] = bias
        if scale is not None:
            kw["scale"] = scale
        return self.op("act", lambda h: h.activation(out=out, in_=in_, func=func, **kw), reads, writes)

    def tt(self, out, in0, in1, op, reads, writes, eng="dve"):
        return self.op(eng, lambda h: h.tensor_tensor(out=out, in0=in0, in1=in1, op=op), reads, writes)

    def ts(self, out, in0, s1, op0, reads, writes, s2=None, op1=None, eng="dve"):
        if op1 is None:
            return self.op(eng, lambda h: h.tensor_scalar(out=out, in0=in0, scalar1=s1, scalar2=None, op0=op0), reads, writes)
        return self.op(eng, lambda h: h.tensor_scalar(out=out, in0=in0, scalar1=s1, scalar2=s2, op0=op0, op1=op1), reads, writes)

    def stt(self, out, in0, scalar, in1, op0, op1, reads, writes):
        return self.op("dve", lambda h: h.scalar_tensor_tensor(out=out, in0=in0, scalar=scalar, in1=in1, op0=op0, op1=op1), reads, writes)

    def copy(self, out, in_, reads, writes, eng="dve"):
        if eng == "act":
            return self.op("act", lambda h: h.copy(out=out, in_=in_), reads, writes)
        return self.op(eng, lambda h: h.tensor_copy(out=out, in_=in_), reads, writes)

    def memset(self, ap, val, writes, eng="dve"):
        return self.op(eng, lambda h: h.memset(ap, val), (), writes)
import numpy as np
from contextlib import ExitStack
import concourse.bass as bass
import concourse.mybir as mybir
from concourse.bass_utils import run_bass_kernel_spmd

D = 1024
NMETA = 16
DFF = 2816
NEXP = 8
ALPHA = 4 ** 0.25
LN_EPS = 1e-5
RWKV_EPS = 64e-5

C_XS, C_B, C_C, C_R, C_K, C_V, C_LO, C_G = 0, 8, 10, 12, 20, 28, 36, 37
NFC = 38


def supertiles(LP):
    out = []
    t = 0
    while t < LP:
        n = min(512, LP - t)
        out.append((t, n))
        t += n
    return out


class Prog:
    def pump_casts(self, n):
        for _ in range(n):
            if self.pending:
                self.pending.pop(0)()

    def __init__(self, LP, dbg=()):
        self.pending = []
        self.LP = LP
        self.dbg = set(dbg)
        self.nc = bass.Bass("TRN2", target_bir_lowering=False)
        self.es = ExitStack()
        self.k = None
        self.dram = {}

    def din(self, name, shape, dt=F32):
        t = self.nc.dram_tensor(name, list(shape), dt, kind="ExternalInput").ap()
        self.dram[name] = t
        return t

    def dscr(self, name, shape, dt=F32):
        kind = "ExternalOutput" if name in self.dbg else "Internal"
        t = self.nc.dram_tensor(name, list(shape), dt, kind=kind).ap()
        self.dram[name] = t
        return t

    def dout(self, name, shape, dt=F32):
        t = self.nc.dram_tensor(name, list(shape), dt, kind="ExternalOutput").ap()
        self.dram[name] = t
        return t


def cast_weights(P, src, dst, nrows, key, defer=False):
    k = P.k
    step = 512
    for r0 in range(0, nrows, step):
        r1 = min(nrows, r0 + step)

        def go(r0=r0, r1=r1):
            k.dma("pool", dst[r0:r1, :], src[r0:r1, :], reads=(), writes=((key, r0 // step),))

        if defer:
            P.pending.append(go)
        else:
            go()


def phase1(P):
    k, LP = P.k, P.LP
    d = P.dram
    xt = [k.sb("xt%d" % i, [128, 1024]) for i in range(4)]
    hT = k.sb("hT", [128, 8, 512], BF16)
    wf = [k.sb("wf%d" % i, [128, 8, 128], BF16) for i in range(3)]
    wz = k.sb("wz", [128, 8, 1040], BF16)
    stg = [k.sb("stg%d" % i, [128, 512]) for i in range(3)]
    zst = [k.sb("zst%d" % i, [128, 1040]) for i in range(2)]
    ident = P.ident
    k.dma("sp", wz[:].rearrange("p a b -> p (a b)"), d["WINZb"][:, :], reads=[("WINZb", i) for i in range(1)], writes=["wz"])
    wi = 0
    si = 0
    zi = 0
    for (t0, n) in supertiles(LP):
        nj = n // 128
        for j in range(nj):
            k.dma("sp", xt[j][:], d["xin"][t0 + j * 128:t0 + (j + 1) * 128, :], reads=[], writes=["xt%d" % j])
        for kc in range(8):
            ps, pk = k.ps()
            for j in range(nj):
                k.tr(ps[:, j * 128:(j + 1) * 128], xt[j][:, kc * 128:(kc + 1) * 128], ident[:], reads=["xt%d" % j, "ident"], writes=[pk])
            k.copy(hT[:, kc, :n], ps[:, :n], reads=[pk], writes=[("hT", kc)], eng=("act" if kc % 2 else "dve"))
        for c in range(NFC):
            w = wf[wi % 3]
            wk = "wf%d" % (wi % 3)
            wi += 1
            k.dma("sp", w[:].rearrange("p a b -> p (a b)"), d["WINFb"][c * 128:(c + 1) * 128, :], reads=[("WINFb", (c * 128) // 512)], writes=[wk])
            ps, pk = k.ps()
            for kc in range(8):
                k.mm(ps[:, :n], w[:, kc, :], hT[:, kc, :n], kc == 0, kc == 7, reads=[wk, ("hT", kc)], writes=[pk])
            s = stg[si % 3]
            sk = "stg%d" % (si % 3)
            si += 1
            k.copy(s[:, :n], ps[:, :n], reads=[pk], writes=[sk], eng=("act" if c % 2 else "dve"))
            k.dma("pool", d["PT"][c * 128:(c + 1) * 128, t0:t0 + n], s[:, :n], reads=[sk], writes=[("PT", c)])
        for j in range(nj):
            z = zst[zi % 2]
            zk = "zst%d" % (zi % 2)
            zi += 1
            for (c0, c1) in ((0, 512), (512, 1024), (1024, 1040)):
                ps, pk = k.ps()
                for kc in range(8):
                    k.mm(ps[:, :c1 - c0], hT[:, kc, j * 128:(j + 1) * 128], wz[:, kc, c0:c1], kc == 0, kc == 7,
                         reads=["wz", ("hT", kc)], writes=[pk])
                k.copy(z[:, c0:c1], ps[:, :c1 - c0], reads=[pk], writes=[zk], eng=("act" if c0 == 512 else "dve"))
            k.dma("pool", d["Z"][t0 + j * 128:t0 + (j + 1) * 128, :], z[:], reads=[zk], writes=[("Z", 0)])


import os
LVL = int(os.environ.get('LVL', '99'))
R3 = int(os.environ.get('R3', '99'))
R3C = int(os.environ.get('R3C', '99'))
R3L = int(os.environ.get('R3L', '99'))
NVT = 32 + 2048
NVF = 64


def build(LP, dbg=(), upto=99):
    P = Prog(LP, dbg)
    nc = P.nc
    P.din("xin", [LP, D])
    P.din("CMd", [128, 640])
    P.din("VTd", [1, NVT])
    P.din("VFd", [128, NVF])
    P.din("WINF", [NFC * 128, 1024])
    P.din("WINZ", [128, 8 * 1040])
    P.din("CRd", [128, NCR])
    P.din("VRd", [128, NVR])
    P.din("WUAd", [128, 1024])
    P.din("GUPd", [128, 1024])
    P.din("LNGd", [128, 512])
    P.din("LNBd", [128, 512])
    P.din("VT2d", [1, 8192])
    P.din("WO", [128, 16 * 1024])
    P.din("WGU0", [44 * 128, 1024])
    P.din("WD0", [128, 22 * 1024])
    P.din("W1IN", [16 * 128, 1024])
    P.din("GWd", [128, 16 * 128])
    P.din("VLd", [128, NVL])
    P.din("W1O", [128, 8 * 1024])
    P.din("WRd", [128, 64])
    for e in range(NEXP):
        P.din("WGUE%d" % e, [44 * 128, 1024])
        P.din("WDE%d" % e, [128, 22 * 1024])
    P.dscr("WOb", [128, 16 * 1024], BF16)
    P.dscr("WGU0b", [44 * 128, 1024], BF16)
    P.dscr("WD0b", [128, 22 * 1024], BF16)
    P.dscr("W1INb", [16 * 128, 1024], BF16)
    P.dscr("W1Ob", [128, 8 * 1024], BF16)
    for e in range(NEXP):
        P.dscr("WGUEb%d" % e, [44 * 128, 1024], BF16)
        P.dscr("WDEb%d" % e, [128, 22 * 1024], BF16)
    P.dscr("H1", [LP, D])
    P.dscr("H1T", [D, LP], BF16)
    P.dscr("H2", [LP, D])
    P.dscr("H2T", [D, LP], BF16)
    P.dscr("XRT", [D, LP])
    P.dscr("Y1T", [D, LP], BF16)
    P.dscr("H3", [LP, D])
    P.dscr("H3T", [D, LP], BF16)
    P.dscr("H3T32", [D, LP])
    P.dout("OUT", [LP, D])
    P.dscr("WINFb", [NFC * 128, 1024], BF16)
    P.dscr("WINZb", [128, 8 * 1040], BF16)
    P.dscr("PT", [NFC * 128, LP])
    P.dscr("Z", [LP, 1040])
    P.dscr("XS", [LP, 1024])
    P.dscr("BTOK", [LP, 256])
    P.dscr("BT", [256, LP])
    P.dscr("CT", [256, LP])
    P.dscr("YT", [2048, LP], BF16)
    with P.es:
        k = KB(nc, P.es)
        P.k = k
        P.CM = k.sb("CM", [128, 640])
        P.ident = P.CM[:, 0:128]
        P.VT = k.sb("VT", [128, NVT])
        P.VF = k.sb("VF", [128, NVF])
        k.dma("sp", P.CM[:], P.dram["CMd"][:, :], writes=["CM", "ident"])
        k.dma("sp", P.VT[:], P.dram["VTd"][0:1, :].partition_broadcast(128), writes=["VT"])
        k.dma("sp", P.VF[:], P.dram["VFd"][:, :], writes=["VF"])
        cast_weights(P, P.dram["WINF"], P.dram["WINFb"], NFC * 128, "WINFb")
        k.dma("pool", P.dram["WINZb"][:, :].rearrange("p (a b) -> p a b", b=1040), P.dram["WINZ"][:, :].rearrange("p (a b) -> p a b", b=1040), writes=[("WINZb", 0)])
        if upto >= 4:
            def cast_rhs(src, dst, nk):
                P.pending.append(lambda: k.dma("pool", P.dram[dst][:, :].rearrange("p (a b) -> p a b", b=1024), P.dram[src][:, :].rearrange("p (a b) -> p a b", b=1024), writes=[(dst, 0)]))
            cast_rhs("WO", "WOb", 16)
            cast_weights(P, P.dram["WGU0"], P.dram["WGU0b"], 44 * 128, "WGU0b", defer=True)
            cast_rhs("WD0", "WD0b", 22)
            cast_weights(P, P.dram["W1IN"], P.dram["W1INb"], 16 * 128, "W1INb", defer=True)
            cast_rhs("W1O", "W1Ob", 8)
            for e in range(NEXP):
                cast_weights(P, P.dram["WGUE%d" % e], P.dram["WGUEb%d" % e], 44 * 128, "WGUEb%d" % e, defer=True)
                cast_rhs("WDE%d" % e, "WDEb%d" % e, 22)
        if upto >= 1:
            with ExitStack() as pes:
                k.pes = pes
                phase1(P)
            k.pes = None
            k.barrier()
        if upto >= 1.5:
            phase2a(P)
        if upto >= 2 and upto != 3.5:
            phase2b(P)
        if upto >= 3:
            phase3(P)
        if upto >= 4:
            P.pump_casts(100000)
            proj_res_ln(P, "p4", "YT", 16, "WOb", "xin", 0, 1024, "H1", "H1T")
            ffn_phase(P, "f0", "H1T", "H1", ["WGU0b"], ["WD0b"], 2048, 3072, "H2", "H2T")
        if upto >= 5:
            phase5(P)
            proj_res_ln(P, "p5", "Y1T", 8, "W1Ob", "H2", 4096, 5120, "H3", "H3T", "H3T32")
        if upto >= 6:
            ffn_phase(P, "f1", "H3T", "H3", ["WGUEb%d" % e for e in range(NEXP)], ["WDEb%d" % e for e in range(NEXP)], 6144, 7168, "OUT", None, router="H3T32", tile_n=1024)
        k.finish("sp")
        k.emit()
    return P


def host_consts(inp):
    t = np.arange(128)
    CM = np.zeros((128, 640), np.float32)
    CM[:, 0:128] = np.eye(128)
    CM[:, 128:256] = (t[:, None] <= t[None, :])
    CM[:, 256:384] = (t[:, None] > t[None, :])
    CM[:, 384:512] = 1.0
    CM[:, 512:640] = np.where(t[None, :] >= t[:, None], 0.0, -30000.0)
    VT = np.zeros((1, NVT), np.float32)
    VT[0, 0:16] = inp["ev_dt_bias"][0]
    VT[0, 16:32] = inp["ev_a_log"][0]
    VT[0, 32:32 + 1024] = np.repeat(inp["ev_d_skip"][0], 64)
    VT[0, 32 + 1024:32 + 2048] = inp["ev_ssm_norm"][0]
    VF = np.zeros((128, NVF), np.float32)
    cw = inp["ev_conv_w"][0]
    VF[:, 0:48] = cw.reshape(4, 12, 128).transpose(2, 1, 0).reshape(128, 48)
    VF[:, 48:60] = inp["ev_conv_b"][0].reshape(12, 128).T
    return CM, VT, VF


def fcols():
    return np.concatenate([np.arange(1024, 2048), np.arange(2048, 2304), np.arange(2304, 2560),
                           np.arange(2576, 3600), np.arange(3600, 4624), np.arange(4624, 5648),
                           np.arange(5648, 5776), np.arange(5776, 5904)])


def zcols():
    return np.concatenate([np.arange(0, 1024), np.arange(2560, 2576)])


def pack_lhsT(w, cols):
    ws = w[:, cols]
    nch = ws.shape[1] // 128
    a = ws.reshape(8, 128, nch, 128)
    return np.ascontiguousarray(a.transpose(2, 1, 0, 3)).reshape(nch * 128, 1024)


def pack_rhs(w, cols):
    ws = w[:, cols]
    n = ws.shape[1]
    return np.ascontiguousarray(ws.reshape(8, 128, n).transpose(1, 0, 2)).reshape(128, 8 * n)


CM_ID, CM_TRI, CM_TRIS, CM_ONES, CM_MB = 0, 128, 256, 384, 512
VT_DTB, VT_ALOG, VT_D, VT_NW = 0, 16, 32, 32 + 1024
VF_CW, VF_CB = 0, 48


def phase2a(P):
    k, LP, d = P.k, P.LP, P.dram
    P.pump_casts(19)
    with ExitStack() as pes:
        k.pes = pes
        raw = [k.sb("c_raw%d" % i, [128, LP + 3]) for i in range(2)]
        acc = [k.sb("c_acc%d" % i, [128, LP]) for i in range(2)]
        stg = [k.sb("c_stg%d" % i, [128, 512]) for i in range(3)]
        VF = P.VF
        for i in range(2):
            k.memset(raw[i][:, 0:3], 0.0, writes=["c_raw%d" % i])
        si = 0
        for c in range(12):
            r, rk = raw[c % 2], "c_raw%d" % (c % 2)
            a, ak = acc[c % 2], "c_acc%d" % (c % 2)
            k.dma("sp", r[:, 3:3 + LP], d["PT"][c * 128:(c + 1) * 128, :], reads=[("PT", c)], writes=[rk])
            cw = VF_CW + c * 4
            k.ts(a[:], r[:, 3:3 + LP], VF[:, cw + 3:cw + 4], OP.mult, reads=[rk, "VF"], writes=[ak])
            for kk in (2, 1, 0):
                k.stt(a[:], r[:, kk:kk + LP], VF[:, cw + kk:cw + kk + 1], a[:], OP.mult, OP.add, reads=[rk, ak, "VF"], writes=[ak])
            k.act(a[:], a[:], AF.Silu, reads=[ak, "VF"], writes=[ak], bias=VF[:, VF_CB + c:VF_CB + c + 1])
            if c >= 8:
                dst = d["BT"] if c < 10 else d["CT"]
                cc = (c - 8) % 2
                k.dma("pool", dst[cc * 128:(cc + 1) * 128, :], a[:], reads=[ak], writes=[("BT" if c < 10 else "CT", cc)])
            if c < 10:
                for (t0, n) in supertiles(LP):
                    nj = n // 128
                    ps, pk = k.ps()
                    for j in range(nj):
                        k.tr(ps[:, j * 128:(j + 1) * 128], a[:, t0 + j * 128:t0 + (j + 1) * 128], P.CM[:, CM_ID:CM_ID + 128], reads=[ak, "CM"], writes=[pk])
                    s, sk = stg[si % 3], "c_stg%d" % (si % 3)
                    si += 1
                    k.copy(s[:, :n], ps[:, :n], reads=[pk], writes=[sk], eng=("act" if si % 2 else "dve"))
                    if c < 8:
                        dd = d["XS"][t0:t0 + n, c * 128:(c + 1) * 128]
                        key = ("XS", c)
                    else:
                        dd = d["BTOK"][t0:t0 + n, (c - 8) * 128:(c - 7) * 128]
                        key = ("BTOK", c - 8)
                    k.dma("pool", dd.rearrange("(j p) m -> p j m", p=128), s[:, :n].rearrange("p (j m) -> p j m", m=128), reads=[sk], writes=[key])
    k.pes = None
    k.barrier()


def phase2b(P):
    k, LP, d = P.k, P.LP, P.dram
    CM, VT = P.CM, P.VT
    with ExitStack() as pes:
        k.pes = pes
        nb = 2
        xs = [k.sb("s_xs%d" % i, [128, 1024]) for i in range(nb)]
        btok = [k.sb("s_btok%d" % i, [128, 256]) for i in range(nb)]
        bt = [k.sb("s_bt%d" % i, [128, 2, 128]) for i in range(nb)]
        ct = [k.sb("s_ct%d" % i, [128, 2, 128]) for i in range(nb)]
        z = [k.sb("s_z%d" % i, [128, 1040]) for i in range(nb)]
        HT = [k.sb("s_HT%d" % g, [128, 512]) for g in range(2)]
        Abc = k.sb("s_Abc", [128, 16])
        dt = k.sb("s_dt", [128, 16])
        dtA = k.sb("s_dtA", [128, 16])
        E = k.sb("s_E", [128, 64])
        ncum = E[:, 48:64]
        LT = k.sb("s_LT", [128, 16, 128])
        SLT = k.sb("s_SLT", [128, 16, 128])
        xdt = k.sb("s_xdt", [128, 1024])
        xde = k.sb("s_xde", [128, 1024])
        y = k.sb("s_y", [128, 1024])
        t1 = k.sb("s_t1", [128, 1024])
        zs = k.sb("s_zs", [128, 1024])
        sq = k.sb("s_sq", [128, 512])
        ss = k.sb("s_ss", [128, 2])
        rstd = k.sb("s_rstd", [128, 2])
        yTs = [k.sb("s_yT%d" % i, [128, 8, 128], BF16) for i in range(2)]
        for g in range(2):
            k.memset(HT[g][:], 0.0, writes=["s_HT%d" % g])
        k.act(Abc[:], VT[:, VT_ALOG:VT_ALOG + 16], AF.Exp, reads=["VT"], writes=["s_Abc"])
        k.ts(Abc[:], Abc[:], -1.0, OP.mult, reads=["s_Abc"], writes=["s_Abc"])
        nch = LP // 128
        for ci in range(nch):
            t0 = ci * 128
            b = ci % nb
            kx, kb, kbt, kct, kz = "s_xs%d" % b, "s_btok%d" % b, "s_bt%d" % b, "s_ct%d" % b, "s_z%d" % b
            k.dma("sp", xs[b][:], d["XS"][t0:t0 + 128, :], reads=[("XS", c) for c in range(8)], writes=[kx])
            k.dma("sp", btok[b][:], d["BTOK"][t0:t0 + 128, :], reads=[("BTOK", 0), ("BTOK", 1)], writes=[kb])
            k.dma("sp", bt[b][:], d["BT"][:, t0:t0 + 128].rearrange("(g n) t -> n g t", n=128), reads=[("BT", 0), ("BT", 1)], writes=[kbt])
            k.dma("sp", ct[b][:], d["CT"][:, t0:t0 + 128].rearrange("(g n) t -> n g t", n=128), reads=[("CT", 0), ("CT", 1)], writes=[kct])
            k.dma("sp", z[b][:], d["Z"][t0:t0 + 128, :], reads=[("Z", 0)], writes=[kz])
            if LVL < 1: continue
            k.tt(dt[:], z[b][:, 1024:1040], VT[:, VT_DTB:VT_DTB + 16], OP.add, reads=[kz, "VT"], writes=["s_dt"])
            k.act(dt[:], dt[:], AF.Exp, reads=["s_dt"], writes=["s_dt"])
            k.act(dt[:], dt[:], AF.Ln, reads=["s_dt"], writes=["s_dt"], bias=1.0)
            k.tt(dtA[:], dt[:], Abc[:], OP.mult, reads=["s_dt", "s_Abc"], writes=["s_dtA"])
            if LVL < 2: continue
            psc, pkc = k.ps()
            k.mm(psc[:, 0:16], CM[:, CM_TRI:CM_TRI + 128], dtA[:], True, True, reads=["CM", "s_dtA"], writes=[pkc])
            k.mm(psc[:, 16:32], CM[:, CM_TRIS:CM_TRIS + 128], dtA[:], True, True, reads=["CM", "s_dtA"], writes=[pkc])
            k.mm(psc[:, 32:48], CM[:, CM_ONES:CM_ONES + 128], dtA[:], True, True, reads=["CM", "s_dtA"], writes=[pkc])
            SUB = int(os.environ.get('SUB', '3'))
            if SUB >= 1:
                k.act(E[:, 0:48], psc[:, 0:48], AF.Exp, reads=[pkc], writes=["s_E"])
            if SUB >= 2:
                k.ts(ncum, psc[:, 0:16], -1.0, OP.mult, reads=[pkc, "s_E"], writes=["s_ncum"])
            if LVL < 3: continue
            k.tt(xdt[:].rearrange("p (h e) -> p h e", e=64), xs[b][:].rearrange("p (h e) -> p h e", e=64),
                 dt[:].unsqueeze(2).broadcast_to([128, 16, 64]), OP.mult, reads=[kx, "s_dt"], writes=["s_xdt"])
            k.tt(xde[:].rearrange("p (h e) -> p h e", e=64), xdt[:].rearrange("p (h e) -> p h e", e=64),
                 E[:, 16:32].unsqueeze(2).broadcast_to([128, 16, 64]), OP.mult, reads=["s_xdt", "s_E"], writes=["s_xde"])
            if LVL < 4: continue
            for hq in range(4):
                psa, pka = k.ps()
                for hh in range(4):
                    h = hq * 4 + hh
                    k.mm(psa[:, hh * 128:(hh + 1) * 128], dtA[:, h:h + 1].broadcast_to([128, 128]), CM[:, CM_TRI:CM_TRI + 128], True, True,
                         reads=["s_dtA", "CM"], writes=[pka])
                for hh in range(4):
                    h = hq * 4 + hh
                    k.stt(LT[:, h, :], psa[:, hh * 128:(hh + 1) * 128], E[:, 48 + h:49 + h], CM[:, CM_MB:CM_MB + 128], OP.add, OP.add,
                          reads=[pka, "s_ncum", "CM"], writes=[("s_LT", hq)])
            for hq in range(4):
                k.act(LT[:, hq * 4:(hq + 1) * 4, :], LT[:, hq * 4:(hq + 1) * 4, :], AF.Exp, reads=[("s_LT", hq)], writes=[("s_LT", hq)])
            if LVL < 5: continue
            pss, pks = k.ps()
            for g in range(2):
                k.mm(pss[:, g * 128:(g + 1) * 128], bt[b][:, g, :], ct[b][:, g, :], True, True, reads=[kbt, kct], writes=[pks])
            for g in range(2):
                k.tt(SLT[:, g * 8:(g + 1) * 8, :], LT[:, g * 8:(g + 1) * 8, :],
                     pss[:, g * 128:(g + 1) * 128].unsqueeze(1).broadcast_to([128, 8, 128]), OP.mult,
                     reads=[("s_LT", 2 * g), ("s_LT", 2 * g + 1), pks], writes=[("s_SLT", g)])
            if LVL < 6: continue
            for g in range(2):
                psy, pky = k.ps()
                for hh in range(8):
                    h = g * 8 + hh
                    k.mm(psy[:, hh * 64:(hh + 1) * 64], SLT[:, h, :], xdt[:, h * 64:(h + 1) * 64], True, True, reads=[("s_SLT", g), "s_xdt"], writes=[pky])
                if R3L < 4: continue
                pso, pko = k.ps()
                k.mm(pso[:, :], ct[b][:, g, :], HT[g][:], True, True, reads=[kct, "s_HT%d" % g], writes=[pko])
                gs = slice(g * 512, (g + 1) * 512)
                k.tt(t1[:, gs].rearrange("p (h e) -> p h e", e=64), pso[:, :].rearrange("p (h e) -> p h e", e=64),
                     E[:, g * 8:(g + 1) * 8].unsqueeze(2).broadcast_to([128, 8, 64]), OP.mult, reads=[pko, "s_E"], writes=[("s_t1", g)])
                k.tt(y[:, gs], psy[:, :], t1[:, gs], OP.add, reads=[pky, ("s_t1", g)], writes=[("s_y", g)])
                psh, pkh = k.ps()
                k.mm(psh[:, :], btok[b][:, g * 128:(g + 1) * 128], xde[:, gs], True, True, reads=[kb, "s_xde"], writes=[pkh])
                k.tt(HT[g][:].rearrange("p (h e) -> p h e", e=64), HT[g][:].rearrange("p (h e) -> p h e", e=64),
                     E[:, 32 + g * 8:32 + (g + 1) * 8].unsqueeze(2).broadcast_to([128, 8, 64]), OP.mult, reads=["s_HT%d" % g, "s_E"], writes=["s_HT%d" % g])
                k.tt(HT[g][:], HT[g][:], psh[:, :], OP.add, reads=["s_HT%d" % g, pkh], writes=["s_HT%d" % g])
            if LVL < 7: continue
            k.tt(t1[:], xs[b][:], VT[:, VT_D:VT_D + 1024], OP.mult, reads=[kx, "VT", ("s_t1", 0), ("s_t1", 1)], writes=[("s_t1", 0), ("s_t1", 1)], eng="pool")
            k.tt(y[:], y[:], t1[:], OP.add, reads=[("s_y", 0), ("s_y", 1), ("s_t1", 0), ("s_t1", 1)], writes=[("s_y", 0), ("s_y", 1)])
            k.act(zs[:], z[b][:, 0:1024], AF.Silu, reads=[kz], writes=["s_zs"])
            k.tt(y[:], y[:], zs[:], OP.mult, reads=[("s_y", 0), ("s_y", 1), "s_zs"], writes=[("s_y", 0), ("s_y", 1)])
            for g in range(2):
                gs = slice(g * 512, (g + 1) * 512)
                k.op("act", lambda h, g=g, gs=gs: h.activation(out=sq[:], in_=y[:, gs], func=AF.Square, accum_out=ss[:, g:g + 1]),
                     reads=[("s_y", 0), ("s_y", 1)], writes=["s_sq", ("s_ss", g)])
            k.act(rstd[:], ss[:], AF.Sqrt, reads=[("s_ss", 0), ("s_ss", 1)], writes=["s_rstd"], bias=LN_EPS, scale=1.0 / 512)
            k.op("dve", lambda h: h.reciprocal(out=rstd[:], in_=rstd[:]), reads=["s_rstd"], writes=["s_rstd"])
            for g in range(2):
                gs = slice(g * 512, (g + 1) * 512)
                k.stt(y[:, gs], y[:, gs], rstd[:, g:g + 1], VT[:, VT_NW + g * 512:VT_NW + (g + 1) * 512], OP.mult, OP.mult,
                      reads=[("s_y", g), "s_rstd", "VT"], writes=[("s_y", g)])
            if LVL < 8: continue
            yT, kyT = yTs[ci % 2], "s_yT%d" % (ci % 2)
            for half in range(2):
                pst, pkt = k.ps()
                for cc in range(4):
                    c = half * 4 + cc
                    k.tr(pst[:, cc * 128:(cc + 1) * 128], y[:, c * 128:(c + 1) * 128], CM[:, CM_ID:CM_ID + 128], reads=[("s_y", 0), ("s_y", 1), "CM"], writes=[pkt])
                k.copy(yT[:, half * 4:(half + 1) * 4, :], pst[:, :].rearrange("p (c t) -> p c t", t=128), reads=[pkt], writes=[kyT], eng=("act" if half else "dve"))
            k.dma("pool", d["YT"][0:1024, t0:t0 + 128].rearrange("(c p) t -> p c t", p=128), yT[:], reads=[kyT], writes=[("YT", 0)])
    k.pes = None
    k.barrier()


CR_M1, CR_M2, CR_SLN, CR_ISEL, CR_BD, CR_BONES, CR_RESET = 0, 256, 512, 640, 704, 832, 960
NCR = 960 + 512
VR_MUR, VR_MUK, VR_MUV, VR_W0, VR_A0, VR_KK, VR_KA, VR_RK, VR_MULO, VR_MUG = 0, 8, 16, 24, 32, 40, 48, 56, 64, 65
NVR = 66


def phase3(P):
    k, LP, d = P.k, P.LP, P.dram
    CM = P.CM
    with ExitStack() as pes:
        k.pes = pes
        CR = k.sb("CR", [128, NCR])
        VR = k.sb("VR", [128, NVR])
        OMKA = k.sb("OMKA", [128, 8])
        WUA = k.sb("WUA", [128, 1024])
        GUP = k.sb("GUP", [128, 1024])
        LNG = k.sb("LNG", [128, 512])
        LNB = k.sb("LNB", [128, 512])
        k.dma("sp", CR[:], d["CRd"][:, :], writes=["CR"])
        k.dma("sp", VR[:], d["VRd"][:, :], writes=["VR"])
        k.dma("sp", WUA[:], d["WUAd"][:, :], writes=["WUA"])
        k.dma("sp", GUP[:], d["GUPd"][:, :], writes=["GUP"])
        k.dma("sp", LNG[:], d["LNGd"][:, :], writes=["LNG"])
        k.dma("sp", LNB[:], d["LNBd"][:, :], writes=["LNB"])
        k.ts(OMKA[:], VR[:, VR_KA:VR_KA + 8], -1.0, OP.mult, reads=["VR"], writes=["OMKA"], s2=1.0, op1=OP.add)
        T = [k.sb("r_T%d" % j, [128, 64]) for j in range(8)]
        for j in range(8):
            k.memset(T[j][:], 0.0, writes=["r_T%d" % j])
        ident = CM[:, CM_ID:CM_ID + 128]
        MASK1 = CR[:, CR_M1:CR_M1 + 256]
        MASK2 = CR[:, CR_M2:CR_M2 + 256]
        MSLN = CR[:, CR_SLN:CR_SLN + 128]
        ISEL = CR[:, CR_ISEL:CR_ISEL + 64]
        BONES = CR[:, CR_BONES:CR_BONES + 128]

        def T_(name, shape, dt=F32):
            return k.sb(name, shape, dt), name

        lo_raw, klo_raw = T_("r_loraw", [128, 513])
        g_raw, kg_raw = T_("r_graw", [128, 513])
        LOm, kLOm = T_("r_LOm", [128, 512])
        Gs, kGs = T_("r_Gs", [128, 512])
        raw = {nm: [T_("r_raw%s%d" % (nm, i), [128, 513]) for i in range(2)] for nm in "rkv"}
        mx = {nm: T_("r_mx" + nm, [128, 512]) for nm in "rkv"}
        tmp, ktmp = T_("r_tmp", [128, 512])
        tmp2, ktmp2 = T_("r_tmp2", [128, 512])
        lw, klw = T_("r_lw", [128, 512])
        av, kav = T_("r_a", [128, 512])
        gate, kgate = T_("r_gate", [128, 512])
        kkv, kkk = T_("r_kk", [128, 512])
        kmod, kkmod = T_("r_kmod", [128, 512])
        bv, kbv = T_("r_b", [128, 512])
        bonus, kbonus = T_("r_bonus", [128, 512])
        cw, kcw = T_("r_cw", [128, 512])
        ecw, kecw = T_("r_ecw", [128, 512])
        ecwx, kecwx = T_("r_ecwx", [128, 512])
        eneg, keneg = T_("r_eneg", [128, 512])
        eend, keend = T_("r_eend", [128, 512])
        prod, kprod = T_("r_prod", [128, 512])
        QRbd, kQR = T_("r_QRbd", [128, 8, 2, 128])
        kibd, kki = T_("r_kibd", [128, 8, 128])
        bibd, kbi = T_("r_bibd", [128, 8, 128])
        vbd, kvb = T_("r_vbd", [128, 8, 128])
        kebd, kke = T_("r_kebd", [128, 8, 128])
        bebd, kbe = T_("r_bebd", [128, 8, 128])
        A1, kA1 = T_("r_A1", [128, 8, 256])
        A2, kA2 = T_("r_A2", [128, 8, 256])
        PP = [T_("r_PP%d" % i, [128, 8, 256]) for i in range(2)]
        QT = [T_("r_QT%d" % i, [128, 8, 128]) for i in range(2)]
        Vs, kVs = T_("r_Vs", [128, 8, 64])
        Rs, kRs = T_("r_Rs", [128, 64])
        Us, kUs = T_("r_Us", [128, 8, 64])
        KT, kKT = T_("r_KT", [128, 8, 256])
        Yall, kY = T_("r_Yall", [128, 8, 64])
        Ysq, kYsq = T_("r_Ysq", [128, 8, 64])
        st1, kst1 = T_("r_st1", [128, 8])
        st2, kst2 = T_("r_st2", [128, 8])
        obd, kobd = T_("r_obd", [128, 8, 128])
        yb = [T_("r_yb%d" % i, [128, 512], BF16) for i in range(2)]
        k.memset(lo_raw[:, 0:1], 0.0, writes=[klo_raw])
        k.memset(g_raw[:, 0:1], 0.0, writes=[kg_raw])
        for nm in "rkv":
            for i in range(2):
                k.memset(raw[nm][i][0][:, 0:1], 0.0, writes=[raw[nm][i][1]])
        BDM = CR[:, CR_BD:CR_BD + 128].rearrange("p (a b) -> p a b", b=64)
        it = 0

        def load_shift(tile, key, chunk, t0, n, mucol, outt, outk):
            if t0 == 0:
                k.dma("sp", tile[:, 1:n + 1], d["PT"][chunk * 128:(chunk + 1) * 128, 0:n], reads=[("PT", chunk)], writes=[key])
            else:
                k.dma("sp", tile[:, 0:n + 1], d["PT"][chunk * 128:(chunk + 1) * 128, t0 - 1:t0 + n], reads=[("PT", chunk)], writes=[key])
            k.tt(outt[:, :n], tile[:, 0:n], tile[:, 1:n + 1], OP.subtract, reads=[key], writes=[outk])
            k.stt(outt[:, :n], outt[:, :n], VR[:, mucol:mucol + 1], tile[:, 1:n + 1], OP.mult, OP.add, reads=[outk, key, "VR"], writes=[outk])

        def bd_expand(dst, kdst, src, ksrc, nc_, eng="dve"):
            k.tt(dst[:, :nc_].rearrange("p c (a b) -> p c a b", b=64) if len(dst.shape) == 3 else dst,
                 src.rearrange("p (c t) -> p c t", t=64).unsqueeze(2).broadcast_to([128, nc_, 2, 64]),
                 BDM.unsqueeze(1).broadcast_to([128, nc_, 2, 64]), OP.mult, reads=[ksrc, "CR"], writes=[kdst], eng=eng)

        for (t0, n) in supertiles(LP):
            nc_ = n // 64
            load_shift(lo_raw, klo_raw, C_LO, t0, n, VR_MULO, LOm, kLOm)
            k.act(LOm[0:64, :n], LOm[0:64, :n], AF.Tanh, reads=[kLOm], writes=[kLOm])
            load_shift(g_raw, kg_raw, C_G, t0, n, VR_MUG, Gs, kGs)
            k.act(Gs[:, :n], Gs[:, :n], AF.Sigmoid, reads=[kGs], writes=[kGs])
            for j in range(8):
                it += 1
                for nm, ch, mu in (("r", C_R, VR_MUR), ("k", C_K, VR_MUK), ("v", C_V, VR_MUV)):
                    rt, rk_ = raw[nm][it % 2]
                    load_shift(rt, rk_, ch + j, t0, n, mu + j, mx[nm][0], mx[nm][1])
                r_, kr_ = mx["r"]
                k_, kk_ = mx["k"]
                v_, kv_ = mx["v"]
                js = slice(j * 128, (j + 1) * 128)
                if R3 < 1: continue
                ps, pk = k.ps()
                k.mm(ps[:, :n], WUA[0:64, js], LOm[0:64, :n], True, True, reads=["WUA", kLOm], writes=[pk])
                k.act(lw[:, :n], ps[:, :n], AF.Sigmoid, reads=[pk, "VR"], writes=[klw], bias=VR[:, VR_W0 + j:VR_W0 + j + 1])
                k.ts(lw[:, :n], lw[:, :n], -float(np.exp(-0.5)), OP.mult, reads=[klw], writes=[klw])
                ps, pk = k.ps()
                k.mm(ps[:, :n], WUA[64:128, js], LOm[64:128, :n], True, True, reads=["WUA", kLOm], writes=[pk])
                k.act(av[:, :n], ps[:, :n], AF.Sigmoid, reads=[pk, "VR"], writes=[kav], bias=VR[:, VR_A0 + j:VR_A0 + j + 1])
                ps, pk = k.ps()
                k.mm(ps[:, :n], GUP[:, js], Gs[:, :n], True, True, reads=["GUP", kGs], writes=[pk])
                k.copy(gate[:, :n], ps[:, :n], reads=[pk], writes=[kgate], eng="act")
                if R3 < 2: continue
                k.ts(kkv[:, :n], k_[:, :n], VR[:, VR_KK + j:VR_KK + j + 1], OP.mult, reads=[kk_, "VR"], writes=[kkk])
                k.tt(tmp[:, :n], kkv[:, :n], kkv[:, :n], OP.mult, reads=[kkk], writes=[ktmp])
                ps, pk = k.ps()
                k.mm(ps[:, :n], BONES, tmp[:, :n], True, True, reads=["CR", ktmp], writes=[pk])
                k.ts(tmp2[:, :n], ps[:, :n], 1e-24, OP.max, reads=[pk], writes=[ktmp2])
                k.act(tmp2[:, :n], tmp2[:, :n], AF.Sqrt, reads=[ktmp2], writes=[ktmp2])
                k.op("dve", lambda h, n=n: h.reciprocal(out=tmp2[:, :n], in_=tmp2[:, :n]), reads=[ktmp2], writes=[ktmp2])
                k.tt(kkv[:, :n], kkv[:, :n], tmp2[:, :n], OP.mult, reads=[kkk, ktmp2], writes=[kkk])
                k.ts(tmp[:, :n], av[:, :n], VR[:, VR_KA + j:VR_KA + j + 1], OP.mult, reads=[kav, "VR", "OMKA"], writes=[ktmp], s2=OMKA[:, j:j + 1], op1=OP.add)
                k.tt(kmod[:, :n], k_[:, :n], tmp[:, :n], OP.mult, reads=[kk_, ktmp], writes=[kkmod])
                k.tt(bv[:, :n], av[:, :n], kkv[:, :n], OP.mult, reads=[kav, kkk], writes=[kbv], eng="pool")
                k.stt(tmp[:, :n], r_[:, :n], VR[:, VR_RK + j:VR_RK + j + 1], kmod[:, :n], OP.mult, OP.mult, reads=[kr_, kkmod, "VR"], writes=[ktmp])
                ps, pk = k.ps()
                k.mm(ps[:, :n], BONES, tmp[:, :n], True, True, reads=["CR", ktmp], writes=[pk])
                k.tt(bonus[:, :n], ps[:, :n], v_[:, :n], OP.mult, reads=[pk, kv_], writes=[kbonus])
                if R3 < 3: continue
                k.op("dve", lambda h, n=n: h.tensor_tensor_scan(out=cw[:, :n], data0=CR[:, CR_RESET:CR_RESET + n], data1=lw[:, :n], initial=0.0, op0=OP.mult, op1=OP.add),
                     reads=["CR", klw], writes=[kcw])
                k.act(ecw[:, :n], cw[:, :n], AF.Exp, reads=[kcw], writes=[kecw])
                k.tt(tmp2[:, :n], cw[:, :n], lw[:, :n], OP.subtract, reads=[kcw, klw], writes=[ktmp2], eng="pool")
                k.act(ecwx[:, :n], tmp2[:, :n], AF.Exp, reads=[ktmp2], writes=[kecwx])
                k.act(eneg[:, :n], cw[:, :n], AF.Exp, reads=[kcw], writes=[keneg], scale=-1.0)
                cw3 = cw[:, :n].rearrange("p (c t) -> p c t", t=64)
                k.tt(tmp[:, :n].rearrange("p (c t) -> p c t", t=64), cw3[:, :, 63:64].broadcast_to([128, nc_, 64]), cw3, OP.subtract, reads=[kcw], writes=[ktmp])
                k.act(eend[:, :n], tmp[:, :n], AF.Exp, reads=[ktmp], writes=[keend])
                if R3 < 4: continue
                k.tt(prod[:, :n], kkv[:, :n], ecwx[:, :n], OP.mult, reads=[kkk, kecwx], writes=[kprod])
                k.tt(QRbd[:, :nc_, 0, :].rearrange("p c (a b) -> p c a b", b=64), prod[:, :n].rearrange("p (c t) -> p c t", t=64).unsqueeze(2).broadcast_to([128, nc_, 2, 64]),
                     BDM.unsqueeze(1).broadcast_to([128, nc_, 2, 64]), OP.mult, reads=[kprod, "CR"], writes=[(kQR, 0)])
                k.tt(prod[:, :n], r_[:, :n], ecw[:, :n], OP.mult, reads=[kr_, kecw, (kQR, 0)], writes=[kprod])
                k.tt(QRbd[:, :nc_, 1, :].rearrange("p c (a b) -> p c a b", b=64), prod[:, :n].rearrange("p (c t) -> p c t", t=64).unsqueeze(2).broadcast_to([128, nc_, 2, 64]),
                     BDM.unsqueeze(1).broadcast_to([128, nc_, 2, 64]), OP.mult, reads=[kprod, "CR"], writes=[(kQR, 1)])
                for (dst, kdst, src, ksrc, ex, kex, neg) in ((kibd, kki, kmod, kkmod, eneg, keneg, False), (bibd, kbi, bv, kbv, eneg, keneg, False),
                                                          (kebd, kke, kmod, kkmod, eend, keend, False), (bebd, kbe, bv, kbv, eend, keend, True)):
                    if neg:
                        k.stt(prod[:, :n], src[:, :n], -1.0, ex[:, :n], OP.mult, OP.mult, reads=[ksrc, kex, kprod, (kQR, 1), kki, kbi, kke], writes=[kprod])
                    else:
                        k.tt(prod[:, :n], src[:, :n], ex[:, :n], OP.mult, reads=[ksrc, kex, kprod, (kQR, 1), kki, kbi, kke], writes=[kprod])
                    k.tt(dst[:, :nc_].rearrange("p c (a b) -> p c a b", b=64), prod[:, :n].rearrange("p (c t) -> p c t", t=64).unsqueeze(2).broadcast_to([128, nc_, 2, 64]),
                         BDM.unsqueeze(1).broadcast_to([128, nc_, 2, 64]), OP.mult, reads=[kprod, "CR"], writes=[kdst])
                k.tt(vbd[:, :nc_].rearrange("p c (a b) -> p c a b", b=64), v_[:, :n].rearrange("p (c t) -> p c t", t=64).unsqueeze(2).broadcast_to([128, nc_, 2, 64]),
                     BDM.unsqueeze(1).broadcast_to([128, nc_, 2, 64]), OP.mult, reads=[kv_, "CR"], writes=[kvb], eng="pool")
                Tj, kTj = T[j], "r_T%d" % j
                for c in range(nc_):
                    QRc = QRbd[:, c].rearrange("p a b -> p (a b)")
                    ps1, pk1 = k.ps()
                    k.mm(ps1[:, 0:256], kibd[:, c], QRc, True, True, reads=[kki, (kQR, 0), (kQR, 1)], writes=[pk1])
                    k.tt(A1[:, c, :], ps1[:, 0:256], MASK1, OP.mult, reads=[pk1, "CR"], writes=[(kA1, c)])
                    ps2, pk2 = k.ps()
                    k.mm(ps2[:, 0:256], bibd[:, c], QRc, True, True, reads=[kbi, (kQR, 0), (kQR, 1)], writes=[pk2])
                    k.tt(A2[:, c, :], ps2[:, 0:256], MASK2, OP.mult, reads=[pk2, "CR"], writes=[(kA2, c)])
                    ps3, pk3 = k.ps()
                    k.mm(ps3[:, 0:128], QRbd[:, c, 0], bibd[:, c], True, True, reads=[kbi, (kQR, 0)], writes=[pk3])
                    k.tt(PP[0][0][:, c, 0:128], ps3[:, 0:128], MSLN, OP.mult, reads=[pk3, "CR"], writes=[(PP[0][1], c)])
                    k.copy(PP[0][0][:, c, 128:256], A2[:, c, 0:128], reads=[(kA2, c), (PP[0][1], c)], writes=[(PP[0][1], c)], eng="pool")
                    k.tt(QT[0][0][:, c, :], A2[:, c, 0:128], ident, OP.add, reads=[(kA2, c), "CM"], writes=[(QT[0][1], c)], eng="pool")
                cur = 0
                for lv in range(1, 6):
                    Pc, kPc = PP[cur]
                    Pn, kPn = PP[1 - cur]
                    Qc, kQc = QT[cur]
                    Qn, kQn = QT[1 - cur]
                    for c0 in range(0, nc_, 2):
                        psq, pkq = k.ps()
                        for ci in range(2):
                            c = c0 + ci
                            k.mm(psq[:, ci * 256:ci * 256 + 128], Pc[:, c, 128:256], Pc[:, c, 0:128], True, True, reads=[(kPc, c)], writes=[pkq])
                            k.mm(psq[:, ci * 256 + 128:ci * 256 + 256], Pc[:, c, 0:128], Pc[:, c, 128:256], True, True, reads=[(kPc, c)], writes=[pkq])
                        k.copy(Pn[:, c0:c0 + 2, :], psq[:, 0:512].rearrange("p (c f) -> p c f", f=256), reads=[pkq], writes=[(kPn, c0), (kPn, c0 + 1)], eng="act")
                    for c0 in range(0, nc_, 4):
                        m = min(4, nc_ - c0)
                        psu, pku = k.ps()
                        for ci in range(m):
                            c = c0 + ci
                            k.mm(psu[:, ci * 128:(ci + 1) * 128], Pn[:, c, 0:128], Qc[:, c, :], True, True, reads=[(kPn, c), (kQc, c)], writes=[pku])
                        k.tt(Qn[:, c0:c0 + m, :], psu[:, 0:m * 128].rearrange("p (c f) -> p c f", f=128), Qc[:, c0:c0 + m, :], OP.add,
                             reads=[pku] + [(kQc, c0 + i) for i in range(m)], writes=[(kQn, c0 + i) for i in range(m)])
                    cur = 1 - cur
                Qf, kQf = QT[cur]
                psv, pkv = k.ps()
                for c in range(nc_):
                    k.mm(psv[:, c * 64:(c + 1) * 64], vbd[:, c], ISEL, True, True, reads=[kvb, "CR"], writes=[pkv])
                k.copy(Vs[:, :nc_, :], psv[:, 0:nc_ * 64].rearrange("p (c f) -> p c f", f=64), reads=[pkv], writes=[kVs], eng="act")
                for c0 in range(0, nc_, 2):
                    pst, pkt = k.ps()
                    for ci in range(2):
                        c = c0 + ci
                        k.tr(pst[:, ci * 256:ci * 256 + 128], kebd[:, c], ident, reads=[kke, "CM"], writes=[pkt])
                        k.tr(pst[:, ci * 256 + 128:ci * 256 + 256], bebd[:, c], ident, reads=[kbe, "CM"], writes=[pkt])
                    k.copy(KT[:, c0:c0 + 2, :], pst[:, 0:512].rearrange("p (c f) -> p c f", f=256), reads=[pkt], writes=[(kKT, c0), (kKT, c0 + 1)])
                psY, pkY = k.psb[7], "psb7"
                for c in range(nc_):
                    psr, pkr = k.ps()
                    k.mm(psr[:, 0:64], QRbd[:, c, 0], Tj[:], True, False, reads=[(kQR, 0), kTj], writes=[pkr])
                    k.mm(psr[:, 0:64], A1[:, c, 0:128], Vs[:, c, :], False, True, reads=[(kA1, c), kVs], writes=[pkr])
                    k.copy(Rs[:], psr[:, 0:64], reads=[pkr], writes=[kRs])
                    psu, pku = k.ps()
                    k.mm(psu[:, 0:64], Qf[:, c, :], Rs[:], True, True, reads=[(kQf, c), kRs], writes=[pku])
                    k.copy(Us[:, c, :], psu[:, 0:64], reads=[pku], writes=[(kUs, c)])
                    psn, pkn = k.ps()
                    k.mm(psn[:, 0:64], KT[:, c, 0:128], Vs[:, c, :], True, False, reads=[(kKT, c), kVs], writes=[pkn])
                    k.mm(psn[:, 0:64], KT[:, c, 128:256], Us[:, c, :], False, True, reads=[(kKT, c), (kUs, c)], writes=[pkn])
                    k.mm(psY[:, c * 64:(c + 1) * 64], QRbd[:, c, 1], Tj[:], True, False, reads=[(kQR, 1), kTj], writes=[pkY])
                    k.mm(psY[:, c * 64:(c + 1) * 64], A1[:, c, 128:256], Vs[:, c, :], False, False, reads=[(kA1, c), kVs], writes=[pkY])
                    k.mm(psY[:, c * 64:(c + 1) * 64], A2[:, c, 128:256], Us[:, c, :], False, True, reads=[(kA2, c), (kUs, c)], writes=[pkY])
                    k.stt(Tj[:], Tj[:], ecw[:, c * 64 + 63:c * 64 + 64], psn[:, 0:64], OP.mult, OP.add, reads=[kTj, kecw, pkn], writes=[kTj])
                k.copy(Yall[:, :nc_, :], psY[:, 0:nc_ * 64].rearrange("p (c f) -> p c f", f=64), reads=[pkY], writes=[kY], eng="act")
                P.pump_casts(2)
                if R3 < 6: continue
                Yv = Yall[:, :nc_, :]
                k.op("dve", lambda h, Yv=Yv, nc_=nc_: h.tensor_reduce(out=st1[:, :nc_], in_=Yv, axis=mybir.AxisListType.X, op=OP.add), reads=[kY], writes=[kst1])
                k.ts(st1[:, :nc_], st1[:, :nc_], 1.0 / 64, OP.mult, reads=[kst1], writes=[kst1])
                k.tt(Yv, Yv, st1[:, :nc_].unsqueeze(2).broadcast_to([128, nc_, 64]), OP.subtract, reads=[kY, kst1], writes=[kY])
                if R3L < 1: continue
                k.tt(Ysq[:, :nc_, :], Yv, Yv, OP.mult, reads=[kY], writes=[kYsq])
                k.op("dve", lambda h, nc_=nc_: h.tensor_reduce(out=st2[:, :nc_], in_=Ysq[:, :nc_, :], axis=mybir.AxisListType.X, op=OP.add), reads=[kYsq], writes=[kst2])
                k.act(st2[:, :nc_], st2[:, :nc_], AF.Sqrt, reads=[kst2], writes=[kst2], bias=RWKV_EPS, scale=1.0 / 64)
                k.op("dve", lambda h, nc_=nc_: h.reciprocal(out=st2[:, :nc_], in_=st2[:, :nc_]), reads=[kst2], writes=[kst2])
                k.tt(Yv, Yv, st2[:, :nc_].unsqueeze(2).broadcast_to([128, nc_, 64]), OP.mult, reads=[kY, kst2], writes=[kY])
                if R3L < 2: continue
                for c in range(nc_):
                    k.tt(Yall[:, c, :], Yall[:, c, :], LNG[:, j * 64:(j + 1) * 64], OP.mult, reads=[kY, "LNG"], writes=[kY])
                    k.tt(Yall[:, c, :], Yall[:, c, :], LNB[:, j * 64:(j + 1) * 64], OP.add, reads=[kY, "LNB"], writes=[kY])
                if R3L < 3: continue
                k.tt(obd[:, :nc_].rearrange("p c (a b) -> p c a b", b=64), Yv.unsqueeze(2).broadcast_to([128, nc_, 2, 64]),
                     BDM.unsqueeze(1).broadcast_to([128, nc_, 2, 64]), OP.mult, reads=[kY, "CR"], writes=[kobd])
                pso, pko = k.ps()
                for c in range(nc_):
                    k.mm(pso[:, c * 64:(c + 1) * 64], obd[:, c], ISEL, True, True, reads=[kobd, "CR"], writes=[pko])
                if R3L < 5: continue
                k.tt(tmp[:, :n], pso[:, :n], bonus[:, :n], OP.add, reads=[pko, kbonus], writes=[ktmp])
                ybt, kyb = yb[it % 2]
                k.tt(ybt[:, :n], tmp[:, :n], gate[:, :n], OP.mult, reads=[ktmp, kgate], writes=[kyb])
                k.dma("pool", d["YT"][1024 + j * 128:1024 + (j + 1) * 128, t0:t0 + n], ybt[:, :n], reads=[kyb], writes=[("YT", 1)])
    k.pes = None
    k.barrier()


def host_consts_rwkv(inp):
    p = np.arange(128)
    q = np.arange(128)
    same = (p[:, None] // 64) == (q[None, :] // 64)
    SU = (same & ((p[:, None] % 64) < (q[None, :] % 64))).astype(np.float32)
    IU = (same & ((p[:, None] % 64) <= (q[None, :] % 64))).astype(np.float32)
    SL = (same & ((q[None, :] % 64) < (p[:, None] % 64))).astype(np.float32)
    CR = np.zeros((128, NCR), np.float32)
    CR[:, 0:128] = SU
    CR[:, 128:256] = IU
    CR[:, 256:384] = -SU
    CR[:, 384:512] = -IU
    CR[:, 512:640] = -SL
    CR[:, 640:704] = ((p[:, None] % 64) == np.arange(64)[None, :])
    CR[:, 704:832] = ((p[:, None] // 64) == (np.arange(128)[None, :] // 64))
    CR[:, 832:960] = same
    CR[:, 960:960 + 512] = (np.arange(512) % 64 != 0)[None, :]
    mu = inp["ev_shift_mu"][0]
    VR = np.zeros((128, NVR), np.float32)

    def pairs(v):
        return v.reshape(8, 128).T

    VR[:, 0:8] = pairs(mu[0:1024])
    VR[:, 8:16] = pairs(mu[1024:2048])
    VR[:, 16:24] = pairs(mu[2048:3072])
    VR[:, 24:32] = pairs(inp["ev_w0"][0])
    VR[:, 32:40] = pairs(inp["ev_a0"][0])
    VR[:, 40:48] = pairs(inp["ev_k_k"][0])
    VR[:, 48:56] = pairs(inp["ev_k_a"][0])
    VR[:, 56:64] = pairs(inp["ev_r_k"][0].reshape(1024))
    VR[:, 64] = mu[3072:3200]
    VR[:, 65] = mu[3200:3328]
    WUA = np.concatenate([inp["ev_w_up"][0], inp["ev_a_up"][0]], 0).astype(np.float32)
    GUP = inp["ev_g_up"][0].astype(np.float32)

    def stack(v):
        a = v.reshape(8, 2, 64)
        return np.ascontiguousarray(np.repeat(a.transpose(1, 0, 2)[:, None], 64, axis=1).reshape(128, 512))

    return dict(CRd=CR, VRd=VR, WUAd=WUA, GUPd=GUP, LNGd=stack(inp["ev_lnx_g"][0]), LNBd=stack(inp["ev_lnx_b"][0]))


def layer_norm_rows(k, pfx, src, ksrc, dst, kdst, gcol, bcol, VT2, tiles):
    stats, mv, rs = tiles
    for i in range(2):
        k.op("dve", lambda h, i=i: h.bn_stats(out=stats[:, i * 6:(i + 1) * 6], in_=src[:, i * 512:(i + 1) * 512]), reads=[ksrc], writes=[pfx + "st"])
    k.op("dve", lambda h: h.bn_aggr(out=mv[:], in_=stats[:]), reads=[pfx + "st"], writes=[pfx + "mv"])
    k.act(rs[:], mv[:, 1:2], AF.Sqrt, reads=[pfx + "mv"], writes=[pfx + "rs"], bias=LN_EPS)
    k.op("dve", lambda h: h.reciprocal(out=rs[:], in_=rs[:]), reads=[pfx + "rs"], writes=[pfx + "rs"])
    k.ts(dst[:], src[:], mv[:, 0:1], OP.subtract, reads=[ksrc, pfx + "mv", pfx + "rs"], writes=[kdst], s2=rs[:, 0:1], op1=OP.mult)
    k.tt(dst[:], dst[:], VT2[:, gcol:gcol + 1024], OP.mult, reads=[kdst, "VT2"], writes=[kdst])
    k.tt(dst[:], dst[:], VT2[:, bcol:bcol + 1024], OP.add, reads=[kdst, "VT2"], writes=[kdst])


def proj_res_ln(P, pfx, srcT, nkc, wname, res_name, gcol, bcol, out_name, outT_name, outT32_name=None):
    k, LP, d = P.k, P.LP, P.dram
    with ExitStack() as pes:
        k.pes = pes
        VT2 = k.sb(pfx + "VT2", [128, 2048])
        k.dma("sp", VT2[:], d["VT2d"][0:1, gcol:gcol + 2048].partition_broadcast(128), writes=["VT2"])
        P.VT2 = VT2
        gcol, bcol = 0, 1024
        W = k.sb(pfx + "W", [128, nkc, 1024], BF16)
        k.dma("sp", W[:].rearrange("p a b -> p (a b)"), d[wname][:, :], reads=[(wname, 0)], writes=[pfx + "W"])
        yT = [k.sb(pfx + "yT%d" % i, [128, nkc, 512], BF16) for i in range(2)]
        xr = [k.sb(pfx + "x%d" % i, [128, 1024]) for i in range(2)]
        hp = k.sb(pfx + "hp", [128, 1024])
        ho = [k.sb(pfx + "ho%d" % i, [128, 1024]) for i in range(2)]
        hT = [k.sb(pfx + "hT%d" % i, [128, 8, 128], BF16) for i in range(2)]
        hT32 = [k.sb(pfx + "hTf%d" % i, [128, 8, 128]) for i in range(2)]
        lnt = (k.sb(pfx + "st", [128, 12]), k.sb(pfx + "mv", [128, 2]), k.sb(pfx + "rs", [128, 1]))
        it = 0
        for si, (t0, n) in enumerate(supertiles(LP)):
            y, ky = yT[si % 2], pfx + "yT%d" % (si % 2)
            k.dma("sp", y[:, :, :n], d[srcT][:, t0:t0 + n].rearrange("(c p) t -> p c t", p=128), reads=[(srcT, 0), (srcT, 1)], writes=[ky])
            for j in range(n // 128):
                it += 1
                x, kx = xr[it % 2], pfx + "x%d" % (it % 2)
                r0 = t0 + j * 128
                k.dma("sp", x[:], d[res_name][r0:r0 + 128, :], reads=[(res_name, 0)], writes=[kx])
                for half in range(2):
                    ps, pk = k.ps()
                    for kc in range(nkc):
                        k.mm(ps[:, :], y[:, kc, j * 128:(j + 1) * 128], W[:, kc, half * 512:(half + 1) * 512], kc == 0, kc == nkc - 1, reads=[ky, pfx + "W"], writes=[pk])
                    k.stt(hp[:, half * 512:(half + 1) * 512], x[:, half * 512:(half + 1) * 512], ALPHA, ps[:, :], OP.mult, OP.add, reads=[kx, pk], writes=[pfx + "hp"])
                o, ko = ho[it % 2], pfx + "ho%d" % (it % 2)
                layer_norm_rows(k, pfx, hp, pfx + "hp", o, ko, gcol, bcol, P.VT2, lnt)
                k.dma("pool", d[out_name][r0:r0 + 128, :], o[:], reads=[ko], writes=[(out_name, 0)])
                t_, kt = hT[it % 2], pfx + "hT%d" % (it % 2)
                tf, ktf = hT32[it % 2], pfx + "hTf%d" % (it % 2)
                for half in range(2):
                    ps, pk = k.ps()
                    for cc in range(4):
                        c = half * 4 + cc
                        k.tr(ps[:, cc * 128:(cc + 1) * 128], o[:, c * 128:(c + 1) * 128], P.ident, reads=[ko, "CM"], writes=[pk])
                    k.copy(t_[:, half * 4:(half + 1) * 4, :], ps[:, :].rearrange("p (c t) -> p c t", t=128), reads=[pk], writes=[kt], eng="act")
                    if outT32_name:
                        k.copy(tf[:, half * 4:(half + 1) * 4, :], ps[:, :].rearrange("p (c t) -> p c t", t=128), reads=[pk], writes=[ktf])
                k.dma("pool", d[outT_name][:, r0:r0 + 128].rearrange("(c p) t -> p c t", p=128), t_[:], reads=[kt], writes=[(outT_name, 0)])
                if outT32_name:
                    k.dma("pool", d[outT32_name][:, r0:r0 + 128].rearrange("(c p) t -> p c t", p=128), tf[:], reads=[ktf], writes=[(outT32_name, 0)])
    k.pes = None
    k.barrier()


def ffn_phase(P, pfx, srcT, res_name, wgu_names, wd_names, gcol, bcol, out_name, outT_name=None, router=None, tile_n=512):
    k, LP, d = P.k, P.LP, P.dram
    ne = len(wgu_names)
    with ExitStack() as pes:
        k.pes = pes
        VT2 = k.sb(pfx + "VT2", [128, 2048])
        k.dma("sp", VT2[:], d["VT2d"][0:1, gcol:gcol + 2048].partition_broadcast(128), writes=["VT2"])
        P.VT2 = VT2
        gcol, bcol = 0, 1024
        nhb = 2 if tile_n == 512 else 1
        hT = [k.sb(pfx + "hT%d" % i, [128, 8, tile_n], BF16) for i in range(nhb)]
        NWG = 6
        wg = [k.sb(pfx + "wg%d" % i, [128, 8, 128], BF16) for i in range(NWG)]
        wdh = [k.sb(pfx + "wdh%d" % i, [128, 22, 512], BF16) for i in range(2)]
        actT = k.sb(pfx + "actT", [128, 22, tile_n], BF16)
        sg = [k.sb(pfx + "sg%d" % i, [128, 512]) for i in range(2)]
        acc = [k.sb(pfx + "acc%d" % i, [128, 1024]) for i in range(tile_n // 128)]
        o = [k.sb(pfx + "o%d" % i, [128, 1024]) for i in range(2)]
        oT = [k.sb(pfx + "oT%d" % i, [128, 8, 128], BF16) for i in range(2)] if outT_name else None
        lnt = (k.sb(pfx + "st", [128, 12]), k.sb(pfx + "mv", [128, 2]), k.sb(pfx + "rs", [128, 1]))
        if router:
            hT32 = [k.sb(pfx + "hT32_%d" % i, [128, 8, 128]) for i in range(2)]
            WR = k.sb(pfx + "WR", [128, 8, 8])
            k.dma("sp", WR[:].rearrange("p a b -> p (a b)"), d["WRd"][:, :], writes=[pfx + "WR"])
            lg = k.sb(pfx + "lg", [128, 8])
            m8 = k.sb(pfx + "m8", [128, 8])
            msk = k.sb(pfx + "msk", [128, 8])
            nm0 = k.sb(pfx + "nm0", [128, 1])
            den = k.sb(pfx + "den", [128, 1])
            G = [k.sb(pfx + "G%d" % i, [128, 8]) for i in range(tile_n // 128)]

        def load_wd(e):
            for hf in range(2):
                k.dma("sp", wdh[hf][:], d[wd_names[e]][:, :].rearrange("p (a b) -> p a b", b=1024)[:, :, hf * 512:(hf + 1) * 512],
                      reads=[(wd_names[e], 0)], writes=[pfx + "wdh%d" % hf])

        if ne == 1:
            load_wd(0)
        wi = 0
        oi = 0
        big = []
        t = 0
        while t < LP:
            nn = min(tile_n, LP - t)
            big.append((t, nn))
            t += nn
        for si, (t0, n) in enumerate(big):
            nj = n // 128
            groups = [(g0, min(512, n - g0)) for g0 in range(0, n, 512)]
            h, kh = hT[si % nhb], pfx + "hT%d" % (si % nhb)
            k.dma("sp", h[:, :, :n], d[srcT][:, t0:t0 + n].rearrange("(c p) t -> p c t", p=128), reads=[(srcT, 0)], writes=[kh])
            for j in range(nj):
                r0 = t0 + j * 128
                k.dma("sp", acc[j][:], d[res_name][r0:r0 + 128, :], reads=[(res_name, 0)], writes=[pfx + "acc%d" % j])
                k.ts(acc[j][:], acc[j][:], ALPHA, OP.mult, reads=[pfx + "acc%d" % j], writes=[pfx + "acc%d" % j], eng="pool")
                if router:
                    h32, kh32 = hT32[j % 2], pfx + "hT32_%d" % (j % 2)
                    k.dma("sp", h32[:], d[router][:, r0:r0 + 128].rearrange("(c p) t -> p c t", p=128), reads=[(router, 0)], writes=[kh32])
                    ps, pk = k.ps()
                    for kc in range(8):
                        k.mm(ps[:, 0:8], h32[:, kc, :], WR[:, kc, :], kc == 0, kc == 7, reads=[kh32, pfx + "WR"], writes=[pk])
                    k.copy(lg[:], ps[:, 0:8], reads=[pk], writes=[pfx + "lg"])
                    k.op("dve", lambda hh: hh.max(out=m8[:], in_=lg[:]), reads=[pfx + "lg"], writes=[pfx + "m8"])
                    k.ts(msk[:], lg[:], m8[:, 1:2], OP.is_ge, reads=[pfx + "lg", pfx + "m8"], writes=[pfx + "msk"])
                    k.ts(nm0[:], m8[:, 0:1], -1.0, OP.mult, reads=[pfx + "m8"], writes=[pfx + "nm0"])
                    k.act(lg[:], lg[:], AF.Exp, reads=[pfx + "lg", pfx + "nm0"], writes=[pfx + "lg"], bias=nm0[:, 0:1])
                    k.tt(lg[:], lg[:], msk[:], OP.mult, reads=[pfx + "lg", pfx + "msk"], writes=[pfx + "lg"])
                    k.op("dve", lambda hh: hh.tensor_reduce(out=den[:], in_=lg[:], axis=mybir.AxisListType.X, op=OP.add), reads=[pfx + "lg"], writes=[pfx + "den"])
                    k.op("dve", lambda hh: hh.reciprocal(out=den[:], in_=den[:]), reads=[pfx + "den"], writes=[pfx + "den"])
                    k.ts(G[j][:], lg[:], den[:, 0:1], OP.mult, reads=[pfx + "lg", pfx + "den"], writes=[pfx + "G%d" % j])
            for e in range(ne):
                for i in range(22):
                    pss = {}
                    for part in range(2):
                        c = part * 22 + i
                        w, kw = wg[wi % NWG], pfx + "wg%d" % (wi % NWG)
                        wi += 1
                        k.dma("sp", w[:].rearrange("p a b -> p (a b)"), d[wgu_names[e]][c * 128:(c + 1) * 128, :], reads=[(wgu_names[e], (c * 128) // 512)], writes=[kw])
                        for gi, (g0, ng) in enumerate(groups):
                            ps, pk = k.ps()
                            for kc in range(8):
                                k.mm(ps[:, :ng], w[:, kc, :], h[:, kc, g0:g0 + ng], kc == 0, kc == 7, reads=[kw, kh], writes=[pk])
                            pss[(gi, part)] = (ps, pk)
                    for gi, (g0, ng) in enumerate(groups):
                        s_, ks = sg[gi % 2], pfx + "sg%d" % (gi % 2)
                        k.act(s_[:, :ng], pss[(gi, 0)][0][:, :ng], AF.Silu, reads=[pss[(gi, 0)][1]], writes=[ks])
                        k.tt(actT[:, i, g0:g0 + ng], s_[:, :ng], pss[(gi, 1)][0][:, :ng], OP.mult, reads=[ks, pss[(gi, 1)][1]], writes=[(pfx + "actT", i)])
                    if ne > 1 and i == 2:
                        load_wd(e)
                for half in range(2):
                    hs = slice(half * 512, (half + 1) * 512)
                    for j in range(nj):
                        ps, pk = k.ps()
                        for i in range(22):
                            k.mm(ps[:, :], actT[:, i, j * 128:(j + 1) * 128], wdh[half][:, i, :], i == 0, i == 21, reads=[(pfx + "actT", i), pfx + "wdh%d" % half], writes=[pk])
                        if router:
                            k.stt(acc[j][:, hs], ps[:, :], G[j][:, e:e + 1], acc[j][:, hs], OP.mult, OP.add, reads=[pk, pfx + "G%d" % j, pfx + "acc%d" % j], writes=[pfx + "acc%d" % j])
                        else:
                            k.tt(acc[j][:, hs], ps[:, :], acc[j][:, hs], OP.add, reads=[pk, pfx + "acc%d" % j], writes=[pfx + "acc%d" % j])
            for j in range(nj):
                r0 = t0 + j * 128
                oi += 1
                ot, ko = o[oi % 2], pfx + "o%d" % (oi % 2)
                layer_norm_rows(k, pfx, acc[j], pfx + "acc%d" % j, ot, ko, gcol, bcol, P.VT2, lnt)
                k.dma("pool", d[out_name][r0:r0 + 128, :], ot[:], reads=[ko], writes=[(out_name, 0)])
                if outT_name:
                    t_, kt = oT[oi % 2], pfx + "oT%d" % (oi % 2)
                    for half in range(2):
                        ps, pk = k.ps()
                        for cc in range(4):
                            c = half * 4 + cc
                            k.tr(ps[:, cc * 128:(cc + 1) * 128], ot[:, c * 128:(c + 1) * 128], P.ident, reads=[ko, "CM"], writes=[pk])
                        k.copy(t_[:, half * 4:(half + 1) * 4, :], ps[:, :].rearrange("p (c t) -> p c t", t=128), reads=[pk], writes=[kt], eng="act")
                    k.dma("pool", d[outT_name][:, r0:r0 + 128].rearrange("(c p) t -> p c t", p=128), t_[:], reads=[kt], writes=[(outT_name, 0)])
    k.pes = None
    k.barrier()


VL_CW, VL_CB, VL_GXB, VL_GAB, VL_LAM = 0, 32, 40, 48, 56
NVL = 64


def phase5(P):
    k, LP, d = P.k, P.LP, P.dram
    with ExitStack() as pes:
        k.pes = pes
        W = k.sb("l_W", [128, 16, 8, 128], BF16)
        k.dma("sp", W[:].rearrange("p c a b -> p c (a b)"), d["W1INb"][:, :].rearrange("(c p) f -> p c f", p=128), reads=[("W1INb", i) for i in range(4)], writes=["l_W"])
        GW = k.sb("l_GW", [128, 16, 128])
        k.dma("sp", GW[:].rearrange("p c b -> p (c b)"), d["GWd"][:, :], writes=["l_GW"])
        VL = k.sb("l_VL", [128, NVL])
        k.dma("sp", VL[:], d["VLd"][:, :], writes=["l_VL"])
        SPm8 = k.sb("l_sp8", [128, 8])
        SPm16 = k.sb("l_sp16", [128, 8])
        k.act(SPm8[:], VL[:, VL_LAM:VL_LAM + 8], AF.Exp, reads=["l_VL"], writes=["l_sp8"], scale=-1.0)
        k.act(SPm8[:], SPm8[:], AF.Ln, reads=["l_sp8"], writes=["l_sp8"], bias=1.0)
        k.ts(SPm16[:], SPm8[:], -16.0, OP.mult, reads=["l_sp8"], writes=["l_sp16"])
        k.ts(SPm8[:], SPm8[:], -8.0, OP.mult, reads=["l_sp8", "l_sp16"], writes=["l_sp8"])
        hT = [k.sb("l_hT%d" % i, [128, 8, 512], BF16) for i in range(2)]
        gb = [k.sb("l_gb%d" % i, [128, 512]) for i in range(2)]
        g2 = k.sb("l_g2", [128, 512])
        xr = [k.sb("l_xr%d" % i, [128, 515]) for i in range(2)]
        xf = k.sb("l_xf", [128, 512])
        gx = k.sb("l_gx", [128, 512])
        ga = k.sb("l_ga", [128, 512])
        av = k.sb("l_a", [128, 512])
        uv = k.sb("l_u", [128, 512])
        hs = [k.sb("l_hs%d" % c, [128, 512]) for c in range(8)]
        carry = [k.sb("l_cy%d" % c, [128, 1]) for c in range(8)]
        yb = [k.sb("l_yb%d" % i, [128, 512], BF16) for i in range(2)]
        for c in range(8):
            k.memset(carry[c][:], 0.0, writes=["l_cy%d" % c])
        it = 0
        for si, (t0, n) in enumerate(supertiles(LP)):
            h, kh = hT[si % 2], "l_hT%d" % (si % 2)
            k.dma("sp", h[:, :, :n], d["H2T"][:, t0:t0 + n].rearrange("(c p) t -> p c t", p=128), reads=[("H2T", 0)], writes=[kh])
            for c in range(8):
                it += 1
                ps, pk = k.ps()
                for kc in range(8):
                    k.mm(ps[:, :n], W[:, c, kc, :], h[:, kc, :n], kc == 0, kc == 7, reads=["l_W", kh], writes=[pk])
                g, kg = gb[it % 2], "l_gb%d" % (it % 2)
                k.copy(g[:, :n], ps[:, :n], reads=[pk], writes=[kg], eng="act")
                k.tt(g2[:, :n], g[:, :n], g[:, :n], OP.mult, reads=[kg], writes=["l_g2"])
                k.ts(g2[:, :n], g2[:, :n], 0.044715, OP.mult, reads=["l_g2"], writes=["l_g2"], s2=1.0, op1=OP.add)
                k.tt(g2[:, :n], g2[:, :n], g[:, :n], OP.mult, reads=["l_g2", kg], writes=["l_g2"])
                k.act(g2[:, :n], g2[:, :n], AF.Sigmoid, reads=["l_g2"], writes=["l_g2"], scale=1.5957691216057308)
                k.tt(g[:, :n], g[:, :n], g2[:, :n], OP.mult, reads=[kg, "l_g2"], writes=[kg])
                x, kx = xr[it % 2], "l_xr%d" % (it % 2)
                ps, pk = k.ps()
                for kc in range(8):
                    k.mm(ps[:, :n], W[:, 8 + c, kc, :], h[:, kc, :n], kc == 0, kc == 7, reads=["l_W", kh], writes=[pk])
                xp, kxp = xr[(it + 1) % 2], "l_xr%d" % ((it + 1) % 2)
                k.copy(x[:, 3:3 + n], ps[:, :n], reads=[pk], writes=[kx])
                k.dma("pool", d["XRT"][c * 128:(c + 1) * 128, t0:t0 + n], x[:, 3:3 + n], reads=[kx], writes=[("XRT", c)])
                if t0 == 0:
                    k.memset(x[:, 0:3], 0.0, writes=[kx])
                else:
                    k.dma("sp", x[:, 0:3], d["XRT"][c * 128:(c + 1) * 128, t0 - 3:t0], reads=[("XRT", c)], writes=[kx])
                cwc = VL_CW + c * 4
                k.ts(xf[:, :n], x[:, 3:3 + n], VL[:, cwc + 3:cwc + 4], OP.mult, reads=[kx, "l_VL"], writes=["l_xf"])
                for kk in (2, 1, 0):
                    k.stt(xf[:, :n], x[:, kk:kk + n], VL[:, cwc + kk:cwc + kk + 1], xf[:, :n], OP.mult, OP.add, reads=[kx, "l_xf", "l_VL"], writes=["l_xf"])
                k.ts(xf[:, :n], xf[:, :n], VL[:, VL_CB + c:VL_CB + c + 1], OP.add, reads=["l_xf", "l_VL"], writes=["l_xf"])
                ps, pk = k.ps()
                k.mm(ps[:, :n], GW[:, c, :], xf[:, :n], True, True, reads=["l_GW", "l_xf"], writes=[pk])
                k.act(gx[:, :n], ps[:, :n], AF.Sigmoid, reads=[pk, "l_VL"], writes=["l_gx"], bias=VL[:, VL_GXB + c:VL_GXB + c + 1])
                ps, pk = k.ps()
                k.mm(ps[:, :n], GW[:, 8 + c, :], xf[:, :n], True, True, reads=["l_GW", "l_xf"], writes=[pk])
                k.act(ga[:, :n], ps[:, :n], AF.Sigmoid, reads=[pk, "l_VL"], writes=["l_ga"], bias=VL[:, VL_GAB + c:VL_GAB + c + 1])
                k.act(av[:, :n], ga[:, :n], AF.Exp, reads=["l_ga", "l_sp8"], writes=["l_a"], scale=SPm8[:, c:c + 1])
                k.act(uv[:, :n], ga[:, :n], AF.Exp, reads=["l_ga", "l_sp16"], writes=["l_u"], scale=SPm16[:, c:c + 1])
                k.act(uv[:, :n], uv[:, :n], AF.Sqrt, reads=["l_u"], writes=["l_u"], scale=-1.0, bias=1.0)
                k.tt(gx[:, :n], gx[:, :n], xf[:, :n], OP.mult, reads=["l_gx", "l_xf"], writes=["l_gx"])
                k.tt(uv[:, :n], uv[:, :n], gx[:, :n], OP.mult, reads=["l_u", "l_gx"], writes=["l_u"])
                k.op("dve", lambda hh, c=c, n=n: hh.tensor_tensor_scan(out=hs[c][:, :n], data0=av[:, :n], data1=uv[:, :n], initial=carry[c][:, 0:1], op0=OP.mult, op1=OP.add),
                     reads=["l_a", "l_u", "l_cy%d" % c], writes=["l_hs%d" % c])
                k.copy(carry[c][:], hs[c][:, n - 1:n], reads=["l_hs%d" % c], writes=["l_cy%d" % c], eng="pool")
                y, ky = yb[it % 2], "l_yb%d" % (it % 2)
                k.tt(y[:, :n], hs[c][:, :n], g[:, :n], OP.mult, reads=["l_hs%d" % c, kg], writes=[ky])
                k.dma("pool", d["Y1T"][c * 128:(c + 1) * 128, t0:t0 + n], y[:, :n], reads=[ky], writes=[("Y1T", 0)])
    k.pes = None
    k.barrier()


def pack_rhs_k(w):
    K = w.shape[0]
    return np.ascontiguousarray(w.reshape(K // 128, 128, w.shape[1]).transpose(1, 0, 2)).reshape(128, -1)


def host_inputs_weights(inp):
    f = np.float32
    im = {}
    CM, VT, VF = host_consts(inp)
    im.update(CMd=CM, VTd=VT, VFd=VF)
    w_in = inp["ev_w_in"][0]
    im["WINF"] = pack_lhsT(w_in, fcols())
    im["WINZ"] = pack_rhs(w_in, zcols())
    im.update(host_consts_rwkv(inp))
    im["VT2d"] = np.concatenate([inp[n][0] for n in ("ev_ln1_g", "ev_ln1_b", "ev_ln2_g", "ev_ln2_b", "od_ln1_g", "od_ln1_b", "od_ln2_g", "od_ln2_b")])[None, :].astype(f)
    im["WO"] = pack_rhs_k(inp["ev_w_out"][0])
    im["WGU0"] = pack_lhsT(inp["ev_ffn_w_gu"][0], np.arange(5632))
    im["WD0"] = pack_rhs_k(inp["ev_ffn_w_down"][0])
    im["W1IN"] = pack_lhsT(inp["od_w_in"][0], np.arange(2048))
    gw = np.concatenate([inp["od_gx_w"][0], inp["od_ga_w"][0]], 0)
    im["GWd"] = np.ascontiguousarray(gw.transpose(1, 0, 2)).reshape(128, 16 * 128).astype(f)
    VL = np.zeros((128, NVL), f)
    VL[:, 0:32] = inp["od_conv_w"][0].reshape(4, 8, 128).transpose(2, 1, 0).reshape(128, 32)
    for off, n in ((32, "od_conv_b"), (40, "od_gx_b"), (48, "od_ga_b"), (56, "od_lambda")):
        VL[:, off:off + 8] = inp[n][0].reshape(8, 128).T
    im["VLd"] = VL
    im["W1O"] = pack_rhs_k(inp["od_w_out"][0])
    im["WRd"] = np.ascontiguousarray(inp["od_router"][0].reshape(8, 128, 8).transpose(1, 0, 2)).reshape(128, 64).astype(f)
    for e in range(NEXP):
        im["WGUE%d" % e] = pack_lhsT(inp["od_exp_w_gu"][0, e], np.arange(5632))
        im["WDE%d" % e] = pack_rhs_k(inp["od_exp_w_down"][0, e])
    return im


_CACHE = {}


def kernel(**inputs):
    inp = {k_: np.asarray(v) for k_, v in inputs.items()}
    x = inp["x"]
    B, S, _ = x.shape
    L = S + NMETA
    LP = ((L + 127) // 128) * 128
    if LP not in _CACHE:
        _CACHE[LP] = build(LP)
    P = _CACHE[LP]
    wim = host_inputs_weights(inp)
    in_maps = []
    for b in range(B):
        xin = np.zeros((LP, D), np.float32)
        xin[:NMETA] = inp["meta"]
        xin[NMETA:L] = x[b]
        m = dict(wim)
        m["xin"] = xin
        in_maps.append(m)
    res = run_bass_kernel_spmd(P.nc, in_maps, core_ids=list(range(B)))
    out = np.stack([np.asarray(r["OUT"])[NMETA:L] for r in res.results], 0)
    return out.astype(np.float32)
```

```python
from contextlib import ExitStack
import concourse.bass as bass
import concourse.mybir as mybir

F32 = mybir.dt.float32
BF16 = mybir.dt.bfloat16
AF = mybir.ActivationFunctionType
OP = mybir.AluOpType
ENGS = ["pe", "dve", "act", "pool", "sp"]
NDMA = 40


class KB:
    def __init__(self, nc, es: ExitStack):
        self.nc = nc
        self.es = es
        self.ops = {e: [] for e in ENGS}
        self.cnt = {e: 0 for e in ENGS}
        self.waited = {e: {} for e in ENGS}
        self.last_w = {}
        self.readers = {}
        self.sem = {e: es.enter_context(nc.semaphore("s_" + e)) for e in ENGS}
        self.dsem = [es.enter_context(nc.semaphore("d%d" % i)) for i in range(NDMA)]
        self.dval = [0] * NDMA
        self.dnext = 0
        self.semid = {}
        for e in ENGS:
            self.semid[("e", e)] = self.sem[e]
        for i in range(NDMA):
            self.semid[("d", i)] = self.dsem[i]
        self.ntile = 0
        self.psb = [self.psum_t("psb%d" % i, [128, 512], F32) for i in range(8)]
        self.psi = 0
        self.final_tokens = []

    def sb(self, name, shape, dt=F32):
        self.ntile += 1
        es = self.pes if getattr(self, "pes", None) is not None else self.es
        return es.enter_context(self.nc.sbuf_tensor(name, list(shape), dt))

    def barrier(self):
        allw = []
        for i in range(NDMA):
            if self.dval[i] > 0:
                allw.append((("d", i), self.dval[i]))
        for e in ENGS:
            if self.cnt[e] > 0:
                allw.append((("e", e), self.cnt[e]))
        semid = self.semid
        for e in ENGS:
            waits = self._waits(e, allw, skip_self=True)

            def run(h, waits=waits):
                for s, v in waits:
                    h.wait_ge(semid[s], v)

            self.ops[e].append(run)

    def psum_t(self, name, shape, dt=F32):
        return self.es.enter_context(self.nc.psum_tensor(name, list(shape), dt))

    def ps(self):
        i = self.psi
        self.psi = (self.psi + 1) % 7
        return self.psb[i], "psb%d" % i

    def _deps(self, reads, writes):
        deps = []
        for k in reads:
            if k in self.last_w:
                deps.append(self.last_w[k])
            if isinstance(k, str) and k.startswith("psb"):
                deps.extend(self.readers.get(k, {}).values())
        for k in writes:
            if k in self.last_w:
                deps.append(self.last_w[k])
            deps.extend(self.readers.get(k, {}).values())
        return deps

    def _waits(self, eng, deps, skip_self=False):
        w = {}
        for (s, v) in deps:
            if skip_self and s == ("e", eng):
                continue
            if self.waited[eng].get(s, 0) < v and w.get(s, 0) < v:
                w[s] = v
        for s, v in w.items():
            self.waited[eng][s] = v
        return list(w.items())

    def _commit(self, tok, reads, writes):
        for k in writes:
            self.last_w[k] = tok
            self.readers[k] = {}
        for k in reads:
            if k in writes:
                continue
            r = self.readers.setdefault(k, {})
            if r.get(tok[0], (None, 0))[1] < tok[1]:
                r[tok[0]] = tok

    def op(self, eng, fn, reads=(), writes=()):
        deps = self._deps(reads, writes)
        waits = self._waits(eng, deps, skip_self=(eng == "pe"))
        self.cnt[eng] += 1
        tok = (("e", eng), self.cnt[eng])
        semid = self.semid
        mysem = self.sem[eng]

        def run(h):
            for s, v in waits:
                h.wait_ge(semid[s], v)
            fn(h).then_inc(mysem, 1)

        self.ops[eng].append(run)
        self._commit(tok, reads, writes)
        return tok

    def dma(self, q, out, in_, reads=(), writes=(), **kw):
        slot = self.dnext
        self.dnext = (self.dnext + 1) % NDMA
        deps = self._deps(reads, writes)
        if self.dval[slot] > 0:
            deps.append((("d", slot), self.dval[slot]))
        waits = self._waits(q, deps)
        self.dval[slot] += 16
        tok = (("d", slot), self.dval[slot])
        semid = self.semid
        ds = self.dsem[slot]

        def run(h):
            for s, v in waits:
                h.wait_ge(semid[s], v)
            h.dma_start(out=out, in_=in_, **kw).then_inc(ds, 16)

        self.ops[q].append(run)
        self._commit(tok, reads, writes)
        return tok

    def finish(self, eng="sp"):
        waits = []
        for i in range(NDMA):
            if self.dval[i] > 0:
                waits.append((("d", i), self.dval[i]))
        for e in ENGS:
            if self.cnt[e] > 0:
                waits.append((("e", e), self.cnt[e]))
        semid = self.semid

        def run(h):
            for s, v in waits:
                h.wait_ge(semid[s], v)

        self.ops[eng].append(run)

    def emit(self):
        nc = self.nc
        with nc.Block() as block:
            @block.tensor
            def _(h):
                for f in self.ops["pe"]:
                    f(h)

            @block.vector
            def _(h):
                for f in self.ops["dve"]:
                    f(h)

            @block.scalar
            def _(h):
                for f in self.ops["act"]:
                    f(h)

            @block.gpsimd
            def _(h):
                for f in self.ops["pool"]:
                    f(h)

            @block.sync
            def _(h):
                for f in self.ops["sp"]:
                    f(h)

    def mm(self, out, lhsT, rhs, start, stop, reads, writes):
        return self.op("pe", lambda h: h.matmul(out, lhsT, rhs, start=start, stop=stop), reads, writes)

    def tr(self, out, in_, ident, reads, writes):
        return self.op("pe", lambda h: h.transpose(out, in_, ident), reads, writes)

    def act(self, out, in_, func, reads, writes, bias=None, scale=None, eng="act"):
        kw = {}
        if bias is not None:
            kw["bias"] = bias
        if scale is not None:
            kw["scale"] = scale
        return self.op("act", lambda h: h.activation(out=out, in_=in_, func=func, **kw), reads, writes)

    def tt(self, out, in0, in1, op, reads, writes, eng="dve"):
        return self.op(eng, lambda h: h.tensor_tensor(out=out, in0=in0, in1=in1, op=op), reads, writes)

    def ts(self, out, in0, s1, op0, reads, writes, s2=None, op1=None, eng="dve"):
        if op1 is None:
            return self.op(eng, lambda h: h.tensor_scalar(out=out, in0=in0, scalar1=s1, scalar2=None, op0=op0), reads, writes)
        return self.op(eng, lambda h: h.tensor_scalar(out=out, in0=in0, scalar1=s1, scalar2=s2, op0=op0, op1=op1), reads, writes)

    def stt(self, out, in0, scalar, in1, op0, op1, reads, writes):
        return self.op("dve", lambda h: h.scalar_tensor_tensor(out=out, in0=in0, scalar=scalar, in1=in1, op0=op0, op1=op1), reads, writes)

    def copy(self, out, in_, reads, writes, eng="dve"):
        if eng == "act":
            return self.op("act", lambda h: h.copy(out=out, in_=in_), reads, writes)
        return self.op(eng, lambda h: h.tensor_copy(out=out, in_=in_), reads, writes)

    def memset(self, ap, val, writes, eng="dve"):
        return self.op(eng, lambda h: h.memset(ap, val), (), writes)
import numpy as np
from contextlib import ExitStack
import concourse.bass as bass
import concourse.mybir as mybir
from concourse.bass_utils import run_bass_kernel_spmd

D = 1024
NMETA = 16
DFF = 2816
NEXP = 8
ALPHA = 4 ** 0.25
LN_EPS = 1e-5
RWKV_EPS = 64e-5

C_XS, C_B, C_C, C_R, C_K, C_V, C_LO, C_G = 0, 8, 10, 12, 20, 28, 36, 37
NFC = 38


def supertiles(LP):
    out = []
    t = 0
    while t < LP:
        n = min(512, LP - t)
        out.append((t, n))
        t += n
    return out


class Prog:
    def pump_casts(self, n):
        for _ in range(n):
            if self.pending:
                self.pending.pop(0)()

    def __init__(self, LP, dbg=()):
        self.pending = []
        self.LP = LP
        self.dbg = set(dbg)
        self.nc = bass.Bass("TRN2", target_bir_lowering=False)
        self.es = ExitStack()
        self.k = None
        self.dram = {}

    def din(self, name, shape, dt=F32):
        t = self.nc.dram_tensor(name, list(shape), dt, kind="ExternalInput").ap()
        self.dram[name] = t
        return t

    def dscr(self, name, shape, dt=F32):
        kind = "ExternalOutput" if name in self.dbg else "Internal"
        t = self.nc.dram_tensor(name, list(shape), dt, kind=kind).ap()
        self.dram[name] = t
        return t

    def dout(self, name, shape, dt=F32):
        t = self.nc.dram_tensor(name, list(shape), dt, kind="ExternalOutput").ap()
        self.dram[name] = t
        return t


def cast_weights(P, src, dst, nrows, key, defer=False):
    k = P.k
    step = 512
    for r0 in range(0, nrows, step):
        r1 = min(nrows, r0 + step)

        def go(r0=r0, r1=r1):
            k.dma("pool", dst[r0:r1, :], src[r0:r1, :], reads=(), writes=((key, r0 // step),))

        if defer:
            P.pending.append(go)
        else:
            go()


def phase1(P):
    k, LP = P.k, P.LP
    d = P.dram
    xt = [k.sb("xt%d" % i, [128, 1024]) for i in range(4)]
    hT = k.sb("hT", [128, 8, 512], BF16)
    wf = [k.sb("wf%d" % i, [128, 8, 128], BF16) for i in range(3)]
    wz = k.sb("wz", [128, 8, 1040], BF16)
    stg = [k.sb("stg%d" % i, [128, 512]) for i in range(3)]
    zst = [k.sb("zst%d" % i, [128, 1040]) for i in range(2)]
    ident = P.ident
    k.dma("sp", wz[:].rearrange("p a b -> p (a b)"), d["WINZb"][:, :], reads=[("WINZb", i) for i in range(1)], writes=["wz"])
    wi = 0
    si = 0
    zi = 0
    for (t0, n) in supertiles(LP):
        nj = n // 128
        for j in range(nj):
            k.dma("sp", xt[j][:], d["xin"][t0 + j * 128:t0 + (j + 1) * 128, :], reads=[], writes=["xt%d" % j])
        for kc in range(8):
            ps, pk = k.ps()
            for j in range(nj):
                k.tr(ps[:, j * 128:(j + 1) * 128], xt[j][:, kc * 128:(kc + 1) * 128], ident[:], reads=["xt%d" % j, "ident"], writes=[pk])
            k.copy(hT[:, kc, :n], ps[:, :n], reads=[pk], writes=[("hT", kc)], eng=("act" if kc % 2 else "dve"))
        for c in range(NFC):
            w = wf[wi % 3]
            wk = "wf%d" % (wi % 3)
            wi += 1
            k.dma("sp", w[:].rearrange("p a b -> p (a b)"), d["WINFb"][c * 128:(c + 1) * 128, :], reads=[("WINFb", (c * 128) // 512)], writes=[wk])
            ps, pk = k.ps()
            for kc in range(8):
                k.mm(ps[:, :n], w[:, kc, :], hT[:, kc, :n], kc == 0, kc == 7, reads=[wk, ("hT", kc)], writes=[pk])
            s = stg[si % 3]
            sk = "stg%d" % (si % 3)
            si += 1
            k.copy(s[:, :n], ps[:, :n], reads=[pk], writes=[sk], eng=("act" if c % 2 else "dve"))
            k.dma("pool", d["PT"][c * 128:(c + 1) * 128, t0:t0 + n], s[:, :n], reads=[sk], writes=[("PT", c)])
        for j in range(nj):
            z = zst[zi % 2]
            zk = "zst%d" % (zi % 2)
            zi += 1
            for (c0, c1) in ((0, 512), (512, 1024), (1024, 1040)):
                ps, pk = k.ps()
                for kc in range(8):
                    k.mm(ps[:, :c1 - c0], hT[:, kc, j * 128:(j + 1) * 128], wz[:, kc, c0:c1], kc == 0, kc == 7,
                         reads=["wz", ("hT", kc)], writes=[pk])
                k.copy(z[:, c0:c1], ps[:, :c1 - c0], reads=[pk], writes=[zk], eng=("act" if c0 == 512 else "dve"))
            k.dma("pool", d["Z"][t0 + j * 128:t0 + (j + 1) * 128, :], z[:], reads=[zk], writes=[("Z", 0)])


import os
LVL = int(os.environ.get('LVL', '99'))
R3 = int(os.environ.get('R3', '99'))
R3C = int(os.environ.get('R3C', '99'))
R3L = int(os.environ.get('R3L', '99'))
NVT = 32 + 2048
NVF = 64


def build(LP, dbg=(), upto=99):
    P = Prog(LP, dbg)
    nc = P.nc
    P.din("xin", [LP, D])
    P.din("CMd", [128, 640])
    P.din("VTd", [1, NVT])
    P.din("VFd", [128, NVF])
    P.din("WINF", [NFC * 128, 1024])
    P.din("WINZ", [128, 8 * 1040])
    P.din("CRd", [128, NCR])
    P.din("VRd", [128, NVR])
    P.din("WUAd", [128, 1024])
    P.din("GUPd", [128, 1024])
    P.din("LNGd", [128, 512])
    P.din("LNBd", [128, 512])
    P.din("VT2d", [1, 8192])
    P.din("WO", [128, 16 * 1024])
    P.din("WGU0", [44 * 128, 1024])
    P.din("WD0", [128, 22 * 1024])
    P.din("W1IN", [16 * 128, 1024])
    P.din("GWd", [128, 16 * 128])
    P.din("VLd", [128, NVL])
    P.din("W1O", [128, 8 * 1024])
    P.din("WRd", [128, 64])
    for e in range(NEXP):
        P.din("WGUE%d" % e, [44 * 128, 1024])
        P.din("WDE%d" % e, [128, 22 * 1024])
    P.dscr("WOb", [128, 16 * 1024], BF16)
    P.dscr("WGU0b", [44 * 128, 1024], BF16)
    P.dscr("WD0b", [128, 22 * 1024], BF16)
    P.dscr("W1INb", [16 * 128, 1024], BF16)
    P.dscr("W1Ob", [128, 8 * 1024], BF16)
    for e in range(NEXP):
        P.dscr("WGUEb%d" % e, [44 * 128, 1024], BF16)
        P.dscr("WDEb%d" % e, [128, 22 * 1024], BF16)
    P.dscr("H1", [LP, D])
    P.dscr("H1T", [D, LP], BF16)
    P.dscr("H2", [LP, D])
    P.dscr("H2T", [D, LP], BF16)
    P.dscr("XRT", [D, LP])
    P.dscr("Y1T", [D, LP], BF16)
    P.dscr("H3", [LP, D])
    P.dscr("H3T", [D, LP], BF16)
    P.dscr("H3T32", [D, LP])
    P.dout("OUT", [LP, D])
    P.dscr("WINFb", [NFC * 128, 1024], BF16)
    P.dscr("WINZb", [128, 8 * 1040], BF16)
    P.dscr("PT", [NFC * 128, LP])
    P.dscr("Z", [LP, 1040])
    P.dscr("XS", [LP, 1024])
    P.dscr("BTOK", [LP, 256])
    P.dscr("BT", [256, LP])
    P.dscr("CT", [256, LP])
    P.dscr("YT", [2048, LP], BF16)
    with P.es:
        k = KB(nc, P.es)
        P.k = k
        P.CM = k.sb("CM", [128, 640])
        P.ident = P.CM[:, 0:128]
        P.VT = k.sb("VT", [128, NVT])
        P.VF = k.sb("VF", [128, NVF])
        k.dma("sp", P.CM[:], P.dram["CMd"][:, :], writes=["CM", "ident"])
        k.dma("sp", P.VT[:], P.dram["VTd"][0:1, :].partition_broadcast(128), writes=["VT"])
        k.dma("sp", P.VF[:], P.dram["VFd"][:, :], writes=["VF"])
        cast_weights(P, P.dram["WINF"], P.dram["WINFb"], NFC * 128, "WINFb")
        k.dma("pool", P.dram["WINZb"][:, :].rearrange("p (a b) -> p a b", b=1040), P.dram["WINZ"][:, :].rearrange("p (a b) -> p a b", b=1040), writes=[("WINZb", 0)])
        if upto >= 4:
            def cast_rhs(src, dst, nk):
                P.pending.append(lambda: k.dma("pool", P.dram[dst][:, :].rearrange("p (a b) -> p a b", b=1024), P.dram[src][:, :].rearrange("p (a b) -> p a b", b=1024), writes=[(dst, 0)]))
            cast_rhs("WO", "WOb", 16)
            cast_weights(P, P.dram["WGU0"], P.dram["WGU0b"], 44 * 128, "WGU0b", defer=True)
            cast_rhs("WD0", "WD0b", 22)
            cast_weights(P, P.dram["W1IN"], P.dram["W1INb"], 16 * 128, "W1INb", defer=True)
            cast_rhs("W1O", "W1Ob", 8)
            for e in range(NEXP):
                cast_weights(P, P.dram["WGUE%d" % e], P.dram["WGUEb%d" % e], 44 * 128, "WGUEb%d" % e, defer=True)
                cast_rhs("WDE%d" % e, "WDEb%d" % e, 22)
        if upto >= 1:
            with ExitStack() as pes:
                k.pes = pes
                phase1(P)
            k.pes = None
            k.barrier()
        if upto >= 1.5:
            phase2a(P)
        if upto >= 2 and upto != 3.5:
            phase2b(P)
        if upto >= 3:
            phase3(P)
        if upto >= 4:
            P.pump_casts(100000)
            proj_res_ln(P, "p4", "YT", 16, "WOb", "xin", 0, 1024, "H1", "H1T")
            ffn_phase(P, "f0", "H1T", "H1", ["WGU0b"], ["WD0b"], 2048, 3072, "H2", "H2T")
        if upto >= 5:
            phase5(P)
            proj_res_ln(P, "p5", "Y1T", 8, "W1Ob", "H2", 4096, 5120, "H3", "H3T", "H3T32")
        if upto >= 6:
            ffn_phase(P, "f1", "H3T", "H3", ["WGUEb%d" % e for e in range(NEXP)], ["WDEb%d" % e for e in range(NEXP)], 6144, 7168, "OUT", None, router="H3T32", tile_n=1024)
        k.finish("sp")
        k.emit()
    return P


def host_consts(inp):
    t = np.arange(128)
    CM = np.zeros((128, 640), np.float32)
    CM[:, 0:128] = np.eye(128)
    CM[:, 128:256] = (t[:, None] <= t[None, :])
    CM[:, 256:384] = (t[:, None] > t[None, :])
    CM[:, 384:512] = 1.0
    CM[:, 512:640] = np.where(t[None, :] >= t[:, None], 0.0, -30000.0)
    VT = np.zeros((1, NVT), np.float32)
    VT[0, 0:16] = inp["ev_dt_bias"][0]
    VT[0, 16:32] = inp["ev_a_log"][0]
    VT[0, 32:32 + 1024] = np.repeat(inp["ev_d_skip"][0], 64)
    VT[0, 32 + 1024:32 + 2048] = inp["ev_ssm_norm"][0]
    VF = np.zeros((128, NVF), np.float32)
    cw = inp["ev_conv_w"][0]
    VF[:, 0:48] = cw.reshape(4, 12, 128).transpose(2, 1, 0).reshape(128, 48)
    VF[:, 48:60] = inp["ev_conv_b"][0].reshape(12, 128).T
    return CM, VT, VF


def fcols():
    return np.concatenate([np.arange(1024, 2048), np.arange(2048, 2304), np.arange(2304, 2560),
                           np.arange(2576, 3600), np.arange(3600, 4624), np.arange(4624, 5648),
                           np.arange(5648, 5776), np.arange(5776, 5904)])


def zcols():
    return np.concatenate([np.arange(0, 1024), np.arange(2560, 2576)])


def pack_lhsT(w, cols):
    ws = w[:, cols]
    nch = ws.shape[1] // 128
    a = ws.reshape(8, 128, nch, 128)
    return np.ascontiguousarray(a.transpose(2, 1, 0, 3)).reshape(nch * 128, 1024)


def pack_rhs(w, cols):
    ws = w[:, cols]
    n = ws.shape[1]
    return np.ascontiguousarray(ws.reshape(8, 128, n).transpose(1, 0, 2)).reshape(128, 8 * n)


CM_ID, CM_TRI, CM_TRIS, CM_ONES, CM_MB = 0, 128, 256, 384, 512
VT_DTB, VT_ALOG, VT_D, VT_NW = 0, 16, 32, 32 + 1024
VF_CW, VF_CB = 0, 48


def phase2a(P):
    k, LP, d = P.k, P.LP, P.dram
    P.pump_casts(19)
    with ExitStack() as pes:
        k.pes = pes
        raw = [k.sb("c_raw%d" % i, [128, LP + 3]) for i in range(2)]
        acc = [k.sb("c_acc%d" % i, [128, LP]) for i in range(2)]
        stg = [k.sb("c_stg%d" % i, [128, 512]) for i in range(3)]
        VF = P.VF
        for i in range(2):
            k.memset(raw[i][:, 0:3], 0.0, writes=["c_raw%d" % i])
        si = 0
        for c in range(12):
            r, rk = raw[c % 2], "c_raw%d" % (c % 2)
            a, ak = acc[c % 2], "c_acc%d" % (c % 2)
            k.dma("sp", r[:, 3:3 + LP], d["PT"][c * 128:(c + 1) * 128, :], reads=[("PT", c)], writes=[rk])
            cw = VF_CW + c * 4
            k.ts(a[:], r[:, 3:3 + LP], VF[:, cw + 3:cw + 4], OP.mult, reads=[rk, "VF"], writes=[ak])
            for kk in (2, 1, 0):
                k.stt(a[:], r[:, kk:kk + LP], VF[:, cw + kk:cw + kk + 1], a[:], OP.mult, OP.add, reads=[rk, ak, "VF"], writes=[ak])
            k.act(a[:], a[:], AF.Silu, reads=[ak, "VF"], writes=[ak], bias=VF[:, VF_CB + c:VF_CB + c + 1])
            if c >= 8:
                dst = d["BT"] if c < 10 else d["CT"]
                cc = (c - 8) % 2
                k.dma("pool", dst[cc * 128:(cc + 1) * 128, :], a[:], reads=[ak], writes=[("BT" if c < 10 else "CT", cc)])
            if c < 10:
                for (t0, n) in supertiles(LP):
                    nj = n // 128
                    ps, pk = k.ps()
                    for j in range(nj):
                        k.tr(ps[:, j * 128:(j + 1) * 128], a[:, t0 + j * 128:t0 + (j + 1) * 128], P.CM[:, CM_ID:CM_ID + 128], reads=[ak, "CM"], writes=[pk])
                    s, sk = stg[si % 3], "c_stg%d" % (si % 3)
                    si += 1
                    k.copy(s[:, :n], ps[:, :n], reads=[pk], writes=[sk], eng=("act" if si % 2 else "dve"))
                    if c < 8:
                        dd = d["XS"][t0:t0 + n, c * 128:(c + 1) * 128]
                        key = ("XS", c)
                    else:
                        dd = d["BTOK"][t0:t0 + n, (c - 8) * 128:(c - 7) * 128]
                        key = ("BTOK", c - 8)
                    k.dma("pool", dd.rearrange("(j p) m -> p j m", p=128), s[:, :n].rearrange("p (j m) -> p j m", m=128), reads=[sk], writes=[key])
    k.pes = None
    k.barrier()


def phase2b(P):
    k, LP, d = P.k, P.LP, P.dram
    CM, VT = P.CM, P.VT
    with ExitStack() as pes:
        k.pes = pes
        nb = 2
        xs = [k.sb("s_xs%d" % i, [128, 1024]) for i in range(nb)]
        btok = [k.sb("s_btok%d" % i, [128, 256]) for i in range(nb)]
        bt = [k.sb("s_bt%d" % i, [128, 2, 128]) for i in range(nb)]
        ct = [k.sb("s_ct%d" % i, [128, 2, 128]) for i in range(nb)]
        z = [k.sb("s_z%d" % i, [128, 1040]) for i in range(nb)]
        HT = [k.sb("s_HT%d" % g, [128, 512]) for g in range(2)]
        Abc = k.sb("s_Abc", [128, 16])
        dt = k.sb("s_dt", [128, 16])
        dtA = k.sb("s_dtA", [128, 16])
        E = k.sb("s_E", [128, 64])
        ncum = E[:, 48:64]
        LT = k.sb("s_LT", [128, 16, 128])
        SLT = k.sb("s_SLT", [128, 16, 128])
        xdt = k.sb("s_xdt", [128, 1024])
        xde = k.sb("s_xde", [128, 1024])
        y = k.sb("s_y", [128, 1024])
        t1 = k.sb("s_t1", [128, 1024])
        zs = k.sb("s_zs", [128, 1024])
        sq = k.sb("s_sq", [128, 512])
        ss = k.sb("s_ss", [128, 2])
        rstd = k.sb("s_rstd", [128, 2])
        yTs = [k.sb("s_yT%d" % i, [128, 8, 128], BF16) for i in range(2)]
        for g in range(2):
            k.memset(HT[g][:], 0.0, writes=["s_HT%d" % g])
        k.act(Abc[:], VT[:, VT_ALOG:VT_ALOG + 16], AF.Exp, reads=["VT"], writes=["s_Abc"])
        k.ts(Abc[:], Abc[:], -1.0, OP.mult, reads=["s_Abc"], writes=["s_Abc"])
        nch = LP // 128
        for ci in range(nch):
            t0 = ci * 128
            b = ci % nb
            kx, kb, kbt, kct, kz = "s_xs%d" % b, "s_btok%d" % b, "s_bt%d" % b, "s_ct%d" % b, "s_z%d" % b
            k.dma("sp", xs[b][:], d["XS"][t0:t0 + 128, :], reads=[("XS", c) for c in range(8)], writes=[kx])
            k.dma("sp", btok[b][:], d["BTOK"][t0:t0 + 128, :], reads=[("BTOK", 0), ("BTOK", 1)], writes=[kb])
            k.dma("sp", bt[b][:], d["BT"][:, t0:t0 + 128].rearrange("(g n) t -> n g t", n=128), reads=[("BT", 0), ("BT", 1)], writes=[kbt])
            k.dma("sp", ct[b][:], d["CT"][:, t0:t0 + 128].rearrange("(g n) t -> n g t", n=128), reads=[("CT", 0), ("CT", 1)], writes=[kct])
            k.dma("sp", z[b][:], d["Z"][t0:t0 + 128, :], reads=[("Z", 0)], writes=[kz])
            if LVL < 1: continue
            k.tt(dt[:], z[b][:, 1024:1040], VT[:, VT_DTB:VT_DTB + 16], OP.add, reads=[kz, "VT"], writes=["s_dt"])
            k.act(dt[:], dt[:], AF.Exp, reads=["s_dt"], writes=["s_dt"])
            k.act(dt[:], dt[:], AF.Ln, reads=["s_dt"], writes=["s_dt"], bias=1.0)
            k.tt(dtA[:], dt[:], Abc[:], OP.mult, reads=["s_dt", "s_Abc"], writes=["s_dtA"])
            if LVL < 2: continue
            psc, pkc = k.ps()
            k.mm(psc[:, 0:16], CM[:, CM_TRI:CM_TRI + 128], dtA[:], True, True, reads=["CM", "s_dtA"], writes=[pkc])
            k.mm(psc[:, 16:32], CM[:, CM_TRIS:CM_TRIS + 128], dtA[:], True, True, reads=["CM", "s_dtA"], writes=[pkc])
            k.mm(psc[:, 32:48], CM[:, CM_ONES:CM_ONES + 128], dtA[:], True, True, reads=["CM", "s_dtA"], writes=[pkc])
            SUB = int(os.environ.get('SUB', '3'))
            if SUB >= 1:
                k.act(E[:, 0:48], psc[:, 0:48], AF.Exp, reads=[pkc], writes=["s_E"])
            if SUB >= 2:
                k.ts(ncum, psc[:, 0:16], -1.0, OP.mult, reads=[pkc, "s_E"], writes=["s_ncum"])
            if LVL < 3: continue
            k.tt(xdt[:].rearrange("p (h e) -> p h e", e=64), xs[b][:].rearrange("p (h e) -> p h e", e=64),
                 dt[:].unsqueeze(2).broadcast_to([128, 16, 64]), OP.mult, reads=[kx, "s_dt"], writes=["s_xdt"])
            k.tt(xde[:].rearrange("p (h e) -> p h e", e=64), xdt[:].rearrange("p (h e) -> p h e", e=64),
                 E[:, 16:32].unsqueeze(2).broadcast_to([128, 16, 64]), OP.mult, reads=["s_xdt", "s_E"], writes=["s_xde"])
            if LVL < 4: continue
            for hq in range(4):
                psa, pka = k.ps()
                for hh in range(4):
                    h = hq * 4 + hh
                    k.mm(psa[:, hh * 128:(hh + 1) * 128], dtA[:, h:h + 1].broadcast_to([128, 128]), CM[:, CM_TRI:CM_TRI + 128], True, True,
                         reads=["s_dtA", "CM"], writes=[pka])
                for hh in range(4):
                    h = hq * 4 + hh
                    k.stt(LT[:, h, :], psa[:, hh * 128:(hh + 1) * 128], E[:, 48 + h:49 + h], CM[:, CM_MB:CM_MB + 128], OP.add, OP.add,
                          reads=[pka, "s_ncum", "CM"], writes=[("s_LT", hq)])
            for hq in range(4):
                k.act(LT[:, hq * 4:(hq + 1) * 4, :], LT[:, hq * 4:(hq + 1) * 4, :], AF.Exp, reads=[("s_LT", hq)], writes=[("s_LT", hq)])
            if LVL < 5: continue
            pss, pks = k.ps()
            for g in range(2):
                k.mm(pss[:, g * 128:(g + 1) * 128], bt[b][:, g, :], ct[b][:, g, :], True, True, reads=[kbt, kct], writes=[pks])
            for g in range(2):
                k.tt(SLT[:, g * 8:(g + 1) * 8, :], LT[:, g * 8:(g + 1) * 8, :],
                     pss[:, g * 128:(g + 1) * 128].unsqueeze(1).broadcast_to([128, 8, 128]), OP.mult,
                     reads=[("s_LT", 2 * g), ("s_LT", 2 * g + 1), pks], writes=[("s_SLT", g)])
            if LVL < 6: continue
            for g in range(2):
                psy, pky = k.ps()
                for hh in range(8):
                    h = g * 8 + hh
                    k.mm(psy[:, hh * 64:(hh + 1) * 64], SLT[:, h, :], xdt[:, h * 64:(h + 1) * 64], True, True, reads=[("s_SLT", g), "s_xdt"], writes=[pky])
                if R3L < 4: continue
                pso, pko = k.ps()
                k.mm(pso[:, :], ct[b][:, g, :], HT[g][:], True, True, reads=[kct, "s_HT%d" % g], writes=[pko])
                gs = slice(g * 512, (g + 1) * 512)
                k.tt(t1[:, gs].rearrange("p (h e) -> p h e", e=64), pso[:, :].rearrange("p (h e) -> p h e", e=64),
                     E[:, g * 8:(g + 1) * 8].unsqueeze(2).broadcast_to([128, 8, 64]), OP.mult, reads=[pko, "s_E"], writes=[("s_t1", g)])
                k.tt(y[:, gs], psy[:, :], t1[:, gs], OP.add, reads=[pky, ("s_t1", g)], writes=[("s_y", g)])
                psh, pkh = k.ps()
                k.mm(psh[:, :], btok[b][:, g * 128:(g + 1) * 128], xde[:, gs], True, True, reads=[kb, "s_xde"], writes=[pkh])
                k.tt(HT[g][:].rearrange("p (h e) -> p h e", e=64), HT[g][:].rearrange("p (h e) -> p h e", e=64),
                     E[:, 32 + g * 8:32 + (g + 1) * 8].unsqueeze(2).broadcast_to([128, 8, 64]), OP.mult, reads=["s_HT%d" % g, "s_E"], writes=["s_HT%d" % g])
                k.tt(HT[g][:], HT[g][:], psh[:, :], OP.add, reads=["s_HT%d" % g, pkh], writes=["s_HT%d" % g])
            if LVL < 7: continue
            k.tt(t1[:], xs[b][:], VT[:, VT_D:VT_D + 1024], OP.mult, reads=[kx, "VT", ("s_t1", 0), ("s_t1", 1)], writes=[("s_t1", 0), ("s_t1", 1)], eng="pool")
            k.tt(y[:], y[:], t1[:], OP.add, reads=[("s_y", 0), ("s_y", 1), ("s_t1", 0), ("s_t1", 1)], writes=[("s_y", 0), ("s_y", 1)])
            k.act(zs[:], z[b][:, 0:1024], AF.Silu, reads=[kz], writes=["s_zs"])
            k.tt(y[:], y[:], zs[:], OP.mult, reads=[("s_y", 0), ("s_y", 1), "s_zs"], writes=[("s_y", 0), ("s_y", 1)])
            for g in range(2):
                gs = slice(g * 512, (g + 1) * 512)
                k.op("act", lambda h, g=g, gs=gs: h.activation(out=sq[:], in_=y[:, gs], func=AF.Square, accum_out=ss[:, g:g + 1]),
                     reads=[("s_y", 0), ("s_y", 1)], writes=["s_sq", ("s_ss", g)])
            k.act(rstd[:], ss[:], AF.Sqrt, reads=[("s_ss", 0), ("s_ss", 1)], writes=["s_rstd"], bias=LN_EPS, scale=1.0 / 512)
            k.op("dve", lambda h: h.reciprocal(out=rstd[:], in_=rstd[:]), reads=["s_rstd"], writes=["s_rstd"])
            for g in range(2):
                gs = slice(g * 512, (g + 1) * 512)
                k.stt(y[:, gs], y[:, gs], rstd[:, g:g + 1], VT[:, VT_NW + g * 512:VT_NW + (g + 1) * 512], OP.mult, OP.mult,
                      reads=[("s_y", g), "s_rstd", "VT"], writes=[("s_y", g)])
            if LVL < 8: continue
            yT, kyT = yTs[ci % 2], "s_yT%d" % (ci % 2)
            for half in range(2):
                pst, pkt = k.ps()
                for cc in range(4):
                    c = half * 4 + cc
                    k.tr(pst[:, cc * 128:(cc + 1) * 128], y[:, c * 128:(c + 1) * 128], CM[:, CM_ID:CM_ID + 128], reads=[("s_y", 0), ("s_y", 1), "CM"], writes=[pkt])
                k.copy(yT[:, half * 4:(half + 1) * 4, :], pst[:, :].rearrange("p (c t) -> p c t", t=128), reads=[pkt], writes=[kyT], eng=("act" if half else "dve"))
            k.dma("pool", d["YT"][0:1024, t0:t0 + 128].rearrange("(c p) t -> p c t", p=128), yT[:], reads=[kyT], writes=[("YT", 0)])
    k.pes = None
    k.barrier()


CR_M1, CR_M2, CR_SLN, CR_ISEL, CR_BD, CR_BONES, CR_RESET = 0, 256, 512, 640, 704, 832, 960
NCR = 960 + 512
VR_MUR, VR_MUK, VR_MUV, VR_W0, VR_A0, VR_KK, VR_KA, VR_RK, VR_MULO, VR_MUG = 0, 8, 16, 24, 32, 40, 48, 56, 64, 65
NVR = 66


def phase3(P):
    k, LP, d = P.k, P.LP, P.dram
    CM = P.CM
    with ExitStack() as pes:
        k.pes = pes
        CR = k.sb("CR", [128, NCR])
        VR = k.sb("VR", [128, NVR])
        OMKA = k.sb("OMKA", [128, 8])
        WUA = k.sb("WUA", [128, 1024])
        GUP = k.sb("GUP", [128, 1024])
        LNG = k.sb("LNG", [128, 512])
        LNB = k.sb("LNB", [128, 512])
        k.dma("sp", CR[:], d["CRd"][:, :], writes=["CR"])
        k.dma("sp", VR[:], d["VRd"][:, :], writes=["VR"])
        k.dma("sp", WUA[:], d["WUAd"][:, :], writes=["WUA"])
        k.dma("sp", GUP[:], d["GUPd"][:, :], writes=["GUP"])
        k.dma("sp", LNG[:], d["LNGd"][:, :], writes=["LNG"])
        k.dma("sp", LNB[:], d["LNBd"][:, :], writes=["LNB"])
        k.ts(OMKA[:], VR[:, VR_KA:VR_KA + 8], -1.0, OP.mult, reads=["VR"], writes=["OMKA"], s2=1.0, op1=OP.add)
        FR = mybir.dt.float32r
        T = [k.sb("r_T%d" % j, [128, 64], FR) for j in range(8)]
        for j in range(8):
            k.memset(T[j][:].bitcast(F32), 0.0, writes=["r_T%d" % j])
        ident = CM[:, CM_ID:CM_ID + 128]
        MASK1 = CR[:, CR_M1:CR_M1 + 256]
        MASK2 = CR[:, CR_M2:CR_M2 + 256]
        MSLN = CR[:, CR_SLN:CR_SLN + 128]
        ISELt = k.sb("r_ISELr", [128, 64], FR)
        k.copy(ISELt[:], CR[:, CR_ISEL:CR_ISEL + 64], reads=["CR"], writes=["CR"])
        ISEL = ISELt[:]
        BONES = CR[:, CR_BONES:CR_BONES + 128]

        def T_(name, shape, dt=F32):
            return k.sb(name, shape, dt), name

        lo_raw, klo_raw = T_("r_loraw", [128, 513])
        g_raw, kg_raw = T_("r_graw", [128, 513])
        LOm, kLOm = T_("r_LOm", [128, 512])
        Gs, kGs = T_("r_Gs", [128, 512])
        raw = {nm: [T_("r_raw%s%d" % (nm, i), [128, 513]) for i in range(2)] for nm in "rkv"}
        mx = {nm: T_("r_mx" + nm, [128, 512]) for nm in "rkv"}
        tmp, ktmp = T_("r_tmp", [128, 512])
        tmp2, ktmp2 = T_("r_tmp2", [128, 512])
        lw, klw = T_("r_lw", [128, 512])
        av, kav = T_("r_a", [128, 512])
        gate, kgate = T_("r_gate", [128, 512])
        kkv, kkk = T_("r_kk", [128, 512])
        kmod, kkmod = T_("r_kmod", [128, 512])
        bv, kbv = T_("r_b", [128, 512])
        bonus, kbonus = T_("r_bonus", [128, 512])
        cw, kcw = T_("r_cw", [128, 512])
        ecw, kecw = T_("r_ecw", [128, 512])
        ecwx, kecwx = T_("r_ecwx", [128, 512])
        eneg, keneg = T_("r_eneg", [128, 512])
        eend, keend = T_("r_eend", [128, 512])
        prod, kprod = T_("r_prod", [128, 512])
        QRbd, kQR = T_("r_QRbd", [128, 8, 2, 128], FR)
        kibd, kki = T_("r_kibd", [128, 8, 128], FR)
        bibd, kbi = T_("r_bibd", [128, 8, 128], FR)
        vbd, kvb = T_("r_vbd", [128, 8, 128], FR)
        kebd, kke = T_("r_kebd", [128, 8, 128], FR)
        bebd, kbe = T_("r_bebd", [128, 8, 128], FR)
        A1, kA1 = T_("r_A1", [128, 8, 256], FR)
        A2, kA2 = T_("r_A2", [128, 8, 256], FR)
        PP = [T_("r_PP%d" % i, [128, 8, 256], FR) for i in range(2)]
        QT = [T_("r_QT%d" % i, [128, 8, 128], FR) for i in range(2)]
        Vs, kVs = T_("r_Vs", [128, 8, 64], FR)
        Rs, kRs = T_("r_Rs", [128, 64], FR)
        Us, kUs = T_("r_Us", [128, 8, 64], FR)
        KT, kKT = T_("r_KT", [128, 8, 256], FR)
        Yall, kY = T_("r_Yall", [128, 8, 64])
        Ysq, kYsq = T_("r_Ysq", [128, 8, 64])
        st1, kst1 = T_("r_st1", [128, 8])
        st2, kst2 = T_("r_st2", [128, 8])
        obd, kobd = T_("r_obd", [128, 8, 128], FR)
        yb = [T_("r_yb%d" % i, [128, 512], BF16) for i in range(2)]
        k.memset(lo_raw[:, 0:1], 0.0, writes=[klo_raw])
        k.memset(g_raw[:, 0:1], 0.0, writes=[kg_raw])
        for nm in "rkv":
            for i in range(2):
                k.memset(raw[nm][i][0][:, 0:1], 0.0, writes=[raw[nm][i][1]])
        BDM = CR[:, CR_BD:CR_BD + 128].rearrange("p (a b) -> p a b", b=64)
        it = 0

        def load_shift(tile, key, chunk, t0, n, mucol, outt, outk):
            if t0 == 0:
                k.dma("sp", tile[:, 1:n + 1], d["PT"][chunk * 128:(chunk + 1) * 128, 0:n], reads=[("PT", chunk)], writes=[key])
            else:
                k.dma("sp", tile[:, 0:n + 1], d["PT"][chunk * 128:(chunk + 1) * 128, t0 - 1:t0 + n], reads=[("PT", chunk)], writes=[key])
            k.tt(outt[:, :n], tile[:, 0:n], tile[:, 1:n + 1], OP.subtract, reads=[key], writes=[outk])
            k.stt(outt[:, :n], outt[:, :n], VR[:, mucol:mucol + 1], tile[:, 1:n + 1], OP.mult, OP.add, reads=[outk, key, "VR"], writes=[outk])

        def bd_expand(dst, kdst, src, ksrc, nc_, eng="dve"):
            k.tt(dst[:, :nc_].rearrange("p c (a b) -> p c a b", b=64) if len(dst.shape) == 3 else dst,
                 src.rearrange("p (c t) -> p c t", t=64).unsqueeze(2).broadcast_to([128, nc_, 2, 64]),
                 BDM.unsqueeze(1).broadcast_to([128, nc_, 2, 64]), OP.mult, reads=[ksrc, "CR"], writes=[kdst], eng=eng)

        for (t0, n) in supertiles(LP):
            nc_ = n // 64
            load_shift(lo_raw, klo_raw, C_LO, t0, n, VR_MULO, LOm, kLOm)
            k.act(LOm[0:64, :n], LOm[0:64, :n], AF.Tanh, reads=[kLOm], writes=[kLOm])
            load_shift(g_raw, kg_raw, C_G, t0, n, VR_MUG, Gs, kGs)
            k.act(Gs[:, :n], Gs[:, :n], AF.Sigmoid, reads=[kGs], writes=[kGs])
            for j in range(8):
                it += 1
                for nm, ch, mu in (("r", C_R, VR_MUR), ("k", C_K, VR_MUK), ("v", C_V, VR_MUV)):
                    rt, rk_ = raw[nm][it % 2]
                    load_shift(rt, rk_, ch + j, t0, n, mu + j, mx[nm][0], mx[nm][1])
                r_, kr_ = mx["r"]
                k_, kk_ = mx["k"]
                v_, kv_ = mx["v"]
                js = slice(j * 128, (j + 1) * 128)
                if R3 < 1: continue
                ps, pk = k.ps()
                k.mm(ps[:, :n], WUA[0:64, js], LOm[0:64, :n], True, True, reads=["WUA", kLOm], writes=[pk])
                k.act(lw[:, :n], ps[:, :n], AF.Sigmoid, reads=[pk, "VR"], writes=[klw], bias=VR[:, VR_W0 + j:VR_W0 + j + 1])
                k.ts(lw[:, :n], lw[:, :n], -float(np.exp(-0.5)), OP.mult, reads=[klw], writes=[klw])
                ps, pk = k.ps()
                k.mm(ps[:, :n], WUA[64:128, js], LOm[64:128, :n], True, True, reads=["WUA", kLOm], writes=[pk])
                k.act(av[:, :n], ps[:, :n], AF.Sigmoid, reads=[pk, "VR"], writes=[kav], bias=VR[:, VR_A0 + j:VR_A0 + j + 1])
                ps, pk = k.ps()
                k.mm(ps[:, :n], GUP[:, js], Gs[:, :n], True, True, reads=["GUP", kGs], writes=[pk])
                k.copy(gate[:, :n], ps[:, :n], reads=[pk], writes=[kgate], eng="act")
                if R3 < 2: continue
                k.ts(kkv[:, :n], k_[:, :n], VR[:, VR_KK + j:VR_KK + j + 1], OP.mult, reads=[kk_, "VR"], writes=[kkk])
                k.tt(tmp[:, :n], kkv[:, :n], kkv[:, :n], OP.mult, reads=[kkk], writes=[ktmp])
                ps, pk = k.ps()
                k.mm(ps[:, :n], BONES, tmp[:, :n], True, True, reads=["CR", ktmp], writes=[pk])
                k.ts(tmp2[:, :n], ps[:, :n], 1e-24, OP.max, reads=[pk], writes=[ktmp2])
                k.act(tmp2[:, :n], tmp2[:, :n], AF.Sqrt, reads=[ktmp2], writes=[ktmp2])
                k.op("dve", lambda h, n=n: h.reciprocal(out=tmp2[:, :n], in_=tmp2[:, :n]), reads=[ktmp2], writes=[ktmp2])
                k.tt(kkv[:, :n], kkv[:, :n], tmp2[:, :n], OP.mult, reads=[kkk, ktmp2], writes=[kkk])
                k.ts(tmp[:, :n], av[:, :n], VR[:, VR_KA + j:VR_KA + j + 1], OP.mult, reads=[kav, "VR", "OMKA"], writes=[ktmp], s2=OMKA[:, j:j + 1], op1=OP.add)
                k.tt(kmod[:, :n], k_[:, :n], tmp[:, :n], OP.mult, reads=[kk_, ktmp], writes=[kkmod])
                k.tt(bv[:, :n], av[:, :n], kkv[:, :n], OP.mult, reads=[kav, kkk], writes=[kbv], eng="pool")
                k.stt(tmp[:, :n], r_[:, :n], VR[:, VR_RK + j:VR_RK + j + 1], kmod[:, :n], OP.mult, OP.mult, reads=[kr_, kkmod, "VR"], writes=[ktmp])
                ps, pk = k.ps()
                k.mm(ps[:, :n], BONES, tmp[:, :n], True, True, reads=["CR", ktmp], writes=[pk])
                k.tt(bonus[:, :n], ps[:, :n], v_[:, :n], OP.mult, reads=[pk, kv_], writes=[kbonus])
                if R3 < 3: continue
                k.op("dve", lambda h, n=n: h.tensor_tensor_scan(out=cw[:, :n], data0=CR[:, CR_RESET:CR_RESET + n], data1=lw[:, :n], initial=0.0, op0=OP.mult, op1=OP.add),
                     reads=["CR", klw], writes=[kcw])
                k.act(ecw[:, :n], cw[:, :n], AF.Exp, reads=[kcw], writes=[kecw])
                k.tt(tmp2[:, :n], cw[:, :n], lw[:, :n], OP.subtract, reads=[kcw, klw], writes=[ktmp2], eng="pool")
                k.act(ecwx[:, :n], tmp2[:, :n], AF.Exp, reads=[ktmp2], writes=[kecwx])
                k.act(eneg[:, :n], cw[:, :n], AF.Exp, reads=[kcw], writes=[keneg], scale=-1.0)
                cw3 = cw[:, :n].rearrange("p (c t) -> p c t", t=64)
                k.tt(tmp[:, :n].rearrange("p (c t) -> p c t", t=64), cw3[:, :, 63:64].broadcast_to([128, nc_, 64]), cw3, OP.subtract, reads=[kcw], writes=[ktmp])
                k.act(eend[:, :n], tmp[:, :n], AF.Exp, reads=[ktmp], writes=[keend])
                if R3 < 4: continue
                k.tt(prod[:, :n], kkv[:, :n], ecwx[:, :n], OP.mult, reads=[kkk, kecwx], writes=[kprod])
                k.tt(QRbd[:, :nc_, 0, :].rearrange("p c (a b) -> p c a b", b=64), prod[:, :n].rearrange("p (c t) -> p c t", t=64).unsqueeze(2).broadcast_to([128, nc_, 2, 64]),
                     BDM.unsqueeze(1).broadcast_to([128, nc_, 2, 64]), OP.mult, reads=[kprod, "CR"], writes=[(kQR, 0)])
                k.tt(prod[:, :n], r_[:, :n], ecw[:, :n], OP.mult, reads=[kr_, kecw, (kQR, 0)], writes=[kprod])
                k.tt(QRbd[:, :nc_, 1, :].rearrange("p c (a b) -> p c a b", b=64), prod[:, :n].rearrange("p (c t) -> p c t", t=64).unsqueeze(2).broadcast_to([128, nc_, 2, 64]),
                     BDM.unsqueeze(1).broadcast_to([128, nc_, 2, 64]), OP.mult, reads=[kprod, "CR"], writes=[(kQR, 1)])
                for (dst, kdst, src, ksrc, ex, kex, neg) in ((kibd, kki, kmod, kkmod, eneg, keneg, False), (bibd, kbi, bv, kbv, eneg, keneg, False),
                                                          (kebd, kke, kmod, kkmod, eend, keend, False), (bebd, kbe, bv, kbv, eend, keend, True)):
                    if neg:
                        k.stt(prod[:, :n], src[:, :n], -1.0, ex[:, :n], OP.mult, OP.mult, reads=[ksrc, kex, kprod, (kQR, 1), kki, kbi, kke], writes=[kprod])
                    else:
                        k.tt(prod[:, :n], src[:, :n], ex[:, :n], OP.mult, reads=[ksrc, kex, kprod, (kQR, 1), kki, kbi, kke], writes=[kprod])
                    k.tt(dst[:, :nc_].rearrange("p c (a b) -> p c a b", b=64), prod[:, :n].rearrange("p (c t) -> p c t", t=64).unsqueeze(2).broadcast_to([128, nc_, 2, 64]),
                         BDM.unsqueeze(1).broadcast_to([128, nc_, 2, 64]), OP.mult, reads=[kprod, "CR"], writes=[kdst])
                k.tt(vbd[:, :nc_].rearrange("p c (a b) -> p c a b", b=64), v_[:, :n].rearrange("p (c t) -> p c t", t=64).unsqueeze(2).broadcast_to([128, nc_, 2, 64]),
                     BDM.unsqueeze(1).broadcast_to([128, nc_, 2, 64]), OP.mult, reads=[kv_, "CR"], writes=[kvb], eng="pool")
                Tj, kTj = T[j], "r_T%d" % j
                for c in range(nc_):
                    QRc = QRbd[:, c].rearrange("p a b -> p (a b)")
                    ps1, pk1 = k.ps()
                    k.mm(ps1[:, 0:256], kibd[:, c], QRc, True, True, reads=[kki, (kQR, 0), (kQR, 1)], writes=[pk1])
                    k.tt(A1[:, c, :], ps1[:, 0:256], MASK1, OP.mult, reads=[pk1, "CR"], writes=[(kA1, c)])
                    ps2, pk2 = k.ps()
                    k.mm(ps2[:, 0:256], bibd[:, c], QRc, True, True, reads=[kbi, (kQR, 0), (kQR, 1)], writes=[pk2])
                    k.tt(A2[:, c, :], ps2[:, 0:256], MASK2, OP.mult, reads=[pk2, "CR"], writes=[(kA2, c)])
                    ps3, pk3 = k.ps()
                    k.mm(ps3[:, 0:128], QRbd[:, c, 0], bibd[:, c], True, True, reads=[kbi, (kQR, 0)], writes=[pk3])
                    k.tt(PP[0][0][:, c, 0:128], ps3[:, 0:128], MSLN, OP.mult, reads=[pk3, "CR"], writes=[(PP[0][1], c)])
                    k.copy(PP[0][0][:, c, 128:256], A2[:, c, 0:128].bitcast(F32), reads=[(kA2, c), (PP[0][1], c)], writes=[(PP[0][1], c)], eng="pool")
                    k.tt(QT[0][0][:, c, :], A2[:, c, 0:128].bitcast(F32), ident, OP.add, reads=[(kA2, c), "CM"], writes=[(QT[0][1], c)], eng="pool")
                cur = 0
                for lv in range(1, 6):
                    Pc, kPc = PP[cur]
                    Pn, kPn = PP[1 - cur]
                    Qc, kQc = QT[cur]
                    Qn, kQn = QT[1 - cur]
                    for c0 in range(0, nc_, 2):
                        psq, pkq = k.ps()
                        for ci in range(2):
                            c = c0 + ci
                            k.mm(psq[:, ci * 256:ci * 256 + 128], Pc[:, c, 128:256], Pc[:, c, 0:128], True, True, reads=[(kPc, c)], writes=[pkq])
                            k.mm(psq[:, ci * 256 + 128:ci * 256 + 256], Pc[:, c, 0:128], Pc[:, c, 128:256], True, True, reads=[(kPc, c)], writes=[pkq])
                        k.copy(Pn[:, c0:c0 + 2, :], psq[:, 0:512].rearrange("p (c f) -> p c f", f=256), reads=[pkq], writes=[(kPn, c0), (kPn, c0 + 1)], eng="act")
                    for c0 in range(0, nc_, 4):
                        m = min(4, nc_ - c0)
                        psu, pku = k.ps()
                        for ci in range(m):
                            c = c0 + ci
                            k.mm(psu[:, ci * 128:(ci + 1) * 128], Pn[:, c, 0:128], Qc[:, c, :], True, True, reads=[(kPn, c), (kQc, c)], writes=[pku])
                        k.tt(Qn[:, c0:c0 + m, :], psu[:, 0:m * 128].rearrange("p (c f) -> p c f", f=128), Qc[:, c0:c0 + m, :].bitcast(F32), OP.add,
                             reads=[pku] + [(kQc, c0 + i) for i in range(m)], writes=[(kQn, c0 + i) for i in range(m)])
                    cur = 1 - cur
                Qf, kQf = QT[cur]
                psv, pkv = k.ps()
                for c in range(nc_):
                    k.mm(psv[:, c * 64:(c + 1) * 64], vbd[:, c], ISEL, True, True, reads=[kvb, "CR"], writes=[pkv])
                k.copy(Vs[:, :nc_, :], psv[:, 0:nc_ * 64].rearrange("p (c f) -> p c f", f=64), reads=[pkv], writes=[kVs], eng="act")
                for c0 in range(0, nc_, 2):
                    pst, pkt = k.ps()
                    for ci in range(2):
                        c = c0 + ci
                        k.tr(pst[:, ci * 256:ci * 256 + 128], kebd[:, c].bitcast(F32), ident, reads=[kke, "CM"], writes=[pkt])
                        k.tr(pst[:, ci * 256 + 128:ci * 256 + 256], bebd[:, c].bitcast(F32), ident, reads=[kbe, "CM"], writes=[pkt])
                    k.copy(KT[:, c0:c0 + 2, :], pst[:, 0:512].rearrange("p (c f) -> p c f", f=256), reads=[pkt], writes=[(kKT, c0), (kKT, c0 + 1)])
                psY, pkY = k.psb[7], "psb7"
                for c in range(nc_):
                    psr, pkr = k.ps()
                    k.mm(psr[:, 0:64], QRbd[:, c, 0], Tj[:], True, False, reads=[(kQR, 0), kTj], writes=[pkr])
                    k.mm(psr[:, 0:64], A1[:, c, 0:128], Vs[:, c, :], False, True, reads=[(kA1, c), kVs], writes=[pkr])
                    k.copy(Rs[:], psr[:, 0:64], reads=[pkr], writes=[kRs])
                    psu, pku = k.ps()
                    k.mm(psu[:, 0:64], Qf[:, c, :], Rs[:], True, True, reads=[(kQf, c), kRs], writes=[pku])
                    k.copy(Us[:, c, :], psu[:, 0:64], reads=[pku], writes=[(kUs, c)])
                    psn, pkn = k.ps()
                    k.mm(psn[:, 0:64], KT[:, c, 0:128], Vs[:, c, :], True, False, reads=[(kKT, c), kVs], writes=[pkn])
                    k.mm(psn[:, 0:64], KT[:, c, 128:256], Us[:, c, :], False, True, reads=[(kKT, c), (kUs, c)], writes=[pkn])
                    k.mm(psY[:, c * 64:(c + 1) * 64], QRbd[:, c, 1], Tj[:], True, False, reads=[(kQR, 1), kTj], writes=[pkY])
                    k.mm(psY[:, c * 64:(c + 1) * 64], A1[:, c, 128:256], Vs[:, c, :], False, False, reads=[(kA1, c), kVs], writes=[pkY])
                    k.mm(psY[:, c * 64:(c + 1) * 64], A2[:, c, 128:256], Us[:, c, :], False, True, reads=[(kA2, c), (kUs, c)], writes=[pkY])
                    k.stt(Tj[:], Tj[:].bitcast(F32), ecw[:, c * 64 + 63:c * 64 + 64], psn[:, 0:64], OP.mult, OP.add, reads=[kTj, kecw, pkn], writes=[kTj])
                k.copy(Yall[:, :nc_, :], psY[:, 0:nc_ * 64].rearrange("p (c f) -> p c f", f=64), reads=[pkY], writes=[kY], eng="act")
                P.pump_casts(2)
                if R3 < 6: continue
                Yv = Yall[:, :nc_, :]
                k.op("dve", lambda h, Yv=Yv, nc_=nc_: h.tensor_reduce(out=st1[:, :nc_], in_=Yv, axis=mybir.AxisListType.X, op=OP.add), reads=[kY], writes=[kst1])
                k.ts(st1[:, :nc_], st1[:, :nc_], 1.0 / 64, OP.mult, reads=[kst1], writes=[kst1])
                k.tt(Yv, Yv, st1[:, :nc_].unsqueeze(2).broadcast_to([128, nc_, 64]), OP.subtract, reads=[kY, kst1], writes=[kY])
                if R3L < 1: continue
                k.tt(Ysq[:, :nc_, :], Yv, Yv, OP.mult, reads=[kY], writes=[kYsq])
                k.op("dve", lambda h, nc_=nc_: h.tensor_reduce(out=st2[:, :nc_], in_=Ysq[:, :nc_, :], axis=mybir.AxisListType.X, op=OP.add), reads=[kYsq], writes=[kst2])
                k.act(st2[:, :nc_], st2[:, :nc_], AF.Sqrt, reads=[kst2], writes=[kst2], bias=RWKV_EPS, scale=1.0 / 64)
                k.op("dve", lambda h, nc_=nc_: h.reciprocal(out=st2[:, :nc_], in_=st2[:, :nc_]), reads=[kst2], writes=[kst2])
                k.tt(Yv, Yv, st2[:, :nc_].unsqueeze(2).broadcast_to([128, nc_, 64]), OP.mult, reads=[kY, kst2], writes=[kY])
                if R3L < 2: continue
                for c in range(nc_):
                    k.tt(Yall[:, c, :], Yall[:, c, :], LNG[:, j * 64:(j + 1) * 64], OP.mult, reads=[kY, "LNG"], writes=[kY])
                    k.tt(Yall[:, c, :], Yall[:, c, :], LNB[:, j * 64:(j + 1) * 64], OP.add, reads=[kY, "LNB"], writes=[kY])
                if R3L < 3: continue
                k.tt(obd[:, :nc_].rearrange("p c (a b) -> p c a b", b=64), Yv.unsqueeze(2).broadcast_to([128, nc_, 2, 64]),
                     BDM.unsqueeze(1).broadcast_to([128, nc_, 2, 64]), OP.mult, reads=[kY, "CR"], writes=[kobd])
                pso, pko = k.ps()
                for c in range(nc_):
                    k.mm(pso[:, c * 64:(c + 1) * 64], obd[:, c], ISEL, True, True, reads=[kobd, "CR"], writes=[pko])
                if R3L < 5: continue
                k.tt(tmp[:, :n], pso[:, :n], bonus[:, :n], OP.add, reads=[pko, kbonus], writes=[ktmp])
                ybt, kyb = yb[it % 2]
                k.tt(ybt[:, :n], tmp[:, :n], gate[:, :n], OP.mult, reads=[ktmp, kgate], writes=[kyb])
                k.dma("pool", d["YT"][1024 + j * 128:1024 + (j + 1) * 128, t0:t0 + n], ybt[:, :n], reads=[kyb], writes=[("YT", 1)])
    k.pes = None
    k.barrier()


def host_consts_rwkv(inp):
    p = np.arange(128)
    q = np.arange(128)
    same = (p[:, None] // 64) == (q[None, :] // 64)
    SU = (same & ((p[:, None] % 64) < (q[None, :] % 64))).astype(np.float32)
    IU = (same & ((p[:, None] % 64) <= (q[None, :] % 64))).astype(np.float32)
    SL = (same & ((q[None, :] % 64) < (p[:, None] % 64))).astype(np.float32)
    CR = np.zeros((128, NCR), np.float32)
    CR[:, 0:128] = SU
    CR[:, 128:256] = IU
    CR[:, 256:384] = -SU
    CR[:, 384:512] = -IU
    CR[:, 512:640] = -SL
    CR[:, 640:704] = ((p[:, None] % 64) == np.arange(64)[None, :])
    CR[:, 704:832] = ((p[:, None] // 64) == (np.arange(128)[None, :] // 64))
    CR[:, 832:960] = same
    CR[:, 960:960 + 512] = (np.arange(512) % 64 != 0)[None, :]
    mu = inp["ev_shift_mu"][0]
    VR = np.zeros((128, NVR), np.float32)

    def pairs(v):
        return v.reshape(8, 128).T

    VR[:, 0:8] = pairs(mu[0:1024])
    VR[:, 8:16] = pairs(mu[1024:2048])
    VR[:, 16:24] = pairs(mu[2048:3072])
    VR[:, 24:32] = pairs(inp["ev_w0"][0])
    VR[:, 32:40] = pairs(inp["ev_a0"][0])
    VR[:, 40:48] = pairs(inp["ev_k_k"][0])
    VR[:, 48:56] = pairs(inp["ev_k_a"][0])
    VR[:, 56:64] = pairs(inp["ev_r_k"][0].reshape(1024))
    VR[:, 64] = mu[3072:3200]
    VR[:, 65] = mu[3200:3328]
    WUA = np.concatenate([inp["ev_w_up"][0], inp["ev_a_up"][0]], 0).astype(np.float32)
    GUP = inp["ev_g_up"][0].astype(np.float32)

    def stack(v):
        a = v.reshape(8, 2, 64)
        return np.ascontiguousarray(np.repeat(a.transpose(1, 0, 2)[:, None], 64, axis=1).reshape(128, 512))

    return dict(CRd=CR, VRd=VR, WUAd=WUA, GUPd=GUP, LNGd=stack(inp["ev_lnx_g"][0]), LNBd=stack(inp["ev_lnx_b"][0]))


def layer_norm_rows(k, pfx, src, ksrc, dst, kdst, gcol, bcol, VT2, tiles):
    stats, mv, rs = tiles
    for i in range(2):
        k.op("dve", lambda h, i=i: h.bn_stats(out=stats[:, i * 6:(i + 1) * 6], in_=src[:, i * 512:(i + 1) * 512]), reads=[ksrc], writes=[pfx + "st"])
    k.op("dve", lambda h: h.bn_aggr(out=mv[:], in_=stats[:]), reads=[pfx + "st"], writes=[pfx + "mv"])
    k.act(rs[:], mv[:, 1:2], AF.Sqrt, reads=[pfx + "mv"], writes=[pfx + "rs"], bias=LN_EPS)
    k.op("dve", lambda h: h.reciprocal(out=rs[:], in_=rs[:]), reads=[pfx + "rs"], writes=[pfx + "rs"])
    k.ts(dst[:], src[:], mv[:, 0:1], OP.subtract, reads=[ksrc, pfx + "mv", pfx + "rs"], writes=[kdst], s2=rs[:, 0:1], op1=OP.mult)
    k.tt(dst[:], dst[:], VT2[:, gcol:gcol + 1024], OP.mult, reads=[kdst, "VT2"], writes=[kdst])
    k.tt(dst[:], dst[:], VT2[:, bcol:bcol + 1024], OP.add, reads=[kdst, "VT2"], writes=[kdst])


def proj_res_ln(P, pfx, srcT, nkc, wname, res_name, gcol, bcol, out_name, outT_name, outT32_name=None):
    k, LP, d = P.k, P.LP, P.dram
    with ExitStack() as pes:
        k.pes = pes
        VT2 = k.sb(pfx + "VT2", [128, 2048])
        k.dma("sp", VT2[:], d["VT2d"][0:1, gcol:gcol + 2048].partition_broadcast(128), writes=["VT2"])
        P.VT2 = VT2
        gcol, bcol = 0, 1024
        W = k.sb(pfx + "W", [128, nkc, 1024], BF16)
        k.dma("sp", W[:].rearrange("p a b -> p (a b)"), d[wname][:, :], reads=[(wname, 0)], writes=[pfx + "W"])
        yT = [k.sb(pfx + "yT%d" % i, [128, nkc, 512], BF16) for i in range(2)]
        xr = [k.sb(pfx + "x%d" % i, [128, 1024]) for i in range(2)]
        hp = k.sb(pfx + "hp", [128, 1024])
        ho = [k.sb(pfx + "ho%d" % i, [128, 1024]) for i in range(2)]
        hT = [k.sb(pfx + "hT%d" % i, [128, 8, 128], BF16) for i in range(2)]
        hT32 = [k.sb(pfx + "hTf%d" % i, [128, 8, 128]) for i in range(2)]
        lnt = (k.sb(pfx + "st", [128, 12]), k.sb(pfx + "mv", [128, 2]), k.sb(pfx + "rs", [128, 1]))
        it = 0
        for si, (t0, n) in enumerate(supertiles(LP)):
            y, ky = yT[si % 2], pfx + "yT%d" % (si % 2)
            k.dma("sp", y[:, :, :n], d[srcT][:, t0:t0 + n].rearrange("(c p) t -> p c t", p=128), reads=[(srcT, 0), (srcT, 1)], writes=[ky])
            for j in range(n // 128):
                it += 1
                x, kx = xr[it % 2], pfx + "x%d" % (it % 2)
                r0 = t0 + j * 128
                k.dma("sp", x[:], d[res_name][r0:r0 + 128, :], reads=[(res_name, 0)], writes=[kx])
                for half in range(2):
                    ps, pk = k.ps()
                    for kc in range(nkc):
                        k.mm(ps[:, :], y[:, kc, j * 128:(j + 1) * 128], W[:, kc, half * 512:(half + 1) * 512], kc == 0, kc == nkc - 1, reads=[ky, pfx + "W"], writes=[pk])
                    k.stt(hp[:, half * 512:(half + 1) * 512], x[:, half * 512:(half + 1) * 512], ALPHA, ps[:, :], OP.mult, OP.add, reads=[kx, pk], writes=[pfx + "hp"])
                o, ko = ho[it % 2], pfx + "ho%d" % (it % 2)
                layer_norm_rows(k, pfx, hp, pfx + "hp", o, ko, gcol, bcol, P.VT2, lnt)
                k.dma("pool", d[out_name][r0:r0 + 128, :], o[:], reads=[ko], writes=[(out_name, 0)])
                t_, kt = hT[it % 2], pfx + "hT%d" % (it % 2)
                tf, ktf = hT32[it % 2], pfx + "hTf%d" % (it % 2)
                for half in range(2):
                    ps, pk = k.ps()
                    for cc in range(4):
                        c = half * 4 + cc
                        k.tr(ps[:, cc * 128:(cc + 1) * 128], o[:, c * 128:(c + 1) * 128], P.ident, reads=[ko, "CM"], writes=[pk])
                    k.copy(t_[:, half * 4:(half + 1) * 4, :], ps[:, :].rearrange("p (c t) -> p c t", t=128), reads=[pk], writes=[kt], eng="act")
                    if outT32_name:
                        k.copy(tf[:, half * 4:(half + 1) * 4, :], ps[:, :].rearrange("p (c t) -> p c t", t=128), reads=[pk], writes=[ktf])
                k.dma("pool", d[outT_name][:, r0:r0 + 128].rearrange("(c p) t -> p c t", p=128), t_[:], reads=[kt], writes=[(outT_name, 0)])
                if outT32_name:
                    k.dma("pool", d[outT32_name][:, r0:r0 + 128].rearrange("(c p) t -> p c t", p=128), tf[:], reads=[ktf], writes=[(outT32_name, 0)])
    k.pes = None
    k.barrier()


def ffn_phase(P, pfx, srcT, res_name, wgu_names, wd_names, gcol, bcol, out_name, outT_name=None, router=None, tile_n=512):
    k, LP, d = P.k, P.LP, P.dram
    ne = len(wgu_names)
    with ExitStack() as pes:
        k.pes = pes
        VT2 = k.sb(pfx + "VT2", [128, 2048])
        k.dma("sp", VT2[:], d["VT2d"][0:1, gcol:gcol + 2048].partition_broadcast(128), writes=["VT2"])
        P.VT2 = VT2
        gcol, bcol = 0, 1024
        nhb = 2 if tile_n == 512 else 1
        hT = [k.sb(pfx + "hT%d" % i, [128, 8, tile_n], BF16) for i in range(nhb)]
        NWG = 6
        wg = [k.sb(pfx + "wg%d" % i, [128, 8, 128], BF16) for i in range(NWG)]
        wdh = [k.sb(pfx + "wdh%d" % i, [128, 22, 512], BF16) for i in range(2)]
        actT = k.sb(pfx + "actT", [128, 22, tile_n], BF16)
        sg = [k.sb(pfx + "sg%d" % i, [128, 512]) for i in range(2)]
        acc = [k.sb(pfx + "acc%d" % i, [128, 1024]) for i in range(tile_n // 128)]
        o = [k.sb(pfx + "o%d" % i, [128, 1024]) for i in range(2)]
        oT = [k.sb(pfx + "oT%d" % i, [128, 8, 128], BF16) for i in range(2)] if outT_name else None
        lnt = (k.sb(pfx + "st", [128, 12]), k.sb(pfx + "mv", [128, 2]), k.sb(pfx + "rs", [128, 1]))
        if router:
            hT32 = [k.sb(pfx + "hT32_%d" % i, [128, 8, 128]) for i in range(2)]
            WR = k.sb(pfx + "WR", [128, 8, 8])
            k.dma("sp", WR[:].rearrange("p a b -> p (a b)"), d["WRd"][:, :], writes=[pfx + "WR"])
            lg = k.sb(pfx + "lg", [128, 8])
            m8 = k.sb(pfx + "m8", [128, 8])
            msk = k.sb(pfx + "msk", [128, 8])
            nm0 = k.sb(pfx + "nm0", [128, 1])
            den = k.sb(pfx + "den", [128, 1])
            G = [k.sb(pfx + "G%d" % i, [128, 8]) for i in range(tile_n // 128)]

        def load_wd(e):
            for hf in range(2):
                k.dma("sp", wdh[hf][:], d[wd_names[e]][:, :].rearrange("p (a b) -> p a b", b=1024)[:, :, hf * 512:(hf + 1) * 512],
                      reads=[(wd_names[e], 0)], writes=[pfx + "wdh%d" % hf])

        if ne == 1:
            load_wd(0)
        wi = 0
        oi = 0
        big = []
        t = 0
        while t < LP:
            nn = min(tile_n, LP - t)
            big.append((t, nn))
            t += nn
        for si, (t0, n) in enumerate(big):
            nj = n // 128
            groups = [(g0, min(512, n - g0)) for g0 in range(0, n, 512)]
            h, kh = hT[si % nhb], pfx + "hT%d" % (si % nhb)
            k.dma("sp", h[:, :, :n], d[srcT][:, t0:t0 + n].rearrange("(c p) t -> p c t", p=128), reads=[(srcT, 0)], writes=[kh])
            for j in range(nj):
                r0 = t0 + j * 128
                k.dma("sp", acc[j][:], d[res_name][r0:r0 + 128, :], reads=[(res_name, 0)], writes=[pfx + "acc%d" % j])
                k.ts(acc[j][:], acc[j][:], ALPHA, OP.mult, reads=[pfx + "acc%d" % j], writes=[pfx + "acc%d" % j], eng="pool")
                if router:
                    h32, kh32 = hT32[j % 2], pfx + "hT32_%d" % (j % 2)
                    k.dma("sp", h32[:], d[router][:, r0:r0 + 128].rearrange("(c p) t -> p c t", p=128), reads=[(router, 0)], writes=[kh32])
                    ps, pk = k.ps()
                    for kc in range(8):
                        k.mm(ps[:, 0:8], h32[:, kc, :], WR[:, kc, :], kc == 0, kc == 7, reads=[kh32, pfx + "WR"], writes=[pk])
                    k.copy(lg[:], ps[:, 0:8], reads=[pk], writes=[pfx + "lg"])
                    k.op("dve", lambda hh: hh.max(out=m8[:], in_=lg[:]), reads=[pfx + "lg"], writes=[pfx + "m8"])
                    k.ts(msk[:], lg[:], m8[:, 1:2], OP.is_ge, reads=[pfx + "lg", pfx + "m8"], writes=[pfx + "msk"])
                    k.ts(nm0[:], m8[:, 0:1], -1.0, OP.mult, reads=[pfx + "m8"], writes=[pfx + "nm0"])
                    k.act(lg[:], lg[:], AF.Exp, reads=[pfx + "lg", pfx + "nm0"], writes=[pfx + "lg"], bias=nm0[:, 0:1])
                    k.tt(lg[:], lg[:], msk[:], OP.mult, reads=[pfx + "lg", pfx + "msk"], writes=[pfx + "lg"])
                    k.op("dve", lambda hh: hh.tensor_reduce(out=den[:], in_=lg[:], axis=mybir.AxisListType.X, op=OP.add), reads=[pfx + "lg"], writes=[pfx + "den"])
                    k.op("dve", lambda hh: hh.reciprocal(out=den[:], in_=den[:]), reads=[pfx + "den"], writes=[pfx + "den"])
                    k.ts(G[j][:], lg[:], den[:, 0:1], OP.mult, reads=[pfx + "lg", pfx + "den"], writes=[pfx + "G%d" % j])
            for e in range(ne):
                for i in range(22):
                    pss = {}
                    for part in range(2):
                        c = part * 22 + i
                        w, kw = wg[wi % NWG], pfx + "wg%d" % (wi % NWG)
                        wi += 1
                        k.dma("sp", w[:].rearrange("p a b -> p (a b)"), d[wgu_names[e]][c * 128:(c + 1) * 128, :], reads=[(wgu_names[e], (c * 128) // 512)], writes=[kw])
                        for gi, (g0, ng) in enumerate(groups):
                            ps, pk = k.ps()
                            for kc in range(8):
                                k.mm(ps[:, :ng], w[:, kc, :], h[:, kc, g0:g0 + ng], kc == 0, kc == 7, reads=[kw, kh], writes=[pk])
                            pss[(gi, part)] = (ps, pk)
                    for gi, (g0, ng) in enumerate(groups):
                        s_, ks = sg[gi % 2], pfx + "sg%d" % (gi % 2)
                        k.act(s_[:, :ng], pss[(gi, 0)][0][:, :ng], AF.Silu, reads=[pss[(gi, 0)][1]], writes=[ks])
                        k.tt(actT[:, i, g0:g0 + ng], s_[:, :ng], pss[(gi, 1)][0][:, :ng], OP.mult, reads=[ks, pss[(gi, 1)][1]], writes=[(pfx + "actT", i)])
                    if ne > 1 and i == 2:
                        load_wd(e)
                for half in range(2):
                    hs = slice(half * 512, (half + 1) * 512)
                    for j in range(nj):
                        ps, pk = k.ps()
                        for i in range(22):
                            k.mm(ps[:, :], actT[:, i, j * 128:(j + 1) * 128], wdh[half][:, i, :], i == 0, i == 21, reads=[(pfx + "actT", i), pfx + "wdh%d" % half], writes=[pk])
                        if router:
                            k.stt(acc[j][:, hs], ps[:, :], G[j][:, e:e + 1], acc[j][:, hs], OP.mult, OP.add, reads=[pk, pfx + "G%d" % j, pfx + "acc%d" % j], writes=[pfx + "acc%d" % j])
                        else:
                            k.tt(acc[j][:, hs], ps[:, :], acc[j][:, hs], OP.add, reads=[pk, pfx + "acc%d" % j], writes=[pfx + "acc%d" % j])
            for j in range(nj):
                r0 = t0 + j * 128
                oi += 1
                ot, ko = o[oi % 2], pfx + "o%d" % (oi % 2)
                layer_norm_rows(k, pfx, acc[j], pfx + "acc%d" % j, ot, ko, gcol, bcol, P.VT2, lnt)
                k.dma("pool", d[out_name][r0:r0 + 128, :], ot[:], reads=[ko], writes=[(out_name, 0)])
                if outT_name:
                    t_, kt = oT[oi % 2], pfx + "oT%d" % (oi % 2)
                    for half in range(2):
                        ps, pk = k.ps()
                        for cc in range(4):
                            c = half * 4 + cc
                            k.tr(ps[:, cc * 128:(cc + 1) * 128], ot[:, c * 128:(c + 1) * 128], P.ident, reads=[ko, "CM"], writes=[pk])
                        k.copy(t_[:, half * 4:(half + 1) * 4, :], ps[:, :].rearrange("p (c t) -> p c t", t=128), reads=[pk], writes=[kt], eng="act")
                    k.dma("pool", d[outT_name][:, r0:r0 + 128].rearrange("(c p) t -> p c t", p=128), t_[:], reads=[kt], writes=[(outT_name, 0)])
    k.pes = None
    k.barrier()


VL_CW, VL_CB, VL_GXB, VL_GAB, VL_LAM = 0, 32, 40, 48, 56
NVL = 64


def phase5(P):
    k, LP, d = P.k, P.LP, P.dram
    with ExitStack() as pes:
        k.pes = pes
        W = k.sb("l_W", [128, 16, 8, 128], BF16)
        k.dma("sp", W[:].rearrange("p c a b -> p c (a b)"), d["W1INb"][:, :].rearrange("(c p) f -> p c f", p=128), reads=[("W1INb", i) for i in range(4)], writes=["l_W"])
        GW = k.sb("l_GW", [128, 16, 128])
        k.dma("sp", GW[:].rearrange("p c b -> p (c b)"), d["GWd"][:, :], writes=["l_GW"])
        VL = k.sb("l_VL", [128, NVL])
        k.dma("sp", VL[:], d["VLd"][:, :], writes=["l_VL"])
        SPm8 = k.sb("l_sp8", [128, 8])
        SPm16 = k.sb("l_sp16", [128, 8])
        k.act(SPm8[:], VL[:, VL_LAM:VL_LAM + 8], AF.Exp, reads=["l_VL"], writes=["l_sp8"], scale=-1.0)
        k.act(SPm8[:], SPm8[:], AF.Ln, reads=["l_sp8"], writes=["l_sp8"], bias=1.0)
        k.ts(SPm16[:], SPm8[:], -16.0, OP.mult, reads=["l_sp8"], writes=["l_sp16"])
        k.ts(SPm8[:], SPm8[:], -8.0, OP.mult, reads=["l_sp8", "l_sp16"], writes=["l_sp8"])
        hT = [k.sb("l_hT%d" % i, [128, 8, 512], BF16) for i in range(2)]
        gb = [k.sb("l_gb%d" % i, [128, 512]) for i in range(2)]
        g2 = k.sb("l_g2", [128, 512])
        xr = [k.sb("l_xr%d" % i, [128, 515]) for i in range(2)]
        xf = k.sb("l_xf", [128, 512])
        gx = k.sb("l_gx", [128, 512])
        ga = k.sb("l_ga", [128, 512])
        av = k.sb("l_a", [128, 512])
        uv = k.sb("l_u", [128, 512])
        hs = [k.sb("l_hs%d" % c, [128, 512]) for c in range(8)]
        carry = [k.sb("l_cy%d" % c, [128, 1]) for c in range(8)]
        yb = [k.sb("l_yb%d" % i, [128, 512], BF16) for i in range(2)]
        for c in range(8):
            k.memset(carry[c][:], 0.0, writes=["l_cy%d" % c])
        it = 0
        for si, (t0, n) in enumerate(supertiles(LP)):
            h, kh = hT[si % 2], "l_hT%d" % (si % 2)
            k.dma("sp", h[:, :, :n], d["H2T"][:, t0:t0 + n].rearrange("(c p) t -> p c t", p=128), reads=[("H2T", 0)], writes=[kh])
            for c in range(8):
                it += 1
                ps, pk = k.ps()
                for kc in range(8):
                    k.mm(ps[:, :n], W[:, c, kc, :], h[:, kc, :n], kc == 0, kc == 7, reads=["l_W", kh], writes=[pk])
                g, kg = gb[it % 2], "l_gb%d" % (it % 2)
                k.copy(g[:, :n], ps[:, :n], reads=[pk], writes=[kg], eng="act")
                k.tt(g2[:, :n], g[:, :n], g[:, :n], OP.mult, reads=[kg], writes=["l_g2"])
                k.ts(g2[:, :n], g2[:, :n], 0.044715, OP.mult, reads=["l_g2"], writes=["l_g2"], s2=1.0, op1=OP.add)
                k.tt(g2[:, :n], g2[:, :n], g[:, :n], OP.mult, reads=["l_g2", kg], writes=["l_g2"])
                k.act(g2[:, :n], g2[:, :n], AF.Sigmoid, reads=["l_g2"], writes=["l_g2"], scale=1.5957691216057308)
                k.tt(g[:, :n], g[:, :n], g2[:, :n], OP.mult, reads=[kg, "l_g2"], writes=[kg])
                x, kx = xr[it % 2], "l_xr%d" % (it % 2)
                ps, pk = k.ps()
                for kc in range(8):
                    k.mm(ps[:, :n], W[:, 8 + c, kc, :], h[:, kc, :n], kc == 0, kc == 7, reads=["l_W", kh], writes=[pk])
                xp, kxp = xr[(it + 1) % 2], "l_xr%d" % ((it + 1) % 2)
                k.copy(x[:, 3:3 + n], ps[:, :n], reads=[pk], writes=[kx])
                k.dma("pool", d["XRT"][c * 128:(c + 1) * 128, t0:t0 + n], x[:, 3:3 + n], reads=[kx], writes=[("XRT", c)])
                if t0 == 0:
                    k.memset(x[:, 0:3], 0.0, writes=[kx])
                else:
                    k.dma("sp", x[:, 0:3], d["XRT"][c * 128:(c + 1) * 128, t0 - 3:t0], reads=[("XRT", c)], writes=[kx])
                cwc = VL_CW + c * 4
                k.ts(xf[:, :n], x[:, 3:3 + n], VL[:, cwc + 3:cwc + 4], OP.mult, reads=[kx, "l_VL"], writes=["l_xf"])
                for kk in (2, 1, 0):
                    k.stt(xf[:, :n], x[:, kk:kk + n], VL[:, cwc + kk:cwc + kk + 1], xf[:, :n], OP.mult, OP.add, reads=[kx, "l_xf", "l_VL"], writes=["l_xf"])
                k.ts(xf[:, :n], xf[:, :n], VL[:, VL_CB + c:VL_CB + c + 1], OP.add, reads=["l_xf", "l_VL"], writes=["l_xf"])
                ps, pk = k.ps()
                k.mm(ps[:, :n], GW[:, c, :], xf[:, :n], True, True, reads=["l_GW", "l_xf"], writes=[pk])
                k.act(gx[:, :n], ps[:, :n], AF.Sigmoid, reads=[pk, "l_VL"], writes=["l_gx"], bias=VL[:, VL_GXB + c:VL_GXB + c + 1])
                ps, pk = k.ps()
                k.mm(ps[:, :n], GW[:, 8 + c, :], xf[:, :n], True, True, reads=["l_GW", "l_xf"], writes=[pk])
                k.act(ga[:, :n], ps[:, :n], AF.Sigmoid, reads=[pk, "l_VL"], writes=["l_ga"], bias=VL[:, VL_GAB + c:VL_GAB + c + 1])
                k.act(av[:, :n], ga[:, :n], AF.Exp, reads=["l_ga", "l_sp8"], writes=["l_a"], scale=SPm8[:, c:c + 1])
                k.act(uv[:, :n], ga[:, :n], AF.Exp, reads=["l_ga", "l_sp16"], writes=["l_u"], scale=SPm16[:, c:c + 1])
                k.act(uv[:, :n], uv[:, :n], AF.Sqrt, reads=["l_u"], writes=["l_u"], scale=-1.0, bias=1.0)
                k.tt(gx[:, :n], gx[:, :n], xf[:, :n], OP.mult, reads=["l_gx", "l_xf"], writes=["l_gx"])
                k.tt(uv[:, :n], uv[:, :n], gx[:, :n], OP.mult, reads=["l_u", "l_gx"], writes=["l_u"])
                k.op("dve", lambda hh, c=c, n=n: hh.tensor_tensor_scan(out=hs[c][:, :n], data0=av[:, :n], data1=uv[:, :n], initial=carry[c][:, 0:1], op0=OP.mult, op1=OP.add),
                     reads=["l_a", "l_u", "l_cy%d" % c], writes=["l_hs%d" % c])
                k.copy(carry[c][:], hs[c][:, n - 1:n], reads=["l_hs%d" % c], writes=["l_cy%d" % c], eng="pool")
                y, ky = yb[it % 2], "l_yb%d" % (it % 2)
                k.tt(y[:, :n], hs[c][:, :n], g[:, :n], OP.mult, reads=["l_hs%d" % c, kg], writes=[ky])
                k.dma("pool", d["Y1T"][c * 128:(c + 1) * 128, t0:t0 + n], y[:, :n], reads=[ky], writes=[("Y1T", 0)])
    k.pes = None
    k.barrier()


def pack_rhs_k(w):
    K = w.shape[0]
    return np.ascontiguousarray(w.reshape(K // 128, 128, w.shape[1]).transpose(1, 0, 2)).reshape(128, -1)


def host_inputs_weights(inp):
    f = np.float32
    im = {}
    CM, VT, VF = host_consts(inp)
    im.update(CMd=CM, VTd=VT, VFd=VF)
    w_in = inp["ev_w_in"][0]
    im["WINF"] = pack_lhsT(w_in, fcols())
    im["WINZ"] = pack_rhs(w_in, zcols())
    im.update(host_consts_rwkv(inp))
    im["VT2d"] = np.concatenate([inp[n][0] for n in ("ev_ln1_g", "ev_ln1_b", "ev_ln2_g", "ev_ln2_b", "od_ln1_g", "od_ln1_b", "od_ln2_g", "od_ln2_b")])[None, :].astype(f)
    im["WO"] = pack_rhs_k(inp["ev_w_out"][0])
    im["WGU0"] = pack_lhsT(inp["ev_ffn_w_gu"][0], np.arange(5632))
    im["WD0"] = pack_rhs_k(inp["ev_ffn_w_down"][0])
    im["W1IN"] = pack_lhsT(inp["od_w_in"][0], np.arange(2048))
    gw = np.concatenate([inp["od_gx_w"][0], inp["od_ga_w"][0]], 0)
    im["GWd"] = np.ascontiguousarray(gw.transpose(1, 0, 2)).reshape(128, 16 * 128).astype(f)
    VL = np.zeros((128, NVL), f)
    VL[:, 0:32] = inp["od_conv_w"][0].reshape(4, 8, 128).transpose(2, 1, 0).reshape(128, 32)
    for off, n in ((32, "od_conv_b"), (40, "od_gx_b"), (48, "od_ga_b"), (56, "od_lambda")):
        VL[:, off:off + 8] = inp[n][0].reshape(8, 128).T
    im["VLd"] = VL
    im["W1O"] = pack_rhs_k(inp["od_w_out"][0])
    im["WRd"] = np.ascontiguousarray(inp["od_router"][0].reshape(8, 128, 8).transpose(1, 0, 2)).reshape(128, 64).astype(f)
    for e in range(NEXP):
        im["WGUE%d" % e] = pack_lhsT(inp["od_exp_w_gu"][0, e], np.arange(5632))
        im["WDE%d" % e] = pack_rhs_k(inp["od_exp_w_down"][0, e])
    return im


_CACHE = {}


def kernel(**inputs):
    inp = {k_: np.asarray(v) for k_, v in inputs.items()}
    x = inp["x"]
    B, S, _ = x.shape
    L = S + NMETA
    LP = ((L + 127) // 128) * 128
    if LP not in _CACHE:
        _CACHE[LP] = build(LP)
    P = _CACHE[LP]
    wim = host_inputs_weights(inp)
    in_maps = []
    for b in range(B):
        xin = np.zeros((LP, D), np.float32)
        xin[:NMETA] = inp["meta"]
        xin[NMETA:L] = x[b]
        m = dict(wim)
        m["xin"] = xin
        in_maps.append(m)
    res = run_bass_kernel_spmd(P.nc, in_maps, core_ids=list(range(B)))
    out = np.stack([np.asarray(r["OUT"])[NMETA:L] for r in res.results], 0)
    return out.astype(np.float32)
```

```python
from contextlib import ExitStack
import concourse.bass as bass
import concourse.mybir as mybir

F32 = mybir.dt.float32
BF16 = mybir.dt.bfloat16
AF = mybir.ActivationFunctionType
OP = mybir.AluOpType
ENGS = ["pe", "dve", "act", "pool", "sp"]
NDMA = 40


class KB:
    def __init__(self, nc, es: ExitStack):
        self.nc = nc
        self.es = es
        self.ops = {e: [] for e in ENGS}
        self.cnt = {e: 0 for e in ENGS}
        self.waited = {e: {} for e in ENGS}
        self.last_w = {}
        self.readers = {}
        self.sem = {e: es.enter_context(nc.semaphore("s_" + e)) for e in ENGS}
        self.dsem = [es.enter_context(nc.semaphore("d%d" % i)) for i in range(NDMA)]
        self.dval = [0] * NDMA
        self.dnext = 0
        self.semid = {}
        for e in ENGS:
            self.semid[("e", e)] = self.sem[e]
        for i in range(NDMA):
            self.semid[("d", i)] = self.dsem[i]
        self.ntile = 0
        self.psb = [self.psum_t("psb%d" % i, [128, 512], F32) for i in range(8)]
        self.psi = 0
        self.final_tokens = []

    def sb(self, name, shape, dt=F32):
        self.ntile += 1
        es = self.pes if getattr(self, "pes", None) is not None else self.es
        return es.enter_context(self.nc.sbuf_tensor(name, list(shape), dt))

    def barrier(self):
        allw = []
        for i in range(NDMA):
            if self.dval[i] > 0:
                allw.append((("d", i), self.dval[i]))
        for e in ENGS:
            if self.cnt[e] > 0:
                allw.append((("e", e), self.cnt[e]))
        semid = self.semid
        for e in ENGS:
            waits = self._waits(e, allw, skip_self=True)

            def run(h, waits=waits):
                for s, v in waits:
                    h.wait_ge(semid[s], v)

            self.ops[e].append(run)

    def psum_t(self, name, shape, dt=F32):
        return self.es.enter_context(self.nc.psum_tensor(name, list(shape), dt))

    def ps(self):
        i = self.psi
        self.psi = (self.psi + 1) % 7
        return self.psb[i], "psb%d" % i

    def _deps(self, reads, writes):
        deps = []
        for k in reads:
            if k in self.last_w:
                deps.append(self.last_w[k])
            if isinstance(k, str) and k.startswith("psb"):
                deps.extend(self.readers.get(k, {}).values())
        for k in writes:
            if k in self.last_w:
                deps.append(self.last_w[k])
            deps.extend(self.readers.get(k, {}).values())
        return deps

    def _waits(self, eng, deps, skip_self=False):
        w = {}
        for (s, v) in deps:
            if skip_self and s == ("e", eng):
                continue
            if self.waited[eng].get(s, 0) < v and w.get(s, 0) < v:
                w[s] = v
        for s, v in w.items():
            self.waited[eng][s] = v
        return list(w.items())

    def _commit(self, tok, reads, writes):
        for k in writes:
            self.last_w[k] = tok
            self.readers[k] = {}
        for k in reads:
            if k in writes:
                continue
            r = self.readers.setdefault(k, {})
            if r.get(tok[0], (None, 0))[1] < tok[1]:
                r[tok[0]] = tok

    def op(self, eng, fn, reads=(), writes=()):
        deps = self._deps(reads, writes)
        waits = self._waits(eng, deps, skip_self=(eng == "pe"))
        self.cnt[eng] += 1
        tok = (("e", eng), self.cnt[eng])
        semid = self.semid
        mysem = self.sem[eng]

        def run(h):
            for s, v in waits:
                h.wait_ge(semid[s], v)
            fn(h).then_inc(mysem, 1)

        self.ops[eng].append(run)
        self._commit(tok, reads, writes)
        self._tick()
        return tok

    def interleave(self, fns, weights):
        import threading
        n = len(fns)
        if n == 1:
            fns[0]()
            return
        st = {"sems": [threading.Semaphore(0) for _ in range(n)], "alive": [True] * n, "cur": 0, "cnt": 0, "err": None,
              "main": threading.Semaphore(0), "w": weights}
        self._il = st

        def body(i):
            st["sems"][i].acquire()
            try:
                fns[i]()
            except BaseException as e:
                st["err"] = e
            st["alive"][i] = False
            m = None
            for dd in range(1, n + 1):
                q = (i + dd) % n
                if st["alive"][q]:
                    m = q
                    break
            if m is None:
                st["main"].release()
            else:
                st["cur"] = m
                st["cnt"] = 0
                st["sems"][m].release()

        ths = [threading.Thread(target=body, args=(i,)) for i in range(n)]
        for t in ths:
            t.start()
        st["sems"][0].release()
        st["main"].acquire()
        for t in ths:
            t.join()
        self._il = None
        if st["err"] is not None:
            raise st["err"]

    def _tick(self):
        st = getattr(self, "_il", None)
        if st is None:
            return
        i = st["cur"]
        st["cnt"] += 1
        if st["cnt"] >= st["w"][i]:
            n = len(st["alive"])
            m = None
            for dd in range(1, n):
                q = (i + dd) % n
                if st["alive"][q]:
                    m = q
                    break
            st["cnt"] = 0
            if m is None:
                return
            st["cur"] = m
            st["sems"][m].release()
            st["sems"][i].acquire()

    def dma(self, q, out, in_, reads=(), writes=(), **kw):
        slot = self.dnext
        self.dnext = (self.dnext + 1) % NDMA
        deps = self._deps(reads, writes)
        if self.dval[slot] > 0:
            deps.append((("d", slot), self.dval[slot]))
        waits = self._waits(q, deps)
        self.dval[slot] += 16
        tok = (("d", slot), self.dval[slot])
        semid = self.semid
        ds = self.dsem[slot]

        def run(h):
            for s, v in waits:
                h.wait_ge(semid[s], v)
            h.dma_start(out=out, in_=in_, **kw).then_inc(ds, 16)

        self.ops[q].append(run)
        self._commit(tok, reads, writes)
        self._tick()
        return tok

    def finish(self, eng="sp"):
        waits = []
        for i in range(NDMA):
            if self.dval[i] > 0:
                waits.append((("d", i), self.dval[i]))
        for e in ENGS:
            if self.cnt[e] > 0:
                waits.append((("e", e), self.cnt[e]))
        semid = self.semid

        def run(h):
            for s, v in waits:
                h.wait_ge(semid[s], v)

        self.ops[eng].append(run)

    def emit(self):
        nc = self.nc
        with nc.Block() as block:
            @block.tensor
            def _(h):
                for f in self.ops["pe"]:
                    f(h)

            @block.vector
            def _(h):
                for f in self.ops["dve"]:
                    f(h)

            @block.scalar
            def _(h):
                for f in self.ops["act"]:
                    f(h)

            @block.gpsimd
            def _(h):
                for f in self.ops["pool"]:
                    f(h)

            @block.sync
            def _(h):
                for f in self.ops["sp"]:
                    f(h)

    def mm(self, out, lhsT, rhs, start, stop, reads, writes):
        return self.op("pe", lambda h: h.matmul(out, lhsT, rhs, start=start, stop=stop), reads, writes)

    def tr(self, out, in_, ident, reads, writes):
        return self.op("pe", lambda h: h.transpose(out, in_, ident), reads, writes)

    def act(self, out, in_, func, reads, writes, bias=None, scale=None, eng="act"):
        kw = {}
        if bias is not None:
            kw["bias"] = bias
        if scale is not None:
            kw["scale"] = scale
        return self.op("act", lambda h: h.activation(out=out, in_=in_, func=func, **kw), reads, writes)

    def tt(self, out, in0, in1, op, reads, writes, eng="dve"):
        return self.op(eng, lambda h: h.tensor_tensor(out=out, in0=in0, in1=in1, op=op), reads, writes)

    def ts(self, out, in0, s1, op0, reads, writes, s2=None, op1=None, eng="dve"):
        if op1 is None:
            return self.op(eng, lambda h: h.tensor_scalar(out=out, in0=in0, scalar1=s1, scalar2=None, op0=op0), reads, writes)
        return self.op(eng, lambda h: h.tensor_scalar(out=out, in0=in0, scalar1=s1, scalar2=s2, op0=op0, op1=op1), reads, writes)

    def stt(self, out, in0, scalar, in1, op0, op1, reads, writes):
        return self.op("dve", lambda h: h.scalar_tensor_tensor(out=out, in0=in0, scalar=scalar, in1=in1, op0=op0, op1=op1), reads, writes)

    def copy(self, out, in_, reads, writes, eng="dve"):
        if eng == "act":
            return self.op("act", lambda h: h.copy(out=out, in_=in_), reads, writes)
        return self.op(eng, lambda h: h.tensor_copy(out=out, in_=in_), reads, writes)

    def memset(self, ap, val, writes, eng="dve"):
        return self.op(eng, lambda h: h.memset(ap, val), (), writes)
import numpy as np
from contextlib import ExitStack
import concourse.bass as bass
import concourse.mybir as mybir
from concourse.bass_utils import run_bass_kernel_spmd

D = 1024
NMETA = 16
DFF = 2816
NEXP = 8
ALPHA = 4 ** 0.25
LN_EPS = 1e-5
RWKV_EPS = 64e-5

C_XS, C_B, C_C, C_R, C_K, C_V, C_LO, C_G = 0, 8, 10, 12, 20, 28, 36, 37
NFC = 38


def supertiles(LP):
    out = []
    t = 0
    while t < LP:
        n = min(512, LP - t)
        out.append((t, n))
        t += n
    return out


class Prog:
    def pump_casts(self, n):
        for _ in range(n):
            if self.pending:
                self.pending.pop(0)()

    def __init__(self, LP, dbg=()):
        self.pending = []
        self.LP = LP
        self.dbg = set(dbg)
        self.nc = bass.Bass("TRN2", target_bir_lowering=False)
        self.es = ExitStack()
        self.k = None
        self.dram = {}

    def din(self, name, shape, dt=F32):
        t = self.nc.dram_tensor(name, list(shape), dt, kind="ExternalInput").ap()
        self.dram[name] = t
        return t

    def dscr(self, name, shape, dt=F32):
        kind = "ExternalOutput" if name in self.dbg else "Internal"
        t = self.nc.dram_tensor(name, list(shape), dt, kind=kind).ap()
        self.dram[name] = t
        return t

    def dout(self, name, shape, dt=F32):
        t = self.nc.dram_tensor(name, list(shape), dt, kind="ExternalOutput").ap()
        self.dram[name] = t
        return t


def cast_weights(P, src, dst, nrows, key, defer=False):
    k = P.k
    step = 512
    for r0 in range(0, nrows, step):
        r1 = min(nrows, r0 + step)

        def go(r0=r0, r1=r1):
            k.dma("pool", dst[r0:r1, :], src[r0:r1, :], reads=(), writes=((key, r0 // step),))

        if defer:
            P.pending.append(go)
        else:
            go()


def phase1(P):
    k, LP = P.k, P.LP
    d = P.dram
    xt = [k.sb("xt%d" % i, [128, 1024]) for i in range(4)]
    hT = k.sb("hT", [128, 8, 512], BF16)
    wf = [k.sb("wf%d" % i, [128, 8, 128], BF16) for i in range(3)]
    wz = k.sb("wz", [128, 8, 1040], BF16)
    stg = [k.sb("stg%d" % i, [128, 512]) for i in range(3)]
    zst = [k.sb("zst%d" % i, [128, 1040]) for i in range(2)]
    ident = P.ident
    k.dma("sp", wz[:].rearrange("p a b -> p (a b)"), d["WINZb"][:, :], reads=[("WINZb", i) for i in range(1)], writes=["wz"])
    wi = 0
    si = 0
    zi = 0
    for (t0, n) in supertiles(LP):
        nj = n // 128
        for j in range(nj):
            k.dma("sp", xt[j][:], d["xin"][t0 + j * 128:t0 + (j + 1) * 128, :], reads=[], writes=["xt%d" % j])
        for kc in range(8):
            ps, pk = k.ps()
            for j in range(nj):
                k.tr(ps[:, j * 128:(j + 1) * 128], xt[j][:, kc * 128:(kc + 1) * 128], ident[:], reads=["xt%d" % j, "ident"], writes=[pk])
            k.copy(hT[:, kc, :n], ps[:, :n], reads=[pk], writes=[("hT", kc)], eng=("act" if kc % 2 else "dve"))
        for c in range(NFC):
            w = wf[wi % 3]
            wk = "wf%d" % (wi % 3)
            wi += 1
            k.dma("sp", w[:].rearrange("p a b -> p (a b)"), d["WINFb"][c * 128:(c + 1) * 128, :], reads=[("WINFb", (c * 128) // 512)], writes=[wk])
            ps, pk = k.ps()
            for kc in range(8):
                k.mm(ps[:, :n], w[:, kc, :], hT[:, kc, :n], kc == 0, kc == 7, reads=[wk, ("hT", kc)], writes=[pk])
            s = stg[si % 3]
            sk = "stg%d" % (si % 3)
            si += 1
            k.copy(s[:, :n], ps[:, :n], reads=[pk], writes=[sk], eng=("act" if c % 2 else "dve"))
            k.dma("pool", d["PT"][c * 128:(c + 1) * 128, t0:t0 + n], s[:, :n], reads=[sk], writes=[("PT", c)])
        for j in range(nj):
            z = zst[zi % 2]
            zk = "zst%d" % (zi % 2)
            zi += 1
            for (c0, c1) in ((0, 512), (512, 1024), (1024, 1040)):
                ps, pk = k.ps()
                for kc in range(8):
                    k.mm(ps[:, :c1 - c0], hT[:, kc, j * 128:(j + 1) * 128], wz[:, kc, c0:c1], kc == 0, kc == 7,
                         reads=["wz", ("hT", kc)], writes=[pk])
                k.copy(z[:, c0:c1], ps[:, :c1 - c0], reads=[pk], writes=[zk], eng=("act" if c0 == 512 else "dve"))
            k.dma("pool", d["Z"][t0 + j * 128:t0 + (j + 1) * 128, :], z[:], reads=[zk], writes=[("Z", 0)])


import os
LVL = int(os.environ.get('LVL', '99'))
R3 = int(os.environ.get('R3', '99'))
R3C = int(os.environ.get('R3C', '99'))
R3L = int(os.environ.get('R3L', '99'))
NVT = 32 + 2048
NVF = 64


def build(LP, dbg=(), upto=99):
    P = Prog(LP, dbg)
    nc = P.nc
    P.din("xin", [LP, D])
    P.din("CMd", [128, 640])
    P.din("VTd", [1, NVT])
    P.din("VFd", [128, NVF])
    P.din("WINF", [NFC * 128, 1024])
    P.din("WINZ", [128, 8 * 1040])
    P.din("CRd", [128, NCR])
    P.din("VRd", [128, NVR])
    P.din("WUAd", [128, 1024])
    P.din("GUPd", [128, 1024])
    P.din("LNGd", [128, 512])
    P.din("LNBd", [128, 512])
    P.din("VT2d", [1, 8192])
    P.din("WO", [128, 16 * 1024])
    P.din("WGU0", [44 * 128, 1024])
    P.din("WD0", [128, 22 * 1024])
    P.din("W1IN", [16 * 128, 1024])
    P.din("GWd", [128, 16 * 128])
    P.din("VLd", [128, NVL])
    P.din("W1O", [128, 8 * 1024])
    P.din("WRd", [128, 64])
    for e in range(NEXP):
        P.din("WGUE%d" % e, [44 * 128, 1024])
        P.din("WDE%d" % e, [128, 22 * 1024])
    P.dscr("WOb", [128, 16 * 1024], BF16)
    P.dscr("WGU0b", [44 * 128, 1024], BF16)
    P.dscr("WD0b", [128, 22 * 1024], BF16)
    P.dscr("W1INb", [16 * 128, 1024], BF16)
    P.dscr("W1Ob", [128, 8 * 1024], BF16)
    for e in range(NEXP):
        P.dscr("WGUEb%d" % e, [44 * 128, 1024], BF16)
        P.dscr("WDEb%d" % e, [128, 22 * 1024], BF16)
    P.dscr("H1", [LP, D])
    P.dscr("H1T", [D, LP], BF16)
    P.dscr("H2", [LP, D])
    P.dscr("H2T", [D, LP], BF16)
    P.dscr("XRT", [D, LP])
    P.dscr("Y1T", [D, LP], BF16)
    P.dscr("H3", [LP, D])
    P.dscr("H3T", [D, LP], BF16)
    P.dscr("H3T32", [D, LP])
    P.dout("OUT", [LP, D])
    P.dscr("WINFb", [NFC * 128, 1024], BF16)
    P.dscr("WINZb", [128, 8 * 1040], BF16)
    P.dscr("PT", [NFC * 128, LP])
    P.dscr("Z", [LP, 1040])
    P.dscr("XS", [LP, 1024])
    P.dscr("BTOK", [LP, 256])
    P.dscr("BT", [256, LP])
    P.dscr("CT", [256, LP])
    P.dscr("YT", [2048, LP], BF16)
    with P.es:
        k = KB(nc, P.es)
        P.k = k
        P.CM = k.sb("CM", [128, 640])
        P.ident = P.CM[:, 0:128]
        P.VT = k.sb("VT", [128, NVT])
        P.VF = k.sb("VF", [128, NVF])
        k.dma("sp", P.CM[:], P.dram["CMd"][:, :], writes=["CM", "ident"])
        k.dma("sp", P.VT[:], P.dram["VTd"][0:1, :].partition_broadcast(128), writes=["VT"])
        k.dma("sp", P.VF[:], P.dram["VFd"][:, :], writes=["VF"])
        cast_weights(P, P.dram["WINF"], P.dram["WINFb"], NFC * 128, "WINFb")
        k.dma("pool", P.dram["WINZb"][:, :].rearrange("p (a b) -> p a b", b=1040), P.dram["WINZ"][:, :].rearrange("p (a b) -> p a b", b=1040), writes=[("WINZb", 0)])
        if upto >= 4:
            def cast_rhs(src, dst, nk):
                P.pending.append(lambda: k.dma("pool", P.dram[dst][:, :].rearrange("p (a b) -> p a b", b=1024), P.dram[src][:, :].rearrange("p (a b) -> p a b", b=1024), writes=[(dst, 0)]))
            cast_rhs("WO", "WOb", 16)
            cast_weights(P, P.dram["WGU0"], P.dram["WGU0b"], 44 * 128, "WGU0b", defer=True)
            cast_rhs("WD0", "WD0b", 22)
            cast_weights(P, P.dram["W1IN"], P.dram["W1INb"], 16 * 128, "W1INb", defer=True)
            cast_rhs("W1O", "W1Ob", 8)
            for e in range(NEXP):
                cast_weights(P, P.dram["WGUE%d" % e], P.dram["WGUEb%d" % e], 44 * 128, "WGUEb%d" % e, defer=True)
                cast_rhs("WDE%d" % e, "WDEb%d" % e, 22)
        if upto >= 1:
            with ExitStack() as pes:
                k.pes = pes
                phase1(P)
            k.pes = None
            k.barrier()
        if upto >= 1.5:
            phase2a(P)
        if upto >= 2 and upto != 3.5:
            phase2b(P)
        if upto >= 3:
            phase3(P)
        if upto >= 4:
            P.pump_casts(100000)
            proj_res_ln(P, "p4", "YT", 16, "WOb", "xin", 0, 1024, "H1", "H1T")
            ffn_phase(P, "f0", "H1T", "H1", ["WGU0b"], ["WD0b"], 2048, 3072, "H2", "H2T")
        if upto >= 5:
            phase5(P)
            proj_res_ln(P, "p5", "Y1T", 8, "W1Ob", "H2", 4096, 5120, "H3", "H3T", "H3T32")
        if upto >= 6:
            ffn_phase(P, "f1", "H3T", "H3", ["WGUEb%d" % e for e in range(NEXP)], ["WDEb%d" % e for e in range(NEXP)], 6144, 7168, "OUT", None, router="H3T32", tile_n=1024)
        k.finish("sp")
        k.emit()
    return P


def host_consts(inp):
    t = np.arange(128)
    CM = np.zeros((128, 640), np.float32)
    CM[:, 0:128] = np.eye(128)
    CM[:, 128:256] = (t[:, None] <= t[None, :])
    CM[:, 256:384] = (t[:, None] > t[None, :])
    CM[:, 384:512] = 1.0
    CM[:, 512:640] = np.where(t[None, :] >= t[:, None], 0.0, -30000.0)
    VT = np.zeros((1, NVT), np.float32)
    VT[0, 0:16] = inp["ev_dt_bias"][0]
    VT[0, 16:32] = inp["ev_a_log"][0]
    VT[0, 32:32 + 1024] = np.repeat(inp["ev_d_skip"][0], 64)
    VT[0, 32 + 1024:32 + 2048] = inp["ev_ssm_norm"][0]
    VF = np.zeros((128, NVF), np.float32)
    cw = inp["ev_conv_w"][0]
    VF[:, 0:48] = cw.reshape(4, 12, 128).transpose(2, 1, 0).reshape(128, 48)
    VF[:, 48:60] = inp["ev_conv_b"][0].reshape(12, 128).T
    return CM, VT, VF


def fcols():
    return np.concatenate([np.arange(1024, 2048), np.arange(2048, 2304), np.arange(2304, 2560),
                           np.arange(2576, 3600), np.arange(3600, 4624), np.arange(4624, 5648),
                           np.arange(5648, 5776), np.arange(5776, 5904)])


def zcols():
    return np.concatenate([np.arange(0, 1024), np.arange(2560, 2576)])


def pack_lhsT(w, cols):
    ws = w[:, cols]
    nch = ws.shape[1] // 128
    a = ws.reshape(8, 128, nch, 128)
    return np.ascontiguousarray(a.transpose(2, 1, 0, 3)).reshape(nch * 128, 1024)


def pack_rhs(w, cols):
    ws = w[:, cols]
    n = ws.shape[1]
    return np.ascontiguousarray(ws.reshape(8, 128, n).transpose(1, 0, 2)).reshape(128, 8 * n)


CM_ID, CM_TRI, CM_TRIS, CM_ONES, CM_MB = 0, 128, 256, 384, 512
VT_DTB, VT_ALOG, VT_D, VT_NW = 0, 16, 32, 32 + 1024
VF_CW, VF_CB = 0, 48


def phase2a(P):
    k, LP, d = P.k, P.LP, P.dram
    P.pump_casts(19)
    with ExitStack() as pes:
        k.pes = pes
        raw = [k.sb("c_raw%d" % i, [128, LP + 3]) for i in range(2)]
        acc = [k.sb("c_acc%d" % i, [128, LP]) for i in range(2)]
        stg = [k.sb("c_stg%d" % i, [128, 512]) for i in range(3)]
        VF = P.VF
        for i in range(2):
            k.memset(raw[i][:, 0:3], 0.0, writes=["c_raw%d" % i])
        si = 0
        for c in range(12):
            r, rk = raw[c % 2], "c_raw%d" % (c % 2)
            a, ak = acc[c % 2], "c_acc%d" % (c % 2)
            k.dma("sp", r[:, 3:3 + LP], d["PT"][c * 128:(c + 1) * 128, :], reads=[("PT", c)], writes=[rk])
            cw = VF_CW + c * 4
            k.ts(a[:], r[:, 3:3 + LP], VF[:, cw + 3:cw + 4], OP.mult, reads=[rk, "VF"], writes=[ak])
            for kk in (2, 1, 0):
                k.stt(a[:], r[:, kk:kk + LP], VF[:, cw + kk:cw + kk + 1], a[:], OP.mult, OP.add, reads=[rk, ak, "VF"], writes=[ak])
            k.act(a[:], a[:], AF.Silu, reads=[ak, "VF"], writes=[ak], bias=VF[:, VF_CB + c:VF_CB + c + 1])
            if c >= 8:
                dst = d["BT"] if c < 10 else d["CT"]
                cc = (c - 8) % 2
                k.dma("pool", dst[cc * 128:(cc + 1) * 128, :], a[:], reads=[ak], writes=[("BT" if c < 10 else "CT", cc)])
            if c < 10:
                for (t0, n) in supertiles(LP):
                    nj = n // 128
                    ps, pk = k.ps()
                    for j in range(nj):
                        k.tr(ps[:, j * 128:(j + 1) * 128], a[:, t0 + j * 128:t0 + (j + 1) * 128], P.CM[:, CM_ID:CM_ID + 128], reads=[ak, "CM"], writes=[pk])
                    s, sk = stg[si % 3], "c_stg%d" % (si % 3)
                    si += 1
                    k.copy(s[:, :n], ps[:, :n], reads=[pk], writes=[sk], eng=("act" if si % 2 else "dve"))
                    if c < 8:
                        dd = d["XS"][t0:t0 + n, c * 128:(c + 1) * 128]
                        key = ("XS", c)
                    else:
                        dd = d["BTOK"][t0:t0 + n, (c - 8) * 128:(c - 7) * 128]
                        key = ("BTOK", c - 8)
                    k.dma("pool", dd.rearrange("(j p) m -> p j m", p=128), s[:, :n].rearrange("p (j m) -> p j m", m=128), reads=[sk], writes=[key])
    k.pes = None
    k.barrier()


def phase2b(P):
    k, LP, d = P.k, P.LP, P.dram
    CM, VT = P.CM, P.VT
    with ExitStack() as pes:
        k.pes = pes
        nb = 2
        xs = [k.sb("s_xs%d" % i, [128, 1024]) for i in range(nb)]
        btok = [k.sb("s_btok%d" % i, [128, 256]) for i in range(nb)]
        bt = [k.sb("s_bt%d" % i, [128, 2, 128]) for i in range(nb)]
        ct = [k.sb("s_ct%d" % i, [128, 2, 128]) for i in range(nb)]
        z = [k.sb("s_z%d" % i, [128, 1040]) for i in range(nb)]
        HT = [k.sb("s_HT%d" % g, [128, 512]) for g in range(2)]
        Abc = k.sb("s_Abc", [128, 16])
        dt = k.sb("s_dt", [128, 16])
        dtA = k.sb("s_dtA", [128, 16])
        E = k.sb("s_E", [128, 64])
        ncum = E[:, 48:64]
        LT = k.sb("s_LT", [128, 16, 128])
        SLT = k.sb("s_SLT", [128, 16, 128])
        xdt = k.sb("s_xdt", [128, 1024])
        xde = k.sb("s_xde", [128, 1024])
        y = k.sb("s_y", [128, 1024])
        t1 = k.sb("s_t1", [128, 1024])
        zs = k.sb("s_zs", [128, 1024])
        sq = k.sb("s_sq", [128, 512])
        ss = k.sb("s_ss", [128, 2])
        rstd = k.sb("s_rstd", [128, 2])
        yTs = [k.sb("s_yT%d" % i, [128, 8, 128], BF16) for i in range(2)]
        for g in range(2):
            k.memset(HT[g][:], 0.0, writes=["s_HT%d" % g])
        k.act(Abc[:], VT[:, VT_ALOG:VT_ALOG + 16], AF.Exp, reads=["VT"], writes=["s_Abc"])
        k.ts(Abc[:], Abc[:], -1.0, OP.mult, reads=["s_Abc"], writes=["s_Abc"])
        nch = LP // 128
        for ci in range(nch):
            t0 = ci * 128
            b = ci % nb
            kx, kb, kbt, kct, kz = "s_xs%d" % b, "s_btok%d" % b, "s_bt%d" % b, "s_ct%d" % b, "s_z%d" % b
            k.dma("sp", xs[b][:], d["XS"][t0:t0 + 128, :], reads=[("XS", c) for c in range(8)], writes=[kx])
            k.dma("sp", btok[b][:], d["BTOK"][t0:t0 + 128, :], reads=[("BTOK", 0), ("BTOK", 1)], writes=[kb])
            k.dma("sp", bt[b][:], d["BT"][:, t0:t0 + 128].rearrange("(g n) t -> n g t", n=128), reads=[("BT", 0), ("BT", 1)], writes=[kbt])
            k.dma("sp", ct[b][:], d["CT"][:, t0:t0 + 128].rearrange("(g n) t -> n g t", n=128), reads=[("CT", 0), ("CT", 1)], writes=[kct])
            k.dma("sp", z[b][:], d["Z"][t0:t0 + 128, :], reads=[("Z", 0)], writes=[kz])
            if LVL < 1: continue
            k.tt(dt[:], z[b][:, 1024:1040], VT[:, VT_DTB:VT_DTB + 16], OP.add, reads=[kz, "VT"], writes=["s_dt"])
            k.act(dt[:], dt[:], AF.Exp, reads=["s_dt"], writes=["s_dt"])
            k.act(dt[:], dt[:], AF.Ln, reads=["s_dt"], writes=["s_dt"], bias=1.0)
            k.tt(dtA[:], dt[:], Abc[:], OP.mult, reads=["s_dt", "s_Abc"], writes=["s_dtA"])
            if LVL < 2: continue
            psc, pkc = k.ps()
            k.mm(psc[:, 0:16], CM[:, CM_TRI:CM_TRI + 128], dtA[:], True, True, reads=["CM", "s_dtA"], writes=[pkc])
            k.mm(psc[:, 16:32], CM[:, CM_TRIS:CM_TRIS + 128], dtA[:], True, True, reads=["CM", "s_dtA"], writes=[pkc])
            k.mm(psc[:, 32:48], CM[:, CM_ONES:CM_ONES + 128], dtA[:], True, True, reads=["CM", "s_dtA"], writes=[pkc])
            SUB = int(os.environ.get('SUB', '3'))
            if SUB >= 1:
                k.act(E[:, 0:48], psc[:, 0:48], AF.Exp, reads=[pkc], writes=["s_E"])
            if SUB >= 2:
                k.ts(ncum, psc[:, 0:16], -1.0, OP.mult, reads=[pkc, "s_E"], writes=["s_ncum"])
            if LVL < 3: continue
            k.tt(xdt[:].rearrange("p (h e) -> p h e", e=64), xs[b][:].rearrange("p (h e) -> p h e", e=64),
                 dt[:].unsqueeze(2).broadcast_to([128, 16, 64]), OP.mult, reads=[kx, "s_dt"], writes=["s_xdt"])
            k.tt(xde[:].rearrange("p (h e) -> p h e", e=64), xdt[:].rearrange("p (h e) -> p h e", e=64),
                 E[:, 16:32].unsqueeze(2).broadcast_to([128, 16, 64]), OP.mult, reads=["s_xdt", "s_E"], writes=["s_xde"])
            if LVL < 4: continue
            for hq in range(4):
                psa, pka = k.ps()
                for hh in range(4):
                    h = hq * 4 + hh
                    k.mm(psa[:, hh * 128:(hh + 1) * 128], dtA[:, h:h + 1].broadcast_to([128, 128]), CM[:, CM_TRI:CM_TRI + 128], True, True,
                         reads=["s_dtA", "CM"], writes=[pka])
                for hh in range(4):
                    h = hq * 4 + hh
                    k.stt(LT[:, h, :], psa[:, hh * 128:(hh + 1) * 128], E[:, 48 + h:49 + h], CM[:, CM_MB:CM_MB + 128], OP.add, OP.add,
                          reads=[pka, "s_ncum", "CM"], writes=[("s_LT", hq)])
            for hq in range(4):
                k.act(LT[:, hq * 4:(hq + 1) * 4, :], LT[:, hq * 4:(hq + 1) * 4, :], AF.Exp, reads=[("s_LT", hq)], writes=[("s_LT", hq)])
            if LVL < 5: continue
            pss, pks = k.ps()
            for g in range(2):
                k.mm(pss[:, g * 128:(g + 1) * 128], bt[b][:, g, :], ct[b][:, g, :], True, True, reads=[kbt, kct], writes=[pks])
            for g in range(2):
                k.tt(SLT[:, g * 8:(g + 1) * 8, :], LT[:, g * 8:(g + 1) * 8, :],
                     pss[:, g * 128:(g + 1) * 128].unsqueeze(1).broadcast_to([128, 8, 128]), OP.mult,
                     reads=[("s_LT", 2 * g), ("s_LT", 2 * g + 1), pks], writes=[("s_SLT", g)])
            if LVL < 6: continue
            for g in range(2):
                psy, pky = k.ps()
                for hh in range(8):
                    h = g * 8 + hh
                    k.mm(psy[:, hh * 64:(hh + 1) * 64], SLT[:, h, :], xdt[:, h * 64:(h + 1) * 64], True, True, reads=[("s_SLT", g), "s_xdt"], writes=[pky])
                if R3L < 4: continue
                pso, pko = k.ps()
                k.mm(pso[:, :], ct[b][:, g, :], HT[g][:], True, True, reads=[kct, "s_HT%d" % g], writes=[pko])
                gs = slice(g * 512, (g + 1) * 512)
                k.tt(t1[:, gs].rearrange("p (h e) -> p h e", e=64), pso[:, :].rearrange("p (h e) -> p h e", e=64),
                     E[:, g * 8:(g + 1) * 8].unsqueeze(2).broadcast_to([128, 8, 64]), OP.mult, reads=[pko, "s_E"], writes=[("s_t1", g)])
                k.tt(y[:, gs], psy[:, :], t1[:, gs], OP.add, reads=[pky, ("s_t1", g)], writes=[("s_y", g)])
                psh, pkh = k.ps()
                k.mm(psh[:, :], btok[b][:, g * 128:(g + 1) * 128], xde[:, gs], True, True, reads=[kb, "s_xde"], writes=[pkh])
                k.tt(HT[g][:].rearrange("p (h e) -> p h e", e=64), HT[g][:].rearrange("p (h e) -> p h e", e=64),
                     E[:, 32 + g * 8:32 + (g + 1) * 8].unsqueeze(2).broadcast_to([128, 8, 64]), OP.mult, reads=["s_HT%d" % g, "s_E"], writes=["s_HT%d" % g])
                k.tt(HT[g][:], HT[g][:], psh[:, :], OP.add, reads=["s_HT%d" % g, pkh], writes=["s_HT%d" % g])
            if LVL < 7: continue
            k.tt(t1[:], xs[b][:], VT[:, VT_D:VT_D + 1024], OP.mult, reads=[kx, "VT", ("s_t1", 0), ("s_t1", 1)], writes=[("s_t1", 0), ("s_t1", 1)], eng="pool")
            k.tt(y[:], y[:], t1[:], OP.add, reads=[("s_y", 0), ("s_y", 1), ("s_t1", 0), ("s_t1", 1)], writes=[("s_y", 0), ("s_y", 1)])
            k.act(zs[:], z[b][:, 0:1024], AF.Silu, reads=[kz], writes=["s_zs"])
            k.tt(y[:], y[:], zs[:], OP.mult, reads=[("s_y", 0), ("s_y", 1), "s_zs"], writes=[("s_y", 0), ("s_y", 1)])
            for g in range(2):
                gs = slice(g * 512, (g + 1) * 512)
                k.op("act", lambda h, g=g, gs=gs: h.activation(out=sq[:], in_=y[:, gs], func=AF.Square, accum_out=ss[:, g:g + 1]),
                     reads=[("s_y", 0), ("s_y", 1)], writes=["s_sq", ("s_ss", g)])
            k.act(rstd[:], ss[:], AF.Sqrt, reads=[("s_ss", 0), ("s_ss", 1)], writes=["s_rstd"], bias=LN_EPS, scale=1.0 / 512)
            k.op("dve", lambda h: h.reciprocal(out=rstd[:], in_=rstd[:]), reads=["s_rstd"], writes=["s_rstd"])
            for g in range(2):
                gs = slice(g * 512, (g + 1) * 512)
                k.stt(y[:, gs], y[:, gs], rstd[:, g:g + 1], VT[:, VT_NW + g * 512:VT_NW + (g + 1) * 512], OP.mult, OP.mult,
                      reads=[("s_y", g), "s_rstd", "VT"], writes=[("s_y", g)])
            if LVL < 8: continue
            yT, kyT = yTs[ci % 2], "s_yT%d" % (ci % 2)
            for half in range(2):
                pst, pkt = k.ps()
                for cc in range(4):
                    c = half * 4 + cc
                    k.tr(pst[:, cc * 128:(cc + 1) * 128], y[:, c * 128:(c + 1) * 128], CM[:, CM_ID:CM_ID + 128], reads=[("s_y", 0), ("s_y", 1), "CM"], writes=[pkt])
                k.copy(yT[:, half * 4:(half + 1) * 4, :], pst[:, :].rearrange("p (c t) -> p c t", t=128), reads=[pkt], writes=[kyT], eng=("act" if half else "dve"))
            k.dma("pool", d["YT"][0:1024, t0:t0 + 128].rearrange("(c p) t -> p c t", p=128), yT[:], reads=[kyT], writes=[("YT", 0)])
    k.pes = None
    k.barrier()


CR_M1, CR_M2, CR_SLN, CR_ISEL, CR_BD, CR_BONES, CR_RESET = 0, 256, 512, 640, 704, 832, 960
NCR = 960 + 512
VR_MUR, VR_MUK, VR_MUV, VR_W0, VR_A0, VR_KK, VR_KA, VR_RK, VR_MULO, VR_MUG = 0, 8, 16, 24, 32, 40, 48, 56, 64, 65
NVR = 66


def phase3(P):
    k, LP, d = P.k, P.LP, P.dram
    CM = P.CM
    with ExitStack() as pes:
        k.pes = pes
        CR = k.sb("CR", [128, NCR])
        VR = k.sb("VR", [128, NVR])
        OMKA = k.sb("OMKA", [128, 8])
        WUA = k.sb("WUA", [128, 1024])
        GUP = k.sb("GUP", [128, 1024])
        LNG = k.sb("LNG", [128, 512])
        LNB = k.sb("LNB", [128, 512])
        k.dma("sp", CR[:], d["CRd"][:, :], writes=["CR"])
        k.dma("sp", VR[:], d["VRd"][:, :], writes=["VR"])
        k.dma("sp", WUA[:], d["WUAd"][:, :], writes=["WUA"])
        k.dma("sp", GUP[:], d["GUPd"][:, :], writes=["GUP"])
        k.dma("sp", LNG[:], d["LNGd"][:, :], writes=["LNG"])
        k.dma("sp", LNB[:], d["LNBd"][:, :], writes=["LNB"])
        k.ts(OMKA[:], VR[:, VR_KA:VR_KA + 8], -1.0, OP.mult, reads=["VR"], writes=["OMKA"], s2=1.0, op1=OP.add)
        FR = mybir.dt.float32r
        T = [k.sb("r_T%d" % j, [128, 64], FR) for j in range(8)]
        for j in range(8):
            k.memset(T[j][:].bitcast(F32), 0.0, writes=["r_T%d" % j])
        ident = CM[:, CM_ID:CM_ID + 128]
        MASK1 = CR[:, CR_M1:CR_M1 + 256]
        MASK2 = CR[:, CR_M2:CR_M2 + 256]
        MSLN = CR[:, CR_SLN:CR_SLN + 128]
        ISELt = k.sb("r_ISELr", [128, 64], FR)
        k.copy(ISELt[:], CR[:, CR_ISEL:CR_ISEL + 64], reads=["CR"], writes=["CR"])
        ISEL = ISELt[:]
        BONES = CR[:, CR_BONES:CR_BONES + 128]

        def T_(name, shape, dt=F32):
            return k.sb(name, shape, dt), name

        lo_raw, klo_raw = T_("r_loraw", [128, 513])
        g_raw, kg_raw = T_("r_graw", [128, 513])
        LOm, kLOm = T_("r_LOm", [128, 512])
        Gs, kGs = T_("r_Gs", [128, 512])
        raw = {nm: [T_("r_raw%s%d" % (nm, i), [128, 513]) for i in range(2)] for nm in "rkv"}
        mx = {nm: T_("r_mx" + nm, [128, 512]) for nm in "rkv"}
        tmp, ktmp = T_("r_tmp", [128, 512])
        tmp2, ktmp2 = T_("r_tmp2", [128, 512])
        lw, klw = T_("r_lw", [128, 512])
        av, kav = T_("r_a", [128, 512])
        gate, kgate = T_("r_gate", [128, 512])
        kkv, kkk = T_("r_kk", [128, 512])
        kmod, kkmod = T_("r_kmod", [128, 512])
        bv, kbv = T_("r_b", [128, 512])
        bonus, kbonus = T_("r_bonus", [128, 512])
        cw, kcw = T_("r_cw", [128, 512])
        ecw, kecw = T_("r_ecw", [128, 512])
        ecwx, kecwx = T_("r_ecwx", [128, 512])
        eneg, keneg = T_("r_eneg", [128, 512])
        eend, keend = T_("r_eend", [128, 512])
        prod, kprod = T_("r_prod", [128, 512])
        QRbd, kQR = T_("r_QRbd", [128, 8, 2, 128], FR)
        kibd, kki = T_("r_kibd", [128, 8, 128], FR)
        bibd, kbi = T_("r_bibd", [128, 8, 128], FR)
        vbd, kvb = T_("r_vbd", [128, 8, 128], FR)
        kebd, kke = T_("r_kebd", [128, 8, 128], FR)
        bebd, kbe = T_("r_bebd", [128, 8, 128], FR)
        A1, kA1 = T_("r_A1", [128, 8, 256], FR)
        A2, kA2 = T_("r_A2", [128, 8, 256], FR)
        PP = [T_("r_PP%d" % i, [128, 8, 256], FR) for i in range(2)]
        QT = [T_("r_QT%d" % i, [128, 8, 128], FR) for i in range(2)]
        Vs, kVs = T_("r_Vs", [128, 8, 64], FR)
        Rs, kRs = T_("r_Rs", [128, 64], FR)
        Us, kUs = T_("r_Us", [128, 8, 64], FR)
        KT, kKT = T_("r_KT", [128, 8, 256], FR)
        Yall, kY = T_("r_Yall", [128, 8, 64])
        Ysq, kYsq = T_("r_Ysq", [128, 8, 64])
        st1, kst1 = T_("r_st1", [128, 8])
        st2, kst2 = T_("r_st2", [128, 8])
        obd, kobd = T_("r_obd", [128, 8, 128], FR)
        yb = [T_("r_yb%d" % i, [128, 512], BF16) for i in range(2)]
        k.memset(lo_raw[:, 0:1], 0.0, writes=[klo_raw])
        k.memset(g_raw[:, 0:1], 0.0, writes=[kg_raw])
        for nm in "rkv":
            for i in range(2):
                k.memset(raw[nm][i][0][:, 0:1], 0.0, writes=[raw[nm][i][1]])
        BDM = CR[:, CR_BD:CR_BD + 128].rearrange("p (a b) -> p a b", b=64)
        it = 0

        def load_shift(tile, key, chunk, t0, n, mucol, outt, outk):
            if t0 == 0:
                k.dma("sp", tile[:, 1:n + 1], d["PT"][chunk * 128:(chunk + 1) * 128, 0:n], reads=[("PT", chunk)], writes=[key])
            else:
                k.dma("sp", tile[:, 0:n + 1], d["PT"][chunk * 128:(chunk + 1) * 128, t0 - 1:t0 + n], reads=[("PT", chunk)], writes=[key])
            k.tt(outt[:, :n], tile[:, 0:n], tile[:, 1:n + 1], OP.subtract, reads=[key], writes=[outk])
            k.stt(outt[:, :n], outt[:, :n], VR[:, mucol:mucol + 1], tile[:, 1:n + 1], OP.mult, OP.add, reads=[outk, key, "VR"], writes=[outk])

        def bd_expand(dst, kdst, src, ksrc, nc_, eng="dve"):
            k.tt(dst[:, :nc_].rearrange("p c (a b) -> p c a b", b=64) if len(dst.shape) == 3 else dst,
                 src.rearrange("p (c t) -> p c t", t=64).unsqueeze(2).broadcast_to([128, nc_, 2, 64]),
                 BDM.unsqueeze(1).broadcast_to([128, nc_, 2, 64]), OP.mult, reads=[ksrc, "CR"], writes=[kdst], eng=eng)

        QR2 = [(QRbd, kQR), T_("r_QRbd_b", [128, 8, 2, 128], FR)]
        bonus2 = [(bonus, kbonus), T_("r_bonus_b", [128, 512])]
        gate2 = [(gate, kgate), T_("r_gate_b", [128, 512])]
        WC2 = [T_("r_WC%d" % i, [128, 8]) for i in range(2)]
        ctr = {"it": 0, "yb": 0}
        Qfin = {}

        def stepA(j, t0, n, nc_, par):
            QRbd, kQR = QR2[par]
            bonus, kbonus = bonus2[par]
            gate, kgate = gate2[par]
            WCp, kWC = WC2[par]
            ctr["it"] += 1
            it = ctr["it"]
            for nm, ch, mu in (("r", C_R, VR_MUR), ("k", C_K, VR_MUK), ("v", C_V, VR_MUV)):
                rt, rk_ = raw[nm][it % 2]
                load_shift(rt, rk_, ch + j, t0, n, mu + j, mx[nm][0], mx[nm][1])
            r_, kr_ = mx["r"]
            k_, kk_ = mx["k"]
            v_, kv_ = mx["v"]
            js = slice(j * 128, (j + 1) * 128)
            ps, pk = k.ps()
            k.mm(ps[:, :n], WUA[0:64, js], LOm[0:64, :n], True, True, reads=["WUA", kLOm], writes=[pk])
            k.act(lw[:, :n], ps[:, :n], AF.Sigmoid, reads=[pk, "VR"], writes=[klw], bias=VR[:, VR_W0 + j:VR_W0 + j + 1])
            k.ts(lw[:, :n], lw[:, :n], -float(np.exp(-0.5)), OP.mult, reads=[klw], writes=[klw])
            ps, pk = k.ps()
            k.mm(ps[:, :n], WUA[64:128, js], LOm[64:128, :n], True, True, reads=["WUA", kLOm], writes=[pk])
            k.act(av[:, :n], ps[:, :n], AF.Sigmoid, reads=[pk, "VR"], writes=[kav], bias=VR[:, VR_A0 + j:VR_A0 + j + 1])
            ps, pk = k.ps()
            k.mm(ps[:, :n], GUP[:, js], Gs[:, :n], True, True, reads=["GUP", kGs], writes=[pk])
            k.copy(gate[:, :n], ps[:, :n], reads=[pk], writes=[kgate], eng="act")
            k.ts(kkv[:, :n], k_[:, :n], VR[:, VR_KK + j:VR_KK + j + 1], OP.mult, reads=[kk_, "VR"], writes=[kkk])
            k.tt(tmp[:, :n], kkv[:, :n], kkv[:, :n], OP.mult, reads=[kkk], writes=[ktmp])
            ps, pk = k.ps()
            k.mm(ps[:, :n], BONES, tmp[:, :n], True, True, reads=["CR", ktmp], writes=[pk])
            k.ts(tmp2[:, :n], ps[:, :n], 1e-24, OP.max, reads=[pk], writes=[ktmp2])
            k.act(tmp2[:, :n], tmp2[:, :n], AF.Sqrt, reads=[ktmp2], writes=[ktmp2])
            k.op("dve", lambda h, n=n: h.reciprocal(out=tmp2[:, :n], in_=tmp2[:, :n]), reads=[ktmp2], writes=[ktmp2])
            k.tt(kkv[:, :n], kkv[:, :n], tmp2[:, :n], OP.mult, reads=[kkk, ktmp2], writes=[kkk])
            k.ts(tmp[:, :n], av[:, :n], VR[:, VR_KA + j:VR_KA + j + 1], OP.mult, reads=[kav, "VR", "OMKA"], writes=[ktmp], s2=OMKA[:, j:j + 1], op1=OP.add)
            k.tt(kmod[:, :n], k_[:, :n], tmp[:, :n], OP.mult, reads=[kk_, ktmp], writes=[kkmod])
            k.tt(bv[:, :n], av[:, :n], kkv[:, :n], OP.mult, reads=[kav, kkk], writes=[kbv], eng="pool")
            k.stt(tmp[:, :n], r_[:, :n], VR[:, VR_RK + j:VR_RK + j + 1], kmod[:, :n], OP.mult, OP.mult, reads=[kr_, kkmod, "VR"], writes=[ktmp])
            ps, pk = k.ps()
            k.mm(ps[:, :n], BONES, tmp[:, :n], True, True, reads=["CR", ktmp], writes=[pk])
            k.tt(bonus[:, :n], ps[:, :n], v_[:, :n], OP.mult, reads=[pk, kv_], writes=[kbonus])
            k.op("dve", lambda h, n=n: h.tensor_tensor_scan(out=cw[:, :n], data0=CR[:, CR_RESET:CR_RESET + n], data1=lw[:, :n], initial=0.0, op0=OP.mult, op1=OP.add),
                 reads=["CR", klw], writes=[kcw])
            k.act(ecw[:, :n], cw[:, :n], AF.Exp, reads=[kcw], writes=[kecw])
            k.tt(tmp2[:, :n], cw[:, :n], lw[:, :n], OP.subtract, reads=[kcw, klw], writes=[ktmp2], eng="pool")
            k.act(ecwx[:, :n], tmp2[:, :n], AF.Exp, reads=[ktmp2], writes=[kecwx])
            k.act(eneg[:, :n], cw[:, :n], AF.Exp, reads=[kcw], writes=[keneg], scale=-1.0)
            cw3 = cw[:, :n].rearrange("p (c t) -> p c t", t=64)
            k.tt(tmp[:, :n].rearrange("p (c t) -> p c t", t=64), cw3[:, :, 63:64].broadcast_to([128, nc_, 64]), cw3, OP.subtract, reads=[kcw], writes=[ktmp])
            k.act(eend[:, :n], tmp[:, :n], AF.Exp, reads=[ktmp], writes=[keend])
            k.tt(prod[:, :n], kkv[:, :n], ecwx[:, :n], OP.mult, reads=[kkk, kecwx], writes=[kprod])
            k.tt(QRbd[:, :nc_, 0, :].rearrange("p c (a b) -> p c a b", b=64), prod[:, :n].rearrange("p (c t) -> p c t", t=64).unsqueeze(2).broadcast_to([128, nc_, 2, 64]),
                 BDM.unsqueeze(1).broadcast_to([128, nc_, 2, 64]), OP.mult, reads=[kprod, "CR"], writes=[(kQR, 0)])
            k.tt(prod[:, :n], r_[:, :n], ecw[:, :n], OP.mult, reads=[kr_, kecw, (kQR, 0)], writes=[kprod])
            k.tt(QRbd[:, :nc_, 1, :].rearrange("p c (a b) -> p c a b", b=64), prod[:, :n].rearrange("p (c t) -> p c t", t=64).unsqueeze(2).broadcast_to([128, nc_, 2, 64]),
                 BDM.unsqueeze(1).broadcast_to([128, nc_, 2, 64]), OP.mult, reads=[kprod, "CR"], writes=[(kQR, 1)])
            for (dst, kdst, src, ksrc, ex, kex, neg) in ((kibd, kki, kmod, kkmod, eneg, keneg, False), (bibd, kbi, bv, kbv, eneg, keneg, False),
                                                      (kebd, kke, kmod, kkmod, eend, keend, False), (bebd, kbe, bv, kbv, eend, keend, True)):
                if neg:
                    k.stt(prod[:, :n], src[:, :n], -1.0, ex[:, :n], OP.mult, OP.mult, reads=[ksrc, kex, kprod, (kQR, 1), kki, kbi, kke], writes=[kprod])
                else:
                    k.tt(prod[:, :n], src[:, :n], ex[:, :n], OP.mult, reads=[ksrc, kex, kprod, (kQR, 1), kki, kbi, kke], writes=[kprod])
                k.tt(dst[:, :nc_].rearrange("p c (a b) -> p c a b", b=64), prod[:, :n].rearrange("p (c t) -> p c t", t=64).unsqueeze(2).broadcast_to([128, nc_, 2, 64]),
                     BDM.unsqueeze(1).broadcast_to([128, nc_, 2, 64]), OP.mult, reads=[kprod, "CR"], writes=[kdst])
            k.tt(vbd[:, :nc_].rearrange("p c (a b) -> p c a b", b=64), v_[:, :n].rearrange("p (c t) -> p c t", t=64).unsqueeze(2).broadcast_to([128, nc_, 2, 64]),
                 BDM.unsqueeze(1).broadcast_to([128, nc_, 2, 64]), OP.mult, reads=[kv_, "CR"], writes=[kvb], eng="pool")
            k.copy(WCp[:, :nc_].unsqueeze(2), ecw[:, :n].rearrange("p (c t) -> p c t", t=64)[:, :, 63:64], reads=[kecw], writes=[kWC], eng="pool")

        def s123(j, t0, n, nc_, par):
            QRbd, kQR = QR2[par]
            bonus, kbonus = bonus2[par]
            gate, kgate = gate2[par]
            WCp, kWC = WC2[par]
            for c in range(nc_):
                QRc = QRbd[:, c].rearrange("p a b -> p (a b)")
                ps1, pk1 = k.ps()
                k.mm(ps1[:, 0:256], kibd[:, c], QRc, True, True, reads=[kki, (kQR, 0), (kQR, 1)], writes=[pk1])
                k.tt(A1[:, c, :], ps1[:, 0:256], MASK1, OP.mult, reads=[pk1, "CR"], writes=[(kA1, c)])
                ps2, pk2 = k.ps()
                k.mm(ps2[:, 0:256], bibd[:, c], QRc, True, True, reads=[kbi, (kQR, 0), (kQR, 1)], writes=[pk2])
                k.tt(A2[:, c, :], ps2[:, 0:256], MASK2, OP.mult, reads=[pk2, "CR"], writes=[(kA2, c)])
                ps3, pk3 = k.ps()
                k.mm(ps3[:, 0:128], QRbd[:, c, 0], bibd[:, c], True, True, reads=[kbi, (kQR, 0)], writes=[pk3])
                k.tt(PP[0][0][:, c, 0:128], ps3[:, 0:128], MSLN, OP.mult, reads=[pk3, "CR"], writes=[(PP[0][1], c)])
                k.copy(PP[0][0][:, c, 128:256], A2[:, c, 0:128].bitcast(F32), reads=[(kA2, c), (PP[0][1], c)], writes=[(PP[0][1], c)], eng="pool")
                k.tt(QT[0][0][:, c, :], A2[:, c, 0:128].bitcast(F32), ident, OP.add, reads=[(kA2, c), "CM"], writes=[(QT[0][1], c)], eng="pool")
            cur = 0
            for lv in range(1, 6):
                Pc, kPc = PP[cur]
                Pn, kPn = PP[1 - cur]
                Qc, kQc = QT[cur]
                Qn, kQn = QT[1 - cur]
                for c0 in range(0, nc_, 2):
                    psq, pkq = k.ps()
                    for ci in range(2):
                        c = c0 + ci
                        k.mm(psq[:, ci * 256:ci * 256 + 128], Pc[:, c, 128:256], Pc[:, c, 0:128], True, True, reads=[(kPc, c)], writes=[pkq])
                        k.mm(psq[:, ci * 256 + 128:ci * 256 + 256], Pc[:, c, 0:128], Pc[:, c, 128:256], True, True, reads=[(kPc, c)], writes=[pkq])
                    k.copy(Pn[:, c0:c0 + 2, :], psq[:, 0:512].rearrange("p (c f) -> p c f", f=256), reads=[pkq], writes=[(kPn, c0), (kPn, c0 + 1)], eng="act")
                for c0 in range(0, nc_, 4):
                    m = min(4, nc_ - c0)
                    psu, pku = k.ps()
                    for ci in range(m):
                        c = c0 + ci
                        k.mm(psu[:, ci * 128:(ci + 1) * 128], Pn[:, c, 0:128], Qc[:, c, :], True, True, reads=[(kPn, c), (kQc, c)], writes=[pku])
                    k.tt(Qn[:, c0:c0 + m, :], psu[:, 0:m * 128].rearrange("p (c f) -> p c f", f=128), Qc[:, c0:c0 + m, :].bitcast(F32), OP.add,
                         reads=[pku] + [(kQc, c0 + i) for i in range(m)], writes=[(kQn, c0 + i) for i in range(m)])
                cur = 1 - cur
            Qf, kQf = QT[cur]
            psv, pkv = k.ps()
            for c in range(nc_):
                k.mm(psv[:, c * 64:(c + 1) * 64], vbd[:, c], ISEL, True, True, reads=[kvb, "CR"], writes=[pkv])
            k.copy(Vs[:, :nc_, :], psv[:, 0:nc_ * 64].rearrange("p (c f) -> p c f", f=64), reads=[pkv], writes=[kVs], eng="act")
            for c0 in range(0, nc_, 2):
                pst, pkt = k.ps()
                for ci in range(2):
                    c = c0 + ci
                    k.tr(pst[:, ci * 256:ci * 256 + 128], kebd[:, c].bitcast(F32), ident, reads=[kke, "CM"], writes=[pkt])
                    k.tr(pst[:, ci * 256 + 128:ci * 256 + 256], bebd[:, c].bitcast(F32), ident, reads=[kbe, "CM"], writes=[pkt])
                k.copy(KT[:, c0:c0 + 2, :], pst[:, 0:512].rearrange("p (c f) -> p c f", f=256), reads=[pkt], writes=[(kKT, c0), (kKT, c0 + 1)])
            Qfin[par] = (Qf, kQf)

        def s4(j, t0, n, nc_, par):
            QRbd, kQR = QR2[par]
            bonus, kbonus = bonus2[par]
            gate, kgate = gate2[par]
            WCp, kWC = WC2[par]
            Tj, kTj = T[j], "r_T%d" % j
            Qf, kQf = Qfin[par]
            psY, pkY = k.psb[7], "psb7"
            for c in range(nc_):
                psr, pkr = k.ps()
                k.mm(psr[:, 0:64], QRbd[:, c, 0], Tj[:], True, False, reads=[(kQR, 0), kTj], writes=[pkr])
                k.mm(psr[:, 0:64], A1[:, c, 0:128], Vs[:, c, :], False, True, reads=[(kA1, c), kVs], writes=[pkr])
                k.copy(Rs[:], psr[:, 0:64], reads=[pkr], writes=[kRs])
                psu, pku = k.ps()
                k.mm(psu[:, 0:64], Qf[:, c, :], Rs[:], True, True, reads=[(kQf, c), kRs], writes=[pku])
                k.copy(Us[:, c, :], psu[:, 0:64], reads=[pku], writes=[(kUs, c)])
                psn, pkn = k.ps()
                k.mm(psn[:, 0:64], KT[:, c, 0:128], Vs[:, c, :], True, False, reads=[(kKT, c), kVs], writes=[pkn])
                k.mm(psn[:, 0:64], KT[:, c, 128:256], Us[:, c, :], False, True, reads=[(kKT, c), (kUs, c)], writes=[pkn])
                k.mm(psY[:, c * 64:(c + 1) * 64], QRbd[:, c, 1], Tj[:], True, False, reads=[(kQR, 1), kTj], writes=[pkY])
                k.mm(psY[:, c * 64:(c + 1) * 64], A1[:, c, 128:256], Vs[:, c, :], False, False, reads=[(kA1, c), kVs], writes=[pkY])
                k.mm(psY[:, c * 64:(c + 1) * 64], A2[:, c, 128:256], Us[:, c, :], False, True, reads=[(kA2, c), (kUs, c)], writes=[pkY])
                k.stt(Tj[:], Tj[:].bitcast(F32), WCp[:, c:c + 1], psn[:, 0:64], OP.mult, OP.add, reads=[kTj, kWC, pkn], writes=[kTj])
            k.copy(Yall[:, :nc_, :], psY[:, 0:nc_ * 64].rearrange("p (c f) -> p c f", f=64), reads=[pkY], writes=[kY], eng="act")

        def tail(j, t0, n, nc_, par):
            QRbd, kQR = QR2[par]
            bonus, kbonus = bonus2[par]
            gate, kgate = gate2[par]
            WCp, kWC = WC2[par]
            ctr["yb"] += 1
            it = ctr["yb"]
            Yv = Yall[:, :nc_, :]
            k.op("dve", lambda h, Yv=Yv, nc_=nc_: h.tensor_reduce(out=st1[:, :nc_], in_=Yv, axis=mybir.AxisListType.X, op=OP.add), reads=[kY], writes=[kst1])
            k.ts(st1[:, :nc_], st1[:, :nc_], 1.0 / 64, OP.mult, reads=[kst1], writes=[kst1])
            k.tt(Yv, Yv, st1[:, :nc_].unsqueeze(2).broadcast_to([128, nc_, 64]), OP.subtract, reads=[kY, kst1], writes=[kY])
            k.tt(Ysq[:, :nc_, :], Yv, Yv, OP.mult, reads=[kY], writes=[kYsq])
            k.op("dve", lambda h, nc_=nc_: h.tensor_reduce(out=st2[:, :nc_], in_=Ysq[:, :nc_, :], axis=mybir.AxisListType.X, op=OP.add), reads=[kYsq], writes=[kst2])
            k.act(st2[:, :nc_], st2[:, :nc_], AF.Sqrt, reads=[kst2], writes=[kst2], bias=RWKV_EPS, scale=1.0 / 64)
            k.op("dve", lambda h, nc_=nc_: h.reciprocal(out=st2[:, :nc_], in_=st2[:, :nc_]), reads=[kst2], writes=[kst2])
            k.tt(Yv, Yv, st2[:, :nc_].unsqueeze(2).broadcast_to([128, nc_, 64]), OP.mult, reads=[kY, kst2], writes=[kY])
            for c in range(nc_):
                k.tt(Yall[:, c, :], Yall[:, c, :], LNG[:, j * 64:(j + 1) * 64], OP.mult, reads=[kY, "LNG"], writes=[kY])
                k.tt(Yall[:, c, :], Yall[:, c, :], LNB[:, j * 64:(j + 1) * 64], OP.add, reads=[kY, "LNB"], writes=[kY])
            k.tt(obd[:, :nc_].rearrange("p c (a b) -> p c a b", b=64), Yv.unsqueeze(2).broadcast_to([128, nc_, 2, 64]),
                 BDM.unsqueeze(1).broadcast_to([128, nc_, 2, 64]), OP.mult, reads=[kY, "CR"], writes=[kobd])
            pso, pko = k.ps()
            for c in range(nc_):
                k.mm(pso[:, c * 64:(c + 1) * 64], obd[:, c], ISEL, True, True, reads=[kobd, "CR"], writes=[pko])
            k.tt(tmp[:, :n], pso[:, :n], bonus[:, :n], OP.add, reads=[pko, kbonus], writes=[ktmp])
            ybt, kyb = yb[it % 2]
            k.tt(ybt[:, :n], tmp[:, :n], gate[:, :n], OP.mult, reads=[ktmp, kgate], writes=[kyb])
            k.dma("pool", d["YT"][1024 + j * 128:1024 + (j + 1) * 128, t0:t0 + n], ybt[:, :n], reads=[kyb], writes=[("YT", 1)])

        for (t0, n) in supertiles(LP):
            nc_ = n // 64
            load_shift(lo_raw, klo_raw, C_LO, t0, n, VR_MULO, LOm, kLOm)
            k.act(LOm[0:64, :n], LOm[0:64, :n], AF.Tanh, reads=[kLOm], writes=[kLOm])
            load_shift(g_raw, kg_raw, C_G, t0, n, VR_MUG, Gs, kGs)
            k.act(Gs[:, :n], Gs[:, :n], AF.Sigmoid, reads=[kGs], writes=[kGs])
            stepA(0, t0, n, nc_, 0)
            s123(0, t0, n, nc_, 0)
            for j in range(8):
                par = j % 2
                fns = [lambda j=j, par=par: s4(j, t0, n, nc_, par)]
                if j < 7:
                    fns.append(lambda j=j, par=par: stepA(j + 1, t0, n, nc_, 1 - par))
                k.interleave(fns, [2, 1])
                P.pump_casts(2)
                tail(j, t0, n, nc_, par)
                if j < 7:
                    s123(j + 1, t0, n, nc_, 1 - par)
    k.pes = None
    k.barrier()


def host_consts_rwkv(inp):
    p = np.arange(128)
    q = np.arange(128)
    same = (p[:, None] // 64) == (q[None, :] // 64)
    SU = (same & ((p[:, None] % 64) < (q[None, :] % 64))).astype(np.float32)
    IU = (same & ((p[:, None] % 64) <= (q[None, :] % 64))).astype(np.float32)
    SL = (same & ((q[None, :] % 64) < (p[:, None] % 64))).astype(np.float32)
    CR = np.zeros((128, NCR), np.float32)
    CR[:, 0:128] = SU
    CR[:, 128:256] = IU
    CR[:, 256:384] = -SU
    CR[:, 384:512] = -IU
    CR[:, 512:640] = -SL
    CR[:, 640:704] = ((p[:, None] % 64) == np.arange(64)[None, :])
    CR[:, 704:832] = ((p[:, None] // 64) == (np.arange(128)[None, :] // 64))
    CR[:, 832:960] = same
    CR[:, 960:960 + 512] = (np.arange(512) % 64 != 0)[None, :]
    mu = inp["ev_shift_mu"][0]
    VR = np.zeros((128, NVR), np.float32)

    def pairs(v):
        return v.reshape(8, 128).T

    VR[:, 0:8] = pairs(mu[0:1024])
    VR[:, 8:16] = pairs(mu[1024:2048])
    VR[:, 16:24] = pairs(mu[2048:3072])
    VR[:, 24:32] = pairs(inp["ev_w0"][0])
    VR[:, 32:40] = pairs(inp["ev_a0"][0])
    VR[:, 40:48] = pairs(inp["ev_k_k"][0])
    VR[:, 48:56] = pairs(inp["ev_k_a"][0])
    VR[:, 56:64] = pairs(inp["ev_r_k"][0].reshape(1024))
    VR[:, 64] = mu[3072:3200]
    VR[:, 65] = mu[3200:3328]
    WUA = np.concatenate([inp["ev_w_up"][0], inp["ev_a_up"][0]], 0).astype(np.float32)
    GUP = inp["ev_g_up"][0].astype(np.float32)

    def stack(v):
        a = v.reshape(8, 2, 64)
        return np.ascontiguousarray(np.repeat(a.transpose(1, 0, 2)[:, None], 64, axis=1).reshape(128, 512))

    return dict(CRd=CR, VRd=VR, WUAd=WUA, GUPd=GUP, LNGd=stack(inp["ev_lnx_g"][0]), LNBd=stack(inp["ev_lnx_b"][0]))


def layer_norm_rows(k, pfx, src, ksrc, dst, kdst, gcol, bcol, VT2, tiles):
    stats, mv, rs = tiles
    for i in range(2):
        k.op("dve", lambda h, i=i: h.bn_stats(out=stats[:, i * 6:(i + 1) * 6], in_=src[:, i * 512:(i + 1) * 512]), reads=[ksrc], writes=[pfx + "st"])
    k.op("dve", lambda h: h.bn_aggr(out=mv[:], in_=stats[:]), reads=[pfx + "st"], writes=[pfx + "mv"])
    k.act(rs[:], mv[:, 1:2], AF.Sqrt, reads=[pfx + "mv"], writes=[pfx + "rs"], bias=LN_EPS)
    k.op("dve", lambda h: h.reciprocal(out=rs[:], in_=rs[:]), reads=[pfx + "rs"], writes=[pfx + "rs"])
    k.ts(dst[:], src[:], mv[:, 0:1], OP.subtract, reads=[ksrc, pfx + "mv", pfx + "rs"], writes=[kdst], s2=rs[:, 0:1], op1=OP.mult)
    k.tt(dst[:], dst[:], VT2[:, gcol:gcol + 1024], OP.mult, reads=[kdst, "VT2"], writes=[kdst])
    k.tt(dst[:], dst[:], VT2[:, bcol:bcol + 1024], OP.add, reads=[kdst, "VT2"], writes=[kdst])


def proj_res_ln(P, pfx, srcT, nkc, wname, res_name, gcol, bcol, out_name, outT_name, outT32_name=None):
    k, LP, d = P.k, P.LP, P.dram
    with ExitStack() as pes:
        k.pes = pes
        VT2 = k.sb(pfx + "VT2", [128, 2048])
        k.dma("sp", VT2[:], d["VT2d"][0:1, gcol:gcol + 2048].partition_broadcast(128), writes=["VT2"])
        P.VT2 = VT2
        gcol, bcol = 0, 1024
        W = k.sb(pfx + "W", [128, nkc, 1024], BF16)
        k.dma("sp", W[:].rearrange("p a b -> p (a b)"), d[wname][:, :], reads=[(wname, 0)], writes=[pfx + "W"])
        yT = [k.sb(pfx + "yT%d" % i, [128, nkc, 512], BF16) for i in range(2)]
        xr = [k.sb(pfx + "x%d" % i, [128, 1024]) for i in range(2)]
        hp = k.sb(pfx + "hp", [128, 1024])
        ho = [k.sb(pfx + "ho%d" % i, [128, 1024]) for i in range(2)]
        hT = [k.sb(pfx + "hT%d" % i, [128, 8, 128], BF16) for i in range(2)]
        hT32 = [k.sb(pfx + "hTf%d" % i, [128, 8, 128]) for i in range(2)]
        lnt = (k.sb(pfx + "st", [128, 12]), k.sb(pfx + "mv", [128, 2]), k.sb(pfx + "rs", [128, 1]))
        it = 0
        for si, (t0, n) in enumerate(supertiles(LP)):
            y, ky = yT[si % 2], pfx + "yT%d" % (si % 2)
            k.dma("sp", y[:, :, :n], d[srcT][:, t0:t0 + n].rearrange("(c p) t -> p c t", p=128), reads=[(srcT, 0), (srcT, 1)], writes=[ky])
            for j in range(n // 128):
                it += 1
                x, kx = xr[it % 2], pfx + "x%d" % (it % 2)
                r0 = t0 + j * 128
                k.dma("sp", x[:], d[res_name][r0:r0 + 128, :], reads=[(res_name, 0)], writes=[kx])
                for half in range(2):
                    ps, pk = k.ps()
                    for kc in range(nkc):
                        k.mm(ps[:, :], y[:, kc, j * 128:(j + 1) * 128], W[:, kc, half * 512:(half + 1) * 512], kc == 0, kc == nkc - 1, reads=[ky, pfx + "W"], writes=[pk])
                    k.stt(hp[:, half * 512:(half + 1) * 512], x[:, half * 512:(half + 1) * 512], ALPHA, ps[:, :], OP.mult, OP.add, reads=[kx, pk], writes=[pfx + "hp"])
                o, ko = ho[it % 2], pfx + "ho%d" % (it % 2)
                layer_norm_rows(k, pfx, hp, pfx + "hp", o, ko, gcol, bcol, P.VT2, lnt)
                k.dma("pool", d[out_name][r0:r0 + 128, :], o[:], reads=[ko], writes=[(out_name, 0)])
                t_, kt = hT[it % 2], pfx + "hT%d" % (it % 2)
                tf, ktf = hT32[it % 2], pfx + "hTf%d" % (it % 2)
                for half in range(2):
                    ps, pk = k.ps()
                    for cc in range(4):
                        c = half * 4 + cc
                        k.tr(ps[:, cc * 128:(cc + 1) * 128], o[:, c * 128:(c + 1) * 128], P.ident, reads=[ko, "CM"], writes=[pk])
                    k.copy(t_[:, half * 4:(half + 1) * 4, :], ps[:, :].rearrange("p (c t) -> p c t", t=128), reads=[pk], writes=[kt], eng="act")
                    if outT32_name:
                        k.copy(tf[:, half * 4:(half + 1) * 4, :], ps[:, :].rearrange("p (c t) -> p c t", t=128), reads=[pk], writes=[ktf])
                k.dma("pool", d[outT_name][:, r0:r0 + 128].rearrange("(c p) t -> p c t", p=128), t_[:], reads=[kt], writes=[(outT_name, 0)])
                if outT32_name:
                    k.dma("pool", d[outT32_name][:, r0:r0 + 128].rearrange("(c p) t -> p c t", p=128), tf[:], reads=[ktf], writes=[(outT32_name, 0)])
    k.pes = None
    k.barrier()


def ffn_phase(P, pfx, srcT, res_name, wgu_names, wd_names, gcol, bcol, out_name, outT_name=None, router=None, tile_n=512):
    k, LP, d = P.k, P.LP, P.dram
    ne = len(wgu_names)
    with ExitStack() as pes:
        k.pes = pes
        VT2 = k.sb(pfx + "VT2", [128, 2048])
        k.dma("sp", VT2[:], d["VT2d"][0:1, gcol:gcol + 2048].partition_broadcast(128), writes=["VT2"])
        P.VT2 = VT2
        gcol, bcol = 0, 1024
        nhb = 2 if tile_n == 512 else 1
        hT = [k.sb(pfx + "hT%d" % i, [128, 8, tile_n], BF16) for i in range(nhb)]
        NWG = 6
        wg = [k.sb(pfx + "wg%d" % i, [128, 8, 128], BF16) for i in range(NWG)]
        wdh = [k.sb(pfx + "wdh%d" % i, [128, 22, 512], BF16) for i in range(2)]
        actT = k.sb(pfx + "actT", [128, 22, tile_n], BF16)
        sg = [k.sb(pfx + "sg%d" % i, [128, 512]) for i in range(2)]
        acc = [k.sb(pfx + "acc%d" % i, [128, 1024]) for i in range(tile_n // 128)]
        o = [k.sb(pfx + "o%d" % i, [128, 1024]) for i in range(2)]
        oT = [k.sb(pfx + "oT%d" % i, [128, 8, 128], BF16) for i in range(2)] if outT_name else None
        lnt = (k.sb(pfx + "st", [128, 12]), k.sb(pfx + "mv", [128, 2]), k.sb(pfx + "rs", [128, 1]))
        if router:
            hT32 = [k.sb(pfx + "hT32_%d" % i, [128, 8, 128]) for i in range(2)]
            WR = k.sb(pfx + "WR", [128, 8, 8])
            k.dma("sp", WR[:].rearrange("p a b -> p (a b)"), d["WRd"][:, :], writes=[pfx + "WR"])
            lg = k.sb(pfx + "lg", [128, 8])
            m8 = k.sb(pfx + "m8", [128, 8])
            msk = k.sb(pfx + "msk", [128, 8])
            nm0 = k.sb(pfx + "nm0", [128, 1])
            den = k.sb(pfx + "den", [128, 1])
            G = [k.sb(pfx + "G%d" % i, [128, 8]) for i in range(tile_n // 128)]

        def load_wd(e):
            for hf in range(2):
                k.dma("sp", wdh[hf][:], d[wd_names[e]][:, :].rearrange("p (a b) -> p a b", b=1024)[:, :, hf * 512:(hf + 1) * 512],
                      reads=[(wd_names[e], 0)], writes=[pfx + "wdh%d" % hf])

        if ne == 1:
            load_wd(0)
        wi = 0
        oi = 0
        big = []
        t = 0
        while t < LP:
            nn = min(tile_n, LP - t)
            big.append((t, nn))
            t += nn
        for si, (t0, n) in enumerate(big):
            nj = n // 128
            groups = [(g0, min(512, n - g0)) for g0 in range(0, n, 512)]
            h, kh = hT[si % nhb], pfx + "hT%d" % (si % nhb)
            k.dma("sp", h[:, :, :n], d[srcT][:, t0:t0 + n].rearrange("(c p) t -> p c t", p=128), reads=[(srcT, 0)], writes=[kh])
            for j in range(nj):
                r0 = t0 + j * 128
                k.dma("sp", acc[j][:], d[res_name][r0:r0 + 128, :], reads=[(res_name, 0)], writes=[pfx + "acc%d" % j])
                k.ts(acc[j][:], acc[j][:], ALPHA, OP.mult, reads=[pfx + "acc%d" % j], writes=[pfx + "acc%d" % j], eng="pool")
                if router:
                    h32, kh32 = hT32[j % 2], pfx + "hT32_%d" % (j % 2)
                    k.dma("sp", h32[:], d[router][:, r0:r0 + 128].rearrange("(c p) t -> p c t", p=128), reads=[(router, 0)], writes=[kh32])
                    ps, pk = k.ps()
                    for kc in range(8):
                        k.mm(ps[:, 0:8], h32[:, kc, :], WR[:, kc, :], kc == 0, kc == 7, reads=[kh32, pfx + "WR"], writes=[pk])
                    k.copy(lg[:], ps[:, 0:8], reads=[pk], writes=[pfx + "lg"])
                    k.op("dve", lambda hh: hh.max(out=m8[:], in_=lg[:]), reads=[pfx + "lg"], writes=[pfx + "m8"])
                    k.ts(msk[:], lg[:], m8[:, 1:2], OP.is_ge, reads=[pfx + "lg", pfx + "m8"], writes=[pfx + "msk"])
                    k.ts(nm0[:], m8[:, 0:1], -1.0, OP.mult, reads=[pfx + "m8"], writes=[pfx + "nm0"])
                    k.act(lg[:], lg[:], AF.Exp, reads=[pfx + "lg", pfx + "nm0"], writes=[pfx + "lg"], bias=nm0[:, 0:1])
                    k.tt(lg[:], lg[:], msk[:], OP.mult, reads=[pfx + "lg", pfx + "msk"], writes=[pfx + "lg"])
                    k.op("dve", lambda hh: hh.tensor_reduce(out=den[:], in_=lg[:], axis=mybir.AxisListType.X, op=OP.add), reads=[pfx + "lg"], writes=[pfx + "den"])
                    k.op("dve", lambda hh: hh.reciprocal(out=den[:], in_=den[:]), reads=[pfx + "den"], writes=[pfx + "den"])
                    k.ts(G[j][:], lg[:], den[:, 0:1], OP.mult, reads=[pfx + "lg", pfx + "den"], writes=[pfx + "G%d" % j])
            for e in range(ne):
                for i in range(22):
                    pss = {}
                    for part in range(2):
                        c = part * 22 + i
                        w, kw = wg[wi % NWG], pfx + "wg%d" % (wi % NWG)
                        wi += 1
                        k.dma("sp", w[:].rearrange("p a b -> p (a b)"), d[wgu_names[e]][c * 128:(c + 1) * 128, :], reads=[(wgu_names[e], (c * 128) // 512)], writes=[kw])
                        for gi, (g0, ng) in enumerate(groups):
                            ps, pk = k.ps()
                            for kc in range(8):
                                k.mm(ps[:, :ng], w[:, kc, :], h[:, kc, g0:g0 + ng], kc == 0, kc == 7, reads=[kw, kh], writes=[pk])
                            pss[(gi, part)] = (ps, pk)
                    for gi, (g0, ng) in enumerate(groups):
                        s_, ks = sg[gi % 2], pfx + "sg%d" % (gi % 2)
                        k.act(s_[:, :ng], pss[(gi, 0)][0][:, :ng], AF.Silu, reads=[pss[(gi, 0)][1]], writes=[ks])
                        k.tt(actT[:, i, g0:g0 + ng], s_[:, :ng], pss[(gi, 1)][0][:, :ng], OP.mult, reads=[ks, pss[(gi, 1)][1]], writes=[(pfx + "actT", i)])
                    if ne > 1 and i == 2:
                        load_wd(e)
                for half in range(2):
                    hs = slice(half * 512, (half + 1) * 512)
                    for j in range(nj):
                        ps, pk = k.ps()
                        for i in range(22):
                            k.mm(ps[:, :], actT[:, i, j * 128:(j + 1) * 128], wdh[half][:, i, :], i == 0, i == 21, reads=[(pfx + "actT", i), pfx + "wdh%d" % half], writes=[pk])
                        if router:
                            k.stt(acc[j][:, hs], ps[:, :], G[j][:, e:e + 1], acc[j][:, hs], OP.mult, OP.add, reads=[pk, pfx + "G%d" % j, pfx + "acc%d" % j], writes=[pfx + "acc%d" % j])
                        else:
                            k.tt(acc[j][:, hs], ps[:, :], acc[j][:, hs], OP.add, reads=[pk, pfx + "acc%d" % j], writes=[pfx + "acc%d" % j])
            for j in range(nj):
                r0 = t0 + j * 128
                oi += 1
                ot, ko = o[oi % 2], pfx + "o%d" % (oi % 2)
                layer_norm_rows(k, pfx, acc[j], pfx + "acc%d" % j, ot, ko, gcol, bcol, P.VT2, lnt)
                k.dma("pool", d[out_name][r0:r0 + 128, :], ot[:], reads=[ko], writes=[(out_name, 0)])
                if outT_name:
                    t_, kt = oT[oi % 2], pfx + "oT%d" % (oi % 2)
                    for half in range(2):
                        ps, pk = k.ps()
                        for cc in range(4):
                            c = half * 4 + cc
                            k.tr(ps[:, cc * 128:(cc + 1) * 128], ot[:, c * 128:(c + 1) * 128], P.ident, reads=[ko, "CM"], writes=[pk])
                        k.copy(t_[:, half * 4:(half + 1) * 4, :], ps[:, :].rearrange("p (c t) -> p c t", t=128), reads=[pk], writes=[kt], eng="act")
                    k.dma("pool", d[outT_name][:, r0:r0 + 128].rearrange("(c p) t -> p c t", p=128), t_[:], reads=[kt], writes=[(outT_name, 0)])
    k.pes = None
    k.barrier()


VL_CW, VL_CB, VL_GXB, VL_GAB, VL_LAM = 0, 32, 40, 48, 56
NVL = 64


def phase5(P):
    k, LP, d = P.k, P.LP, P.dram
    with ExitStack() as pes:
        k.pes = pes
        W = k.sb("l_W", [128, 16, 8, 128], BF16)
        k.dma("sp", W[:].rearrange("p c a b -> p c (a b)"), d["W1INb"][:, :].rearrange("(c p) f -> p c f", p=128), reads=[("W1INb", i) for i in range(4)], writes=["l_W"])
        GW = k.sb("l_GW", [128, 16, 128])
        k.dma("sp", GW[:].rearrange("p c b -> p (c b)"), d["GWd"][:, :], writes=["l_GW"])
        VL = k.sb("l_VL", [128, NVL])
        k.dma("sp", VL[:], d["VLd"][:, :], writes=["l_VL"])
        SPm8 = k.sb("l_sp8", [128, 8])
        SPm16 = k.sb("l_sp16", [128, 8])
        k.act(SPm8[:], VL[:, VL_LAM:VL_LAM + 8], AF.Exp, reads=["l_VL"], writes=["l_sp8"], scale=-1.0)
        k.act(SPm8[:], SPm8[:], AF.Ln, reads=["l_sp8"], writes=["l_sp8"], bias=1.0)
        k.ts(SPm16[:], SPm8[:], -16.0, OP.mult, reads=["l_sp8"], writes=["l_sp16"])
        k.ts(SPm8[:], SPm8[:], -8.0, OP.mult, reads=["l_sp8", "l_sp16"], writes=["l_sp8"])
        hT = [k.sb("l_hT%d" % i, [128, 8, 512], BF16) for i in range(2)]
        gb = [k.sb("l_gb%d" % i, [128, 512]) for i in range(2)]
        g2_2 = [k.sb("l_g2_%d" % i, [128, 512]) for i in range(2)]
        xr = [k.sb("l_xr%d" % i, [128, 515]) for i in range(2)]
        xf_2 = [k.sb("l_xf_%d" % i, [128, 512]) for i in range(2)]
        gx_2 = [k.sb("l_gx_%d" % i, [128, 512]) for i in range(2)]
        ga_2 = [k.sb("l_ga_%d" % i, [128, 512]) for i in range(2)]
        av_2 = [k.sb("l_a_%d" % i, [128, 512]) for i in range(2)]
        uv_2 = [k.sb("l_u_%d" % i, [128, 512]) for i in range(2)]
        hs = [k.sb("l_hs%d" % c, [128, 512]) for c in range(8)]
        carry = [k.sb("l_cy%d" % c, [128, 1]) for c in range(8)]
        yb = [k.sb("l_yb%d" % i, [128, 512], BF16) for i in range(2)]
        for c in range(8):
            k.memset(carry[c][:], 0.0, writes=["l_cy%d" % c])
        it = 0
        for si, (t0, n) in enumerate(supertiles(LP)):
            h, kh = hT[si % 2], "l_hT%d" % (si % 2)
            k.dma("sp", h[:, :, :n], d["H2T"][:, t0:t0 + n].rearrange("(c p) t -> p c t", p=128), reads=[("H2T", 0)], writes=[kh])
            def body(c, par, h=h, kh=kh, t0=t0, n=n):
                it = par
                g2, xf, gx, ga, av, uv = g2_2[par], xf_2[par], gx_2[par], ga_2[par], av_2[par], uv_2[par]
                sfx = "_%d" % par
                ps, pk = k.ps()
                for kc in range(8):
                    k.mm(ps[:, :n], W[:, c, kc, :], h[:, kc, :n], kc == 0, kc == 7, reads=["l_W", kh], writes=[pk])
                g, kg = gb[it % 2], "l_gb%d" % (it % 2)
                k.copy(g[:, :n], ps[:, :n], reads=[pk], writes=[kg], eng="act")
                k.tt(g2[:, :n], g[:, :n], g[:, :n], OP.mult, reads=[kg], writes=["l_g2" + sfx])
                k.ts(g2[:, :n], g2[:, :n], 0.044715, OP.mult, reads=["l_g2" + sfx], writes=["l_g2" + sfx], s2=1.0, op1=OP.add)
                k.tt(g2[:, :n], g2[:, :n], g[:, :n], OP.mult, reads=["l_g2" + sfx, kg], writes=["l_g2" + sfx])
                k.act(g2[:, :n], g2[:, :n], AF.Sigmoid, reads=["l_g2" + sfx], writes=["l_g2" + sfx], scale=1.5957691216057308)
                k.tt(g[:, :n], g[:, :n], g2[:, :n], OP.mult, reads=[kg, "l_g2" + sfx], writes=[kg])
                x, kx = xr[it % 2], "l_xr%d" % (it % 2)
                ps, pk = k.ps()
                for kc in range(8):
                    k.mm(ps[:, :n], W[:, 8 + c, kc, :], h[:, kc, :n], kc == 0, kc == 7, reads=["l_W", kh], writes=[pk])
                xp, kxp = xr[(it + 1) % 2], "l_xr%d" % ((it + 1) % 2)
                k.copy(x[:, 3:3 + n], ps[:, :n], reads=[pk], writes=[kx])
                k.dma("pool", d["XRT"][c * 128:(c + 1) * 128, t0:t0 + n], x[:, 3:3 + n], reads=[kx], writes=[("XRT", c)])
                if t0 == 0:
                    k.memset(x[:, 0:3], 0.0, writes=[kx])
                else:
                    k.dma("sp", x[:, 0:3], d["XRT"][c * 128:(c + 1) * 128, t0 - 3:t0], reads=[("XRT", c)], writes=[kx])
                cwc = VL_CW + c * 4
                k.ts(xf[:, :n], x[:, 3:3 + n], VL[:, cwc + 3:cwc + 4], OP.mult, reads=[kx, "l_VL"], writes=["l_xf" + sfx])
                for kk in (2, 1, 0):
                    k.stt(xf[:, :n], x[:, kk:kk + n], VL[:, cwc + kk:cwc + kk + 1], xf[:, :n], OP.mult, OP.add, reads=[kx, "l_xf" + sfx, "l_VL"], writes=["l_xf" + sfx])
                k.ts(xf[:, :n], xf[:, :n], VL[:, VL_CB + c:VL_CB + c + 1], OP.add, reads=["l_xf" + sfx, "l_VL"], writes=["l_xf" + sfx])
                ps, pk = k.ps()
                k.mm(ps[:, :n], GW[:, c, :], xf[:, :n], True, True, reads=["l_GW", "l_xf" + sfx], writes=[pk])
                k.act(gx[:, :n], ps[:, :n], AF.Sigmoid, reads=[pk, "l_VL"], writes=["l_gx" + sfx], bias=VL[:, VL_GXB + c:VL_GXB + c + 1])
                ps, pk = k.ps()
                k.mm(ps[:, :n], GW[:, 8 + c, :], xf[:, :n], True, True, reads=["l_GW", "l_xf" + sfx], writes=[pk])
                k.act(ga[:, :n], ps[:, :n], AF.Sigmoid, reads=[pk, "l_VL"], writes=["l_ga" + sfx], bias=VL[:, VL_GAB + c:VL_GAB + c + 1])
                k.act(av[:, :n], ga[:, :n], AF.Exp, reads=["l_ga" + sfx, "l_sp8"], writes=["l_a" + sfx], scale=SPm8[:, c:c + 1])
                k.act(uv[:, :n], ga[:, :n], AF.Exp, reads=["l_ga" + sfx, "l_sp16"], writes=["l_u" + sfx], scale=SPm16[:, c:c + 1])
                k.act(uv[:, :n], uv[:, :n], AF.Sqrt, reads=["l_u" + sfx], writes=["l_u" + sfx], scale=-1.0, bias=1.0)
                k.tt(gx[:, :n], gx[:, :n], xf[:, :n], OP.mult, reads=["l_gx" + sfx, "l_xf" + sfx], writes=["l_gx" + sfx])
                k.tt(uv[:, :n], uv[:, :n], gx[:, :n], OP.mult, reads=["l_u" + sfx, "l_gx" + sfx], writes=["l_u" + sfx])
                k.op("dve", lambda hh, c=c, n=n: hh.tensor_tensor_scan(out=hs[c][:, :n], data0=av[:, :n], data1=uv[:, :n], initial=carry[c][:, 0:1], op0=OP.mult, op1=OP.add),
                     reads=["l_a" + sfx, "l_u" + sfx, "l_cy%d" % c], writes=["l_hs%d" % c])
                k.copy(carry[c][:], hs[c][:, n - 1:n], reads=["l_hs%d" % c], writes=["l_cy%d" % c], eng="pool")
                y, ky = yb[it % 2], "l_yb%d" % (it % 2)
                k.tt(y[:, :n], hs[c][:, :n], g[:, :n], OP.mult, reads=["l_hs%d" % c, kg], writes=[ky])
                k.dma("pool", d["Y1T"][c * 128:(c + 1) * 128, t0:t0 + n], y[:, :n], reads=[ky], writes=[("Y1T", 0)])

            for c in range(0, 8, 2):
                k.interleave([lambda c=c: body(c, 0), lambda c=c: body(c + 1, 1)], [1, 1])
    k.pes = None
    k.barrier()


def pack_rhs_k(w):
    K = w.shape[0]
    return np.ascontiguousarray(w.reshape(K // 128, 128, w.shape[1]).transpose(1, 0, 2)).reshape(128, -1)


def host_inputs_weights(inp):
    f = np.float32
    im = {}
    CM, VT, VF = host_consts(inp)
    im.update(CMd=CM, VTd=VT, VFd=VF)
    w_in = inp["ev_w_in"][0]
    im["WINF"] = pack_lhsT(w_in, fcols())
    im["WINZ"] = pack_rhs(w_in, zcols())
    im.update(host_consts_rwkv(inp))
    im["VT2d"] = np.concatenate([inp[n][0] for n in ("ev_ln1_g", "ev_ln1_b", "ev_ln2_g", "ev_ln2_b", "od_ln1_g", "od_ln1_b", "od_ln2_g", "od_ln2_b")])[None, :].astype(f)
    im["WO"] = pack_rhs_k(inp["ev_w_out"][0])
    im["WGU0"] = pack_lhsT(inp["ev_ffn_w_gu"][0], np.arange(5632))
    im["WD0"] = pack_rhs_k(inp["ev_ffn_w_down"][0])
    im["W1IN"] = pack_lhsT(inp["od_w_in"][0], np.arange(2048))
    gw = np.concatenate([inp["od_gx_w"][0], inp["od_ga_w"][0]], 0)
    im["GWd"] = np.ascontiguousarray(gw.transpose(1, 0, 2)).reshape(128, 16 * 128).astype(f)
    VL = np.zeros((128, NVL), f)
    VL[:, 0:32] = inp["od_conv_w"][0].reshape(4, 8, 128).transpose(2, 1, 0).reshape(128, 32)
    for off, n in ((32, "od_conv_b"), (40, "od_gx_b"), (48, "od_ga_b"), (56, "od_lambda")):
        VL[:, off:off + 8] = inp[n][0].reshape(8, 128).T
    im["VLd"] = VL
    im["W1O"] = pack_rhs_k(inp["od_w_out"][0])
    im["WRd"] = np.ascontiguousarray(inp["od_router"][0].reshape(8, 128, 8).transpose(1, 0, 2)).reshape(128, 64).astype(f)
    for e in range(NEXP):
        im["WGUE%d" % e] = pack_lhsT(inp["od_exp_w_gu"][0, e], np.arange(5632))
        im["WDE%d" % e] = pack_rhs_k(inp["od_exp_w_down"][0, e])
    return im


_CACHE = {}


def kernel(**inputs):
    inp = {k_: np.asarray(v) for k_, v in inputs.items()}
    x = inp["x"]
    B, S, _ = x.shape
    L = S + NMETA
    LP = ((L + 127) // 128) * 128
    if LP not in _CACHE:
        _CACHE[LP] = build(LP)
    P = _CACHE[LP]
    wim = host_inputs_weights(inp)
    in_maps = []
    for b in range(B):
        xin = np.zeros((LP, D), np.float32)
        xin[:NMETA] = inp["meta"]
        xin[NMETA:L] = x[b]
        m = dict(wim)
        m["xin"] = xin
        in_maps.append(m)
    res = run_bass_kernel_spmd(P.nc, in_maps, core_ids=list(range(B)))
    out = np.stack([np.asarray(r["OUT"])[NMETA:L] for r in res.results], 0)
    return out.astype(np.float32)
```

```python
from contextlib import ExitStack
import concourse.bass as bass
import concourse.mybir as mybir

F32 = mybir.dt.float32
BF16 = mybir.dt.bfloat16
AF = mybir.ActivationFunctionType
OP = mybir.AluOpType
ENGS = ["pe", "dve", "act", "pool", "sp"]
NDMA = 40


class KB:
    def __init__(self, nc, es: ExitStack):
        self.nc = nc
        self.es = es
        self.ops = {e: [] for e in ENGS}
        self.cnt = {e: 0 for e in ENGS}
        self.waited = {e: {} for e in ENGS}
        self.last_w = {}
        self.readers = {}
        self.sem = {e: es.enter_context(nc.semaphore("s_" + e)) for e in ENGS}
        self.dsem = [es.enter_context(nc.semaphore("d%d" % i)) for i in range(NDMA)]
        self.dval = [0] * NDMA
        self.dnext = 0
        self.semid = {}
        for e in ENGS:
            self.semid[("e", e)] = self.sem[e]
        for i in range(NDMA):
            self.semid[("d", i)] = self.dsem[i]
        self.ntile = 0
        self.psb = [self.psum_t("psb%d" % i, [128, 512], F32) for i in range(8)]
        self.psi = 0
        self.final_tokens = []

    def sb(self, name, shape, dt=F32):
        self.ntile += 1
        es = self.pes if getattr(self, "pes", None) is not None else self.es
        return es.enter_context(self.nc.sbuf_tensor(name, list(shape), dt))

    def barrier(self):
        allw = []
        for i in range(NDMA):
            if self.dval[i] > 0:
                allw.append((("d", i), self.dval[i]))
        for e in ENGS:
            if self.cnt[e] > 0:
                allw.append((("e", e), self.cnt[e]))
        semid = self.semid
        for e in ENGS:
            waits = self._waits(e, allw, skip_self=True)

            def run(h, waits=waits):
                for s, v in waits:
                    h.wait_ge(semid[s], v)

            self.ops[e].append(run)

    def psum_t(self, name, shape, dt=F32):
        return self.es.enter_context(self.nc.psum_tensor(name, list(shape), dt))

    def ps(self):
        i = self.psi
        self.psi = (self.psi + 1) % 7
        return self.psb[i], "psb%d" % i

    def _deps(self, reads, writes):
        deps = []
        for k in reads:
            if k in self.last_w:
                deps.append(self.last_w[k])
            if isinstance(k, str) and k.startswith("psb"):
                deps.extend(self.readers.get(k, {}).values())
        for k in writes:
            if k in self.last_w:
                deps.append(self.last_w[k])
            deps.extend(self.readers.get(k, {}).values())
        return deps

    def _waits(self, eng, deps, skip_self=False):
        w = {}
        for (s, v) in deps:
            if skip_self and s == ("e", eng):
                continue
            if self.waited[eng].get(s, 0) < v and w.get(s, 0) < v:
                w[s] = v
        for s, v in w.items():
            self.waited[eng][s] = v
        return list(w.items())

    def _commit(self, tok, reads, writes):
        for k in writes:
            self.last_w[k] = tok
            self.readers[k] = {}
        for k in reads:
            if k in writes:
                continue
            r = self.readers.setdefault(k, {})
            if r.get(tok[0], (None, 0))[1] < tok[1]:
                r[tok[0]] = tok

    def op(self, eng, fn, reads=(), writes=()):
        deps = self._deps(reads, writes)
        waits = self._waits(eng, deps, skip_self=(eng == "pe"))
        self.cnt[eng] += 1
        tok = (("e", eng), self.cnt[eng])
        semid = self.semid
        mysem = self.sem[eng]

        def run(h):
            for s, v in waits:
                h.wait_ge(semid[s], v)
            fn(h).then_inc(mysem, 1)

        self.ops[eng].append(run)
        self._commit(tok, reads, writes)
        self._tick()
        return tok

    def interleave(self, fns, weights):
        import threading
        n = len(fns)
        if n == 1:
            fns[0]()
            return
        st = {"sems": [threading.Semaphore(0) for _ in range(n)], "alive": [True] * n, "cur": 0, "cnt": 0, "err": None,
              "main": threading.Semaphore(0), "w": weights}
        self._il = st

        def body(i):
            st["sems"][i].acquire()
            try:
                fns[i]()
            except BaseException as e:
                st["err"] = e
            st["alive"][i] = False
            m = None
            for dd in range(1, n + 1):
                q = (i + dd) % n
                if st["alive"][q]:
                    m = q
                    break
            if m is None:
                st["main"].release()
            else:
                st["cur"] = m
                st["cnt"] = 0
                st["sems"][m].release()

        ths = [threading.Thread(target=body, args=(i,)) for i in range(n)]
        for t in ths:
            t.start()
        st["sems"][0].release()
        st["main"].acquire()
        for t in ths:
            t.join()
        self._il = None
        if st["err"] is not None:
            raise st["err"]

    def _tick(self):
        st = getattr(self, "_il", None)
        if st is None:
            return
        i = st["cur"]
        st["cnt"] += 1
        if st["cnt"] >= st["w"][i]:
            n = len(st["alive"])
            m = None
            for dd in range(1, n):
                q = (i + dd) % n
                if st["alive"][q]:
                    m = q
                    break
            st["cnt"] = 0
            if m is None:
                return
            st["cur"] = m
            st["sems"][m].release()
            st["sems"][i].acquire()

    def dma(self, q, out, in_, reads=(), writes=(), **kw):
        slot = self.dnext
        self.dnext = (self.dnext + 1) % NDMA
        deps = self._deps(reads, writes)
        if self.dval[slot] > 0:
            deps.append((("d", slot), self.dval[slot]))
        waits = self._waits(q, deps)
        self.dval[slot] += 16
        tok = (("d", slot), self.dval[slot])
        semid = self.semid
        ds = self.dsem[slot]

        def run(h):
            for s, v in waits:
                h.wait_ge(semid[s], v)
            h.dma_start(out=out, in_=in_, **kw).then_inc(ds, 16)

        self.ops[q].append(run)
        self._commit(tok, reads, writes)
        self._tick()
        return tok

    def finish(self, eng="sp"):
        waits = []
        for i in range(NDMA):
            if self.dval[i] > 0:
                waits.append((("d", i), self.dval[i]))
        for e in ENGS:
            if self.cnt[e] > 0:
                waits.append((("e", e), self.cnt[e]))
        semid = self.semid

        def run(h):
            for s, v in waits:
                h.wait_ge(semid[s], v)

        self.ops[eng].append(run)

    def emit(self):
        nc = self.nc
        with nc.Block() as block:
            @block.tensor
            def _(h):
                for f in self.ops["pe"]:
                    f(h)

            @block.vector
            def _(h):
                for f in self.ops["dve"]:
                    f(h)

            @block.scalar
            def _(h):
                for f in self.ops["act"]:
                    f(h)

            @block.gpsimd
            def _(h):
                for f in self.ops["pool"]:
                    f(h)

            @block.sync
            def _(h):
                for f in self.ops["sp"]:
                    f(h)

    def mm(self, out, lhsT, rhs, start, stop, reads, writes):
        return self.op("pe", lambda h: h.matmul(out, lhsT, rhs, start=start, stop=stop), reads, writes)

    def tr(self, out, in_, ident, reads, writes):
        return self.op("pe", lambda h: h.transpose(out, in_, ident), reads, writes)

    def act(self, out, in_, func, reads, writes, bias=None, scale=None, eng="act"):
        kw = {}
        if bias is not None:
            kw["bias"] = bias
        if scale is not None:
            kw["scale"] = scale
        return self.op("act", lambda h: h.activation(out=out, in_=in_, func=func, **kw), reads, writes)

    def tt(self, out, in0, in1, op, reads, writes, eng="dve"):
        return self.op(eng, lambda h: h.tensor_tensor(out=out, in0=in0, in1=in1, op=op), reads, writes)

    def ts(self, out, in0, s1, op0, reads, writes, s2=None, op1=None, eng="dve"):
        if op1 is None:
            return self.op(eng, lambda h: h.tensor_scalar(out=out, in0=in0, scalar1=s1, scalar2=None, op0=op0), reads, writes)
        return self.op(eng, lambda h: h.tensor_scalar(out=out, in0=in0, scalar1=s1, scalar2=s2, op0=op0, op1=op1), reads, writes)

    def stt(self, out, in0, scalar, in1, op0, op1, reads, writes):
        return self.op("dve", lambda h: h.scalar_tensor_tensor(out=out, in0=in0, scalar=scalar, in1=in1, op0=op0, op1=op1), reads, writes)

    def copy(self, out, in_, reads, writes, eng="dve"):
        if eng == "act":
            return self.op("act", lambda h: h.copy(out=out, in_=in_), reads, writes)
        return self.op(eng, lambda h: h.tensor_copy(out=out, in_=in_), reads, writes)

    def memset(self, ap, val, writes, eng="dve"):
        return self.op(eng, lambda h: h.memset(ap, val), (), writes)
import numpy as np
from contextlib import ExitStack
import concourse.bass as bass
import concourse.mybir as mybir
from concourse.bass_utils import run_bass_kernel_spmd

D = 1024
NMETA = 16
DFF = 2816
NEXP = 8
ALPHA = 4 ** 0.25
LN_EPS = 1e-5
RWKV_EPS = 64e-5

C_XS, C_B, C_C, C_R, C_K, C_V, C_LO, C_G = 0, 8, 10, 12, 20, 28, 36, 37
NFC = 38


def supertiles(LP):
    out = []
    t = 0
    while t < LP:
        n = min(512, LP - t)
        out.append((t, n))
        t += n
    return out


class Prog:
    def pump_casts(self, n):
        for _ in range(n):
            if self.pending:
                self.pending.pop(0)()

    def __init__(self, LP, dbg=()):
        self.pending = []
        self.LP = LP
        self.dbg = set(dbg)
        self.nc = bass.Bass("TRN2", target_bir_lowering=False)
        self.es = ExitStack()
        self.k = None
        self.dram = {}

    def din(self, name, shape, dt=F32):
        t = self.nc.dram_tensor(name, list(shape), dt, kind="ExternalInput").ap()
        self.dram[name] = t
        return t

    def dscr(self, name, shape, dt=F32):
        kind = "ExternalOutput" if name in self.dbg else "Internal"
        t = self.nc.dram_tensor(name, list(shape), dt, kind=kind).ap()
        self.dram[name] = t
        return t

    def dout(self, name, shape, dt=F32):
        t = self.nc.dram_tensor(name, list(shape), dt, kind="ExternalOutput").ap()
        self.dram[name] = t
        return t


def cast_weights(P, src, dst, nrows, key, defer=False):
    k = P.k
    step = 512
    for r0 in range(0, nrows, step):
        r1 = min(nrows, r0 + step)

        def go(r0=r0, r1=r1):
            k.dma("pool", dst[r0:r1, :], src[r0:r1, :], reads=(), writes=((key, r0 // step),))

        if defer:
            P.pending.append(go)
        else:
            go()


def phase1(P):
    k, LP = P.k, P.LP
    d = P.dram
    xt = [k.sb("xt%d" % i, [128, 1024]) for i in range(4)]
    hT = k.sb("hT", [128, 8, 512], BF16)
    wf = [k.sb("wf%d" % i, [128, 8, 128], BF16) for i in range(3)]
    wz = k.sb("wz", [128, 8, 1040], BF16)
    stg = [k.sb("stg%d" % i, [128, 512]) for i in range(3)]
    zst = [k.sb("zst%d" % i, [128, 1040]) for i in range(2)]
    ident = P.ident
    k.dma("sp", wz[:].rearrange("p a b -> p (a b)"), d["WINZb"][:, :], reads=[("WINZb", i) for i in range(1)], writes=["wz"])
    wi = 0
    si = 0
    zi = 0
    for (t0, n) in supertiles(LP):
        nj = n // 128
        for j in range(nj):
            k.dma("sp", xt[j][:], d["xin"][t0 + j * 128:t0 + (j + 1) * 128, :], reads=[], writes=["xt%d" % j])
        for kc in range(8):
            ps, pk = k.ps()
            for j in range(nj):
                k.tr(ps[:, j * 128:(j + 1) * 128], xt[j][:, kc * 128:(kc + 1) * 128], ident[:], reads=["xt%d" % j, "ident"], writes=[pk])
            k.copy(hT[:, kc, :n], ps[:, :n], reads=[pk], writes=[("hT", kc)], eng=("act" if kc % 2 else "dve"))
        for c in range(NFC):
            w = wf[wi % 3]
            wk = "wf%d" % (wi % 3)
            wi += 1
            k.dma("sp", w[:].rearrange("p a b -> p (a b)"), d["WINFb"][c * 128:(c + 1) * 128, :], reads=[("WINFb", (c * 128) // 512)], writes=[wk])
            ps, pk = k.ps()
            for kc in range(8):
                k.mm(ps[:, :n], w[:, kc, :], hT[:, kc, :n], kc == 0, kc == 7, reads=[wk, ("hT", kc)], writes=[pk])
            s = stg[si % 3]
            sk = "stg%d" % (si % 3)
            si += 1
            k.copy(s[:, :n], ps[:, :n], reads=[pk], writes=[sk], eng=("act" if c % 2 else "dve"))
            k.dma("pool", d["PT"][c * 128:(c + 1) * 128, t0:t0 + n], s[:, :n], reads=[sk], writes=[("PT", c)])
        for j in range(nj):
            z = zst[zi % 2]
            zk = "zst%d" % (zi % 2)
            zi += 1
            for (c0, c1) in ((0, 512), (512, 1024), (1024, 1040)):
                ps, pk = k.ps()
                for kc in range(8):
                    k.mm(ps[:, :c1 - c0], hT[:, kc, j * 128:(j + 1) * 128], wz[:, kc, c0:c1], kc == 0, kc == 7,
                         reads=["wz", ("hT", kc)], writes=[pk])
                k.copy(z[:, c0:c1], ps[:, :c1 - c0], reads=[pk], writes=[zk], eng=("act" if c0 == 512 else "dve"))
            k.dma("pool", d["Z"][t0 + j * 128:t0 + (j + 1) * 128, :], z[:], reads=[zk], writes=[("Z", 0)])


import os
LVL = int(os.environ.get('LVL', '99'))
R3 = int(os.environ.get('R3', '99'))
R3C = int(os.environ.get('R3C', '99'))
R3L = int(os.environ.get('R3L', '99'))
NVT = 32 + 2048
NVF = 64


def build(LP, dbg=(), upto=99):
    P = Prog(LP, dbg)
    nc = P.nc
    P.din("xin", [LP, D])
    P.din("CMd", [128, 640])
    P.din("VTd", [1, NVT])
    P.din("VFd", [128, NVF])
    P.din("WINF", [NFC * 128, 1024])
    P.din("WINZ", [128, 8 * 1040])
    P.din("CRd", [128, NCR])
    P.din("VRd", [128, NVR])
    P.din("WUAd", [128, 1024])
    P.din("GUPd", [128, 1024])
    P.din("LNGd", [128, 512])
    P.din("LNBd", [128, 512])
    P.din("VT2d", [1, 8192])
    P.din("WO", [128, 16 * 1024])
    P.din("WGU0", [44 * 128, 1024])
    P.din("WD0", [128, 22 * 1024])
    P.din("W1IN", [16 * 128, 1024])
    P.din("GWd", [128, 16 * 128])
    P.din("VLd", [128, NVL])
    P.din("W1O", [128, 8 * 1024])
    P.din("WRd", [128, 64])
    for e in range(NEXP):
        P.din("WGUE%d" % e, [44 * 128, 1024])
        P.din("WDE%d" % e, [128, 22 * 1024])
    P.dscr("WOb", [128, 16 * 1024], BF16)
    P.dscr("WGU0b", [44 * 128, 1024], BF16)
    P.dscr("WD0b", [128, 22 * 1024], BF16)
    P.dscr("W1INb", [16 * 128, 1024], BF16)
    P.dscr("W1Ob", [128, 8 * 1024], BF16)
    for e in range(NEXP):
        P.dscr("WGUEb%d" % e, [44 * 128, 1024], BF16)
        P.dscr("WDEb%d" % e, [128, 22 * 1024], BF16)
    P.dscr("H1", [LP, D])
    P.dscr("H1T", [D, LP], BF16)
    P.dscr("H2", [LP, D])
    P.dscr("H2T", [D, LP], BF16)
    P.dscr("XRT", [D, LP])
    P.dscr("Y1T", [D, LP], BF16)
    P.dscr("H3", [LP, D])
    P.dscr("H3T", [D, LP], BF16)
    P.dscr("H3T32", [D, LP])
    P.dout("OUT", [LP, D])
    P.dscr("WINFb", [NFC * 128, 1024], BF16)
    P.dscr("WINZb", [128, 8 * 1040], BF16)
    P.dscr("PT", [NFC * 128, LP])
    P.dscr("Z", [LP, 1040])
    P.dscr("XS", [LP, 1024])
    P.dscr("BTOK", [LP, 256])
    P.dscr("BT", [256, LP])
    P.dscr("CT", [256, LP])
    P.dscr("YT", [2048, LP], BF16)
    with P.es:
        k = KB(nc, P.es)
        P.k = k
        P.CM = k.sb("CM", [128, 640])
        P.ident = P.CM[:, 0:128]
        P.VT = k.sb("VT", [128, NVT])
        P.VF = k.sb("VF", [128, NVF])
        k.dma("sp", P.CM[:], P.dram["CMd"][:, :], writes=["CM", "ident"])
        k.dma("sp", P.VT[:], P.dram["VTd"][0:1, :].partition_broadcast(128), writes=["VT"])
        k.dma("sp", P.VF[:], P.dram["VFd"][:, :], writes=["VF"])
        cast_weights(P, P.dram["WINF"], P.dram["WINFb"], NFC * 128, "WINFb")
        k.dma("pool", P.dram["WINZb"][:, :].rearrange("p (a b) -> p a b", b=1040), P.dram["WINZ"][:, :].rearrange("p (a b) -> p a b", b=1040), writes=[("WINZb", 0)])
        if upto >= 4:
            def cast_rhs(src, dst, nk):
                P.pending.append(lambda: k.dma("pool", P.dram[dst][:, :].rearrange("p (a b) -> p a b", b=1024), P.dram[src][:, :].rearrange("p (a b) -> p a b", b=1024), writes=[(dst, 0)]))
            cast_rhs("WO", "WOb", 16)
            cast_weights(P, P.dram["WGU0"], P.dram["WGU0b"], 44 * 128, "WGU0b", defer=True)
            cast_rhs("WD0", "WD0b", 22)
            cast_weights(P, P.dram["W1IN"], P.dram["W1INb"], 16 * 128, "W1INb", defer=True)
            cast_rhs("W1O", "W1Ob", 8)
            for e in range(NEXP):
                cast_weights(P, P.dram["WGUE%d" % e], P.dram["WGUEb%d" % e], 44 * 128, "WGUEb%d" % e, defer=True)
                cast_rhs("WDE%d" % e, "WDEb%d" % e, 22)
        if upto >= 1:
            with ExitStack() as pes:
                k.pes = pes
                phase1(P)
            k.pes = None
            k.barrier()
        if upto >= 1.5:
            phase2a(P)
        if upto >= 2 and upto != 3.5:
            phase2b(P)
        if upto >= 3:
            phase3(P)
        if upto >= 4:
            P.pump_casts(100000)
            proj_res_ln(P, "p4", "YT", 16, "WOb", "xin", 0, 1024, "H1", "H1T")
            ffn_phase(P, "f0", "H1T", "H1", ["WGU0b"], ["WD0b"], 2048, 3072, "H2", "H2T")
        if upto >= 5:
            phase5(P)
            proj_res_ln(P, "p5", "Y1T", 8, "W1Ob", "H2", 4096, 5120, "H3", "H3T", "H3T32")
        if upto >= 6:
            ffn_phase(P, "f1", "H3T", "H3", ["WGUEb%d" % e for e in range(NEXP)], ["WDEb%d" % e for e in range(NEXP)], 6144, 7168, "OUT", None, router="H3T32", tile_n=1024)
        k.finish("sp")
        k.emit()
    return P


def host_consts(inp):
    t = np.arange(128)
    CM = np.zeros((128, 640), np.float32)
    CM[:, 0:128] = np.eye(128)
    CM[:, 128:256] = (t[:, None] <= t[None, :])
    CM[:, 256:384] = (t[:, None] > t[None, :])
    CM[:, 384:512] = 1.0
    CM[:, 512:640] = np.where(t[None, :] >= t[:, None], 0.0, -30000.0)
    VT = np.zeros((1, NVT), np.float32)
    VT[0, 0:16] = inp["ev_dt_bias"][0]
    VT[0, 16:32] = inp["ev_a_log"][0]
    VT[0, 32:32 + 1024] = np.repeat(inp["ev_d_skip"][0], 64)
    VT[0, 32 + 1024:32 + 2048] = inp["ev_ssm_norm"][0]
    VF = np.zeros((128, NVF), np.float32)
    cw = inp["ev_conv_w"][0]
    VF[:, 0:48] = cw.reshape(4, 12, 128).transpose(2, 1, 0).reshape(128, 48)
    VF[:, 48:60] = inp["ev_conv_b"][0].reshape(12, 128).T
    return CM, VT, VF


def fcols():
    return np.concatenate([np.arange(1024, 2048), np.arange(2048, 2304), np.arange(2304, 2560),
                           np.arange(2576, 3600), np.arange(3600, 4624), np.arange(4624, 5648),
                           np.arange(5648, 5776), np.arange(5776, 5904)])


def zcols():
    return np.concatenate([np.arange(0, 1024), np.arange(2560, 2576)])


def pack_lhsT(w, cols):
    ws = w[:, cols]
    nch = ws.shape[1] // 128
    a = ws.reshape(8, 128, nch, 128)
    return np.ascontiguousarray(a.transpose(2, 1, 0, 3)).reshape(nch * 128, 1024)


def pack_rhs(w, cols):
    ws = w[:, cols]
    n = ws.shape[1]
    return np.ascontiguousarray(ws.reshape(8, 128, n).transpose(1, 0, 2)).reshape(128, 8 * n)


CM_ID, CM_TRI, CM_TRIS, CM_ONES, CM_MB = 0, 128, 256, 384, 512
VT_DTB, VT_ALOG, VT_D, VT_NW = 0, 16, 32, 32 + 1024
VF_CW, VF_CB = 0, 48


def phase2a(P):
    k, LP, d = P.k, P.LP, P.dram
    P.pump_casts(19)
    with ExitStack() as pes:
        k.pes = pes
        raw = [k.sb("c_raw%d" % i, [128, LP + 3]) for i in range(2)]
        acc = [k.sb("c_acc%d" % i, [128, LP]) for i in range(2)]
        stg = [k.sb("c_stg%d" % i, [128, 512]) for i in range(3)]
        VF = P.VF
        for i in range(2):
            k.memset(raw[i][:, 0:3], 0.0, writes=["c_raw%d" % i])
        si = 0
        for c in range(12):
            r, rk = raw[c % 2], "c_raw%d" % (c % 2)
            a, ak = acc[c % 2], "c_acc%d" % (c % 2)
            k.dma("sp", r[:, 3:3 + LP], d["PT"][c * 128:(c + 1) * 128, :], reads=[("PT", c)], writes=[rk])
            cw = VF_CW + c * 4
            k.ts(a[:], r[:, 3:3 + LP], VF[:, cw + 3:cw + 4], OP.mult, reads=[rk, "VF"], writes=[ak])
            for kk in (2, 1, 0):
                k.stt(a[:], r[:, kk:kk + LP], VF[:, cw + kk:cw + kk + 1], a[:], OP.mult, OP.add, reads=[rk, ak, "VF"], writes=[ak])
            k.act(a[:], a[:], AF.Silu, reads=[ak, "VF"], writes=[ak], bias=VF[:, VF_CB + c:VF_CB + c + 1])
            if c >= 8:
                dst = d["BT"] if c < 10 else d["CT"]
                cc = (c - 8) % 2
                k.dma("pool", dst[cc * 128:(cc + 1) * 128, :], a[:], reads=[ak], writes=[("BT" if c < 10 else "CT", cc)])
            if c < 10:
                for (t0, n) in supertiles(LP):
                    nj = n // 128
                    ps, pk = k.ps()
                    for j in range(nj):
                        k.tr(ps[:, j * 128:(j + 1) * 128], a[:, t0 + j * 128:t0 + (j + 1) * 128], P.CM[:, CM_ID:CM_ID + 128], reads=[ak, "CM"], writes=[pk])
                    s, sk = stg[si % 3], "c_stg%d" % (si % 3)
                    si += 1
                    k.copy(s[:, :n], ps[:, :n], reads=[pk], writes=[sk], eng=("act" if si % 2 else "dve"))
                    if c < 8:
                        dd = d["XS"][t0:t0 + n, c * 128:(c + 1) * 128]
                        key = ("XS", c)
                    else:
                        dd = d["BTOK"][t0:t0 + n, (c - 8) * 128:(c - 7) * 128]
                        key = ("BTOK", c - 8)
                    k.dma("pool", dd.rearrange("(j p) m -> p j m", p=128), s[:, :n].rearrange("p (j m) -> p j m", m=128), reads=[sk], writes=[key])
    k.pes = None
    k.barrier()


def phase2b(P):
    k, LP, d = P.k, P.LP, P.dram
    CM, VT = P.CM, P.VT
    with ExitStack() as pes:
        k.pes = pes
        nb = 2
        xs = [k.sb("s_xs%d" % i, [128, 1024]) for i in range(nb)]
        btok = [k.sb("s_btok%d" % i, [128, 256]) for i in range(nb)]
        bt = [k.sb("s_bt%d" % i, [128, 2, 128]) for i in range(nb)]
        ct = [k.sb("s_ct%d" % i, [128, 2, 128]) for i in range(nb)]
        z = [k.sb("s_z%d" % i, [128, 1040]) for i in range(nb)]
        HT = [k.sb("s_HT%d" % g, [128, 512]) for g in range(2)]
        Abc = k.sb("s_Abc", [128, 16])
        dt = k.sb("s_dt", [128, 16])
        dtA = k.sb("s_dtA", [128, 16])
        E = k.sb("s_E", [128, 64])
        ncum = E[:, 48:64]
        LT = k.sb("s_LT", [128, 16, 128])
        SLT = k.sb("s_SLT", [128, 16, 128])
        xdt = k.sb("s_xdt", [128, 1024])
        xde = k.sb("s_xde", [128, 1024])
        y = k.sb("s_y", [128, 1024])
        t1 = k.sb("s_t1", [128, 1024])
        zs = k.sb("s_zs", [128, 1024])
        sq = k.sb("s_sq", [128, 512])
        ss = k.sb("s_ss", [128, 2])
        rstd = k.sb("s_rstd", [128, 2])
        yTs = [k.sb("s_yT%d" % i, [128, 8, 128], BF16) for i in range(2)]
        for g in range(2):
            k.memset(HT[g][:], 0.0, writes=["s_HT%d" % g])
        k.act(Abc[:], VT[:, VT_ALOG:VT_ALOG + 16], AF.Exp, reads=["VT"], writes=["s_Abc"])
        k.ts(Abc[:], Abc[:], -1.0, OP.mult, reads=["s_Abc"], writes=["s_Abc"])
        nch = LP // 128
        for ci in range(nch):
            t0 = ci * 128
            b = ci % nb
            kx, kb, kbt, kct, kz = "s_xs%d" % b, "s_btok%d" % b, "s_bt%d" % b, "s_ct%d" % b, "s_z%d" % b
            k.dma("sp", xs[b][:], d["XS"][t0:t0 + 128, :], reads=[("XS", c) for c in range(8)], writes=[kx])
            k.dma("sp", btok[b][:], d["BTOK"][t0:t0 + 128, :], reads=[("BTOK", 0), ("BTOK", 1)], writes=[kb])
            k.dma("sp", bt[b][:], d["BT"][:, t0:t0 + 128].rearrange("(g n) t -> n g t", n=128), reads=[("BT", 0), ("BT", 1)], writes=[kbt])
            k.dma("sp", ct[b][:], d["CT"][:, t0:t0 + 128].rearrange("(g n) t -> n g t", n=128), reads=[("CT", 0), ("CT", 1)], writes=[kct])
            k.dma("sp", z[b][:], d["Z"][t0:t0 + 128, :], reads=[("Z", 0)], writes=[kz])
            if LVL < 1: continue
            k.tt(dt[:], z[b][:, 1024:1040], VT[:, VT_DTB:VT_DTB + 16], OP.add, reads=[kz, "VT"], writes=["s_dt"])
            k.act(dt[:], dt[:], AF.Exp, reads=["s_dt"], writes=["s_dt"])
            k.act(dt[:], dt[:], AF.Ln, reads=["s_dt"], writes=["s_dt"], bias=1.0)
            k.tt(dtA[:], dt[:], Abc[:], OP.mult, reads=["s_dt", "s_Abc"], writes=["s_dtA"])
            if LVL < 2: continue
            psc, pkc = k.ps()
            k.mm(psc[:, 0:16], CM[:, CM_TRI:CM_TRI + 128], dtA[:], True, True, reads=["CM", "s_dtA"], writes=[pkc])
            k.mm(psc[:, 16:32], CM[:, CM_TRIS:CM_TRIS + 128], dtA[:], True, True, reads=["CM", "s_dtA"], writes=[pkc])
            k.mm(psc[:, 32:48], CM[:, CM_ONES:CM_ONES + 128], dtA[:], True, True, reads=["CM", "s_dtA"], writes=[pkc])
            SUB = int(os.environ.get('SUB', '3'))
            if SUB >= 1:
                k.act(E[:, 0:48], psc[:, 0:48], AF.Exp, reads=[pkc], writes=["s_E"])
            if SUB >= 2:
                k.ts(ncum, psc[:, 0:16], -1.0, OP.mult, reads=[pkc, "s_E"], writes=["s_ncum"])
            if LVL < 3: continue
            k.tt(xdt[:].rearrange("p (h e) -> p h e", e=64), xs[b][:].rearrange("p (h e) -> p h e", e=64),
                 dt[:].unsqueeze(2).broadcast_to([128, 16, 64]), OP.mult, reads=[kx, "s_dt"], writes=["s_xdt"])
            k.tt(xde[:].rearrange("p (h e) -> p h e", e=64), xdt[:].rearrange("p (h e) -> p h e", e=64),
                 E[:, 16:32].unsqueeze(2).broadcast_to([128, 16, 64]), OP.mult, reads=["s_xdt", "s_E"], writes=["s_xde"])
            if LVL < 4: continue
            for hq in range(4):
                psa, pka = k.ps()
                for hh in range(4):
                    h = hq * 4 + hh
                    k.mm(psa[:, hh * 128:(hh + 1) * 128], dtA[:, h:h + 1].broadcast_to([128, 128]), CM[:, CM_TRI:CM_TRI + 128], True, True,
                         reads=["s_dtA", "CM"], writes=[pka])
                for hh in range(4):
                    h = hq * 4 + hh
                    k.stt(LT[:, h, :], psa[:, hh * 128:(hh + 1) * 128], E[:, 48 + h:49 + h], CM[:, CM_MB:CM_MB + 128], OP.add, OP.add,
                          reads=[pka, "s_ncum", "CM"], writes=[("s_LT", hq)])
            for hq in range(4):
                k.act(LT[:, hq * 4:(hq + 1) * 4, :], LT[:, hq * 4:(hq + 1) * 4, :], AF.Exp, reads=[("s_LT", hq)], writes=[("s_LT", hq)])
            if LVL < 5: continue
            pss, pks = k.ps()
            for g in range(2):
                k.mm(pss[:, g * 128:(g + 1) * 128], bt[b][:, g, :], ct[b][:, g, :], True, True, reads=[kbt, kct], writes=[pks])
            for g in range(2):
                k.tt(SLT[:, g * 8:(g + 1) * 8, :], LT[:, g * 8:(g + 1) * 8, :],
                     pss[:, g * 128:(g + 1) * 128].unsqueeze(1).broadcast_to([128, 8, 128]), OP.mult,
                     reads=[("s_LT", 2 * g), ("s_LT", 2 * g + 1), pks], writes=[("s_SLT", g)])
            if LVL < 6: continue
            for g in range(2):
                psy, pky = k.ps()
                for hh in range(8):
                    h = g * 8 + hh
                    k.mm(psy[:, hh * 64:(hh + 1) * 64], SLT[:, h, :], xdt[:, h * 64:(h + 1) * 64], True, True, reads=[("s_SLT", g), "s_xdt"], writes=[pky])
                if R3L < 4: continue
                pso, pko = k.ps()
                k.mm(pso[:, :], ct[b][:, g, :], HT[g][:], True, True, reads=[kct, "s_HT%d" % g], writes=[pko])
                gs = slice(g * 512, (g + 1) * 512)
                k.tt(t1[:, gs].rearrange("p (h e) -> p h e", e=64), pso[:, :].rearrange("p (h e) -> p h e", e=64),
                     E[:, g * 8:(g + 1) * 8].unsqueeze(2).broadcast_to([128, 8, 64]), OP.mult, reads=[pko, "s_E"], writes=[("s_t1", g)])
                k.tt(y[:, gs], psy[:, :], t1[:, gs], OP.add, reads=[pky, ("s_t1", g)], writes=[("s_y", g)])
                psh, pkh = k.ps()
                k.mm(psh[:, :], btok[b][:, g * 128:(g + 1) * 128], xde[:, gs], True, True, reads=[kb, "s_xde"], writes=[pkh])
                k.tt(HT[g][:].rearrange("p (h e) -> p h e", e=64), HT[g][:].rearrange("p (h e) -> p h e", e=64),
                     E[:, 32 + g * 8:32 + (g + 1) * 8].unsqueeze(2).broadcast_to([128, 8, 64]), OP.mult, reads=["s_HT%d" % g, "s_E"], writes=["s_HT%d" % g])
                k.tt(HT[g][:], HT[g][:], psh[:, :], OP.add, reads=["s_HT%d" % g, pkh], writes=["s_HT%d" % g])
            if LVL < 7: continue
            k.tt(t1[:], xs[b][:], VT[:, VT_D:VT_D + 1024], OP.mult, reads=[kx, "VT", ("s_t1", 0), ("s_t1", 1)], writes=[("s_t1", 0), ("s_t1", 1)], eng="pool")
            k.tt(y[:], y[:], t1[:], OP.add, reads=[("s_y", 0), ("s_y", 1), ("s_t1", 0), ("s_t1", 1)], writes=[("s_y", 0), ("s_y", 1)])
            k.act(zs[:], z[b][:, 0:1024], AF.Silu, reads=[kz], writes=["s_zs"])
            k.tt(y[:], y[:], zs[:], OP.mult, reads=[("s_y", 0), ("s_y", 1), "s_zs"], writes=[("s_y", 0), ("s_y", 1)])
            for g in range(2):
                gs = slice(g * 512, (g + 1) * 512)
                k.op("act", lambda h, g=g, gs=gs: h.activation(out=sq[:], in_=y[:, gs], func=AF.Square, accum_out=ss[:, g:g + 1]),
                     reads=[("s_y", 0), ("s_y", 1)], writes=["s_sq", ("s_ss", g)])
            k.act(rstd[:], ss[:], AF.Sqrt, reads=[("s_ss", 0), ("s_ss", 1)], writes=["s_rstd"], bias=LN_EPS, scale=1.0 / 512)
            k.op("dve", lambda h: h.reciprocal(out=rstd[:], in_=rstd[:]), reads=["s_rstd"], writes=["s_rstd"])
            for g in range(2):
                gs = slice(g * 512, (g + 1) * 512)
                k.stt(y[:, gs], y[:, gs], rstd[:, g:g + 1], VT[:, VT_NW + g * 512:VT_NW + (g + 1) * 512], OP.mult, OP.mult,
                      reads=[("s_y", g), "s_rstd", "VT"], writes=[("s_y", g)])
            if LVL < 8: continue
            yT, kyT = yTs[ci % 2], "s_yT%d" % (ci % 2)
            for half in range(2):
                pst, pkt = k.ps()
                for cc in range(4):
                    c = half * 4 + cc
                    k.tr(pst[:, cc * 128:(cc + 1) * 128], y[:, c * 128:(c + 1) * 128], CM[:, CM_ID:CM_ID + 128], reads=[("s_y", 0), ("s_y", 1), "CM"], writes=[pkt])
                k.copy(yT[:, half * 4:(half + 1) * 4, :], pst[:, :].rearrange("p (c t) -> p c t", t=128), reads=[pkt], writes=[kyT], eng=("act" if half else "dve"))
            k.dma("pool", d["YT"][0:1024, t0:t0 + 128].rearrange("(c p) t -> p c t", p=128), yT[:], reads=[kyT], writes=[("YT", 0)])
    k.pes = None
    k.barrier()


CR_M1, CR_M2, CR_SLN, CR_ISEL, CR_BD, CR_BONES, CR_RESET = 0, 256, 512, 640, 704, 832, 960
NCR = 960 + 512
VR_MUR, VR_MUK, VR_MUV, VR_W0, VR_A0, VR_KK, VR_KA, VR_RK, VR_MULO, VR_MUG = 0, 8, 16, 24, 32, 40, 48, 56, 64, 65
NVR = 66


def phase3(P):
    k, LP, d = P.k, P.LP, P.dram
    CM = P.CM
    with ExitStack() as pes:
        k.pes = pes
        CR = k.sb("CR", [128, NCR])
        VR = k.sb("VR", [128, NVR])
        OMKA = k.sb("OMKA", [128, 8])
        WUA = k.sb("WUA", [128, 1024])
        GUP = k.sb("GUP", [128, 1024])
        LNG = k.sb("LNG", [128, 512])
        LNB = k.sb("LNB", [128, 512])
        k.dma("sp", CR[:], d["CRd"][:, :], writes=["CR"])
        k.dma("sp", VR[:], d["VRd"][:, :], writes=["VR"])
        k.dma("sp", WUA[:], d["WUAd"][:, :], writes=["WUA"])
        k.dma("sp", GUP[:], d["GUPd"][:, :], writes=["GUP"])
        k.dma("sp", LNG[:], d["LNGd"][:, :], writes=["LNG"])
        k.dma("sp", LNB[:], d["LNBd"][:, :], writes=["LNB"])
        k.ts(OMKA[:], VR[:, VR_KA:VR_KA + 8], -1.0, OP.mult, reads=["VR"], writes=["OMKA"], s2=1.0, op1=OP.add)
        FR = mybir.dt.float32r
        T = [k.sb("r_T%d" % j, [128, 64], FR) for j in range(8)]
        for j in range(8):
            k.memset(T[j][:].bitcast(F32), 0.0, writes=["r_T%d" % j])
        ident = CM[:, CM_ID:CM_ID + 128]
        MASK1 = CR[:, CR_M1:CR_M1 + 256]
        MASK2 = CR[:, CR_M2:CR_M2 + 256]
        MSLN = CR[:, CR_SLN:CR_SLN + 128]
        ISELt = k.sb("r_ISELr", [128, 64], FR)
        k.copy(ISELt[:], CR[:, CR_ISEL:CR_ISEL + 64], reads=["CR"], writes=["CR"])
        ISEL = ISELt[:]
        BONES = CR[:, CR_BONES:CR_BONES + 128]

        def T_(name, shape, dt=F32):
            return k.sb(name, shape, dt), name

        lo_raw, klo_raw = T_("r_loraw", [128, 513])
        g_raw, kg_raw = T_("r_graw", [128, 513])
        LOm, kLOm = T_("r_LOm", [128, 512])
        Gs, kGs = T_("r_Gs", [128, 512])
        raw = {nm: [T_("r_raw%s%d" % (nm, i), [128, 513]) for i in range(2)] for nm in "rkv"}
        mx = {nm: T_("r_mx" + nm, [128, 512]) for nm in "rkv"}
        tmp, ktmp = T_("r_tmp", [128, 512])
        tmp2, ktmp2 = T_("r_tmp2", [128, 512])
        lw, klw = T_("r_lw", [128, 512])
        av, kav = T_("r_a", [128, 512])
        gate, kgate = T_("r_gate", [128, 512])
        kkv, kkk = T_("r_kk", [128, 512])
        kmod, kkmod = T_("r_kmod", [128, 512])
        bv, kbv = T_("r_b", [128, 512])
        bonus, kbonus = T_("r_bonus", [128, 512])
        cw, kcw = T_("r_cw", [128, 512])
        ecw, kecw = T_("r_ecw", [128, 512])
        ecwx, kecwx = T_("r_ecwx", [128, 512])
        eneg, keneg = T_("r_eneg", [128, 512])
        eend, keend = T_("r_eend", [128, 512])
        prod, kprod = T_("r_prod", [128, 512])
        QRbd, kQR = T_("r_QRbd", [128, 8, 2, 128], FR)
        kibd, kki = T_("r_kibd", [128, 8, 128], FR)
        bibd, kbi = T_("r_bibd", [128, 8, 128], FR)
        vbd, kvb = T_("r_vbd", [128, 8, 128], FR)
        kebd, kke = T_("r_kebd", [128, 8, 128], FR)
        bebd, kbe = T_("r_bebd", [128, 8, 128], FR)
        A1, kA1 = T_("r_A1", [128, 8, 256], FR)
        A2, kA2 = T_("r_A2", [128, 8, 256], FR)
        PP = [T_("r_PP%d" % i, [128, 8, 256], FR) for i in range(2)]
        QT = [T_("r_QT%d" % i, [128, 8, 128], FR) for i in range(2)]
        Vs, kVs = T_("r_Vs", [128, 8, 64], FR)
        Rs, kRs = T_("r_Rs", [128, 64], FR)
        Us, kUs = T_("r_Us", [128, 8, 64], FR)
        KT, kKT = T_("r_KT", [128, 8, 256], FR)
        Yall, kY = T_("r_Yall", [128, 8, 64])
        Ysq, kYsq = T_("r_Ysq", [128, 8, 64])
        st1, kst1 = T_("r_st1", [128, 8])
        st2, kst2 = T_("r_st2", [128, 8])
        obd, kobd = T_("r_obd", [128, 8, 128], FR)
        yb = [T_("r_yb%d" % i, [128, 512], BF16) for i in range(2)]
        k.memset(lo_raw[:, 0:1], 0.0, writes=[klo_raw])
        k.memset(g_raw[:, 0:1], 0.0, writes=[kg_raw])
        for nm in "rkv":
            for i in range(2):
                k.memset(raw[nm][i][0][:, 0:1], 0.0, writes=[raw[nm][i][1]])
        BDM = CR[:, CR_BD:CR_BD + 128].rearrange("p (a b) -> p a b", b=64)
        it = 0

        def load_shift(tile, key, chunk, t0, n, mucol, outt, outk):
            if t0 == 0:
                k.dma("sp", tile[:, 1:n + 1], d["PT"][chunk * 128:(chunk + 1) * 128, 0:n], reads=[("PT", chunk)], writes=[key])
            else:
                k.dma("sp", tile[:, 0:n + 1], d["PT"][chunk * 128:(chunk + 1) * 128, t0 - 1:t0 + n], reads=[("PT", chunk)], writes=[key])
            k.tt(outt[:, :n], tile[:, 0:n], tile[:, 1:n + 1], OP.subtract, reads=[key], writes=[outk])
            k.stt(outt[:, :n], outt[:, :n], VR[:, mucol:mucol + 1], tile[:, 1:n + 1], OP.mult, OP.add, reads=[outk, key, "VR"], writes=[outk])

        def bd_expand(dst, kdst, src, ksrc, nc_, eng="dve"):
            k.tt(dst[:, :nc_].rearrange("p c (a b) -> p c a b", b=64) if len(dst.shape) == 3 else dst,
                 src.rearrange("p (c t) -> p c t", t=64).unsqueeze(2).broadcast_to([128, nc_, 2, 64]),
                 BDM.unsqueeze(1).broadcast_to([128, nc_, 2, 64]), OP.mult, reads=[ksrc, "CR"], writes=[kdst], eng=eng)

        QR2 = [(QRbd, kQR), T_("r_QRbd_b", [128, 8, 2, 128], FR)]
        bonus2 = [(bonus, kbonus), T_("r_bonus_b", [128, 512])]
        gate2 = [(gate, kgate), T_("r_gate_b", [128, 512])]
        WC2 = [T_("r_WC%d" % i, [128, 8]) for i in range(2)]
        ctr = {"it": 0, "yb": 0}
        Qfin = {}

        def stepA(j, t0, n, nc_, par):
            QRbd, kQR = QR2[par]
            bonus, kbonus = bonus2[par]
            gate, kgate = gate2[par]
            WCp, kWC = WC2[par]
            ctr["it"] += 1
            it = ctr["it"]
            for nm, ch, mu in (("r", C_R, VR_MUR), ("k", C_K, VR_MUK), ("v", C_V, VR_MUV)):
                rt, rk_ = raw[nm][it % 2]
                load_shift(rt, rk_, ch + j, t0, n, mu + j, mx[nm][0], mx[nm][1])
            r_, kr_ = mx["r"]
            k_, kk_ = mx["k"]
            v_, kv_ = mx["v"]
            js = slice(j * 128, (j + 1) * 128)
            ps, pk = k.ps()
            k.mm(ps[:, :n], WUA[0:64, js], LOm[0:64, :n], True, True, reads=["WUA", kLOm], writes=[pk])
            k.act(lw[:, :n], ps[:, :n], AF.Sigmoid, reads=[pk, "VR"], writes=[klw], bias=VR[:, VR_W0 + j:VR_W0 + j + 1])
            k.ts(lw[:, :n], lw[:, :n], -float(np.exp(-0.5)), OP.mult, reads=[klw], writes=[klw])
            ps, pk = k.ps()
            k.mm(ps[:, :n], WUA[64:128, js], LOm[64:128, :n], True, True, reads=["WUA", kLOm], writes=[pk])
            k.act(av[:, :n], ps[:, :n], AF.Sigmoid, reads=[pk, "VR"], writes=[kav], bias=VR[:, VR_A0 + j:VR_A0 + j + 1])
            ps, pk = k.ps()
            k.mm(ps[:, :n], GUP[:, js], Gs[:, :n], True, True, reads=["GUP", kGs], writes=[pk])
            k.copy(gate[:, :n], ps[:, :n], reads=[pk], writes=[kgate], eng="act")
            k.ts(kkv[:, :n], k_[:, :n], VR[:, VR_KK + j:VR_KK + j + 1], OP.mult, reads=[kk_, "VR"], writes=[kkk])
            k.tt(tmp[:, :n], kkv[:, :n], kkv[:, :n], OP.mult, reads=[kkk], writes=[ktmp])
            ps, pk = k.ps()
            k.mm(ps[:, :n], BONES, tmp[:, :n], True, True, reads=["CR", ktmp], writes=[pk])
            k.ts(tmp2[:, :n], ps[:, :n], 1e-24, OP.max, reads=[pk], writes=[ktmp2])
            k.act(tmp2[:, :n], tmp2[:, :n], AF.Sqrt, reads=[ktmp2], writes=[ktmp2])
            k.op("dve", lambda h, n=n: h.reciprocal(out=tmp2[:, :n], in_=tmp2[:, :n]), reads=[ktmp2], writes=[ktmp2])
            k.tt(kkv[:, :n], kkv[:, :n], tmp2[:, :n], OP.mult, reads=[kkk, ktmp2], writes=[kkk])
            k.ts(tmp[:, :n], av[:, :n], VR[:, VR_KA + j:VR_KA + j + 1], OP.mult, reads=[kav, "VR", "OMKA"], writes=[ktmp], s2=OMKA[:, j:j + 1], op1=OP.add)
            k.tt(kmod[:, :n], k_[:, :n], tmp[:, :n], OP.mult, reads=[kk_, ktmp], writes=[kkmod])
            k.tt(bv[:, :n], av[:, :n], kkv[:, :n], OP.mult, reads=[kav, kkk], writes=[kbv], eng="pool")
            k.stt(tmp[:, :n], r_[:, :n], VR[:, VR_RK + j:VR_RK + j + 1], kmod[:, :n], OP.mult, OP.mult, reads=[kr_, kkmod, "VR"], writes=[ktmp])
            ps, pk = k.ps()
            k.mm(ps[:, :n], BONES, tmp[:, :n], True, True, reads=["CR", ktmp], writes=[pk])
            k.tt(bonus[:, :n], ps[:, :n], v_[:, :n], OP.mult, reads=[pk, kv_], writes=[kbonus])
            k.op("dve", lambda h, n=n: h.tensor_tensor_scan(out=cw[:, :n], data0=CR[:, CR_RESET:CR_RESET + n], data1=lw[:, :n], initial=0.0, op0=OP.mult, op1=OP.add),
                 reads=["CR", klw], writes=[kcw])
            k.act(ecw[:, :n], cw[:, :n], AF.Exp, reads=[kcw], writes=[kecw])
            k.tt(tmp2[:, :n], cw[:, :n], lw[:, :n], OP.subtract, reads=[kcw, klw], writes=[ktmp2], eng="pool")
            k.act(ecwx[:, :n], tmp2[:, :n], AF.Exp, reads=[ktmp2], writes=[kecwx])
            k.act(eneg[:, :n], cw[:, :n], AF.Exp, reads=[kcw], writes=[keneg], scale=-1.0)
            cw3 = cw[:, :n].rearrange("p (c t) -> p c t", t=64)
            k.tt(tmp[:, :n].rearrange("p (c t) -> p c t", t=64), cw3[:, :, 63:64].broadcast_to([128, nc_, 64]), cw3, OP.subtract, reads=[kcw], writes=[ktmp])
            k.act(eend[:, :n], tmp[:, :n], AF.Exp, reads=[ktmp], writes=[keend])
            k.tt(prod[:, :n], kkv[:, :n], ecwx[:, :n], OP.mult, reads=[kkk, kecwx], writes=[kprod])
            k.tt(QRbd[:, :nc_, 0, :].rearrange("p c (a b) -> p c a b", b=64), prod[:, :n].rearrange("p (c t) -> p c t", t=64).unsqueeze(2).broadcast_to([128, nc_, 2, 64]),
                 BDM.unsqueeze(1).broadcast_to([128, nc_, 2, 64]), OP.mult, reads=[kprod, "CR"], writes=[(kQR, 0)])
            k.tt(prod[:, :n], r_[:, :n], ecw[:, :n], OP.mult, reads=[kr_, kecw, (kQR, 0)], writes=[kprod])
            k.tt(QRbd[:, :nc_, 1, :].rearrange("p c (a b) -> p c a b", b=64), prod[:, :n].rearrange("p (c t) -> p c t", t=64).unsqueeze(2).broadcast_to([128, nc_, 2, 64]),
                 BDM.unsqueeze(1).broadcast_to([128, nc_, 2, 64]), OP.mult, reads=[kprod, "CR"], writes=[(kQR, 1)])
            for (dst, kdst, src, ksrc, ex, kex, neg) in ((kibd, kki, kmod, kkmod, eneg, keneg, False), (bibd, kbi, bv, kbv, eneg, keneg, False),
                                                      (kebd, kke, kmod, kkmod, eend, keend, False), (bebd, kbe, bv, kbv, eend, keend, True)):
                if neg:
                    k.stt(prod[:, :n], src[:, :n], -1.0, ex[:, :n], OP.mult, OP.mult, reads=[ksrc, kex, kprod, (kQR, 1), kki, kbi, kke], writes=[kprod])
                else:
                    k.tt(prod[:, :n], src[:, :n], ex[:, :n], OP.mult, reads=[ksrc, kex, kprod, (kQR, 1), kki, kbi, kke], writes=[kprod])
                k.tt(dst[:, :nc_].rearrange("p c (a b) -> p c a b", b=64), prod[:, :n].rearrange("p (c t) -> p c t", t=64).unsqueeze(2).broadcast_to([128, nc_, 2, 64]),
                     BDM.unsqueeze(1).broadcast_to([128, nc_, 2, 64]), OP.mult, reads=[kprod, "CR"], writes=[kdst])
            k.tt(vbd[:, :nc_].rearrange("p c (a b) -> p c a b", b=64), v_[:, :n].rearrange("p (c t) -> p c t", t=64).unsqueeze(2).broadcast_to([128, nc_, 2, 64]),
                 BDM.unsqueeze(1).broadcast_to([128, nc_, 2, 64]), OP.mult, reads=[kv_, "CR"], writes=[kvb], eng="pool")
            k.copy(WCp[:, :nc_].unsqueeze(2), ecw[:, :n].rearrange("p (c t) -> p c t", t=64)[:, :, 63:64], reads=[kecw], writes=[kWC], eng="pool")

        def s123(j, t0, n, nc_, par):
            QRbd, kQR = QR2[par]
            bonus, kbonus = bonus2[par]
            gate, kgate = gate2[par]
            WCp, kWC = WC2[par]
            for c in range(nc_):
                QRc = QRbd[:, c].rearrange("p a b -> p (a b)")
                ps1, pk1 = k.ps()
                k.mm(ps1[:, 0:256], kibd[:, c], QRc, True, True, reads=[kki, (kQR, 0), (kQR, 1)], writes=[pk1])
                k.tt(A1[:, c, :], ps1[:, 0:256], MASK1, OP.mult, reads=[pk1, "CR"], writes=[(kA1, c)])
                ps2, pk2 = k.ps()
                k.mm(ps2[:, 0:256], bibd[:, c], QRc, True, True, reads=[kbi, (kQR, 0), (kQR, 1)], writes=[pk2])
                k.tt(A2[:, c, :], ps2[:, 0:256], MASK2, OP.mult, reads=[pk2, "CR"], writes=[(kA2, c)])
                ps3, pk3 = k.ps()
                k.mm(ps3[:, 0:128], QRbd[:, c, 0], bibd[:, c], True, True, reads=[kbi, (kQR, 0)], writes=[pk3])
                k.tt(PP[0][0][:, c, 0:128], ps3[:, 0:128], MSLN, OP.mult, reads=[pk3, "CR"], writes=[(PP[0][1], c)])
                k.copy(PP[0][0][:, c, 128:256], A2[:, c, 0:128].bitcast(F32), reads=[(kA2, c), (PP[0][1], c)], writes=[(PP[0][1], c)], eng="pool")
                k.tt(QT[0][0][:, c, :], A2[:, c, 0:128].bitcast(F32), ident, OP.add, reads=[(kA2, c), "CM"], writes=[(QT[0][1], c)], eng="pool")
            cur = 0
            for lv in range(1, 6):
                Pc, kPc = PP[cur]
                Pn, kPn = PP[1 - cur]
                Qc, kQc = QT[cur]
                Qn, kQn = QT[1 - cur]
                for c0 in range(0, nc_, 2):
                    psq, pkq = k.ps()
                    for ci in range(2):
                        c = c0 + ci
                        k.mm(psq[:, ci * 256:ci * 256 + 128], Pc[:, c, 128:256], Pc[:, c, 0:128], True, True, reads=[(kPc, c)], writes=[pkq])
                        k.mm(psq[:, ci * 256 + 128:ci * 256 + 256], Pc[:, c, 0:128], Pc[:, c, 128:256], True, True, reads=[(kPc, c)], writes=[pkq])
                    k.copy(Pn[:, c0:c0 + 2, :], psq[:, 0:512].rearrange("p (c f) -> p c f", f=256), reads=[pkq], writes=[(kPn, c0), (kPn, c0 + 1)], eng="act")
                for c0 in range(0, nc_, 4):
                    m = min(4, nc_ - c0)
                    psu, pku = k.ps()
                    for ci in range(m):
                        c = c0 + ci
                        k.mm(psu[:, ci * 128:(ci + 1) * 128], Pn[:, c, 0:128], Qc[:, c, :], True, True, reads=[(kPn, c), (kQc, c)], writes=[pku])
                    k.tt(Qn[:, c0:c0 + m, :], psu[:, 0:m * 128].rearrange("p (c f) -> p c f", f=128), Qc[:, c0:c0 + m, :].bitcast(F32), OP.add,
                         reads=[pku] + [(kQc, c0 + i) for i in range(m)], writes=[(kQn, c0 + i) for i in range(m)])
                cur = 1 - cur
            Qf, kQf = QT[cur]
            psv, pkv = k.ps()
            for c in range(nc_):
                k.mm(psv[:, c * 64:(c + 1) * 64], vbd[:, c], ISEL, True, True, reads=[kvb, "CR"], writes=[pkv])
            k.copy(Vs[:, :nc_, :], psv[:, 0:nc_ * 64].rearrange("p (c f) -> p c f", f=64), reads=[pkv], writes=[kVs], eng="act")
            for c0 in range(0, nc_, 2):
                pst, pkt = k.ps()
                for ci in range(2):
                    c = c0 + ci
                    k.tr(pst[:, ci * 256:ci * 256 + 128], kebd[:, c].bitcast(F32), ident, reads=[kke, "CM"], writes=[pkt])
                    k.tr(pst[:, ci * 256 + 128:ci * 256 + 256], bebd[:, c].bitcast(F32), ident, reads=[kbe, "CM"], writes=[pkt])
                k.copy(KT[:, c0:c0 + 2, :], pst[:, 0:512].rearrange("p (c f) -> p c f", f=256), reads=[pkt], writes=[(kKT, c0), (kKT, c0 + 1)])
            Qfin[par] = (Qf, kQf)

        def s4(j, t0, n, nc_, par):
            QRbd, kQR = QR2[par]
            bonus, kbonus = bonus2[par]
            gate, kgate = gate2[par]
            WCp, kWC = WC2[par]
            Tj, kTj = T[j], "r_T%d" % j
            Qf, kQf = Qfin[par]
            psY, pkY = k.psb[7], "psb7"
            for c in range(nc_):
                psr, pkr = k.ps()
                k.mm(psr[:, 0:64], QRbd[:, c, 0], Tj[:], True, False, reads=[(kQR, 0), kTj], writes=[pkr])
                k.mm(psr[:, 0:64], A1[:, c, 0:128], Vs[:, c, :], False, True, reads=[(kA1, c), kVs], writes=[pkr])
                k.copy(Rs[:], psr[:, 0:64], reads=[pkr], writes=[kRs])
                psu, pku = k.ps()
                k.mm(psu[:, 0:64], Qf[:, c, :], Rs[:], True, True, reads=[(kQf, c), kRs], writes=[pku])
                k.copy(Us[:, c, :], psu[:, 0:64], reads=[pku], writes=[(kUs, c)])
                psn, pkn = k.ps()
                k.mm(psn[:, 0:64], KT[:, c, 0:128], Vs[:, c, :], True, False, reads=[(kKT, c), kVs], writes=[pkn])
                k.mm(psn[:, 0:64], KT[:, c, 128:256], Us[:, c, :], False, True, reads=[(kKT, c), (kUs, c)], writes=[pkn])
                k.mm(psY[:, c * 64:(c + 1) * 64], QRbd[:, c, 1], Tj[:], True, False, reads=[(kQR, 1), kTj], writes=[pkY])
                k.mm(psY[:, c * 64:(c + 1) * 64], A1[:, c, 128:256], Vs[:, c, :], False, False, reads=[(kA1, c), kVs], writes=[pkY])
                k.mm(psY[:, c * 64:(c + 1) * 64], A2[:, c, 128:256], Us[:, c, :], False, True, reads=[(kA2, c), (kUs, c)], writes=[pkY])
                k.stt(Tj[:], Tj[:].bitcast(F32), WCp[:, c:c + 1], psn[:, 0:64], OP.mult, OP.add, reads=[kTj, kWC, pkn], writes=[kTj])
            k.copy(Yall[:, :nc_, :], psY[:, 0:nc_ * 64].rearrange("p (c f) -> p c f", f=64), reads=[pkY], writes=[kY], eng="act")

        def tail(j, t0, n, nc_, par):
            QRbd, kQR = QR2[par]
            bonus, kbonus = bonus2[par]
            gate, kgate = gate2[par]
            WCp, kWC = WC2[par]
            ctr["yb"] += 1
            it = ctr["yb"]
            Yv = Yall[:, :nc_, :]
            k.op("dve", lambda h, Yv=Yv, nc_=nc_: h.tensor_reduce(out=st1[:, :nc_], in_=Yv, axis=mybir.AxisListType.X, op=OP.add), reads=[kY], writes=[kst1])
            k.ts(st1[:, :nc_], st1[:, :nc_], 1.0 / 64, OP.mult, reads=[kst1], writes=[kst1])
            k.tt(Yv, Yv, st1[:, :nc_].unsqueeze(2).broadcast_to([128, nc_, 64]), OP.subtract, reads=[kY, kst1], writes=[kY])
            k.tt(Ysq[:, :nc_, :], Yv, Yv, OP.mult, reads=[kY], writes=[kYsq])
            k.op("dve", lambda h, nc_=nc_: h.tensor_reduce(out=st2[:, :nc_], in_=Ysq[:, :nc_, :], axis=mybir.AxisListType.X, op=OP.add), reads=[kYsq], writes=[kst2])
            k.act(st2[:, :nc_], st2[:, :nc_], AF.Sqrt, reads=[kst2], writes=[kst2], bias=RWKV_EPS, scale=1.0 / 64)
            k.op("dve", lambda h, nc_=nc_: h.reciprocal(out=st2[:, :nc_], in_=st2[:, :nc_]), reads=[kst2], writes=[kst2])
            k.tt(Yv, Yv, st2[:, :nc_].unsqueeze(2).broadcast_to([128, nc_, 64]), OP.mult, reads=[kY, kst2], writes=[kY])
            for c in range(nc_):
                k.tt(Yall[:, c, :], Yall[:, c, :], LNG[:, j * 64:(j + 1) * 64], OP.mult, reads=[kY, "LNG"], writes=[kY])
                k.tt(Yall[:, c, :], Yall[:, c, :], LNB[:, j * 64:(j + 1) * 64], OP.add, reads=[kY, "LNB"], writes=[kY])
            k.tt(obd[:, :nc_].rearrange("p c (a b) -> p c a b", b=64), Yv.unsqueeze(2).broadcast_to([128, nc_, 2, 64]),
                 BDM.unsqueeze(1).broadcast_to([128, nc_, 2, 64]), OP.mult, reads=[kY, "CR"], writes=[kobd])
            pso, pko = k.ps()
            for c in range(nc_):
                k.mm(pso[:, c * 64:(c + 1) * 64], obd[:, c], ISEL, True, True, reads=[kobd, "CR"], writes=[pko])
            k.tt(tmp[:, :n], pso[:, :n], bonus[:, :n], OP.add, reads=[pko, kbonus], writes=[ktmp])
            ybt, kyb = yb[it % 2]
            k.tt(ybt[:, :n], tmp[:, :n], gate[:, :n], OP.mult, reads=[ktmp, kgate], writes=[kyb])
            k.dma("pool", d["YT"][1024 + j * 128:1024 + (j + 1) * 128, t0:t0 + n], ybt[:, :n], reads=[kyb], writes=[("YT", 1)])

        for (t0, n) in supertiles(LP):
            nc_ = n // 64
            load_shift(lo_raw, klo_raw, C_LO, t0, n, VR_MULO, LOm, kLOm)
            k.act(LOm[0:64, :n], LOm[0:64, :n], AF.Tanh, reads=[kLOm], writes=[kLOm])
            load_shift(g_raw, kg_raw, C_G, t0, n, VR_MUG, Gs, kGs)
            k.act(Gs[:, :n], Gs[:, :n], AF.Sigmoid, reads=[kGs], writes=[kGs])
            stepA(0, t0, n, nc_, 0)
            s123(0, t0, n, nc_, 0)
            for j in range(8):
                par = j % 2
                fns = [lambda j=j, par=par: s4(j, t0, n, nc_, par)]
                if j < 7:
                    fns.append(lambda j=j, par=par: stepA(j + 1, t0, n, nc_, 1 - par))
                k.interleave(fns, [2, 1])
                P.pump_casts(2)
                if j < 7:
                    k.interleave([lambda j=j, par=par: tail(j, t0, n, nc_, par), lambda j=j, par=par: s123(j + 1, t0, n, nc_, 1 - par)], [1, 3])
                else:
                    tail(j, t0, n, nc_, par)
    k.pes = None
    k.barrier()


def host_consts_rwkv(inp):
    p = np.arange(128)
    q = np.arange(128)
    same = (p[:, None] // 64) == (q[None, :] // 64)
    SU = (same & ((p[:, None] % 64) < (q[None, :] % 64))).astype(np.float32)
    IU = (same & ((p[:, None] % 64) <= (q[None, :] % 64))).astype(np.float32)
    SL = (same & ((q[None, :] % 64) < (p[:, None] % 64))).astype(np.float32)
    CR = np.zeros((128, NCR), np.float32)
    CR[:, 0:128] = SU
    CR[:, 128:256] = IU
    CR[:, 256:384] = -SU
    CR[:, 384:512] = -IU
    CR[:, 512:640] = -SL
    CR[:, 640:704] = ((p[:, None] % 64) == np.arange(64)[None, :])
    CR[:, 704:832] = ((p[:, None] // 64) == (np.arange(128)[None, :] // 64))
    CR[:, 832:960] = same
    CR[:, 960:960 + 512] = (np.arange(512) % 64 != 0)[None, :]
    mu = inp["ev_shift_mu"][0]
    VR = np.zeros((128, NVR), np.float32)

    def pairs(v):
        return v.reshape(8, 128).T

    VR[:, 0:8] = pairs(mu[0:1024])
    VR[:, 8:16] = pairs(mu[1024:2048])
    VR[:, 16:24] = pairs(mu[2048:3072])
    VR[:, 24:32] = pairs(inp["ev_w0"][0])
    VR[:, 32:40] = pairs(inp["ev_a0"][0])
    VR[:, 40:48] = pairs(inp["ev_k_k"][0])
    VR[:, 48:56] = pairs(inp["ev_k_a"][0])
    VR[:, 56:64] = pairs(inp["ev_r_k"][0].reshape(1024))
    VR[:, 64] = mu[3072:3200]
    VR[:, 65] = mu[3200:3328]
    WUA = np.concatenate([inp["ev_w_up"][0], inp["ev_a_up"][0]], 0).astype(np.float32)
    GUP = inp["ev_g_up"][0].astype(np.float32)

    def stack(v):
        a = v.reshape(8, 2, 64)
        return np.ascontiguousarray(np.repeat(a.transpose(1, 0, 2)[:, None], 64, axis=1).reshape(128, 512))

    return dict(CRd=CR, VRd=VR, WUAd=WUA, GUPd=GUP, LNGd=stack(inp["ev_lnx_g"][0]), LNBd=stack(inp["ev_lnx_b"][0]))


def layer_norm_rows(k, pfx, src, ksrc, dst, kdst, gcol, bcol, VT2, tiles):
    stats, mv, rs = tiles
    for i in range(2):
        k.op("dve", lambda h, i=i: h.bn_stats(out=stats[:, i * 6:(i + 1) * 6], in_=src[:, i * 512:(i + 1) * 512]), reads=[ksrc], writes=[pfx + "st"])
    k.op("dve", lambda h: h.bn_aggr(out=mv[:], in_=stats[:]), reads=[pfx + "st"], writes=[pfx + "mv"])
    k.act(rs[:], mv[:, 1:2], AF.Sqrt, reads=[pfx + "mv"], writes=[pfx + "rs"], bias=LN_EPS)
    k.op("dve", lambda h: h.reciprocal(out=rs[:], in_=rs[:]), reads=[pfx + "rs"], writes=[pfx + "rs"])
    k.ts(dst[:], src[:], mv[:, 0:1], OP.subtract, reads=[ksrc, pfx + "mv", pfx + "rs"], writes=[kdst], s2=rs[:, 0:1], op1=OP.mult)
    k.tt(dst[:], dst[:], VT2[:, gcol:gcol + 1024], OP.mult, reads=[kdst, "VT2"], writes=[kdst])
    k.tt(dst[:], dst[:], VT2[:, bcol:bcol + 1024], OP.add, reads=[kdst, "VT2"], writes=[kdst])


def proj_res_ln(P, pfx, srcT, nkc, wname, res_name, gcol, bcol, out_name, outT_name, outT32_name=None):
    k, LP, d = P.k, P.LP, P.dram
    with ExitStack() as pes:
        k.pes = pes
        VT2 = k.sb(pfx + "VT2", [128, 2048])
        k.dma("sp", VT2[:], d["VT2d"][0:1, gcol:gcol + 2048].partition_broadcast(128), writes=["VT2"])
        P.VT2 = VT2
        gcol, bcol = 0, 1024
        W = k.sb(pfx + "W", [128, nkc, 1024], BF16)
        k.dma("sp", W[:].rearrange("p a b -> p (a b)"), d[wname][:, :], reads=[(wname, 0)], writes=[pfx + "W"])
        yT = [k.sb(pfx + "yT%d" % i, [128, nkc, 512], BF16) for i in range(2)]
        xr = [k.sb(pfx + "x%d" % i, [128, 1024]) for i in range(2)]
        hp = k.sb(pfx + "hp", [128, 1024])
        ho = [k.sb(pfx + "ho%d" % i, [128, 1024]) for i in range(2)]
        hT = [k.sb(pfx + "hT%d" % i, [128, 8, 128], BF16) for i in range(2)]
        hT32 = [k.sb(pfx + "hTf%d" % i, [128, 8, 128]) for i in range(2)]
        lnt = (k.sb(pfx + "st", [128, 12]), k.sb(pfx + "mv", [128, 2]), k.sb(pfx + "rs", [128, 1]))
        it = 0
        for si, (t0, n) in enumerate(supertiles(LP)):
            y, ky = yT[si % 2], pfx + "yT%d" % (si % 2)
            k.dma("sp", y[:, :, :n], d[srcT][:, t0:t0 + n].rearrange("(c p) t -> p c t", p=128), reads=[(srcT, 0), (srcT, 1)], writes=[ky])
            for j in range(n // 128):
                it += 1
                x, kx = xr[it % 2], pfx + "x%d" % (it % 2)
                r0 = t0 + j * 128
                k.dma("sp", x[:], d[res_name][r0:r0 + 128, :], reads=[(res_name, 0)], writes=[kx])
                for half in range(2):
                    ps, pk = k.ps()
                    for kc in range(nkc):
                        k.mm(ps[:, :], y[:, kc, j * 128:(j + 1) * 128], W[:, kc, half * 512:(half + 1) * 512], kc == 0, kc == nkc - 1, reads=[ky, pfx + "W"], writes=[pk])
                    k.stt(hp[:, half * 512:(half + 1) * 512], x[:, half * 512:(half + 1) * 512], ALPHA, ps[:, :], OP.mult, OP.add, reads=[kx, pk], writes=[pfx + "hp"])
                o, ko = ho[it % 2], pfx + "ho%d" % (it % 2)
                layer_norm_rows(k, pfx, hp, pfx + "hp", o, ko, gcol, bcol, P.VT2, lnt)
                k.dma("pool", d[out_name][r0:r0 + 128, :], o[:], reads=[ko], writes=[(out_name, 0)])
                t_, kt = hT[it % 2], pfx + "hT%d" % (it % 2)
                tf, ktf = hT32[it % 2], pfx + "hTf%d" % (it % 2)
                for half in range(2):
                    ps, pk = k.ps()
                    for cc in range(4):
                        c = half * 4 + cc
                        k.tr(ps[:, cc * 128:(cc + 1) * 128], o[:, c * 128:(c + 1) * 128], P.ident, reads=[ko, "CM"], writes=[pk])
                    k.copy(t_[:, half * 4:(half + 1) * 4, :], ps[:, :].rearrange("p (c t) -> p c t", t=128), reads=[pk], writes=[kt], eng="act")
                    if outT32_name:
                        k.copy(tf[:, half * 4:(half + 1) * 4, :], ps[:, :].rearrange("p (c t) -> p c t", t=128), reads=[pk], writes=[ktf])
                k.dma("pool", d[outT_name][:, r0:r0 + 128].rearrange("(c p) t -> p c t", p=128), t_[:], reads=[kt], writes=[(outT_name, 0)])
                if outT32_name:
                    k.dma("pool", d[outT32_name][:, r0:r0 + 128].rearrange("(c p) t -> p c t", p=128), tf[:], reads=[ktf], writes=[(outT32_name, 0)])
    k.pes = None
    k.barrier()


def ffn_phase(P, pfx, srcT, res_name, wgu_names, wd_names, gcol, bcol, out_name, outT_name=None, router=None, tile_n=512):
    k, LP, d = P.k, P.LP, P.dram
    ne = len(wgu_names)
    with ExitStack() as pes:
        k.pes = pes
        VT2 = k.sb(pfx + "VT2", [128, 2048])
        k.dma("sp", VT2[:], d["VT2d"][0:1, gcol:gcol + 2048].partition_broadcast(128), writes=["VT2"])
        P.VT2 = VT2
        gcol, bcol = 0, 1024
        nhb = 2 if tile_n == 512 else 1
        hT = [k.sb(pfx + "hT%d" % i, [128, 8, tile_n], BF16) for i in range(nhb)]
        NWG = 6
        wg = [k.sb(pfx + "wg%d" % i, [128, 8, 128], BF16) for i in range(NWG)]
        wdh = [k.sb(pfx + "wdh%d" % i, [128, 22, 512], BF16) for i in range(2)]
        actT = k.sb(pfx + "actT", [128, 22, tile_n], BF16)
        sg = [k.sb(pfx + "sg%d" % i, [128, 512]) for i in range(2)]
        acc = [k.sb(pfx + "acc%d" % i, [128, 1024]) for i in range(tile_n // 128)]
        o = [k.sb(pfx + "o%d" % i, [128, 1024]) for i in range(2)]
        oT = [k.sb(pfx + "oT%d" % i, [128, 8, 128], BF16) for i in range(2)] if outT_name else None
        lnt = (k.sb(pfx + "st", [128, 12]), k.sb(pfx + "mv", [128, 2]), k.sb(pfx + "rs", [128, 1]))
        if router:
            hT32 = [k.sb(pfx + "hT32_%d" % i, [128, 8, 128]) for i in range(2)]
            WR = k.sb(pfx + "WR", [128, 8, 8])
            k.dma("sp", WR[:].rearrange("p a b -> p (a b)"), d["WRd"][:, :], writes=[pfx + "WR"])
            lg = k.sb(pfx + "lg", [128, 8])
            m8 = k.sb(pfx + "m8", [128, 8])
            msk = k.sb(pfx + "msk", [128, 8])
            nm0 = k.sb(pfx + "nm0", [128, 1])
            den = k.sb(pfx + "den", [128, 1])
            G = [k.sb(pfx + "G%d" % i, [128, 8]) for i in range(tile_n // 128)]

        def load_wd(e):
            for hf in range(2):
                k.dma("sp", wdh[hf][:], d[wd_names[e]][:, :].rearrange("p (a b) -> p a b", b=1024)[:, :, hf * 512:(hf + 1) * 512],
                      reads=[(wd_names[e], 0)], writes=[pfx + "wdh%d" % hf])

        if ne == 1:
            load_wd(0)
        wi = 0
        oi = 0
        big = []
        t = 0
        while t < LP:
            nn = min(tile_n, LP - t)
            big.append((t, nn))
            t += nn
        for si, (t0, n) in enumerate(big):
            nj = n // 128
            groups = [(g0, min(512, n - g0)) for g0 in range(0, n, 512)]
            h, kh = hT[si % nhb], pfx + "hT%d" % (si % nhb)
            k.dma("sp", h[:, :, :n], d[srcT][:, t0:t0 + n].rearrange("(c p) t -> p c t", p=128), reads=[(srcT, 0)], writes=[kh])
            for j in range(nj):
                r0 = t0 + j * 128
                k.dma("sp", acc[j][:], d[res_name][r0:r0 + 128, :], reads=[(res_name, 0)], writes=[pfx + "acc%d" % j])
                k.ts(acc[j][:], acc[j][:], ALPHA, OP.mult, reads=[pfx + "acc%d" % j], writes=[pfx + "acc%d" % j], eng="pool")
                if router:
                    h32, kh32 = hT32[j % 2], pfx + "hT32_%d" % (j % 2)
                    k.dma("sp", h32[:], d[router][:, r0:r0 + 128].rearrange("(c p) t -> p c t", p=128), reads=[(router, 0)], writes=[kh32])
                    ps, pk = k.ps()
                    for kc in range(8):
                        k.mm(ps[:, 0:8], h32[:, kc, :], WR[:, kc, :], kc == 0, kc == 7, reads=[kh32, pfx + "WR"], writes=[pk])
                    k.copy(lg[:], ps[:, 0:8], reads=[pk], writes=[pfx + "lg"])
                    k.op("dve", lambda hh: hh.max(out=m8[:], in_=lg[:]), reads=[pfx + "lg"], writes=[pfx + "m8"])
                    k.ts(msk[:], lg[:], m8[:, 1:2], OP.is_ge, reads=[pfx + "lg", pfx + "m8"], writes=[pfx + "msk"])
                    k.ts(nm0[:], m8[:, 0:1], -1.0, OP.mult, reads=[pfx + "m8"], writes=[pfx + "nm0"])
                    k.act(lg[:], lg[:], AF.Exp, reads=[pfx + "lg", pfx + "nm0"], writes=[pfx + "lg"], bias=nm0[:, 0:1])
                    k.tt(lg[:], lg[:], msk[:], OP.mult, reads=[pfx + "lg", pfx + "msk"], writes=[pfx + "lg"])
                    k.op("dve", lambda hh: hh.tensor_reduce(out=den[:], in_=lg[:], axis=mybir.AxisListType.X, op=OP.add), reads=[pfx + "lg"], writes=[pfx + "den"])
                    k.op("dve", lambda hh: hh.reciprocal(out=den[:], in_=den[:]), reads=[pfx + "den"], writes=[pfx + "den"])
                    k.ts(G[j][:], lg[:], den[:, 0:1], OP.mult, reads=[pfx + "lg", pfx + "den"], writes=[pfx + "G%d" % j])
            for e in range(ne):
                for i in range(22):
                    pss = {}
                    for part in range(2):
                        c = part * 22 + i
                        w, kw = wg[wi % NWG], pfx + "wg%d" % (wi % NWG)
                        wi += 1
                        k.dma("sp", w[:].rearrange("p a b -> p (a b)"), d[wgu_names[e]][c * 128:(c + 1) * 128, :], reads=[(wgu_names[e], (c * 128) // 512)], writes=[kw])
                        for gi, (g0, ng) in enumerate(groups):
                            ps, pk = k.ps()
                            for kc in range(8):
                                k.mm(ps[:, :ng], w[:, kc, :], h[:, kc, g0:g0 + ng], kc == 0, kc == 7, reads=[kw, kh], writes=[pk])
                            pss[(gi, part)] = (ps, pk)
                    for gi, (g0, ng) in enumerate(groups):
                        s_, ks = sg[gi % 2], pfx + "sg%d" % (gi % 2)
                        k.act(s_[:, :ng], pss[(gi, 0)][0][:, :ng], AF.Silu, reads=[pss[(gi, 0)][1]], writes=[ks])
                        k.tt(actT[:, i, g0:g0 + ng], s_[:, :ng], pss[(gi, 1)][0][:, :ng], OP.mult, reads=[ks, pss[(gi, 1)][1]], writes=[(pfx + "actT", i)])
                    if ne > 1 and i == 2:
                        load_wd(e)
                for half in range(2):
                    hs = slice(half * 512, (half + 1) * 512)
                    for j in range(nj):
                        ps, pk = k.ps()
                        for i in range(22):
                            k.mm(ps[:, :], actT[:, i, j * 128:(j + 1) * 128], wdh[half][:, i, :], i == 0, i == 21, reads=[(pfx + "actT", i), pfx + "wdh%d" % half], writes=[pk])
                        if router:
                            k.stt(acc[j][:, hs], ps[:, :], G[j][:, e:e + 1], acc[j][:, hs], OP.mult, OP.add, reads=[pk, pfx + "G%d" % j, pfx + "acc%d" % j], writes=[pfx + "acc%d" % j])
                        else:
                            k.tt(acc[j][:, hs], ps[:, :], acc[j][:, hs], OP.add, reads=[pk, pfx + "acc%d" % j], writes=[pfx + "acc%d" % j])
            for j in range(nj):
                r0 = t0 + j * 128
                oi += 1
                ot, ko = o[oi % 2], pfx + "o%d" % (oi % 2)
                layer_norm_rows(k, pfx, acc[j], pfx + "acc%d" % j, ot, ko, gcol, bcol, P.VT2, lnt)
                k.dma("pool", d[out_name][r0:r0 + 128, :], ot[:], reads=[ko], writes=[(out_name, 0)])
                if outT_name:
                    t_, kt = oT[oi % 2], pfx + "oT%d" % (oi % 2)
                    for half in range(2):
                        ps, pk = k.ps()
                        for cc in range(4):
                            c = half * 4 + cc
                            k.tr(ps[:, cc * 128:(cc + 1) * 128], ot[:, c * 128:(c + 1) * 128], P.ident, reads=[ko, "CM"], writes=[pk])
                        k.copy(t_[:, half * 4:(half + 1) * 4, :], ps[:, :].rearrange("p (c t) -> p c t", t=128), reads=[pk], writes=[kt], eng="act")
                    k.dma("pool", d[outT_name][:, r0:r0 + 128].rearrange("(c p) t -> p c t", p=128), t_[:], reads=[kt], writes=[(outT_name, 0)])
    k.pes = None
    k.barrier()


VL_CW, VL_CB, VL_GXB, VL_GAB, VL_LAM = 0, 32, 40, 48, 56
NVL = 64


def phase5(P):
    k, LP, d = P.k, P.LP, P.dram
    with ExitStack() as pes:
        k.pes = pes
        W = k.sb("l_W", [128, 16, 8, 128], BF16)
        k.dma("sp", W[:].rearrange("p c a b -> p c (a b)"), d["W1INb"][:, :].rearrange("(c p) f -> p c f", p=128), reads=[("W1INb", i) for i in range(4)], writes=["l_W"])
        GW = k.sb("l_GW", [128, 16, 128])
        k.dma("sp", GW[:].rearrange("p c b -> p (c b)"), d["GWd"][:, :], writes=["l_GW"])
        VL = k.sb("l_VL", [128, NVL])
        k.dma("sp", VL[:], d["VLd"][:, :], writes=["l_VL"])
        SPm8 = k.sb("l_sp8", [128, 8])
        SPm16 = k.sb("l_sp16", [128, 8])
        k.act(SPm8[:], VL[:, VL_LAM:VL_LAM + 8], AF.Exp, reads=["l_VL"], writes=["l_sp8"], scale=-1.0)
        k.act(SPm8[:], SPm8[:], AF.Ln, reads=["l_sp8"], writes=["l_sp8"], bias=1.0)
        k.ts(SPm16[:], SPm8[:], -16.0, OP.mult, reads=["l_sp8"], writes=["l_sp16"])
        k.ts(SPm8[:], SPm8[:], -8.0, OP.mult, reads=["l_sp8", "l_sp16"], writes=["l_sp8"])
        hT = [k.sb("l_hT%d" % i, [128, 8, 512], BF16) for i in range(2)]
        gb = [k.sb("l_gb%d" % i, [128, 512]) for i in range(2)]
        g2_2 = [k.sb("l_g2_%d" % i, [128, 512]) for i in range(2)]
        xr = [k.sb("l_xr%d" % i, [128, 515]) for i in range(2)]
        xf_2 = [k.sb("l_xf_%d" % i, [128, 512]) for i in range(2)]
        gx_2 = [k.sb("l_gx_%d" % i, [128, 512]) for i in range(2)]
        ga_2 = [k.sb("l_ga_%d" % i, [128, 512]) for i in range(2)]
        av_2 = [k.sb("l_a_%d" % i, [128, 512]) for i in range(2)]
        uv_2 = [k.sb("l_u_%d" % i, [128, 512]) for i in range(2)]
        hs = [k.sb("l_hs%d" % c, [128, 512]) for c in range(8)]
        carry = [k.sb("l_cy%d" % c, [128, 1]) for c in range(8)]
        yb = [k.sb("l_yb%d" % i, [128, 512], BF16) for i in range(2)]
        for c in range(8):
            k.memset(carry[c][:], 0.0, writes=["l_cy%d" % c])
        it = 0
        for si, (t0, n) in enumerate(supertiles(LP)):
            h, kh = hT[si % 2], "l_hT%d" % (si % 2)
            k.dma("sp", h[:, :, :n], d["H2T"][:, t0:t0 + n].rearrange("(c p) t -> p c t", p=128), reads=[("H2T", 0)], writes=[kh])
            def body(c, par, h=h, kh=kh, t0=t0, n=n):
                it = par
                g2, xf, gx, ga, av, uv = g2_2[par], xf_2[par], gx_2[par], ga_2[par], av_2[par], uv_2[par]
                sfx = "_%d" % par
                ps, pk = k.ps()
                for kc in range(8):
                    k.mm(ps[:, :n], W[:, c, kc, :], h[:, kc, :n], kc == 0, kc == 7, reads=["l_W", kh], writes=[pk])
                g, kg = gb[it % 2], "l_gb%d" % (it % 2)
                k.copy(g[:, :n], ps[:, :n], reads=[pk], writes=[kg], eng="act")
                k.tt(g2[:, :n], g[:, :n], g[:, :n], OP.mult, reads=[kg], writes=["l_g2" + sfx])
                k.ts(g2[:, :n], g2[:, :n], 0.044715, OP.mult, reads=["l_g2" + sfx], writes=["l_g2" + sfx], s2=1.0, op1=OP.add)
                k.tt(g2[:, :n], g2[:, :n], g[:, :n], OP.mult, reads=["l_g2" + sfx, kg], writes=["l_g2" + sfx])
                k.act(g2[:, :n], g2[:, :n], AF.Sigmoid, reads=["l_g2" + sfx], writes=["l_g2" + sfx], scale=1.5957691216057308)
                k.tt(g[:, :n], g[:, :n], g2[:, :n], OP.mult, reads=[kg, "l_g2" + sfx], writes=[kg])
                x, kx = xr[it % 2], "l_xr%d" % (it % 2)
                ps, pk = k.ps()
                for kc in range(8):
                    k.mm(ps[:, :n], W[:, 8 + c, kc, :], h[:, kc, :n], kc == 0, kc == 7, reads=["l_W", kh], writes=[pk])
                xp, kxp = xr[(it + 1) % 2], "l_xr%d" % ((it + 1) % 2)
                k.copy(x[:, 3:3 + n], ps[:, :n], reads=[pk], writes=[kx])
                k.dma("pool", d["XRT"][c * 128:(c + 1) * 128, t0:t0 + n], x[:, 3:3 + n], reads=[kx], writes=[("XRT", c)])
                if t0 == 0:
                    k.memset(x[:, 0:3], 0.0, writes=[kx])
                else:
                    k.dma("sp", x[:, 0:3], d["XRT"][c * 128:(c + 1) * 128, t0 - 3:t0], reads=[("XRT", c)], writes=[kx])
                cwc = VL_CW + c * 4
                k.ts(xf[:, :n], x[:, 3:3 + n], VL[:, cwc + 3:cwc + 4], OP.mult, reads=[kx, "l_VL"], writes=["l_xf" + sfx])
                for kk in (2, 1, 0):
                    k.stt(xf[:, :n], x[:, kk:kk + n], VL[:, cwc + kk:cwc + kk + 1], xf[:, :n], OP.mult, OP.add, reads=[kx, "l_xf" + sfx, "l_VL"], writes=["l_xf" + sfx])
                k.ts(xf[:, :n], xf[:, :n], VL[:, VL_CB + c:VL_CB + c + 1], OP.add, reads=["l_xf" + sfx, "l_VL"], writes=["l_xf" + sfx])
                ps, pk = k.ps()
                k.mm(ps[:, :n], GW[:, c, :], xf[:, :n], True, True, reads=["l_GW", "l_xf" + sfx], writes=[pk])
                k.act(gx[:, :n], ps[:, :n], AF.Sigmoid, reads=[pk, "l_VL"], writes=["l_gx" + sfx], bias=VL[:, VL_GXB + c:VL_GXB + c + 1])
                ps, pk = k.ps()
                k.mm(ps[:, :n], GW[:, 8 + c, :], xf[:, :n], True, True, reads=["l_GW", "l_xf" + sfx], writes=[pk])
                k.act(ga[:, :n], ps[:, :n], AF.Sigmoid, reads=[pk, "l_VL"], writes=["l_ga" + sfx], bias=VL[:, VL_GAB + c:VL_GAB + c + 1])
                k.act(av[:, :n], ga[:, :n], AF.Exp, reads=["l_ga" + sfx, "l_sp8"], writes=["l_a" + sfx], scale=SPm8[:, c:c + 1])
                k.act(uv[:, :n], ga[:, :n], AF.Exp, reads=["l_ga" + sfx, "l_sp16"], writes=["l_u" + sfx], scale=SPm16[:, c:c + 1])
                k.act(uv[:, :n], uv[:, :n], AF.Sqrt, reads=["l_u" + sfx], writes=["l_u" + sfx], scale=-1.0, bias=1.0)
                k.tt(gx[:, :n], gx[:, :n], xf[:, :n], OP.mult, reads=["l_gx" + sfx, "l_xf" + sfx], writes=["l_gx" + sfx])
                k.tt(uv[:, :n], uv[:, :n], gx[:, :n], OP.mult, reads=["l_u" + sfx, "l_gx" + sfx], writes=["l_u" + sfx])
                k.op("dve", lambda hh, c=c, n=n: hh.tensor_tensor_scan(out=hs[c][:, :n], data0=av[:, :n], data1=uv[:, :n], initial=carry[c][:, 0:1], op0=OP.mult, op1=OP.add),
                     reads=["l_a" + sfx, "l_u" + sfx, "l_cy%d" % c], writes=["l_hs%d" % c])
                k.copy(carry[c][:], hs[c][:, n - 1:n], reads=["l_hs%d" % c], writes=["l_cy%d" % c], eng="pool")
                y, ky = yb[it % 2], "l_yb%d" % (it % 2)
                k.tt(y[:, :n], hs[c][:, :n], g[:, :n], OP.mult, reads=["l_hs%d" % c, kg], writes=[ky])
                k.dma("pool", d["Y1T"][c * 128:(c + 1) * 128, t0:t0 + n], y[:, :n], reads=[ky], writes=[("Y1T", 0)])

            for c in range(0, 8, 2):
                k.interleave([lambda c=c: body(c, 0), lambda c=c: body(c + 1, 1)], [1, 1])
    k.pes = None
    k.barrier()


def pack_rhs_k(w):
    K = w.shape[0]
    return np.ascontiguousarray(w.reshape(K // 128, 128, w.shape[1]).transpose(1, 0, 2)).reshape(128, -1)


def host_inputs_weights(inp):
    f = np.float32
    im = {}
    CM, VT, VF = host_consts(inp)
    im.update(CMd=CM, VTd=VT, VFd=VF)
    w_in = inp["ev_w_in"][0]
    im["WINF"] = pack_lhsT(w_in, fcols())
    im["WINZ"] = pack_rhs(w_in, zcols())
    im.update(host_consts_rwkv(inp))
    im["VT2d"] = np.concatenate([inp[n][0] for n in ("ev_ln1_g", "ev_ln1_b", "ev_ln2_g", "ev_ln2_b", "od_ln1_g", "od_ln1_b", "od_ln2_g", "od_ln2_b")])[None, :].astype(f)
    im["WO"] = pack_rhs_k(inp["ev_w_out"][0])
    im["WGU0"] = pack_lhsT(inp["ev_ffn_w_gu"][0], np.arange(5632))
    im["WD0"] = pack_rhs_k(inp["ev_ffn_w_down"][0])
    im["W1IN"] = pack_lhsT(inp["od_w_in"][0], np.arange(2048))
    gw = np.concatenate([inp["od_gx_w"][0], inp["od_ga_w"][0]], 0)
    im["GWd"] = np.ascontiguousarray(gw.transpose(1, 0, 2)).reshape(128, 16 * 128).astype(f)
    VL = np.zeros((128, NVL), f)
    VL[:, 0:32] = inp["od_conv_w"][0].reshape(4, 8, 128).transpose(2, 1, 0).reshape(128, 32)
    for off, n in ((32, "od_conv_b"), (40, "od_gx_b"), (48, "od_ga_b"), (56, "od_lambda")):
        VL[:, off:off + 8] = inp[n][0].reshape(8, 128).T
    im["VLd"] = VL
    im["W1O"] = pack_rhs_k(inp["od_w_out"][0])
    im["WRd"] = np.ascontiguousarray(inp["od_router"][0].reshape(8, 128, 8).transpose(1, 0, 2)).reshape(128, 64).astype(f)
    for e in range(NEXP):
        im["WGUE%d" % e] = pack_lhsT(inp["od_exp_w_gu"][0, e], np.arange(5632))
        im["WDE%d" % e] = pack_rhs_k(inp["od_exp_w_down"][0, e])
    return im


_CACHE = {}


def kernel(**inputs):
    inp = {k_: np.asarray(v) for k_, v in inputs.items()}
    x = inp["x"]
    B, S, _ = x.shape
    L = S + NMETA
    LP = ((L + 127) // 128) * 128
    if LP not in _CACHE:
        _CACHE[LP] = build(LP)
    P = _CACHE[LP]
    wim = host_inputs_weights(inp)
    in_maps = []
    for b in range(B):
        xin = np.zeros((LP, D), np.float32)
        xin[:NMETA] = inp["meta"]
        xin[NMETA:L] = x[b]
        m = dict(wim)
        m["xin"] = xin
        in_maps.append(m)
    res = run_bass_kernel_spmd(P.nc, in_maps, core_ids=list(range(B)))
    out = np.stack([np.asarray(r["OUT"])[NMETA:L] for r in res.results], 0)
    return out.astype(np.float32)
```
